# Optimizing a Trainium2 kernel written in Bass

```python
import math
import jax, jax.numpy as jnp
from jax import lax
import numpy as np

D_MODEL = 1024
BATCH = 8
SEQ = 4096
DEPTH = 4

CHUNK = 64
Q_BLOCK = 128
EPS = 1e-6

SSM_WIDTH = 256
SSM_GROUP = 16
N_SSM_GROUPS = SSM_WIDTH // SSM_GROUP
SSM_STATE = 64
DT_MIN = 1e-3
DT_MAX = 1e-1

MLA_HEADS = 6
MLA_Q_RANK = 256
MLA_KV_RANK = 128
MLA_NOPE = 64
MLA_ROPE = 32
MLA_V = 64
MLA_QK = MLA_NOPE + MLA_ROPE
MLA_WIDTH = MLA_HEADS * MLA_V
ROPE_BASE = 10000.0

FOX_HEADS = 6
FOX_HEAD_DIM = 64
FOX_WIDTH = FOX_HEADS * FOX_HEAD_DIM

D_MIX = SSM_WIDTH + MLA_WIDTH + FOX_WIDTH
IN_SPLITS = (
    SSM_WIDTH,
    SSM_WIDTH + MLA_Q_RANK,
    SSM_WIDTH + MLA_Q_RANK + MLA_KV_RANK,
    SSM_WIDTH + MLA_Q_RANK + MLA_KV_RANK + MLA_ROPE,
    SSM_WIDTH + MLA_Q_RANK + MLA_KV_RANK + MLA_ROPE + FOX_WIDTH,
    SSM_WIDTH + MLA_Q_RANK + MLA_KV_RANK + MLA_ROPE + 2 * FOX_WIDTH,
    SSM_WIDTH + MLA_Q_RANK + MLA_KV_RANK + MLA_ROPE + 3 * FOX_WIDTH,
)
IN_COLS = SSM_WIDTH + MLA_Q_RANK + MLA_KV_RANK + MLA_ROPE + 3 * FOX_WIDTH + FOX_HEADS

D_FF = 2816
N_EXPERTS = 8
TOP_K = 2
D_FF_EXPERT = 1408
N_DENSE = (DEPTH + 1) // 2
N_MOE = DEPTH // 2

kernel_name = "hybrid_s5_mla_fox_moe_encoder"


def rms_norm(x, g):
    xf = x.astype(jnp.float32)
    y = xf * lax.rsqrt(jnp.mean(xf * xf, axis=-1, keepdims=True) + EPS)
    return (y * g.astype(jnp.float32)).astype(x.dtype)


def modulate(h, shift, scale):
    return h * (1.0 + scale[:, None, :]) + shift[:, None, :]


def rope_tables(positions):
    half = MLA_ROPE // 2
    inv = ROPE_BASE ** (-jnp.arange(half, dtype=jnp.float32) / half)
    ang = positions.astype(jnp.float32)[..., None] * inv
    return jnp.cos(ang), jnp.sin(ang)


def apply_rope(x, cos, sin):
    half = x.shape[-1] // 2
    x1 = x[..., :half].astype(jnp.float32)
    x2 = x[..., half:].astype(jnp.float32)
    cs = cos[:, :, None, :]
    sn = sin[:, :, None, :]
    return jnp.concatenate([x1 * cs - x2 * sn, x1 * sn + x2 * cs], axis=-1).astype(x.dtype)


def chunk_causal(t_pos, s_pos):
    return (s_pos // CHUNK) <= (t_pos // CHUNK)


def frame_causal(t_pos, s_pos):
    return s_pos <= t_pos


def block_sweep(q, k, v, scale, mask_fn, log_decay=None):
    L = q.shape[2]
    outs = []
    for qb in range(L // Q_BLOCK):
        q0, q1 = qb * Q_BLOCK, (qb + 1) * Q_BLOCK
        s = jnp.einsum("bhqd,bhkd->bhqk", q[:, :, q0:q1], k[:, :, :q1]).astype(jnp.float32) * scale
        if log_decay is not None:
            s = s + log_decay[:, :, q0:q1, None] - log_decay[:, :, None, :q1]
        t_pos = jnp.arange(q0, q1)[:, None]
        s_pos = jnp.arange(q1)[None, :]
        s = jnp.where(mask_fn(t_pos, s_pos), s, -jnp.inf)
        p = jax.nn.softmax(s, axis=-1)
        outs.append(jnp.einsum("bhqk,bhkd->bhqd", p.astype(v.dtype), v[:, :, :q1]))
    return jnp.concatenate(outs, axis=2)


def s5_mixer(u, lam_re, lam_im, log_dt, b_re, b_im, c_re, c_im, d_skip, w_glu, b_glu):
    Bn, L, _ = u.shape
    f32 = jnp.float32
    uf = u.astype(f32)
    ug = uf.reshape(Bn, L, N_SSM_GROUPS, SSM_GROUP)
    lam = lax.complex(lam_re.astype(f32), lam_im.astype(f32))
    dt = jnp.exp(log_dt.astype(f32))[:, None]
    lam_bar = jnp.exp(lam * dt)
    b = lax.complex(b_re.astype(f32), b_im.astype(f32))
    b_bar = ((lam_bar - 1.0) / lam)[..., None] * b
    bu = jnp.einsum("gpc,blgc->blgp", b_bar, ug)
    a = jnp.broadcast_to(lam_bar, bu.shape)

    def combine(left, right):
        a_l, b_l = left
        a_r, b_r = right
        return a_r * a_l, a_r * b_l + b_r

    _, states = lax.associative_scan(combine, (a, bu), axis=1)
    cmat = lax.complex(c_re.astype(f32), c_im.astype(f32))
    y = jnp.real(jnp.einsum("gcp,blgp->blgc", cmat, states)).reshape(Bn, L, SSM_WIDTH)
    y = jax.nn.gelu(y + d_skip.astype(f32) * uf)
    out = y * jax.nn.sigmoid(y @ w_glu.astype(f32) + b_glu.astype(f32))
    return out.astype(u.dtype)


def hybrid_mixer(h, cos, sin, w_in, lam_re, lam_im, log_dt, b_re, b_im, c_re, c_im, d_skip,
                 w_glu, b_glu, q_norm, kv_norm, w_uq, w_ukv, mla_gq, mla_gk,
                 fox_bf, fox_gq, fox_gk, out_norm, w_out):
    Bn, L, _ = h.shape
    proj = h @ w_in
    u, cq, ckv, kr, fq, fk, fv, fg = jnp.split(proj, IN_SPLITS, axis=-1)

    o_ssm = s5_mixer(u, lam_re, lam_im, log_dt, b_re, b_im, c_re, c_im, d_skip, w_glu, b_glu)

    q = (rms_norm(cq, q_norm) @ w_uq).reshape(Bn, L, MLA_HEADS, MLA_QK)
    kv = (rms_norm(ckv, kv_norm) @ w_ukv).reshape(Bn, L, MLA_HEADS, MLA_NOPE + MLA_V)
    k_nope, v_mla = kv[..., :MLA_NOPE], kv[..., MLA_NOPE:]
    k_rope = jnp.broadcast_to(kr[:, :, None, :], (Bn, L, MLA_HEADS, MLA_ROPE))
    k = jnp.concatenate([k_nope, k_rope], axis=-1)
    q = rms_norm(q, mla_gq)
    k = rms_norm(k, mla_gk)
    q = jnp.concatenate([q[..., :MLA_NOPE], apply_rope(q[..., MLA_NOPE:], cos, sin)], axis=-1)
    k = jnp.concatenate([k[..., :MLA_NOPE], apply_rope(k[..., MLA_NOPE:], cos, sin)], axis=-1)
    o_mla = block_sweep(q.transpose(0, 2, 1, 3), k.transpose(0, 2, 1, 3), v_mla.transpose(0, 2, 1, 3),
                        1.0 / math.sqrt(MLA_QK), chunk_causal)
    o_mla = o_mla.transpose(0, 2, 1, 3).reshape(Bn, L, MLA_WIDTH)

    fqh = rms_norm(fq.reshape(Bn, L, FOX_HEADS, FOX_HEAD_DIM), fox_gq)
    fkh = rms_norm(fk.reshape(Bn, L, FOX_HEADS, FOX_HEAD_DIM), fox_gk)
    fvh = fv.reshape(Bn, L, FOX_HEADS, FOX_HEAD_DIM)
    log_f = jax.nn.log_sigmoid(fg.astype(jnp.float32) + fox_bf.astype(jnp.float32))
    cum_log_f = jnp.cumsum(log_f, axis=1).transpose(0, 2, 1)
    o_fox = block_sweep(fqh.transpose(0, 2, 1, 3), fkh.transpose(0, 2, 1, 3), fvh.transpose(0, 2, 1, 3),
                        1.0 / math.sqrt(FOX_HEAD_DIM), frame_causal, log_decay=cum_log_f)
    o_fox = o_fox.transpose(0, 2, 1, 3).reshape(Bn, L, FOX_WIDTH)

    e1 = SSM_WIDTH
    e2 = SSM_WIDTH + MLA_WIDTH
    merged = jnp.concatenate([
        rms_norm(o_ssm, out_norm[:e1]),
        rms_norm(o_mla.astype(h.dtype), out_norm[e1:e2]),
        rms_norm(o_fox.astype(h.dtype), out_norm[e2:]),
    ], axis=-1)
    return merged @ w_out


def swiglu(t, w_gate, w_up, w_down):
    return (jax.nn.silu(t @ w_gate) * (t @ w_up)) @ w_down


def moe_ffn(h, w_router, b_router, w_gate, w_up, w_down):
    Bn, L, D = h.shape
    t = h.reshape(Bn * L, D)
    logits = (t @ w_router).astype(jnp.float32) + b_router.astype(jnp.float32)
    top_v, top_i = lax.top_k(logits, TOP_K)
    p = jax.nn.softmax(top_v, axis=-1)
    combine = jnp.sum(jax.nn.one_hot(top_i, N_EXPERTS, dtype=jnp.float32) * p[..., None], axis=1)
    out = jnp.zeros_like(t)
    for e in range(N_EXPERTS):
        out = out + combine[:, e:e + 1].astype(t.dtype) * swiglu(t, w_gate[e], w_up[e], w_down[e])
    return out.reshape(Bn, L, D)


def setup_inputs(seed: int = 0) -> dict:
    key = jax.random.key(seed)
    keys = iter(jax.random.split(key, 48))
    f32 = jnp.float32

    def nrm(shape, std):
        return std * jax.random.normal(next(keys), shape, f32)

    def gain(shape):
        return 1.0 + nrm(shape, 0.02)

    x = jax.random.normal(next(keys), (BATCH, SEQ, D_MODEL), f32)
    c = jax.random.normal(next(keys), (BATCH, D_MODEL), f32)
    offset = jax.random.randint(next(keys), (BATCH,), 0, 64, jnp.int32) * CHUNK
    positions = (offset[:, None] + jnp.arange(SEQ, dtype=jnp.int32)[None, :]).astype(jnp.int32)

    G, P = N_SSM_GROUPS, SSM_STATE
    lam_im_base = jnp.pi * jnp.arange(P, dtype=f32)
    log_dt = jax.random.uniform(next(keys), (DEPTH, G), f32, math.log(DT_MIN), math.log(DT_MAX))

    return {
        "x": x,
        "c": c,
        "positions": positions,
        "norm_mix": gain((DEPTH, D_MODEL)),
        "norm_ffn": gain((DEPTH, D_MODEL)),
        "w_ada": nrm((DEPTH, D_MODEL, 6 * D_MODEL), 0.5 * D_MODEL ** -0.5),
        "b_ada": nrm((DEPTH, 6 * D_MODEL), 0.02),
        "w_in": nrm((DEPTH, D_MODEL, IN_COLS), D_MODEL ** -0.5),
        "ssm_lam_re": -0.5 + nrm((DEPTH, G, P), 0.01),
        "ssm_lam_im": lam_im_base + nrm((DEPTH, G, P), 0.01),
        "ssm_log_dt": log_dt,
        "ssm_b_re": nrm((DEPTH, G, P, SSM_GROUP), (2 * SSM_GROUP) ** -0.5),
        "ssm_b_im": nrm((DEPTH, G, P, SSM_GROUP), (2 * SSM_GROUP) ** -0.5),
        "ssm_c_re": nrm((DEPTH, G, SSM_GROUP, P), 0.5),
        "ssm_c_im": nrm((DEPTH, G, SSM_GROUP, P), 0.5),
        "ssm_d": nrm((DEPTH, SSM_WIDTH), 1.0),
        "ssm_w_glu": nrm((DEPTH, SSM_WIDTH, SSM_WIDTH), SSM_WIDTH ** -0.5),
        "ssm_b_glu": nrm((DEPTH, SSM_WIDTH), 0.02),
        "mla_q_norm": gain((DEPTH, MLA_Q_RANK)),
        "mla_kv_norm": gain((DEPTH, MLA_KV_RANK)),
        "mla_w_uq": nrm((DEPTH, MLA_Q_RANK, MLA_HEADS * MLA_QK), MLA_Q_RANK ** -0.5),
        "mla_w_ukv": nrm((DEPTH, MLA_KV_RANK, MLA_HEADS * (MLA_NOPE + MLA_V)), MLA_KV_RANK ** -0.5),
        "mla_qk_gq": gain((DEPTH, MLA_QK)),
        "mla_qk_gk": gain((DEPTH, MLA_QK)),
        "fox_b_f": 3.0 + nrm((DEPTH, FOX_HEADS), 0.5),
        "fox_qk_gq": gain((DEPTH, FOX_HEAD_DIM)),
        "fox_qk_gk": gain((DEPTH, FOX_HEAD_DIM)),
        "out_norm": gain((DEPTH, D_MIX)),
        "w_out": nrm((DEPTH, D_MIX, D_MODEL), D_MIX ** -0.5),
        "ffn_w_gate": nrm((N_DENSE, D_MODEL, D_FF), D_MODEL ** -0.5),
        "ffn_w_up": nrm((N_DENSE, D_MODEL, D_FF), D_MODEL ** -0.5),
        "ffn_w_down": nrm((N_DENSE, D_FF, D_MODEL), D_FF ** -0.5),
        "moe_w_router": nrm((N_MOE, D_MODEL, N_EXPERTS), D_MODEL ** -0.5),
        "moe_b_router": nrm((N_MOE, N_EXPERTS), 0.01),
        "moe_w_gate": nrm((N_MOE, N_EXPERTS, D_MODEL, D_FF_EXPERT), D_MODEL ** -0.5),
        "moe_w_up": nrm((N_MOE, N_EXPERTS, D_MODEL, D_FF_EXPERT), D_MODEL ** -0.5),
        "moe_w_down": nrm((N_MOE, N_EXPERTS, D_FF_EXPERT, D_MODEL), D_FF_EXPERT ** -0.5),
    }


def reference(x, c, positions, norm_mix, norm_ffn, w_ada, b_ada, w_in,
              ssm_lam_re, ssm_lam_im, ssm_log_dt, ssm_b_re, ssm_b_im, ssm_c_re, ssm_c_im,
              ssm_d, ssm_w_glu, ssm_b_glu,
              mla_q_norm, mla_kv_norm, mla_w_uq, mla_w_ukv, mla_qk_gq, mla_qk_gk,
              fox_b_f, fox_qk_gq, fox_qk_gk, out_norm, w_out,
              ffn_w_gate, ffn_w_up, ffn_w_down,
              moe_w_router, moe_b_router, moe_w_gate, moe_w_up, moe_w_down):
    cos, sin = rope_tables(positions)
    c_act = jax.nn.silu(c)
    for i in range(DEPTH):
        ada = c_act @ w_ada[i] + b_ada[i]
        sh1, sc1, g1, sh2, sc2, g2 = jnp.split(ada, 6, axis=-1)

        h = modulate(rms_norm(x, norm_mix[i]), sh1, sc1)
        mix = hybrid_mixer(h, cos, sin, w_in[i],
                           ssm_lam_re[i], ssm_lam_im[i], ssm_log_dt[i], ssm_b_re[i], ssm_b_im[i],
                           ssm_c_re[i], ssm_c_im[i], ssm_d[i], ssm_w_glu[i], ssm_b_glu[i],
                           mla_q_norm[i], mla_kv_norm[i], mla_w_uq[i], mla_w_ukv[i],
                           mla_qk_gq[i], mla_qk_gk[i],
                           fox_b_f[i], fox_qk_gq[i], fox_qk_gk[i], out_norm[i], w_out[i])
        x = x + g1[:, None, :] * mix

        h = modulate(rms_norm(x, norm_ffn[i]), sh2, sc2)
        j = i // 2
        if i % 2 == 0:
            ff = swiglu(h, ffn_w_gate[j], ffn_w_up[j], ffn_w_down[j])
        else:
            ff = moe_ffn(h, moe_w_router[j], moe_b_router[j], moe_w_gate[j], moe_w_up[j], moe_w_down[j])
        x = x + g2[:, None, :] * ff
    return x
```

```python
import contextlib
import math
import sys
import numpy as np
import concourse.bass as bass
import concourse.mybir as mybir
from concourse.bass_utils import run_bass_kernel_spmd

F32 = mybir.dt.float32
BF16 = mybir.dt.bfloat16
I32 = mybir.dt.int32
AF = mybir.ActivationFunctionType
ALU = mybir.AluOpType
AX = mybir.AxisListType

ENGS = ("pe", "act", "dve", "pool", "sp")

D = 1024
KD = 8
DEPTH = 4
EPS = 1e-6
IN_COLS = 1830
NH = 6
DFE = 1408
NFC = 11
SUB = 256


class Tl:
    __slots__ = ("t", "name", "w", "r", "excl")

    def __init__(self, t, name):
        self.t = t
        self.name = name
        self.excl = False
        self.w = {}
        self.r = {}

    def __getitem__(self, idx):
        return self.t[idx]


class Prog:
    max_ops = 10 ** 9
    log = None

    def __init__(self, nc, ring_sizes=None):
        self.nc = nc
        self.es = contextlib.ExitStack()
        self.cnt = {e: 0 for e in ENGS}
        self.sems = {}
        self.seen = {e: {} for e in ENGS}
        for e in ENGS:
            self.sems[("eng", e)] = self.es.enter_context(nc.semaphore("s_" + e))
        ring_sizes = ring_sizes or {"sp": 16, "pool": 8, "act": 2}
        self.rings = {}
        self.ring_i = {}
        for e, k in ring_sizes.items():
            self.rings[e] = []
            for i in range(k):
                key = ("ring", e, i)
                self.sems[key] = self.es.enter_context(nc.semaphore("r_%s%d" % (e, i)))
                self.rings[e].append([key, 0])
            self.ring_i[e] = 0
        self.n_inst = 0
        self.scopes = [self.es]
        self.E = {"pe": nc.tensor, "act": nc.scalar, "dve": nc.vector, "pool": nc.gpsimd, "sp": nc.sync}

    def sb(self, name, shape, dt):
        self._uid = getattr(self, "_uid", 0) + 1
        return Tl(self.scopes[-1].enter_context(self.nc.sbuf_tensor("%s_%d" % (name, self._uid), list(shape), dt)), name)

    def push(self):
        self.scopes.append(contextlib.ExitStack())

    def barrier(self):
        need = {}
        for e, ring in self.rings.items():
            for key, v in ring:
                if v > 0:
                    need[key] = v
        for e in ENGS:
            if self.cnt[e] > 0:
                need[("eng", e)] = self.cnt[e]
        for eng in ENGS:
            seen = self.seen[eng]
            for k, v in need.items():
                if k == ("eng", eng) or seen.get(k, 0) >= v:
                    continue
                seen[k] = v
                self.E[eng].wait_ge(self.sems[k], v)

    def pop(self):
        self.barrier()
        self.scopes.pop().close()

    def ps(self, name, shape, dt):
        t = Tl(self.es.enter_context(self.nc.psum_tensor(name, list(shape), dt)), name)
        t.excl = True
        return t

    def dram(self, name, shape, dt, kind="Internal"):
        return Tl(self.nc.dram_tensor(name, list(shape), dt, kind=kind).ap(), name)

    def _collect(self, eng, reads, writes):
        need = {}
        me = ("eng", eng)
        for t in reads:
            for k, v in t.w.items():
                if need.get(k, 0) < v:
                    need[k] = v
            if t.excl:
                for k, v in t.r.items():
                    if k != me and need.get(k, 0) < v:
                        need[k] = v
        for t in writes:
            for k, v in t.w.items():
                if need.get(k, 0) < v:
                    need[k] = v
            for k, v in t.r.items():
                if need.get(k, 0) < v:
                    need[k] = v
        waits = []
        seen = self.seen[eng]
        for k, v in need.items():
            if eng == "pe" and k == ("eng", "pe"):
                continue
            if seen.get(k, 0) >= v:
                continue
            seen[k] = v
            waits.append((self.sems[k], v))
        return waits

    def _commit(self, reads, writes, key, val):
        for t in reads:
            if t.r.get(key, 0) < val:
                t.r[key] = val
        for t in writes:
            t.w = {key: val}
            t.r = {}

    def op(self, eng, fn, reads=(), writes=()):
        self.n_inst += 1
        if Prog.log is not None:
            Prog.log.append((self.n_inst, eng, sys._getframe(1).f_lineno))
        if self.n_inst > Prog.max_ops:
            return
        waits = self._collect(eng, reads, writes)
        self.cnt[eng] += 1
        key = ("eng", eng)
        val = self.cnt[eng]
        self._commit(reads, writes, key, val)
        e = self.E[eng]
        for s, v in waits:
            e.wait_ge(s, v)
        fn(e).then_inc(self.sems[key], 1)

    def dma(self, eng, out, in_, reads=(), writes=(), **kw):
        self.n_inst += 1
        if Prog.log is not None:
            Prog.log.append((self.n_inst, "dma-" + eng, sys._getframe(1).f_lineno))
        if self.n_inst > Prog.max_ops:
            return
        ring = self.rings[eng]
        slot = ring[self.ring_i[eng] % len(ring)]
        self.ring_i[eng] += 1
        key, pv = slot
        waits = self._collect(eng, reads, writes)
        seen = self.seen[eng]
        if pv > 0 and seen.get(key, 0) < pv:
            seen[key] = pv
            waits.append((self.sems[key], pv))
        val = pv + 16
        slot[1] = val
        self._commit(reads, writes, key, val)
        e = self.E[eng]
        for s, v in waits:
            e.wait_ge(s, v)
        e.dma_start(out=out, in_=in_, **kw).then_inc(self.sems[key], 16)

    def finish(self, eng="sp"):
        need = {}
        for e, ring in self.rings.items():
            for key, v in ring:
                if v > 0:
                    need[key] = v
        for e in ENGS:
            if self.cnt[e] > 0:
                need[("eng", e)] = self.cnt[e]
        E = self.E[eng]
        for k, v in need.items():
            E.wait_ge(self.sems[k], v)
        self.es.close()


LAYER_PARAMS = [
    ("norm_mix_c", [128, KD]), ("norm_ffn_c", [128, KD]),
    ("w_ada", [D, 6 * D]), ("b_ada", [1, 6 * D]),
    ("w_in", [D, IN_COLS]),
    ("lam_re", [128, 8]), ("lam_im", [128, 8]), ("log_dt", [128, 8]),
    ("b_re", [8, 128, 128]), ("b_im", [8, 128, 128]),
    ("c_re", [8, 128, 128]), ("c_im", [8, 128, 128]),
    ("ssm_d", [128, 2]), ("w_glu", [256, 256]), ("b_glu", [128, 2]),
    ("q_norm", [128, 2]), ("kv_norm", [128, 1]),
    ("w_uq", [256, 576]), ("w_ukv", [128, 768]),
    ("gq_m", [128, 96]), ("gk_m", [128, 96]),
    ("fox_bf", [NH, 1]), ("gq_f", [128, 64]), ("gk_f", [128, 64]),
    ("out_norm", [128, KD]), ("w_out", [D, D]),
]


def _alloc_a(P, L):
    NT = L // 128
    w_in_sb = P.sb("w_in_sb", [128, KD, IN_COLS], BF16)
    w_uq_sb = P.sb("w_uq_sb", [128, 2, 576], BF16)
    w_ukv_sb = P.sb("w_ukv_sb", [128, 768], BF16)
    w_glu_sb = P.sb("w_glu_sb", [128, 2, 256], BF16)
    qn_c = P.sb("qn_c", [128, 2], F32)
    kvn_c = P.sb("kvn_c", [128, 1], F32)
    gqm = P.sb("gqm", [128, 96], F32)
    gkm = P.sb("gkm", [128, 96], F32)
    gqf = P.sb("gqf", [128, 64], F32)
    gkf = P.sb("gkf", [128, 64], F32)
    nbf = P.sb("nbf", [NH, 1], F32)
    dcol = P.sb("dcol", [128, 2], F32)
    bglu_c = P.sb("bglu_c", [128, 2], F32)
    nbglu_c = P.sb("nbglu_c", [128, 2], F32)
    s5p = P.sb("s5p", [128, 24, 8], F32)
    BreT = P.sb("BreT", [128, 8, 128], BF16)
    BimT = P.sb("BimT", [128, 8, 128], BF16)
    CreT = P.sb("CreT", [128, 8, 128], BF16)
    nCimT = P.sb("nCimT", [128, 8, 128], BF16)
    Ddiag = P.sb("Ddiag", [128, 2, 128], BF16)
    Ctab = P.sb("Ctab", [128, 8, SUB], F32)
    Stab = P.sb("Stab", [128, 8, SUB], F32)
    Rtab = P.sb("Rtab", [128, 8, SUB], F32)
    blk_f = P.sb("blk_f", [128, 2, 128], F32)
    blk_o = P.sb("blk_o", [128, 2, 128], F32)
    hT = [P.sb("hT%d" % i, [128, KD, 512], BF16) for i in range(2)]
    uT = P.sb("uT", [128, 2, 512], BF16)
    cq_h = P.sb("cq_h", [128, 256], BF16)
    ckv_h = P.sb("ckv_h", [128, 128], BF16)
    cqT = P.sb("cqT", [128, 2, 128], BF16)
    ckvT = P.sb("ckvT", [128, 128], BF16)
    qn = P.sb("qn", [128, NH, 96], F32)
    kn = P.sb("kn", [128, NH, 96], F32)
    rt = [P.sb("rt%d" % i, [128, NH, 16], F32) for i in range(4)]
    qfin = P.sb("qfin", [128, NH, 96], BF16)
    kfin = P.sb("kfin", [128, NH, 96], BF16)
    fqn = P.sb("fqn", [128, NH, 64], F32)
    fqb = P.sb("fqb", [128, NH, 64], BF16)
    fkb = P.sb("fkb", [128, NH, 64], BF16)
    QTm_st = [P.sb("QTm_st%d" % i, [96, NH, 128], BF16) for i in range(2)]
    KTm_st = [P.sb("KTm_st%d" % i, [96, NH, 128], BF16) for i in range(2)]
    QTf_st = [P.sb("QTf_st%d" % i, [64, NH, 128], BF16) for i in range(2)]
    KTf_st = [P.sb("KTf_st%d" % i, [64, NH, 128], BF16) for i in range(2)]
    Vm_st = [P.sb("Vm_st%d" % i, [128, NH, 65], BF16) for i in range(2)]
    Vf_st = [P.sb("Vf_st%d" % i, [128, NH, 65], BF16) for i in range(2)]
    for t_ in Vm_st + Vf_st:
        P.op("pool", lambda e, t_=t_: e.memset(t_[:], 1.0), writes=[t_])
    fg_e = P.sb("fg_e", [NH, 512], F32)
    fg_sp = P.sb("fg_sp", [NH, 512], F32)
    fg_cum = P.sb("fg_cum", [NH, 512], F32)
    fg_carry = P.sb("fg_carry", [NH, 1], F32)
    fg_hi = P.sb("fg_hi", [NH, 512], BF16)
    fg_lo = P.sb("fg_lo", [NH, 512], BF16)
    fg_nhi = P.sb("fg_nhi", [NH, 512], BF16)
    fg_nlo = P.sb("fg_nlo", [NH, 512], BF16)
    W_re = P.sb("W_re", [128, 4, SUB], F32)
    W_im = P.sb("W_im", [128, 4, SUB], F32)
    wlast = P.sb("wlast", [128, 2, 8], F32)
    pre_c = P.sb("pre_c", [128, 2, SUB], F32)
    pre_s = P.sb("pre_s", [128, 2, SUB], F32)
    pin_re = P.sb("pin_re", [128, SUB], F32)
    pin_im = P.sb("pin_im", [128, SUB], F32)
    w0 = P.sb("w0", [128, 2, 8], F32)
    w0t = P.sb("w0t", [128, 4, 8], F32)
    pt = [P.sb("pt%d" % i, [128, 4, SUB], F32) for i in range(2)]
    s_re = P.sb("s_re", [128, 4, SUB], BF16)
    s_im = P.sb("s_im", [128, 4, SUB], BF16)
    yg = P.sb("yg", [128, 2, SUB], F32)
    yt1 = P.sb("yt1", [128, 2, SUB], F32)
    yt2 = P.sb("yt2", [128, 2, SUB], F32)
    yTb = P.sb("yTb", [128, 2, SUB], BF16)
    o2 = P.sb("o2", [128, 2, SUB], F32)
    osm = P.sb("osm", [128, 2, SUB], F32)
    rs_bc = P.sb("rs_bc", [128, SUB], F32)
    msm = P.sb("msm", [128, 2, SUB], BF16)
    return locals()


def _alloc_bc(P, L):
    NT = L // 128
    o_attn = P.sb("o_attn", [128, NT, 768], BF16)
    return locals()


def _alloc_b(P, L):
    NT = L // 128
    QT_sb = [P.sb("QT_sb%d" % i, [96, L], BF16) for i in range(2)]
    KT_sb = [P.sb("KT_sb%d" % i, [96, L], BF16) for i in range(2)]
    V_sb = P.sb("V_sb", [128, NT, NH * 65], BF16)
    PT = [P.sb("PT%d" % i, [128, 512], BF16) for i in range(3)]
    rden = [P.sb("rden%d" % i, [128, 1], F32) for i in range(4)]
    return locals()


def _alloc_c(P, L):
    NT = L // 128
    w_out_sb = P.sb("w_out_sb", [128, KD, D], BF16)
    wstc = [P.sb("wstc%d" % i, [128, D], F32) for i in range(2)]
    onorm_c = P.sb("onorm_c", [128, KD], F32)
    mat = P.sb("mat", [128, 768], BF16)
    mT = P.sb("mT", [128, KD, 128], BF16)
    xnew = [P.sb("xnew%d" % i, [128, D], F32) for i in range(2)]
    h2T_st = P.sb("h2T_st", [128, KD, 128], BF16)
    xhf = P.sb("xhf", [128, D], F32)
    h2Tf = P.sb("h2Tf", [128, KD, 128], F32)
    wr_sb = P.sb("wr_sb", [128, KD, 8], F32)
    br_sb = P.sb("br_sb", [1, 8], F32)
    rtmp = [P.sb("rtmp%d" % i, [128, 8], F32) for i in range(4)]
    rsc = [P.sb("rsc%d" % i, [128, 1], F32) for i in range(4)]
    return locals()


def _alloc_d(P, L):
    Wg_sb = [P.sb("Wg_sb%d" % i, [128, KD, DFE], BF16) for i in range(2)]
    Wu_sb = [P.sb("Wu_sb%d" % i, [128, KD, DFE], BF16) for i in range(2)]
    Wd_sb = [P.sb("Wd_sb%d" % i, [128, NFC, D], BF16) for i in range(2)]
    h2T_sb = [P.sb("h2T_sb%d" % i, [128, KD, 512], BF16) for i in range(2)]
    sg = [P.sb("sg%d" % i, [128, 512], BF16) for i in range(2)]
    aT = P.sb("aT", [128, NFC, 512], BF16)
    ost = [P.sb("ost%d" % i, [128, D], F32) for i in range(2)]
    return locals()


def build(L, n_layers=DEPTH, debug=False):
    NT = L // 128
    NB = L // 512
    nc = bass.Bass("TRN2", target_bir_lowering=False)
    P = Prog(nc)
    A = {}

    def din(name, shape, dt=F32):
        A[name] = P.dram(name, shape, dt, kind="ExternalInput")
        return A[name]

    x_in = din("x", [L, D])
    c_in = din("c_col", [128, KD])
    pos_in = din("pos", [128, NT], I32)
    inv_in = din("inv_bc", [128, 16])
    for name, shp in LAYER_PARAMS:
        din(name, [DEPTH] + shp)
    din("ffn_wg", [2, D, 2 * DFE])
    din("ffn_wu", [2, D, 2 * DFE])
    din("ffn_wd", [2, 2 * DFE, D])
    din("moe_wr", [2, D, 8])
    din("moe_br", [2, 1, 8])
    din("moe_wg", [2, 8, D, DFE])
    din("moe_wu", [2, 8, D, DFE])
    din("moe_wd", [2, 8, DFE, D])

    y = P.dram("y", [L, D], F32, kind="ExternalOutput")
    skind = "ExternalOutput" if debug else "Internal"
    QTm = P.dram("QTm", [NH, 96, L], BF16, kind=skind)
    KTm = P.dram("KTm", [NH, 96, L], BF16, kind=skind)
    Vm = P.dram("Vm", [L, NH * 65], BF16, kind=skind)
    QTf = P.dram("QTf", [NH, 68, L], BF16, kind=skind)
    KTf = P.dram("KTf", [NH, 68, L], BF16, kind=skind)
    Vf = P.dram("Vf", [L, NH * 65], BF16, kind=skind)
    mssm = P.dram("mssm", [2, 128, L], BF16, kind=skind)
    h2T_d = P.dram("h2T", [KD, 128, L], BF16, kind=skind)
    g12 = P.dram("g12", [DEPTH, 2, 128, D], F32, kind=skind)
    ytile = [Tl(y.t, "y%d" % t) for t in range(NT)]

    ident = P.sb("ident", [128, 128], BF16)
    identf = P.sb("identf", [128, 128], F32)
    ones_f = P.sb("ones_f", [128, 512], F32)
    ones_b = P.sb("ones_b", [128, 128], BF16)
    for t_, dt_ in ((ident, BF16), (identf, F32)):
        P.op("pool", lambda e, t_=t_: e.memset(t_[:], 1.0), writes=[t_])
        P.op("pool", lambda e, t_=t_: e.affine_select(out=t_[:], in_=t_[:], pattern=[[1, 128]], compare_op=ALU.is_equal,
                                                    fill=0.0, base=0, channel_multiplier=-1), reads=[t_], writes=[t_])
    P.op("pool", lambda e: e.memset(ones_f[:], 1.0), writes=[ones_f])
    P.op("pool", lambda e: e.memset(ones_b[:], 1.0), writes=[ones_b])

    TB = [P.ps("TB%d" % i, [128, 1024], BF16) for i in range(2)]
    FB = [P.ps("FB%d" % i, [128, 512], F32) for i in range(6)]

    def rstd_chain(ss_ap, n, nfeat, tmp, out, reads, eng_r="dve"):
        (tt, tv), (ot, ov) = tmp, out
        P.op("act", lambda e: e.activation(out=tv, in_=ss_ap, func=AF.Sqrt, scale=1.0 / nfeat, bias=eps_c[:, 0:1]),
             reads=list(reads) + [eps_c], writes=[tt])
        P.op("dve", lambda e: e.reciprocal(out=ov, in_=tv), reads=[tt], writes=[ot])

    eps_c = P.sb("eps_c", [128, 1], F32)
    P.op("pool", lambda e: e.memset(eps_c[:], EPS), writes=[eps_c])

    G_bc = P.sb("G_bc", [128, D], F32)
    xs = [P.sb("xs%d" % i, [128, D], F32) for i in range(2)]
    sq_junk = P.sb("sq_junk", [128, D], F32)
    xh = [P.sb("xh%d" % i, [128, D], BF16) for i in range(2)]
    stat = [P.sb("stat%d" % i, [128, 16], F32) for i in range(4)]
    comb_all = P.sb("comb_all", [128, NT, 8], F32)

    modc = P.sb("modc", [128, DEPTH, 4, KD], F32)
    nmc = P.sb("nmc", [128, DEPTH, 2, KD], F32)
    cosT = P.sb("cosT", [128, NT, 16], F32)
    sinT = P.sb("sinT", [128, NT, 16], F32)
    P.push()
    c_col = P.sb("c_colsb", [128, KD], F32)
    P.dma("sp", c_col[:], c_in[:], writes=[c_col])
    c_e = P.sb("c_e", [128, KD], F32)
    c_act = P.sb("c_act", [128, KD], F32)
    P.op("act", lambda e: e.activation(out=c_e[:], in_=c_col[:], func=AF.Exp, scale=-1.0), reads=[c_col], writes=[c_e])
    P.op("dve", lambda e: e.tensor_scalar(out=c_e[:], in0=c_e[:], scalar1=1.0, scalar2=None, op0=ALU.add), reads=[c_e], writes=[c_e])
    P.op("dve", lambda e: e.reciprocal(out=c_e[:], in_=c_e[:]), reads=[c_e], writes=[c_e])
    P.op("dve", lambda e: e.tensor_tensor(out=c_act[:], in0=c_col[:], in1=c_e[:], op=ALU.mult), reads=[c_col, c_e], writes=[c_act])
    C_bc = P.sb("C_bc", [128, KD, 128], F32)
    for k in range(KD):
        P.op("dve", lambda e, k=k: e.tensor_scalar(out=C_bc[:, k, :], in0=ones_f[:, 0:128], scalar1=c_act[:, k:k + 1], scalar2=None,
                                                   op0=ALU.mult), reads=[ones_f, c_act], writes=[C_bc])
    for l in range(n_layers):
        P.dma("sp", nmc[:, l, 0, :], A["norm_mix_c"][l], writes=[nmc])
        P.dma("sp", nmc[:, l, 1, :], A["norm_ffn_c"][l], writes=[nmc])
    wst = [P.sb("wst%d" % i, [128, 2048], F32) for i in range(3)]
    wst_i = [0]

    def next_wst():
        t = wst[wst_i[0] % 3]
        wst_i[0] += 1
        return t
    ada_sb = P.sb("ada_sb", [128, 2048], F32)
    brow = P.sb("brow", [1, 2048], F32)
    for l in range(n_layers):
        for ng in range(3):
            P.dma("sp", brow[:], A["b_ada"][l][:, ng * 2048:(ng + 1) * 2048], writes=[brow])
            for k in range(KD):
                st = next_wst()
                P.dma("sp", st[:], A["w_ada"][l, k * 128:(k + 1) * 128, ng * 2048:(ng + 1) * 2048], writes=[st])
                for j in range(4):
                    P.op("pe", lambda e, st=st, j=j, k=k: e.matmul(FB[j][:], lhsT=C_bc[:, k, :], rhs=st[:, j * 512:(j + 1) * 512],
                                                                 start=(k == 0), stop=False), reads=[st, C_bc], writes=[FB[j]])
            for j in range(4):
                c0 = j * 512
                P.op("pe", lambda e, j=j, c0=c0: e.matmul(FB[j][:], lhsT=ones_f[0:1, 0:128], rhs=brow[0:1, c0:c0 + 512],
                                                        start=False, stop=True), reads=[ones_f, brow], writes=[FB[j]])
                P.op("act", lambda e, j=j: e.activation(out=ada_sb[:, j * 512:(j + 1) * 512], in_=FB[j][:], func=AF.Copy),
                     reads=[FB[j]], writes=[ada_sb])
            for half in range(2):
                seg = 2 * ng + half
                src = ada_sb[:, half * 1024:(half + 1) * 1024]
                if seg in (2, 5):
                    P.dma("sp", g12[l, 0 if seg == 2 else 1], src, reads=[ada_sb], writes=[g12])
                else:
                    v = {0: 1, 1: 0, 3: 3, 4: 2}[seg]
                    for k in range(KD):
                        P.op("pe", lambda e, half=half, k=k: e.transpose(FB[4][:, 0:128], ada_sb[:, half * 1024 + k * 128: half * 1024 + (k + 1) * 128], identf[:]),
                             reads=[ada_sb, identf], writes=[FB[4]])
                        if seg in (1, 4):
                            nm = 0 if seg == 1 else 1
                            P.op("dve", lambda e, l=l, v=v, k=k, nm=nm: e.scalar_tensor_tensor(
                                out=modc[:, l, v, k:k + 1], in0=FB[4][:, 0:1], scalar=1.0, in1=nmc[:, l, nm, k:k + 1],
                                op0=ALU.add, op1=ALU.mult), reads=[FB[4], nmc], writes=[modc])
                        else:
                            P.op("dve", lambda e, l=l, v=v, k=k: e.tensor_copy(out=modc[:, l, v, k:k + 1], in_=FB[4][:, 0:1]),
                                 reads=[FB[4]], writes=[modc])

    posi = P.sb("posi", [128, NT], I32)
    posf = P.sb("posf", [128, NT], F32)
    inv_bc = P.sb("inv_bcs", [128, 16], F32)
    ang = P.sb("ang", [128, NT, 16], F32)
    kk = P.sb("kk", [128, NT, 16], F32)
    P.dma("sp", posi[:], pos_in[:], writes=[posi])
    P.dma("sp", inv_bc[:], inv_in[:], writes=[inv_bc])
    P.op("dve", lambda e: e.tensor_copy(out=posf[:], in_=posi[:]), reads=[posi], writes=[posf])
    for t in range(NT):
        P.op("dve", lambda e, t=t: e.tensor_scalar(out=ang[:, t, :], in0=inv_bc[:], scalar1=posf[:, t:t + 1], scalar2=None, op0=ALU.mult),
             reads=[inv_bc, posf], writes=[ang])

    MAGIC = 12582912.0
    C1 = 6.28125
    C2 = 2.0 * math.pi - C1

    def sin_reduced(dst, src_t, src_v, shape_v, shift, tmp_t):
        tv = tmp_t[:] if shape_v is None else shape_v(tmp_t)
        P.op("dve", lambda e: e.tensor_scalar(out=tv, in0=src_v, scalar1=shift, scalar2=1.0 / (2 * math.pi), op0=ALU.add, op1=ALU.mult),
             reads=[src_t], writes=[tmp_t])
        P.op("dve", lambda e: e.tensor_scalar(out=tv, in0=tv, scalar1=MAGIC, scalar2=None, op0=ALU.add), reads=[tmp_t], writes=[tmp_t])
        P.op("dve", lambda e: e.tensor_scalar(out=tv, in0=tv, scalar1=-MAGIC, scalar2=None, op0=ALU.add), reads=[tmp_t], writes=[tmp_t])
        P.op("dve", lambda e: e.scalar_tensor_tensor(out=dst[0][:] if shape_v is None else shape_v(dst[0]), in0=tv, scalar=-C1, in1=src_v,
                                                     op0=ALU.mult, op1=ALU.add), reads=[tmp_t, src_t], writes=[dst[0]])
        dv = dst[0][:] if shape_v is None else shape_v(dst[0])
        P.op("dve", lambda e: e.scalar_tensor_tensor(out=dv, in0=tv, scalar=-C2, in1=dv, op0=ALU.mult, op1=ALU.add),
             reads=[tmp_t, dst[0]], writes=[dst[0]])
        P.op("dve", lambda e: e.tensor_scalar(out=dv, in0=dv, scalar1=shift, scalar2=3.14159, op0=ALU.add, op1=ALU.min), reads=[dst[0]], writes=[dst[0]])
        P.op("dve", lambda e: e.tensor_scalar(out=dv, in0=dv, scalar1=-3.14159, scalar2=None, op0=ALU.max), reads=[dst[0]], writes=[dst[0]])
        P.op("act", lambda e: e.activation(out=dv, in_=dv, func=AF.Sin), reads=[dst[0]], writes=[dst[0]])

    sin_reduced((sinT,), ang, ang[:], None, 0.0, kk)
    sin_reduced((cosT,), ang, ang[:], None, math.pi / 2, kk)
    onesrow = P.sb("onesrow", [NH, L], BF16)
    P.op("pool", lambda e: e.memset(onesrow[:], 1.0), writes=[onesrow])
    for r_ in (66, 67):
        P.dma("sp", QTf[:, r_, :], onesrow[:], reads=[onesrow], writes=[QTf])
    for r_ in (64, 65):
        P.dma("sp", KTf[:, r_, :], onesrow[:], reads=[onesrow], writes=[KTf])
    P.pop()

    def load_layer(l):
        lp = lambda n: A[n][l]
        P.dma("pool", w_in_sb[:], lp("w_in").rearrange("(k p) n -> p k n", p=128), writes=[w_in_sb])
        P.dma("pool", w_uq_sb[:], lp("w_uq").rearrange("(k p) n -> p k n", p=128), writes=[w_uq_sb])
        P.dma("pool", w_ukv_sb[:], lp("w_ukv"), writes=[w_ukv_sb])
        P.dma("pool", w_glu_sb[:], lp("w_glu").rearrange("(k p) n -> p k n", p=128), writes=[w_glu_sb])
        for t_, n_ in ((qn_c, "q_norm"), (kvn_c, "kv_norm"), (gqm, "gq_m"), (gkm, "gk_m"), (gqf, "gq_f"), (gkf, "gk_f"),
                       (dcol, "ssm_d"), (bglu_c, "b_glu")):
            P.dma("sp", t_[:], lp(n_), writes=[t_])
        P.dma("sp", nbf[:], lp("fox_bf"), writes=[nbf])
        P.op("dve", lambda e: e.tensor_scalar(out=nbf[:], in0=nbf[:], scalar1=-1.0, scalar2=None, op0=ALU.mult), reads=[nbf], writes=[nbf])
        P.op("dve", lambda e: e.tensor_scalar(out=nbglu_c[:], in0=bglu_c[:], scalar1=-1.0, scalar2=None, op0=ALU.mult), reads=[bglu_c], writes=[nbglu_c])
        for m in range(2):
            P.op("dve", lambda e, m=m: e.tensor_scalar(out=Ddiag[:, m, :], in0=identf[:], scalar1=dcol[:, m:m + 1], scalar2=None, op0=ALU.mult),
                 reads=[identf, dcol], writes=[Ddiag])
        V = lambda i: s5p[:, i, :]
        LRE, LIM, LDT, DT, ZRE, ZIM, MAG, SN, CS, LBR, LBI, DEN, KR, KI, NKI, T1, T2, CK, SK, CK2, SK2 = range(21)
        P.dma("sp", V(LRE), lp("lam_re"), writes=[s5p])
        P.dma("sp", V(LIM), lp("lam_im"), writes=[s5p])
        P.dma("sp", V(LDT), lp("log_dt"), writes=[s5p])
        sop = lambda fn: P.op("dve", fn, reads=[s5p], writes=[s5p])
        P.op("act", lambda e: e.activation(out=V(DT), in_=V(LDT), func=AF.Exp), reads=[s5p], writes=[s5p])
        sop(lambda e: e.tensor_tensor(out=V(ZRE), in0=V(LRE), in1=V(DT), op=ALU.mult))
        sop(lambda e: e.tensor_tensor(out=V(ZIM), in0=V(LIM), in1=V(DT), op=ALU.mult))
        P.op("act", lambda e: e.activation(out=V(MAG), in_=V(ZRE), func=AF.Exp), reads=[s5p], writes=[s5p])
        sv = lambda i: (lambda t: t[:, i, :])
        sin_reduced((s5p,), s5p, V(ZIM), sv(SN), 0.0, s5p) if False else None
        for dst_i, shift in ((SN, 0.0), (CS, math.pi / 2)):
            sop(lambda e, shift=shift: e.tensor_scalar(out=V(T1), in0=V(ZIM), scalar1=shift, scalar2=1.0 / (2 * math.pi), op0=ALU.add, op1=ALU.mult))
            sop(lambda e: e.tensor_scalar(out=V(T1), in0=V(T1), scalar1=MAGIC, scalar2=None, op0=ALU.add))
            sop(lambda e: e.tensor_scalar(out=V(T1), in0=V(T1), scalar1=-MAGIC, scalar2=None, op0=ALU.add))
            sop(lambda e, dst_i=dst_i: e.scalar_tensor_tensor(out=V(dst_i), in0=V(T1), scalar=-C1, in1=V(ZIM), op0=ALU.mult, op1=ALU.add))
            sop(lambda e, dst_i=dst_i: e.scalar_tensor_tensor(out=V(dst_i), in0=V(T1), scalar=-C2, in1=V(dst_i), op0=ALU.mult, op1=ALU.add))
            sop(lambda e, dst_i=dst_i, shift=shift: e.tensor_scalar(out=V(dst_i), in0=V(dst_i), scalar1=shift, scalar2=3.14159, op0=ALU.add, op1=ALU.min))
            sop(lambda e, dst_i=dst_i: e.tensor_scalar(out=V(dst_i), in0=V(dst_i), scalar1=-3.14159, scalar2=None, op0=ALU.max))
            P.op("act", lambda e, dst_i=dst_i: e.activation(out=V(dst_i), in_=V(dst_i), func=AF.Sin), reads=[s5p], writes=[s5p])
        sop(lambda e: e.tensor_tensor(out=V(LBR), in0=V(MAG), in1=V(CS), op=ALU.mult))
        sop(lambda e: e.tensor_tensor(out=V(LBI), in0=V(MAG), in1=V(SN), op=ALU.mult))
        sop(lambda e: e.tensor_tensor(out=V(DEN), in0=V(LRE), in1=V(LRE), op=ALU.mult))
        sop(lambda e: e.tensor_tensor(out=V(T1), in0=V(LIM), in1=V(LIM), op=ALU.mult))
        sop(lambda e: e.tensor_tensor(out=V(DEN), in0=V(DEN), in1=V(T1), op=ALU.add))
        sop(lambda e: e.reciprocal(out=V(DEN), in_=V(DEN)))
        sop(lambda e: e.tensor_scalar(out=V(T2), in0=V(LBR), scalar1=-1.0, scalar2=None, op0=ALU.add))
        sop(lambda e: e.tensor_tensor(out=V(KR), in0=V(T2), in1=V(LRE), op=ALU.mult))
        sop(lambda e: e.tensor_tensor(out=V(T1), in0=V(LBI), in1=V(LIM), op=ALU.mult))
        sop(lambda e: e.tensor_tensor(out=V(KR), in0=V(KR), in1=V(T1), op=ALU.add))
        sop(lambda e: e.tensor_tensor(out=V(KR), in0=V(KR), in1=V(DEN), op=ALU.mult))
        sop(lambda e: e.tensor_tensor(out=V(KI), in0=V(LBI), in1=V(LRE), op=ALU.mult))
        sop(lambda e: e.tensor_tensor(out=V(T1), in0=V(T2), in1=V(LIM), op=ALU.mult))
        sop(lambda e: e.tensor_tensor(out=V(KI), in0=V(KI), in1=V(T1), op=ALU.subtract))
        sop(lambda e: e.tensor_tensor(out=V(KI), in0=V(KI), in1=V(DEN), op=ALU.mult))
        sop(lambda e: e.tensor_scalar(out=V(NKI), in0=V(KI), scalar1=-1.0, scalar2=None, op0=ALU.mult))
        for j in range(8):
            P.dma("sp", blk_f[:, 0, :], lp("b_re")[j], writes=[blk_f])
            P.dma("sp", blk_f[:, 1, :], lp("b_im")[j], writes=[blk_f])
            P.op("dve", lambda e, j=j: e.tensor_scalar(out=blk_o[:, 0, :], in0=blk_f[:, 0, :], scalar1=s5p[:, KR, j:j + 1], scalar2=None, op0=ALU.mult),
                 reads=[blk_f, s5p], writes=[blk_o])
            P.op("dve", lambda e, j=j: e.scalar_tensor_tensor(out=blk_o[:, 0, :], in0=blk_f[:, 1, :], scalar=s5p[:, NKI, j:j + 1], in1=blk_o[:, 0, :],
                                                              op0=ALU.mult, op1=ALU.add), reads=[blk_f, s5p, blk_o], writes=[blk_o])
            P.op("dve", lambda e, j=j: e.tensor_scalar(out=blk_o[:, 1, :], in0=blk_f[:, 1, :], scalar1=s5p[:, KR, j:j + 1], scalar2=None, op0=ALU.mult),
                 reads=[blk_f, s5p], writes=[blk_o])
            P.op("dve", lambda e, j=j: e.scalar_tensor_tensor(out=blk_o[:, 1, :], in0=blk_f[:, 0, :], scalar=s5p[:, KI, j:j + 1], in1=blk_o[:, 1, :],
                                                              op0=ALU.mult, op1=ALU.add), reads=[blk_f, s5p, blk_o], writes=[blk_o])
            for ri, dstT in ((0, BreT), (1, BimT)):
                P.op("pe", lambda e, ri=ri: e.transpose(FB[5][:, 0:128], blk_o[:, ri, :], identf[:]), reads=[blk_o, identf], writes=[FB[5]])
                P.op("act", lambda e, dstT=dstT, j=j: e.activation(out=dstT[:, j, :], in_=FB[5][:, 0:128], func=AF.Copy), reads=[FB[5]], writes=[dstT])
        P.dma("pool", CreT[:], lp("c_re").rearrange("j p f -> p j f"), writes=[CreT])
        P.dma("pool", nCimT[:], lp("c_im").rearrange("j p f -> p j f"), writes=[nCimT])
        P.op("dve", lambda e: e.tensor_scalar(out=nCimT[:], in0=nCimT[:], scalar1=-1.0, scalar2=None, op0=ALU.mult), reads=[nCimT], writes=[nCimT])
        P.op("dve", lambda e: e.memset(Ctab[:, :, 0:1], 1.0), writes=[Ctab])
        P.op("dve", lambda e: e.memset(Stab[:, :, 0:1], 0.0), writes=[Stab])
        sop(lambda e: e.tensor_copy(out=V(CK), in_=V(CS)))
        sop(lambda e: e.tensor_copy(out=V(SK), in_=V(SN)))
        n = 1
        while n < SUB:
            tabt_v = pt[0][:].rearrange("p a b -> p (a b)")[:, 0:8 * n].rearrange("p (j n) -> p j n", j=8)
            tabu_v = pt[1][:].rearrange("p a b -> p (a b)")[:, 0:8 * n].rearrange("p (j n) -> p j n", j=8)
            tabt = pt[0]
            tabu = pt[1]
            ckb = s5p[:, CK, :].unsqueeze(2).to_broadcast([128, 8, n])
            skb = s5p[:, SK, :].unsqueeze(2).to_broadcast([128, 8, n])
            P.op("dve", lambda e, n=n, ckb=ckb: e.tensor_tensor(out=tabt_v, in0=Ctab[:, :, 0:n], in1=ckb, op=ALU.mult), reads=[Ctab, s5p], writes=[tabt])
            P.op("dve", lambda e, n=n, skb=skb: e.tensor_tensor(out=tabu_v, in0=Stab[:, :, 0:n], in1=skb, op=ALU.mult), reads=[Stab, s5p], writes=[tabu])
            P.op("dve", lambda e, n=n: e.tensor_tensor(out=Ctab[:, :, n:2 * n], in0=tabt_v, in1=tabu_v, op=ALU.subtract), reads=[tabt, tabu], writes=[Ctab])
            P.op("dve", lambda e, n=n, skb=skb: e.tensor_tensor(out=tabt_v, in0=Ctab[:, :, 0:n], in1=skb, op=ALU.mult), reads=[Ctab, s5p], writes=[tabt])
            P.op("dve", lambda e, n=n, ckb=ckb: e.tensor_tensor(out=tabu_v, in0=Stab[:, :, 0:n], in1=ckb, op=ALU.mult), reads=[Stab, s5p], writes=[tabu])
            P.op("dve", lambda e, n=n: e.tensor_tensor(out=Stab[:, :, n:2 * n], in0=tabt_v, in1=tabu_v, op=ALU.add), reads=[tabt, tabu], writes=[Stab])
            sop(lambda e: e.tensor_tensor(out=V(T1), in0=V(CK), in1=V(CK), op=ALU.mult))
            sop(lambda e: e.tensor_tensor(out=V(T2), in0=V(SK), in1=V(SK), op=ALU.mult))
            sop(lambda e: e.tensor_tensor(out=V(SK2), in0=V(CK), in1=V(SK), op=ALU.mult))
            sop(lambda e: e.tensor_tensor(out=V(CK), in0=V(T1), in1=V(T2), op=ALU.subtract))
            sop(lambda e: e.tensor_scalar(out=V(SK), in0=V(SK2), scalar1=2.0, scalar2=None, op0=ALU.mult))
            n *= 2
        P.op("dve", lambda e: e.tensor_copy(out=Rtab[:], in_=s5p[:, MAG, :].unsqueeze(2).to_broadcast([128, 8, SUB])), reads=[s5p], writes=[Rtab])
        return dict(CK=CK, SK=SK)

    def load_c(l):
        P.dma("sp", onorm_c[:], A["out_norm"][l], writes=[onorm_c])
        P.dma("sp", G_bc[:], g12[l, 0], reads=[g12], writes=[G_bc])
        for k in range(KD):
            st = wstc[k % 2]
            P.dma("sp", st[:], A["w_out"][l][k * 128:(k + 1) * 128, :], writes=[st])
            P.op("dve", lambda e, st=st, k=k: e.scalar_tensor_tensor(out=w_out_sb[:, k, :], in0=st[:], scalar=onorm_c[:, k:k + 1], in1=G_bc[:],
                                                                     op0=ALU.mult, op1=ALU.mult), reads=[st, onorm_c, G_bc], writes=[w_out_sb])

    def norm_transpose(src_t, src_v, l, v_a, v_b, dst_t, dst_slice, si, fp32_path=None):
        st_ = stat[si % 4]
        P.op("act", lambda e: e.activation(out=sq_junk[:], in_=src_v, func=AF.Square, accum_out=st_[:, 0:1]), reads=[src_t], writes=[sq_junk, st_])
        P.op("act", lambda e: e.activation(out=st_[:, 1:2], in_=st_[:, 0:1], func=AF.Sqrt, scale=1.0 / D, bias=eps_c[:, 0:1]), reads=[st_, eps_c], writes=[st_])
        P.op("dve", lambda e: e.reciprocal(out=st_[:, 2:3], in_=st_[:, 1:2]), reads=[st_], writes=[st_])
        xh_ = xh[si % 2]
        P.op("dve", lambda e: e.tensor_scalar(out=xh_[:], in0=src_v, scalar1=st_[:, 2:3], scalar2=None, op0=ALU.mult), reads=[src_t, st_], writes=[xh_])
        tb = TB[si % 2]
        for k in range(KD):
            P.op("pe", lambda e, k=k: e.transpose(tb[:, k * 128:(k + 1) * 128], xh_[:, k * 128:(k + 1) * 128], ident[:]), reads=[xh_, ident], writes=[tb])
        for k in range(KD):
            P.op("act", lambda e, k=k: e.activation(out=dst_t[:, k, dst_slice], in_=tb[:, k * 128:(k + 1) * 128], func=AF.Identity,
                                                    scale=modc[:, l, v_a, k:k + 1], bias=modc[:, l, v_b, k:k + 1]), reads=[tb, modc], writes=[dst_t])
        if fp32_path is not None:
            xhf_, dstf = fp32_path
            P.op("dve", lambda e: e.tensor_scalar(out=xhf_[:], in0=src_v, scalar1=st_[:, 2:3], scalar2=None, op0=ALU.mult), reads=[src_t, st_], writes=[xhf_])
            for k in range(KD):
                fb = FB[k % 2]
                P.op("pe", lambda e, k=k, fb=fb: e.transpose(fb[:, 0:128], xhf_[:, k * 128:(k + 1) * 128], identf[:]), reads=[xhf_, identf], writes=[fb])
                P.op("act", lambda e, k=k, fb=fb: e.activation(out=dstf[:, k, :], in_=fb[:, 0:128], func=AF.Identity,
                                                             scale=modc[:, l, v_a, k:k + 1], bias=modc[:, l, v_b, k:k + 1]), reads=[fb, modc], writes=[dstf])

    def head_rms(src_t, src_v3, nh, hd, gain_t, dst_t, dst_v3, si, extra_ss=None):
        st_ = stat[si % 4]
        P.op("act", lambda e: e.activation(out=sq_junk[:, 0:nh * hd].rearrange("p (h d) -> p h d", h=nh), in_=src_v3, func=AF.Square), reads=[src_t], writes=[sq_junk])
        P.op("dve", lambda e: e.tensor_reduce(out=st_[:, 0:nh], in_=sq_junk[:, 0:nh * hd].rearrange("p (h d) -> p h d", h=nh), axis=AX.X, op=ALU.add),
             reads=[sq_junk], writes=[st_])
        tot = hd
        if extra_ss is not None:
            et, ev, en = extra_ss
            P.op("dve", lambda e: e.tensor_scalar(out=st_[:, 0:nh], in0=st_[:, 0:nh], scalar1=ev, scalar2=None, op0=ALU.add), reads=[st_, et], writes=[st_])
            tot = hd + en
        P.op("act", lambda e: e.activation(out=st_[:, 6:6 + nh], in_=st_[:, 0:nh], func=AF.Sqrt, scale=1.0 / tot, bias=eps_c[:, 0:1]), reads=[st_, eps_c], writes=[st_])
        P.op("dve", lambda e: e.reciprocal(out=st_[:, 6:6 + nh], in_=st_[:, 6:6 + nh]), reads=[st_], writes=[st_])
        return st_

    def rope(src_t, dst_t, cos_v, sin_v):
        x1 = src_t[:, :, 64:80]
        x2 = src_t[:, :, 80:96]
        cb = cos_v.unsqueeze(1).to_broadcast([128, NH, 16])
        sb_ = sin_v.unsqueeze(1).to_broadcast([128, NH, 16])
        P.op("dve", lambda e: e.tensor_copy(out=dst_t[:, :, 0:64], in_=src_t[:, :, 0:64]), reads=[src_t], writes=[dst_t])
        P.op("dve", lambda e: e.tensor_tensor(out=rt[0][:], in0=x1, in1=cb, op=ALU.mult), reads=[src_t, cosT], writes=[rt[0]])
        P.op("dve", lambda e: e.tensor_tensor(out=rt[1][:], in0=x2, in1=sb_, op=ALU.mult), reads=[src_t, sinT], writes=[rt[1]])
        P.op("dve", lambda e: e.tensor_tensor(out=dst_t[:, :, 64:80], in0=rt[0][:], in1=rt[1][:], op=ALU.subtract), reads=[rt[0], rt[1]], writes=[dst_t])
        P.op("dve", lambda e: e.tensor_tensor(out=rt[2][:], in0=x1, in1=sb_, op=ALU.mult), reads=[src_t, sinT], writes=[rt[2]])
        P.op("dve", lambda e: e.tensor_tensor(out=rt[3][:], in0=x2, in1=cb, op=ALU.mult), reads=[src_t, cosT], writes=[rt[3]])
        P.op("dve", lambda e: e.tensor_tensor(out=dst_t[:, :, 80:96], in0=rt[2][:], in1=rt[3][:], op=ALU.add), reads=[rt[2], rt[3]], writes=[dst_t])

    def phase_a(l, s5c):
        src = x_in if l == 0 else None
        for b in range(NB):
            hTb = hT[b % 2]
            for ti in range(4):
                t = b * 4 + ti
                xs_ = xs[t % 2]
                if l == 0:
                    P.dma("sp", xs_[:], x_in[t * 128:(t + 1) * 128, :], writes=[xs_])
                else:
                    P.dma("sp", xs_[:], y[t * 128:(t + 1) * 128, :], reads=[ytile[t]], writes=[xs_])
                norm_transpose(xs_, xs_[:], l, 0, 1, hTb, slice(ti * 128, (ti + 1) * 128), t)
            for m in range(2):
                for k in range(KD):
                    P.op("pe", lambda e, m=m, k=k: e.matmul(FB[m][:], lhsT=w_in_sb[:, k, m * 128:(m + 1) * 128], rhs=hTb[:, k, :], start=(k == 0), stop=(k == KD - 1)),
                         reads=[w_in_sb, hTb], writes=[FB[m]])
                P.op("act", lambda e, m=m: e.activation(out=uT[:, m, :], in_=FB[m][:], func=AF.Copy), reads=[FB[m]], writes=[uT])
            for k in range(KD):
                P.op("pe", lambda e, k=k: e.matmul(FB[2][0:NH, :], lhsT=w_in_sb[:, k, 1824:1830], rhs=hTb[:, k, :], start=(k == 0), stop=(k == KD - 1)),
                     reads=[w_in_sb, hTb], writes=[FB[2]])
            P.op("act", lambda e: e.activation(out=fg_e[:], in_=FB[2][0:NH, :], func=AF.Exp, scale=-1.0, bias=nbf[:, 0:1]), reads=[FB[2], nbf], writes=[fg_e])
            P.op("act", lambda e: e.activation(out=fg_sp[:], in_=fg_e[:], func=AF.Ln, bias=ones_f[0:NH, 0:1]), reads=[fg_e, ones_f], writes=[fg_sp])
            if b == 0:
                P.op("dve", lambda e: e.tensor_tensor_scan(out=fg_cum[:], data0=ones_f[0:NH, 0:512], data1=fg_sp[:], initial=0.0, op0=ALU.mult, op1=ALU.add),
                     reads=[ones_f, fg_sp], writes=[fg_cum])
            else:
                P.op("dve", lambda e: e.tensor_tensor_scan(out=fg_cum[:], data0=ones_f[0:NH, 0:512], data1=fg_sp[:], initial=fg_carry[:, 0:1], op0=ALU.mult, op1=ALU.add),
                     reads=[ones_f, fg_sp, fg_carry], writes=[fg_cum])
            P.op("dve", lambda e: e.tensor_copy(out=fg_carry[:], in_=fg_cum[:, 511:512]), reads=[fg_cum], writes=[fg_carry])
            P.op("dve", lambda e: e.tensor_scalar(out=fg_sp[:], in0=fg_cum[:], scalar1=8.0, scalar2=None, op0=ALU.mult), reads=[fg_cum], writes=[fg_sp])
            P.op("dve", lambda e: e.tensor_copy(out=fg_hi[:], in_=fg_sp[:]), reads=[fg_sp], writes=[fg_hi])
            P.op("dve", lambda e: e.tensor_tensor(out=fg_lo[:], in0=fg_sp[:], in1=fg_hi[:], op=ALU.subtract), reads=[fg_sp, fg_hi], writes=[fg_lo])
            P.op("dve", lambda e: e.tensor_scalar(out=fg_nhi[:], in0=fg_hi[:], scalar1=-1.0, scalar2=None, op0=ALU.mult), reads=[fg_hi], writes=[fg_nhi])
            P.op("dve", lambda e: e.tensor_scalar(out=fg_nlo[:], in0=fg_lo[:], scalar1=-1.0, scalar2=None, op0=ALU.mult), reads=[fg_lo], writes=[fg_nlo])
            bs = slice(b * 512, (b + 1) * 512)
            P.dma("sp", QTf[:, 64, bs], fg_nhi[:], reads=[fg_nhi], writes=[QTf])
            P.dma("sp", QTf[:, 65, bs], fg_nlo[:], reads=[fg_nlo], writes=[QTf])
            P.dma("sp", KTf[:, 66, bs], fg_hi[:], reads=[fg_hi], writes=[KTf])
            P.dma("sp", KTf[:, 67, bs], fg_lo[:], reads=[fg_lo], writes=[KTf])

            for ti in range(4):
                t = b * 4 + ti
                tsl = slice(ti * 128, (ti + 1) * 128)
                segs = ((FB[2], 256, 416), (FB[3], 672, 384), (FB[4], 1056, 384), (FB[5], 1440, 384))
                for fb, c0, w in segs:
                    for k in range(KD):
                        P.op("pe", lambda e, fb=fb, c0=c0, w=w, k=k: e.matmul(fb[:, 0:w], lhsT=hTb[:, k, tsl], rhs=w_in_sb[:, k, c0:c0 + w], start=(k == 0), stop=(k == KD - 1)),
                             reads=[hTb, w_in_sb], writes=[fb])
                st_ = stat[0]
                P.op("act", lambda e: e.activation(out=sq_junk[:, 0:256], in_=FB[2][:, 0:256], func=AF.Square, accum_out=st_[:, 12:13]), reads=[FB[2]], writes=[sq_junk, st_])
                P.op("act", lambda e: e.activation(out=st_[:, 13:14], in_=st_[:, 12:13], func=AF.Sqrt, scale=1.0 / 256, bias=eps_c[:, 0:1]), reads=[st_, eps_c], writes=[st_])
                P.op("dve", lambda e: e.reciprocal(out=st_[:, 13:14], in_=st_[:, 13:14]), reads=[st_], writes=[st_])
                P.op("dve", lambda e: e.tensor_scalar(out=cq_h[:], in0=FB[2][:, 0:256], scalar1=st_[:, 13:14], scalar2=None, op0=ALU.mult), reads=[FB[2], st_], writes=[cq_h])
                st1 = stat[1]
                P.op("act", lambda e: e.activation(out=sq_junk[:, 256:384], in_=FB[2][:, 256:384], func=AF.Square, accum_out=st1[:, 12:13]), reads=[FB[2]], writes=[sq_junk, st1])
                P.op("act", lambda e: e.activation(out=st1[:, 13:14], in_=st1[:, 12:13], func=AF.Sqrt, scale=1.0 / 128, bias=eps_c[:, 0:1]), reads=[st1, eps_c], writes=[st1])
                P.op("dve", lambda e: e.reciprocal(out=st1[:, 13:14], in_=st1[:, 13:14]), reads=[st1], writes=[st1])
                P.op("dve", lambda e: e.tensor_scalar(out=ckv_h[:], in0=FB[2][:, 256:384], scalar1=st1[:, 13:14], scalar2=None, op0=ALU.mult), reads=[FB[2], st1], writes=[ckv_h])
                P.op("act", lambda e: e.activation(out=kn[:, 0, 64:96], in_=FB[2][:, 384:416], func=AF.Copy), reads=[FB[2]], writes=[kn])
                P.op("act", lambda e: e.activation(out=sq_junk[:, 384:416], in_=FB[2][:, 384:416], func=AF.Square, accum_out=st1[:, 14:15]), reads=[FB[2]], writes=[sq_junk, st1])
                tb = TB[0]
                for j in range(2):
                    P.op("pe", lambda e, j=j: e.transpose(tb[:, j * 128:(j + 1) * 128], cq_h[:, j * 128:(j + 1) * 128], ident[:]), reads=[cq_h, ident], writes=[tb])
                P.op("pe", lambda e: e.transpose(tb[:, 256:384], ckv_h[:], ident[:]), reads=[ckv_h, ident], writes=[tb])
                for j in range(2):
                    P.op("act", lambda e, j=j: e.activation(out=cqT[:, j, :], in_=tb[:, j * 128:(j + 1) * 128], func=AF.Copy, scale=qn_c[:, j:j + 1]), reads=[tb, qn_c], writes=[cqT])
                P.op("act", lambda e: e.activation(out=ckvT[:], in_=tb[:, 256:384], func=AF.Copy, scale=kvn_c[:, 0:1]), reads=[tb, kvn_c], writes=[ckvT])
                for j in range(2):
                    P.op("pe", lambda e, j=j: e.matmul(FB[0][:], lhsT=cqT[:, j, :], rhs=w_uq_sb[:, j, 0:512], start=(j == 0), stop=(j == 1)), reads=[cqT, w_uq_sb], writes=[FB[0]])
                for j in range(2):
                    P.op("pe", lambda e, j=j: e.matmul(FB[1][:, 0:64], lhsT=cqT[:, j, :], rhs=w_uq_sb[:, j, 512:576], start=(j == 0), stop=(j == 1)), reads=[cqT, w_uq_sb], writes=[FB[1]])
                P.op("act", lambda e: e.activation(out=qn[:].rearrange("p h d -> p (h d)")[:, 0:512], in_=FB[0][:], func=AF.Copy), reads=[FB[0]], writes=[qn])
                P.op("act", lambda e: e.activation(out=qn[:].rearrange("p h d -> p (h d)")[:, 512:576], in_=FB[1][:, 0:64], func=AF.Copy), reads=[FB[1]], writes=[qn])
                sq = head_rms(qn, qn[:], NH, 96, gqm, None, None, 2)
                P.op("dve", lambda e, sq=sq: e.tensor_tensor(out=qn[:], in0=qn[:], in1=sq[:, 6:12].unsqueeze(2).to_broadcast([128, NH, 96]), op=ALU.mult), reads=[qn, sq], writes=[qn])
                P.op("dve", lambda e: e.tensor_tensor(out=qn[:], in0=qn[:], in1=gqm[:].unsqueeze(1).to_broadcast([128, NH, 96]), op=ALU.mult), reads=[qn, gqm], writes=[qn])
                rope(qn, qfin, cosT[:, t, :], sinT[:, t, :])
                P.op("pe", lambda e: e.matmul(FB[0][:], lhsT=ckvT[:], rhs=w_ukv_sb[:, 0:512], start=True, stop=True), reads=[ckvT, w_ukv_sb], writes=[FB[0]])
                P.op("pe", lambda e: e.matmul(FB[1][:, 0:256], lhsT=ckvT[:], rhs=w_ukv_sb[:, 512:768], start=True, stop=True), reads=[ckvT, w_ukv_sb], writes=[FB[1]])
                vst = Vm_st[t % 2]
                kv0 = FB[0][:].rearrange("p (h d) -> p h d", h=4)
                kv1 = FB[1][:, 0:256].rearrange("p (h d) -> p h d", h=2)
                P.op("act", lambda e: e.activation(out=kn[:, 0:4, 0:64], in_=kv0[:, :, 0:64], func=AF.Copy), reads=[FB[0]], writes=[kn])
                P.op("act", lambda e: e.activation(out=kn[:, 4:6, 0:64], in_=kv1[:, :, 0:64], func=AF.Copy), reads=[FB[1]], writes=[kn])
                P.op("dve", lambda e: e.tensor_copy(out=vst[:, 0:4, 0:64], in_=kv0[:, :, 64:128]), reads=[FB[0]], writes=[vst])
                P.op("dve", lambda e: e.tensor_copy(out=vst[:, 4:6, 0:64], in_=kv1[:, :, 64:128]), reads=[FB[1]], writes=[vst])
                P.dma("sp", Vm[t * 128:(t + 1) * 128, :], vst[:].rearrange("p h d -> p (h d)"), reads=[vst], writes=[Vm])
                P.op("dve", lambda e: e.tensor_copy(out=kn[:, 1:6, 64:96], in_=kn[:, 0:1, 64:96].to_broadcast([128, 5, 32])), reads=[kn], writes=[kn])
                st3 = stat[3]
                P.op("act", lambda e: e.activation(out=sq_junk[:, 0:384].rearrange("p (h d) -> p h d", h=NH), in_=kn[:, :, 0:64], func=AF.Square), reads=[kn], writes=[sq_junk])
                P.op("dve", lambda e: e.tensor_reduce(out=st3[:, 0:NH], in_=sq_junk[:, 0:384].rearrange("p (h d) -> p h d", h=NH), axis=AX.X, op=ALU.add), reads=[sq_junk], writes=[st3])
                P.op("dve", lambda e: e.tensor_scalar(out=st3[:, 0:NH], in0=st3[:, 0:NH], scalar1=st1[:, 14:15], scalar2=None, op0=ALU.add), reads=[st3, st1], writes=[st3])
                P.op("act", lambda e: e.activation(out=st3[:, 6:12], in_=st3[:, 0:NH], func=AF.Sqrt, scale=1.0 / 96, bias=eps_c[:, 0:1]), reads=[st3, eps_c], writes=[st3])
                P.op("dve", lambda e: e.reciprocal(out=st3[:, 6:12], in_=st3[:, 6:12]), reads=[st3], writes=[st3])
                P.op("dve", lambda e: e.tensor_tensor(out=kn[:], in0=kn[:], in1=st3[:, 6:12].unsqueeze(2).to_broadcast([128, NH, 96]), op=ALU.mult), reads=[kn, st3], writes=[kn])
                P.op("dve", lambda e: e.tensor_tensor(out=kn[:], in0=kn[:], in1=gkm[:].unsqueeze(1).to_broadcast([128, NH, 96]), op=ALU.mult), reads=[kn, gkm], writes=[kn])
                rope(kn, kfin, cosT[:, t, :], sinT[:, t, :])
                gsl = slice(t * 128, (t + 1) * 128)
                for src_, st_l, dst_d in ((qfin, QTm_st, QTm), (kfin, KTm_st, KTm)):
                    tb2 = TB[1]
                    st_t = st_l[t % 2]
                    for h in range(NH):
                        P.op("pe", lambda e, h=h, src_=src_: e.transpose(tb2[0:96, h * 128:(h + 1) * 128], src_[:, h, :], ident[:]), reads=[src_, ident], writes=[tb2])
                    P.op("act", lambda e, st_t=st_t: e.activation(out=st_t[:], in_=tb2[0:96, 0:768].rearrange("p (h t) -> p h t", h=NH), func=AF.Copy), reads=[tb2], writes=[st_t])
                    P.dma("sp", dst_d[:, :, gsl].rearrange("h p t -> p h t"), st_t[:], reads=[st_t], writes=[dst_d])
                for fb, g_t, dstb, st_l, dst_d in ((FB[3], gqf, fqb, QTf_st, QTf), (FB[4], gkf, fkb, KTf_st, KTf)):
                    st_t = st_l[t % 2]
                    P.op("act", lambda e, fb=fb: e.activation(out=fqn[:].rearrange("p h d -> p (h d)"), in_=fb[:, 0:384], func=AF.Copy), reads=[fb], writes=[fqn])
                    sq = head_rms(fqn, fqn[:], NH, 64, g_t, None, None, 2)
                    P.op("dve", lambda e, sq=sq: e.tensor_tensor(out=fqn[:], in0=fqn[:], in1=sq[:, 6:12].unsqueeze(2).to_broadcast([128, NH, 64]), op=ALU.mult), reads=[fqn, sq], writes=[fqn])
                    P.op("dve", lambda e, g_t=g_t, dstb=dstb: e.tensor_tensor(out=dstb[:], in0=fqn[:], in1=g_t[:].unsqueeze(1).to_broadcast([128, NH, 64]), op=ALU.mult), reads=[fqn, g_t], writes=[dstb])
                    tb2 = TB[1]
                    for h in range(NH):
                        P.op("pe", lambda e, h=h, dstb=dstb: e.transpose(tb2[0:64, h * 128:(h + 1) * 128], dstb[:, h, :], ident[:]), reads=[dstb, ident], writes=[tb2])
                    P.op("act", lambda e, st_t=st_t: e.activation(out=st_t[:], in_=tb2[0:64, 0:768].rearrange("p (h t) -> p h t", h=NH), func=AF.Copy), reads=[tb2], writes=[st_t])
                    P.dma("sp", dst_d[:, 0:64, gsl].rearrange("h p t -> p h t"), st_t[:], reads=[st_t], writes=[dst_d])
                vst = Vf_st[t % 2]
                P.op("dve", lambda e, vst=vst: e.tensor_copy(out=vst[:, :, 0:64], in_=FB[5][:, 0:384].rearrange("p (h d) -> p h d", h=NH)), reads=[FB[5]], writes=[vst])
                P.dma("sp", Vf[t * 128:(t + 1) * 128, :], vst[:].rearrange("p h d -> p (h d)"), reads=[vst], writes=[Vf])
            for sc in range(512 // SUB):
                first = (b == 0 and sc == 0)
                ss_ = slice(sc * SUB, (sc + 1) * SUB)
                gs = slice(b * 512 + sc * SUB, b * 512 + (sc + 1) * SUB)
                if not first:
                    wl_re = wlast[:, 0, :]
                    wl_im = wlast[:, 1, :]
                    ck = s5p[:, s5c["CK"], :]
                    sk = s5p[:, s5c["SK"], :]
                    P.op("dve", lambda e: e.tensor_tensor(out=w0t[:, 0, :], in0=wl_re, in1=ck, op=ALU.mult), reads=[wlast, s5p], writes=[w0t])
                    P.op("dve", lambda e: e.tensor_tensor(out=w0t[:, 1, :], in0=wl_im, in1=sk, op=ALU.mult), reads=[wlast, s5p], writes=[w0t])
                    P.op("dve", lambda e: e.tensor_tensor(out=w0t[:, 2, :], in0=wl_re, in1=sk, op=ALU.mult), reads=[wlast, s5p], writes=[w0t])
                    P.op("dve", lambda e: e.tensor_tensor(out=w0t[:, 3, :], in0=wl_im, in1=ck, op=ALU.mult), reads=[wlast, s5p], writes=[w0t])
                    P.op("dve", lambda e: e.tensor_tensor(out=w0[:, 0, :], in0=w0t[:, 0, :], in1=w0t[:, 1, :], op=ALU.subtract), reads=[w0t], writes=[w0])
                    P.op("dve", lambda e: e.tensor_tensor(out=w0[:, 1, :], in0=w0t[:, 2, :], in1=w0t[:, 3, :], op=ALU.add), reads=[w0t], writes=[w0])
                for m in range(2):
                    hs = slice(4 * m, 4 * m + 4)
                    for jj in range(4):
                        j = 4 * m + jj
                        fbp = FB[j % 2]
                        P.op("pe", lambda e: e.matmul(fbp[:, 0:SUB], lhsT=BreT[:, j, :], rhs=uT[:, m, ss_], start=True, stop=True), reads=[BreT, uT], writes=[fbp])
                        P.op("pe", lambda e: e.matmul(fbp[:, SUB:2 * SUB], lhsT=BimT[:, j, :], rhs=uT[:, m, ss_], start=True, stop=True), reads=[BimT, uT], writes=[fbp])
                        b2 = fbp[:, 0:2 * SUB].rearrange("p (r t) -> p r t", r=2)
                        cb = Ctab[:, j, :].unsqueeze(1).to_broadcast([128, 2, SUB])
                        sb_ = Stab[:, j, :].unsqueeze(1).to_broadcast([128, 2, SUB])
                        P.op("dve", lambda e: e.tensor_tensor(out=pre_c[:], in0=b2, in1=cb, op=ALU.mult), reads=[fbp, Ctab], writes=[pre_c])
                        P.op("dve", lambda e: e.tensor_tensor(out=pre_s[:], in0=b2, in1=sb_, op=ALU.mult), reads=[fbp, Stab], writes=[pre_s])
                        P.op("dve", lambda e: e.tensor_tensor(out=pin_re[:], in0=pre_c[:, 0, :], in1=pre_s[:, 1, :], op=ALU.add), reads=[pre_c, pre_s], writes=[pin_re])
                        P.op("dve", lambda e: e.tensor_tensor(out=pin_im[:], in0=pre_c[:, 1, :], in1=pre_s[:, 0, :], op=ALU.subtract), reads=[pre_c, pre_s], writes=[pin_im])
                        for pin, Wt, ri in ((pin_re, W_re, 0), (pin_im, W_im, 1)):
                            if first:
                                P.op("dve", lambda e: e.tensor_tensor_scan(out=Wt[:, jj, :], data0=Rtab[:, j, :], data1=pin[:], initial=0.0, op0=ALU.mult, op1=ALU.add),
                                     reads=[Rtab, pin], writes=[Wt])
                            else:
                                P.op("dve", lambda e: e.tensor_tensor_scan(out=Wt[:, jj, :], data0=Rtab[:, j, :], data1=pin[:], initial=w0[:, ri, j:j + 1],
                                                                           op0=ALU.mult, op1=ALU.add), reads=[Rtab, pin, w0], writes=[Wt])
                    P.op("dve", lambda e: e.tensor_copy(out=wlast[:, 0, hs], in_=W_re[:, :, SUB - 1]), reads=[W_re], writes=[wlast])
                    P.op("dve", lambda e: e.tensor_copy(out=wlast[:, 1, hs], in_=W_im[:, :, SUB - 1]), reads=[W_im], writes=[wlast])
                    Ch = Ctab[:, hs, :]
                    Sh = Stab[:, hs, :]
                    P.op("dve", lambda e: e.tensor_tensor(out=pt[0][:], in0=W_re[:], in1=Ch, op=ALU.mult), reads=[W_re, Ctab], writes=[pt[0]])
                    P.op("dve", lambda e: e.tensor_tensor(out=pt[1][:], in0=W_im[:], in1=Sh, op=ALU.mult), reads=[W_im, Stab], writes=[pt[1]])
                    P.op("dve", lambda e: e.tensor_tensor(out=s_re[:], in0=pt[0][:], in1=pt[1][:], op=ALU.subtract), reads=[pt[0], pt[1]], writes=[s_re])
                    P.op("dve", lambda e: e.tensor_tensor(out=pt[0][:], in0=W_re[:], in1=Sh, op=ALU.mult), reads=[W_re, Stab], writes=[pt[0]])
                    P.op("dve", lambda e: e.tensor_tensor(out=pt[1][:], in0=W_im[:], in1=Ch, op=ALU.mult), reads=[W_im, Stab], writes=[pt[1]])
                    P.op("dve", lambda e: e.tensor_tensor(out=s_im[:], in0=pt[0][:], in1=pt[1][:], op=ALU.add), reads=[pt[0], pt[1]], writes=[s_im])
                    osl = slice(m * SUB, (m + 1) * SUB)
                    for jj in range(4):
                        j = 4 * m + jj
                        P.op("pe", lambda e: e.matmul(FB[2][:, osl], lhsT=CreT[:, j, :], rhs=s_re[:, jj, :], start=(jj == 0), stop=False), reads=[CreT, s_re], writes=[FB[2]])
                        P.op("pe", lambda e: e.matmul(FB[2][:, osl], lhsT=nCimT[:, j, :], rhs=s_im[:, jj, :], start=False, stop=False), reads=[nCimT, s_im], writes=[FB[2]])
                    P.op("pe", lambda e: e.matmul(FB[2][:, osl], lhsT=Ddiag[:, m, :], rhs=uT[:, m, ss_], start=False, stop=True), reads=[Ddiag, uT], writes=[FB[2]])
                yv = FB[2][:, 0:2 * SUB].rearrange("p (m t) -> p m t", m=2)
                P.op("act", lambda e: e.activation(out=yg[:], in_=yv, func=AF.Copy), reads=[FB[2]], writes=[yg])
                P.op("dve", lambda e: e.tensor_tensor(out=yt1[:], in0=yg[:], in1=yg[:], op=ALU.mult), reads=[yg], writes=[yt1])
                P.op("dve", lambda e: e.tensor_scalar(out=yt1[:], in0=yt1[:], scalar1=0.044715, scalar2=1.0, op0=ALU.mult, op1=ALU.add), reads=[yt1], writes=[yt1])
                P.op("dve", lambda e: e.tensor_tensor(out=yt1[:], in0=yt1[:], in1=yg[:], op=ALU.mult), reads=[yt1, yg], writes=[yt1])
                P.op("dve", lambda e: e.tensor_scalar(out=yt1[:], in0=yt1[:], scalar1=-45.0, scalar2=None, op0=ALU.max), reads=[yt1], writes=[yt1])
                P.op("act", lambda e: e.activation(out=yt1[:], in_=yt1[:], func=AF.Exp, scale=-1.5957691216), reads=[yt1], writes=[yt1])
                P.op("dve", lambda e: e.tensor_scalar(out=yt1[:], in0=yt1[:], scalar1=1.0, scalar2=None, op0=ALU.add), reads=[yt1], writes=[yt1])
                P.op("dve", lambda e: e.reciprocal(out=yt1[:], in_=yt1[:]), reads=[yt1], writes=[yt1])
                P.op("dve", lambda e: e.tensor_tensor(out=yg[:], in0=yg[:], in1=yt1[:], op=ALU.mult), reads=[yg, yt1], writes=[yg])
                P.op("dve", lambda e: e.tensor_copy(out=yTb[:], in_=yg[:]), reads=[yg], writes=[yTb])
                for mo in range(2):
                    osl = slice(mo * SUB, (mo + 1) * SUB)
                    for k in range(2):
                        P.op("pe", lambda e, mo=mo, k=k, osl=osl: e.matmul(FB[3][:, osl], lhsT=w_glu_sb[:, k, mo * 128:(mo + 1) * 128], rhs=yTb[:, k, :], start=(k == 0), stop=(k == 1)),
                             reads=[w_glu_sb, yTb], writes=[FB[3]])
                    P.op("act", lambda e, mo=mo, osl=osl: e.activation(out=yt2[:, mo, :], in_=FB[3][:, osl], func=AF.Exp, scale=-1.0, bias=nbglu_c[:, mo:mo + 1]), reads=[FB[3], nbglu_c], writes=[yt2])
                P.op("dve", lambda e: e.tensor_scalar(out=yt2[:], in0=yt2[:], scalar1=1.0, scalar2=None, op0=ALU.add), reads=[yt2], writes=[yt2])
                P.op("dve", lambda e: e.reciprocal(out=yt2[:], in_=yt2[:]), reads=[yt2], writes=[yt2])
                P.op("dve", lambda e: e.tensor_tensor(out=osm[:], in0=yg[:], in1=yt2[:], op=ALU.mult), reads=[yg, yt2], writes=[osm])
                P.op("dve", lambda e: e.tensor_tensor(out=o2[:], in0=osm[:], in1=osm[:], op=ALU.mult), reads=[osm], writes=[o2])
                for m in range(2):
                    P.op("pe", lambda e, m=m: e.matmul(FB[3][:, 0:SUB], lhsT=ones_f[:, 0:128], rhs=o2[:, m, :], start=(m == 0), stop=(m == 1)), reads=[ones_f, o2], writes=[FB[3]])
                P.op("act", lambda e: e.activation(out=rs_bc[:], in_=FB[3][:, 0:SUB], func=AF.Sqrt, scale=1.0 / 256, bias=eps_c[:, 0:1]), reads=[FB[3], eps_c], writes=[rs_bc])
                P.op("dve", lambda e: e.reciprocal(out=rs_bc[:], in_=rs_bc[:]), reads=[rs_bc], writes=[rs_bc])
                P.op("dve", lambda e: e.tensor_tensor(out=msm[:], in0=osm[:], in1=rs_bc[:].unsqueeze(1).to_broadcast([128, 2, SUB]), op=ALU.mult), reads=[osm, rs_bc], writes=[msm])
                P.dma("sp", mssm[:, :, gs].rearrange("m p t -> p m t"), msm[:], reads=[msm], writes=[mssm])

    def phase_b(l):
        hi = 0
        for mixer in range(2):
            QTd, KTd, Vd, dk, scale = ((QTm, KTm, Vm, 96, 1.0 / math.sqrt(96.0)), (QTf, KTf, Vf, 68, 0.125))[mixer]
            P.dma("sp", V_sb[:], Vd[:].rearrange("(t p) c -> p t c", p=128), reads=[Vd], writes=[V_sb])
            for h in range(NH):
                Qs = QT_sb[hi % 2]
                Ks = KT_sb[hi % 2]
                hi += 1
                P.dma("sp", Qs[0:dk, :], QTd[h], reads=[QTd], writes=[Qs])
                P.dma("sp", Ks[0:dk, :], KTd[h], reads=[KTd], writes=[Ks])
                it = 0
                for b in range(NB):
                    nk = 4 * b + 4
                    for kt in range(nk):
                        j = kt - 4 * b
                        q0 = 0 if j <= 0 else 128 * j
                        Sb = FB[it % 2]
                        pt_ = PT[it % 3]
                        it += 1
                        qsl = slice(b * 512 + q0, (b + 1) * 512)
                        P.op("pe", lambda e, Sb=Sb, kt=kt, qsl=qsl, q0=q0: e.matmul(Sb[:, q0:512], lhsT=Ks[0:dk, kt * 128:(kt + 1) * 128], rhs=Qs[0:dk, qsl], start=True, stop=True),
                             reads=[Ks, Qs], writes=[Sb])
                        P.op("act", lambda e, Sb=Sb, pt_=pt_, q0=q0: e.activation(out=pt_[:, q0:512], in_=Sb[:, q0:512], func=AF.Exp, scale=scale), reads=[Sb], writes=[pt_])
                        if j >= 0:
                            if mixer == 0:
                                P.op("pool", lambda e, pt_=pt_, q0=q0: e.memset(pt_[64:128, q0:q0 + 64], 0.0), writes=[pt_])
                            else:
                                P.op("pool", lambda e, pt_=pt_, q0=q0: e.affine_select(out=pt_[:, q0:q0 + 128], in_=pt_[:, q0:q0 + 128], pattern=[[1, 128]], compare_op=ALU.is_ge,
                                                                                     fill=0.0, base=0, channel_multiplier=-1), reads=[pt_], writes=[pt_])
                        for qi in range(max(j, 0), 4):
                            Ob = FB[2 + qi]
                            P.op("pe", lambda e, Ob=Ob, pt_=pt_, qi=qi, kt=kt: e.matmul(Ob[:, 0:65], lhsT=pt_[:, qi * 128:(qi + 1) * 128], rhs=V_sb[:, kt, h * 65:(h + 1) * 65],
                                                                                       start=(kt == 0), stop=(kt == 4 * b + qi)), reads=[pt_, V_sb], writes=[Ob])
                    for qi in range(4):
                        Ob = FB[2 + qi]
                        rd = rden[qi]
                        t = 4 * b + qi
                        col = mixer * 384 + h * 64
                        P.op("dve", lambda e, Ob=Ob, rd=rd: e.reciprocal(out=rd[:], in_=Ob[:, 64:65]), reads=[Ob], writes=[rd])
                        P.op("dve", lambda e, Ob=Ob, rd=rd, t=t, col=col: e.tensor_scalar(out=o_attn[:, t, col:col + 64], in0=Ob[:, 0:64], scalar1=rd[:, 0:1], scalar2=None, op0=ALU.mult),
                             reads=[Ob, rd], writes=[o_attn])

    def phase_c(l, moe):
        j2 = l // 2
        if moe:
            P.dma("sp", wr_sb[:], A["moe_wr"][j2].rearrange("(k p) n -> p k n", p=128), writes=[wr_sb])
            P.dma("sp", br_sb[:], A["moe_br"][j2], writes=[br_sb])
        for t in range(NT):
            xs_ = xs[t % 2]
            if l == 0:
                P.dma("sp", xs_[:], x_in[t * 128:(t + 1) * 128, :], writes=[xs_])
            else:
                P.dma("sp", xs_[:], y[t * 128:(t + 1) * 128, :], reads=[ytile[t]], writes=[xs_])
            st_ = stat[t % 4]
            for mx in range(2):
                P.op("act", lambda e, mx=mx: e.activation(out=sq_junk[:, mx * 384:(mx + 1) * 384], in_=o_attn[:, t, mx * 384:(mx + 1) * 384], func=AF.Square, accum_out=st_[:, mx:mx + 1]),
                     reads=[o_attn], writes=[sq_junk, st_])
            P.op("act", lambda e: e.activation(out=st_[:, 2:4], in_=st_[:, 0:2], func=AF.Sqrt, scale=1.0 / 384, bias=eps_c[:, 0:1]), reads=[st_, eps_c], writes=[st_])
            P.op("dve", lambda e: e.reciprocal(out=st_[:, 2:4], in_=st_[:, 2:4]), reads=[st_], writes=[st_])
            for mx in range(2):
                P.op("dve", lambda e, mx=mx: e.tensor_scalar(out=mat[:, mx * 384:(mx + 1) * 384], in0=o_attn[:, t, mx * 384:(mx + 1) * 384], scalar1=st_[:, 2 + mx:3 + mx], scalar2=None, op0=ALU.mult),
                     reads=[o_attn, st_], writes=[mat])
            tb = TB[t % 2]
            for k in range(6):
                P.op("pe", lambda e, k=k: e.transpose(tb[:, k * 128:(k + 1) * 128], mat[:, k * 128:(k + 1) * 128], ident[:]), reads=[mat, ident], writes=[tb])
            P.op("act", lambda e: e.activation(out=mT[:, 2:8, :], in_=tb[:, 0:768].rearrange("p (k t) -> p k t", k=6), func=AF.Copy), reads=[tb], writes=[mT])
            P.dma("sp", mT[:, 0:2, :], mssm[:, :, t * 128:(t + 1) * 128].rearrange("m p t -> p m t"), reads=[mssm], writes=[mT])
            xn = xnew[t % 2]
            for hf in range(2):
                fb = FB[hf]
                for k in range(KD):
                    P.op("pe", lambda e, fb=fb, k=k, hf=hf: e.matmul(fb[:], lhsT=mT[:, k, :], rhs=w_out_sb[:, k, hf * 512:(hf + 1) * 512], start=(k == 0), stop=(k == KD - 1)),
                         reads=[mT, w_out_sb], writes=[fb])
                P.op("dve", lambda e, fb=fb, hf=hf: e.tensor_tensor(out=xn[:, hf * 512:(hf + 1) * 512], in0=fb[:], in1=xs_[:, hf * 512:(hf + 1) * 512], op=ALU.add), reads=[fb, xs_], writes=[xn])
            P.dma("sp", y[t * 128:(t + 1) * 128, :], xn[:], reads=[xn], writes=[ytile[t]])
            norm_transpose(xn, xn[:], l, 2, 3, h2T_st, slice(0, 128), t, fp32_path=(xhf, h2Tf) if moe else None)
            P.dma("sp", h2T_d[:, :, t * 128:(t + 1) * 128].rearrange("k p t -> p k t"), h2T_st[:], reads=[h2T_st], writes=[h2T_d])
            if moe:
                fb = FB[2]
                for k in range(KD):
                    P.op("pe", lambda e, k=k: e.matmul(fb[:, 0:8], lhsT=h2Tf[:, k, :], rhs=wr_sb[:, k, :], start=(k == 0), stop=False), reads=[h2Tf, wr_sb], writes=[fb])
                P.op("pe", lambda e: e.matmul(fb[:, 0:8], lhsT=ones_f[0:1, 0:128], rhs=br_sb[0:1, :], start=False, stop=True), reads=[ones_f, br_sb], writes=[fb])
                lg, m1, m2, lg2 = rtmp
                s1, s2, s3, s4 = rsc
                P.op("dve", lambda e: e.tensor_copy(out=lg[:], in_=fb[:, 0:8]), reads=[fb], writes=[lg])
                P.op("dve", lambda e: e.tensor_reduce(out=s1[:], in_=lg[:], axis=AX.X, op=ALU.max), reads=[lg], writes=[s1])
                P.op("dve", lambda e: e.tensor_scalar(out=m1[:], in0=lg[:], scalar1=s1[:, 0:1], scalar2=None, op0=ALU.is_equal), reads=[lg, s1], writes=[m1])
                P.op("dve", lambda e: e.scalar_tensor_tensor(out=lg2[:], in0=m1[:], scalar=-1e30, in1=lg[:], op0=ALU.mult, op1=ALU.add), reads=[m1, lg], writes=[lg2])
                P.op("dve", lambda e: e.tensor_reduce(out=s2[:], in_=lg2[:], axis=AX.X, op=ALU.max), reads=[lg2], writes=[s2])
                P.op("dve", lambda e: e.tensor_scalar(out=m2[:], in0=lg2[:], scalar1=s2[:, 0:1], scalar2=None, op0=ALU.is_equal), reads=[lg2, s2], writes=[m2])
                P.op("dve", lambda e: e.tensor_tensor(out=s3[:], in0=s2[:], in1=s1[:], op=ALU.subtract), reads=[s1, s2], writes=[s3])
                P.op("act", lambda e: e.activation(out=s3[:], in_=s3[:], func=AF.Exp), reads=[s3], writes=[s3])
                P.op("dve", lambda e: e.tensor_scalar(out=s3[:], in0=s3[:], scalar1=1.0, scalar2=None, op0=ALU.add), reads=[s3], writes=[s3])
                P.op("dve", lambda e: e.reciprocal(out=s3[:], in_=s3[:]), reads=[s3], writes=[s3])
                P.op("dve", lambda e: e.tensor_scalar(out=s4[:], in0=s3[:], scalar1=-1.0, scalar2=1.0, op0=ALU.mult, op1=ALU.add), reads=[s3], writes=[s4])
                P.op("dve", lambda e: e.tensor_scalar(out=m1[:], in0=m1[:], scalar1=s3[:, 0:1], scalar2=None, op0=ALU.mult), reads=[m1, s3], writes=[m1])
                P.op("dve", lambda e, t=t: e.scalar_tensor_tensor(out=comb_all[:, t, :], in0=m2[:], scalar=s4[:, 0:1], in1=m1[:], op0=ALU.mult, op1=ALU.add), reads=[m2, s4, m1], writes=[comb_all])

    def phase_d(l, moe):
        j2 = l // 2
        P.dma("sp", G_bc[:], g12[l, 1], reads=[g12], writes=[G_bc])
        ne = 8 if moe else 2
        it = 0
        oi = 0
        for ex in range(ne):
            if moe:
                wg = A["moe_wg"][j2, ex]
                wu = A["moe_wu"][j2, ex]
                wd = A["moe_wd"][j2, ex]
            else:
                wg = A["ffn_wg"][j2][:, ex * DFE:(ex + 1) * DFE]
                wu = A["ffn_wu"][j2][:, ex * DFE:(ex + 1) * DFE]
                wd = A["ffn_wd"][j2][ex * DFE:(ex + 1) * DFE, :]
            Wg_, Wu_, Wd_ = Wg_sb[ex % 2], Wu_sb[ex % 2], Wd_sb[ex % 2]
            P.dma("pool", Wg_[:], wg.rearrange("(k p) f -> p k f", p=128), writes=[Wg_])
            P.dma("pool", Wu_[:], wu.rearrange("(k p) f -> p k f", p=128), writes=[Wu_])
            P.dma("pool", Wd_[:], wd.rearrange("(c p) d -> p c d", p=128), writes=[Wd_])
            for b in range(NB):
                hb = h2T_sb[(ex * NB + b) % 2]
                P.dma("sp", hb[:], h2T_d[:, :, b * 512:(b + 1) * 512].rearrange("k p t -> p k t"), reads=[h2T_d], writes=[hb])
                for c in range(NFC):
                    gb = FB[(it % 2) * 2]
                    ub = FB[(it % 2) * 2 + 1]
                    sg_ = sg[it % 2]
                    it += 1
                    for k in range(KD):
                        P.op("pe", lambda e, gb=gb, k=k, c=c: e.matmul(gb[:], lhsT=Wg_[:, k, c * 128:(c + 1) * 128], rhs=hb[:, k, :], start=(k == 0), stop=(k == KD - 1)), reads=[Wg_, hb], writes=[gb])
                    for k in range(KD):
                        P.op("pe", lambda e, ub=ub, k=k, c=c: e.matmul(ub[:], lhsT=Wu_[:, k, c * 128:(c + 1) * 128], rhs=hb[:, k, :], start=(k == 0), stop=(k == KD - 1)), reads=[Wu_, hb], writes=[ub])
                    P.op("act", lambda e, gb=gb, sg_=sg_: e.activation(out=sg_[:], in_=gb[:], func=AF.Silu), reads=[gb], writes=[sg_])
                    P.op("dve", lambda e, ub=ub, sg_=sg_, c=c: e.tensor_tensor(out=aT[:, c, :], in0=ub[:], in1=sg_[:], op=ALU.mult), reads=[ub, sg_], writes=[aT])
                for ti in range(4):
                    t = b * 4 + ti
                    os_ = ost[oi % 2]
                    oi += 1
                    for hf in range(2):
                        fb = FB[4 + hf]
                        for c in range(NFC):
                            P.op("pe", lambda e, fb=fb, c=c, ti=ti, hf=hf: e.matmul(fb[:], lhsT=aT[:, c, ti * 128:(ti + 1) * 128], rhs=Wd_[:, c, hf * 512:(hf + 1) * 512], start=(c == 0), stop=(c == NFC - 1)),
                                 reads=[aT, Wd_], writes=[fb])
                        if moe:
                            P.op("dve", lambda e, fb=fb, hf=hf, t=t, ex=ex, os_=os_: e.scalar_tensor_tensor(out=os_[:, hf * 512:(hf + 1) * 512], in0=fb[:], scalar=comb_all[:, t, ex:ex + 1],
                                                                                                       in1=G_bc[:, hf * 512:(hf + 1) * 512], op0=ALU.mult, op1=ALU.mult), reads=[fb, comb_all, G_bc], writes=[os_])
                        else:
                            P.op("dve", lambda e, fb=fb, hf=hf, os_=os_: e.tensor_tensor(out=os_[:, hf * 512:(hf + 1) * 512], in0=fb[:], in1=G_bc[:, hf * 512:(hf + 1) * 512], op=ALU.mult),
                                 reads=[fb, G_bc], writes=[os_])
                    P.dma("pool", y[t * 128:(t + 1) * 128, :], os_[:], reads=[os_, ytile[t]], writes=[ytile[t]], accum_op=ALU.add)

    stop = build.stop_after
    G = globals()

    def use(dct):
        for k_, v_ in dct.items():
            if k_ not in ("P", "L", "NT"):
                G[k_] = v_
    dbg = P.dram("dbg_oattn", [128, NT * 768], BF16, kind="ExternalOutput") if debug else None
    for l in range(n_layers):
        moe = (l % 2 == 1)
        P.push()
        use(_alloc_a(P, L))
        s5c = load_layer(l)
        phase_a(l, s5c)
        P.pop()
        if stop == "a":
            break
        P.push()
        use(_alloc_bc(P, L))
        P.push()
        use(_alloc_b(P, L))
        phase_b(l)
        P.pop()
        if debug and (stop == "b" or l == n_layers - 1):
            P.dma("sp", dbg[:], o_attn[:].rearrange("p t c -> p (t c)"), reads=[o_attn], writes=[dbg])
        if stop == "b":
            P.pop()
            break
        P.push()
        use(_alloc_c(P, L))
        load_c(l)
        phase_c(l, moe)
        P.pop()
        P.pop()
        if stop == "c":
            break
        P.push()
        use(_alloc_d(P, L))
        phase_d(l, moe)
        P.pop()
    P.finish()
    build.n_inst = P.n_inst
    return nc


build.stop_after = None


def prep_shared(inp, n_layers=DEPTH):
    f = lambda a: np.ascontiguousarray(np.asarray(a, dtype=np.float32))
    col = lambda a, k: f(np.asarray(a).reshape(DEPTH, k, 128).transpose(0, 2, 1))
    out = {}
    out["norm_mix_c"] = col(inp["norm_mix"], KD)
    out["norm_ffn_c"] = col(inp["norm_ffn"], KD)
    out["w_ada"] = f(inp["w_ada"])
    out["b_ada"] = f(np.asarray(inp["b_ada"]).reshape(DEPTH, 1, 6 * D))
    out["w_in"] = f(inp["w_in"])
    sm = lambda a: f(np.asarray(a).reshape(DEPTH, 8, 128).transpose(0, 2, 1))
    out["lam_re"] = sm(inp["ssm_lam_re"])
    out["lam_im"] = sm(inp["ssm_lam_im"])
    out["log_dt"] = sm(np.repeat(np.asarray(inp["ssm_log_dt"])[:, :, None], 64, axis=2))
    b_re = np.asarray(inp["ssm_b_re"]); b_im = np.asarray(inp["ssm_b_im"])
    c_re = np.asarray(inp["ssm_c_re"]); c_im = np.asarray(inp["ssm_c_im"])
    blk = {k: np.zeros((DEPTH, 8, 128, 128), np.float32) for k in ("b_re", "b_im", "c_re", "c_im")}
    for g in range(16):
        j, gg = g // 2, g % 2
        fc = (g % 8) * 16
        blk["b_re"][:, j, gg * 64:(gg + 1) * 64, fc:fc + 16] = b_re[:, g]
        blk["b_im"][:, j, gg * 64:(gg + 1) * 64, fc:fc + 16] = b_im[:, g]
        blk["c_re"][:, j, gg * 64:(gg + 1) * 64, fc:fc + 16] = c_re[:, g].transpose(0, 2, 1)
        blk["c_im"][:, j, gg * 64:(gg + 1) * 64, fc:fc + 16] = c_im[:, g].transpose(0, 2, 1)
    out.update(blk)
    out["ssm_d"] = col(inp["ssm_d"], 2)
    out["w_glu"] = f(inp["ssm_w_glu"])
    out["b_glu"] = col(inp["ssm_b_glu"], 2)
    out["q_norm"] = col(inp["mla_q_norm"], 2)
    out["kv_norm"] = col(inp["mla_kv_norm"], 1)
    out["w_uq"] = f(inp["mla_w_uq"])
    out["w_ukv"] = f(inp["mla_w_ukv"])
    rep = lambda a: f(np.broadcast_to(np.asarray(a)[:, None, :], (DEPTH, 128, np.asarray(a).shape[1])))
    out["gq_m"] = rep(inp["mla_qk_gq"])
    out["gk_m"] = rep(inp["mla_qk_gk"])
    out["fox_bf"] = f(np.asarray(inp["fox_b_f"]).reshape(DEPTH, NH, 1))
    out["gq_f"] = rep(inp["fox_qk_gq"])
    out["gk_f"] = rep(inp["fox_qk_gk"])
    out["out_norm"] = col(inp["out_norm"], KD)
    out["w_out"] = f(inp["w_out"])
    out["ffn_wg"] = f(inp["ffn_w_gate"])
    out["ffn_wu"] = f(inp["ffn_w_up"])
    out["ffn_wd"] = f(inp["ffn_w_down"])
    out["moe_wr"] = f(inp["moe_w_router"])
    out["moe_br"] = f(np.asarray(inp["moe_b_router"]).reshape(2, 1, 8))
    out["moe_wg"] = f(inp["moe_w_gate"])
    out["moe_wu"] = f(inp["moe_w_up"])
    out["moe_wd"] = f(inp["moe_w_down"])
    half = 16
    inv = (10000.0 ** (-np.arange(half, dtype=np.float32) / half)).astype(np.float32)
    out["inv_bc"] = f(np.broadcast_to(inv[None, :], (128, 16)))
    return out


def prep_core(inp, b, L):
    NT = L // 128
    m = {}
    m["x"] = np.ascontiguousarray(np.asarray(inp["x"])[b, :L, :], dtype=np.float32)
    m["c_col"] = np.ascontiguousarray(np.asarray(inp["c"], dtype=np.float32)[b].reshape(KD, 128).T)
    m["pos"] = np.ascontiguousarray(np.asarray(inp["positions"])[b, :L].astype(np.int32).reshape(NT, 128).T)
    return m


_CACHE = {}


def run(inp, L, n_layers=DEPTH, debug=False, cores=8, trace=False):
    key = (L, n_layers, debug, build.stop_after)
    if key not in _CACHE:
        _CACHE[key] = build(L, n_layers, debug)
    nc = _CACHE[key]
    shared = prep_shared(inp, n_layers)
    in_maps = []
    for b in range(cores):
        m = dict(shared)
        m.update(prep_core(inp, b, L))
        in_maps.append(m)
    res = run_bass_kernel_spmd(nc, in_maps, core_ids=list(range(cores)))
    return res


def kernel(**inputs):
    L = np.asarray(inputs["x"]).shape[1]
    res = run(inputs, L)
    out = np.stack([np.asarray(r["y"], dtype=np.float32) for r in res.results], axis=0)
    return out
```

```python
import contextlib
import math
import sys
import numpy as np
import concourse.bass as bass
import concourse.mybir as mybir
from concourse.bass_utils import run_bass_kernel_spmd

F32 = mybir.dt.float32
BF16 = mybir.dt.bfloat16
I32 = mybir.dt.int32
AF = mybir.ActivationFunctionType
ALU = mybir.AluOpType
AX = mybir.AxisListType

ENGS = ("pe", "act", "dve", "pool", "sp")

D = 1024
KD = 8
DEPTH = 4
EPS = 1e-6
IN_COLS = 1830
NH = 6
DFE = 1408
NFC = 11
SUB = 256


class Tl:
    __slots__ = ("t", "name", "w", "r", "excl")

    def __init__(self, t, name):
        self.t = t
        self.name = name
        self.excl = False
        self.w = {}
        self.r = {}

    def __getitem__(self, idx):
        return self.t[idx]


class Prog:
    max_ops = 10 ** 9
    log = None

    def __init__(self, nc, ring_sizes=None):
        self.nc = nc
        self.es = contextlib.ExitStack()
        self.cnt = {e: 0 for e in ENGS}
        self.sems = {}
        self.seen = {e: {} for e in ENGS}
        for e in ENGS:
            self.sems[("eng", e)] = self.es.enter_context(nc.semaphore("s_" + e))
        ring_sizes = ring_sizes or {"sp": 16, "pool": 8, "act": 2}
        self.rings = {}
        self.ring_i = {}
        for e, k in ring_sizes.items():
            self.rings[e] = []
            for i in range(k):
                key = ("ring", e, i)
                self.sems[key] = self.es.enter_context(nc.semaphore("r_%s%d" % (e, i)))
                self.rings[e].append([key, 0])
            self.ring_i[e] = 0
        self.n_inst = 0
        self.scopes = [self.es]
        self.E = {"pe": nc.tensor, "act": nc.scalar, "dve": nc.vector, "pool": nc.gpsimd, "sp": nc.sync}

    def sb(self, name, shape, dt):
        self._uid = getattr(self, "_uid", 0) + 1
        return Tl(self.scopes[-1].enter_context(self.nc.sbuf_tensor("%s_%d" % (name, self._uid), list(shape), dt)), name)

    def push(self):
        self.scopes.append(contextlib.ExitStack())

    def barrier(self):
        need = {}
        for e, ring in self.rings.items():
            for key, v in ring:
                if v > 0:
                    need[key] = v
        for e in ENGS:
            if self.cnt[e] > 0:
                need[("eng", e)] = self.cnt[e]
        for eng in ENGS:
            seen = self.seen[eng]
            for k, v in need.items():
                if k == ("eng", eng) or seen.get(k, 0) >= v:
                    continue
                seen[k] = v
                self.E[eng].wait_ge(self.sems[k], v)

    def pop(self):
        self.barrier()
        self.scopes.pop().close()

    def ps(self, name, shape, dt):
        t = Tl(self.es.enter_context(self.nc.psum_tensor(name, list(shape), dt)), name)
        t.excl = True
        return t

    def dram(self, name, shape, dt, kind="Internal"):
        return Tl(self.nc.dram_tensor(name, list(shape), dt, kind=kind).ap(), name)

    def _collect(self, eng, reads, writes):
        need = {}
        me = ("eng", eng)
        for t in reads:
            for k, v in t.w.items():
                if need.get(k, 0) < v:
                    need[k] = v
            if t.excl:
                for k, v in t.r.items():
                    if k != me and need.get(k, 0) < v:
                        need[k] = v
        for t in writes:
            for k, v in t.w.items():
                if need.get(k, 0) < v:
                    need[k] = v
            for k, v in t.r.items():
                if need.get(k, 0) < v:
                    need[k] = v
        waits = []
        seen = self.seen[eng]
        for k, v in need.items():
            if eng == "pe" and k == ("eng", "pe"):
                continue
            if seen.get(k, 0) >= v:
                continue
            seen[k] = v
            waits.append((self.sems[k], v))
        return waits

    def _commit(self, reads, writes, key, val):
        for t in reads:
            if t.r.get(key, 0) < val:
                t.r[key] = val
        for t in writes:
            t.w = {key: val}
            t.r = {}

    def op(self, eng, fn, reads=(), writes=()):
        self.n_inst += 1
        if Prog.log is not None:
            Prog.log.append((self.n_inst, eng, sys._getframe(1).f_lineno))
        if self.n_inst > Prog.max_ops:
            return
        waits = self._collect(eng, reads, writes)
        self.cnt[eng] += 1
        key = ("eng", eng)
        val = self.cnt[eng]
        self._commit(reads, writes, key, val)
        e = self.E[eng]
        for s, v in waits:
            e.wait_ge(s, v)
        fn(e).then_inc(self.sems[key], 1)

    def dma(self, eng, out, in_, reads=(), writes=(), **kw):
        self.n_inst += 1
        if Prog.log is not None:
            Prog.log.append((self.n_inst, "dma-" + eng, sys._getframe(1).f_lineno))
        if self.n_inst > Prog.max_ops:
            return
        ring = self.rings[eng]
        slot = ring[self.ring_i[eng] % len(ring)]
        self.ring_i[eng] += 1
        key, pv = slot
        waits = self._collect(eng, reads, writes)
        seen = self.seen[eng]
        if pv > 0 and seen.get(key, 0) < pv:
            seen[key] = pv
            waits.append((self.sems[key], pv))
        val = pv + 16
        slot[1] = val
        self._commit(reads, writes, key, val)
        e = self.E[eng]
        for s, v in waits:
            e.wait_ge(s, v)
        e.dma_start(out=out, in_=in_, **kw).then_inc(self.sems[key], 16)

    def finish(self, eng="sp"):
        need = {}
        for e, ring in self.rings.items():
            for key, v in ring:
                if v > 0:
                    need[key] = v
        for e in ENGS:
            if self.cnt[e] > 0:
                need[("eng", e)] = self.cnt[e]
        E = self.E[eng]
        for k, v in need.items():
            E.wait_ge(self.sems[k], v)
        self.es.close()


LAYER_PARAMS = [
    ("norm_mix_c", [128, KD]), ("norm_ffn_c", [128, KD]),
    ("w_ada", [D, 6 * D]), ("b_ada", [1, 6 * D]),
    ("w_in", [D, IN_COLS]),
    ("lam_re", [128, 8]), ("lam_im", [128, 8]), ("log_dt", [128, 8]),
    ("b_re", [8, 128, 128]), ("b_im", [8, 128, 128]),
    ("c_re", [8, 128, 128]), ("c_im", [8, 128, 128]),
    ("ssm_d", [128, 2]), ("w_glu", [256, 256]), ("b_glu", [128, 2]),
    ("q_norm", [128, 2]), ("kv_norm", [128, 1]),
    ("w_uq", [256, 576]), ("w_ukv", [128, 768]),
    ("gq_m", [128, 96]), ("gk_m", [128, 96]),
    ("fox_bf", [NH, 1]), ("gq_f", [128, 64]), ("gk_f", [128, 64]),
    ("out_norm", [128, KD]), ("w_out", [D, D]),
]


def _alloc_a(P, L):
    NT = L // 128
    w_in_sb = P.sb("w_in_sb", [128, KD, IN_COLS], BF16)
    w_uq_sb = P.sb("w_uq_sb", [128, 2, 576], BF16)
    w_ukv_sb = P.sb("w_ukv_sb", [128, 768], BF16)
    w_glu_sb = P.sb("w_glu_sb", [128, 2, 256], BF16)
    qn_c = P.sb("qn_c", [128, 2], F32)
    kvn_c = P.sb("kvn_c", [128, 1], F32)
    gqm = P.sb("gqm", [128, 96], F32)
    gkm = P.sb("gkm", [128, 96], F32)
    gqf = P.sb("gqf", [128, 64], F32)
    gkf = P.sb("gkf", [128, 64], F32)
    nbf = P.sb("nbf", [NH, 1], F32)
    dcol = P.sb("dcol", [128, 2], F32)
    bglu_c = P.sb("bglu_c", [128, 2], F32)
    nbglu_c = P.sb("nbglu_c", [128, 2], F32)
    s5p = P.sb("s5p", [128, 24, 8], F32)
    BreT = P.sb("BreT", [128, 8, 128], BF16)
    BimT = P.sb("BimT", [128, 8, 128], BF16)
    CreT = P.sb("CreT", [128, 8, 128], BF16)
    nCimT = P.sb("nCimT", [128, 8, 128], BF16)
    Ddiag = P.sb("Ddiag", [128, 2, 128], BF16)
    Ctab = P.sb("Ctab", [128, 8, SUB], F32)
    Stab = P.sb("Stab", [128, 8, SUB], F32)
    Rtab = P.sb("Rtab", [128, 8, SUB], F32)
    blk_f = P.sb("blk_f", [128, 2, 128], F32)
    blk_o = P.sb("blk_o", [128, 2, 128], F32)
    hT = [P.sb("hT%d" % i, [128, KD, 512], BF16) for i in range(2)]
    uT = P.sb("uT", [128, 2, 512], BF16)
    cq_h = P.sb("cq_h", [128, 256], BF16)
    ckv_h = P.sb("ckv_h", [128, 128], BF16)
    cqT = P.sb("cqT", [128, 2, 128], BF16)
    ckvT = P.sb("ckvT", [128, 128], BF16)
    qn = P.sb("qn", [128, NH, 96], F32)
    kn = P.sb("kn", [128, NH, 96], F32)
    rt = [P.sb("rt%d" % i, [128, NH, 16], F32) for i in range(4)]
    qfin = P.sb("qfin", [128, NH, 96], BF16)
    kfin = P.sb("kfin", [128, NH, 96], BF16)
    fqn = P.sb("fqn", [128, NH, 64], F32)
    fqb = P.sb("fqb", [128, NH, 64], BF16)
    fkb = P.sb("fkb", [128, NH, 64], BF16)
    QTm_st = [P.sb("QTm_st%d" % i, [96, NH, 128], BF16) for i in range(2)]
    KTm_st = [P.sb("KTm_st%d" % i, [96, NH, 128], BF16) for i in range(2)]
    QTf_st = [P.sb("QTf_st%d" % i, [64, NH, 128], BF16) for i in range(2)]
    KTf_st = [P.sb("KTf_st%d" % i, [64, NH, 128], BF16) for i in range(2)]
    Vm_st = [P.sb("Vm_st%d" % i, [128, NH, 65], BF16) for i in range(2)]
    Vf_st = [P.sb("Vf_st%d" % i, [128, NH, 65], BF16) for i in range(2)]
    for t_ in Vm_st + Vf_st:
        P.op("pool", lambda e, t_=t_: e.memset(t_[:], 1.0), writes=[t_])
    fg_e = P.sb("fg_e", [NH, 512], F32)
    fg_sp = P.sb("fg_sp", [NH, 512], F32)
    fg_cum = P.sb("fg_cum", [NH, 512], F32)
    fg_carry = P.sb("fg_carry", [NH, 1], F32)
    fg_hi = P.sb("fg_hi", [NH, 512], BF16)
    fg_lo = P.sb("fg_lo", [NH, 512], BF16)
    fg_nhi = P.sb("fg_nhi", [NH, 512], BF16)
    fg_nlo = P.sb("fg_nlo", [NH, 512], BF16)
    W_re = P.sb("W_re", [128, 4, SUB], F32)
    W_im = P.sb("W_im", [128, 4, SUB], F32)
    wlast = P.sb("wlast", [128, 2, 8], F32)
    pre_c = P.sb("pre_c", [128, 2, SUB], F32)
    pre_s = P.sb("pre_s", [128, 2, SUB], F32)
    pin_re = P.sb("pin_re", [128, SUB], F32)
    pin_im = P.sb("pin_im", [128, SUB], F32)
    w0 = P.sb("w0", [128, 2, 8], F32)
    w0t = P.sb("w0t", [128, 4, 8], F32)
    pt = [P.sb("pt%d" % i, [128, 4, SUB], F32) for i in range(2)]
    s_re = P.sb("s_re", [128, 4, SUB], BF16)
    s_im = P.sb("s_im", [128, 4, SUB], BF16)
    yg = P.sb("yg", [128, 2, SUB], F32)
    yt1 = P.sb("yt1", [128, 2, SUB], F32)
    yt2 = P.sb("yt2", [128, 2, SUB], F32)
    yTb = P.sb("yTb", [128, 2, SUB], BF16)
    o2 = P.sb("o2", [128, 2, SUB], F32)
    osm = P.sb("osm", [128, 2, SUB], F32)
    rs_bc = P.sb("rs_bc", [128, SUB], F32)
    msm = P.sb("msm", [128, 2, SUB], BF16)
    return locals()


def _alloc_bc(P, L):
    NT = L // 128
    o_attn = P.sb("o_attn", [128, NT, 768], BF16)
    return locals()


def _alloc_b(P, L):
    NT = L // 128
    QT_sb = [P.sb("QT_sb%d" % i, [96, L], BF16) for i in range(2)]
    KT_sb = [P.sb("KT_sb%d" % i, [96, L], BF16) for i in range(2)]
    V_sb = P.sb("V_sb", [128, NT, NH * 65], BF16)
    PT = [P.sb("PT%d" % i, [128, 512], BF16) for i in range(3)]
    rden = [P.sb("rden%d" % i, [128, 1], F32) for i in range(4)]
    return locals()


def _alloc_c(P, L):
    NT = L // 128
    w_out_sb = P.sb("w_out_sb", [128, KD, D], BF16)
    wstc = [P.sb("wstc%d" % i, [128, D], F32) for i in range(2)]
    onorm_c = P.sb("onorm_c", [128, KD], F32)
    mat = P.sb("mat", [128, 768], BF16)
    mT = P.sb("mT", [128, KD, 128], BF16)
    xnew = [P.sb("xnew%d" % i, [128, D], F32) for i in range(2)]
    h2T_st = P.sb("h2T_st", [128, KD, 128], BF16)
    xhf = P.sb("xhf", [128, D], F32)
    h2Tf = P.sb("h2Tf", [128, KD, 128], F32)
    wr_sb = P.sb("wr_sb", [128, KD, 8], F32)
    br_sb = P.sb("br_sb", [128, 8], F32)
    rtmp = [P.sb("rtmp%d" % i, [128, 8], F32) for i in range(4)]
    rsc = [P.sb("rsc%d" % i, [128, 1], F32) for i in range(4)]
    return locals()


def _alloc_d(P, L):
    Wg_sb = [P.sb("Wg_sb%d" % i, [128, KD, DFE], BF16) for i in range(2)]
    Wu_sb = [P.sb("Wu_sb%d" % i, [128, KD, DFE], BF16) for i in range(2)]
    Wd_sb = [P.sb("Wd_sb%d" % i, [128, NFC, D], BF16) for i in range(2)]
    h2T_sb = [P.sb("h2T_sb%d" % i, [128, KD, 512], BF16) for i in range(2)]
    sg = [P.sb("sg%d" % i, [128, 512], BF16) for i in range(2)]
    aT = P.sb("aT", [128, NFC, 512], BF16)
    ost = [P.sb("ost%d" % i, [128, D], F32) for i in range(2)]
    return locals()


def build(L, n_layers=DEPTH, debug=False):
    NT = L // 128
    NB = L // 512
    nc = bass.Bass("TRN2", target_bir_lowering=False)
    P = Prog(nc)
    A = {}

    def din(name, shape, dt=F32):
        A[name] = P.dram(name, shape, dt, kind="ExternalInput")
        return A[name]

    x_in = din("x", [L, D])
    c_in = din("c_col", [128, KD])
    pos_in = din("pos", [128, NT], I32)
    inv_in = din("inv_bc", [128, 16])
    for name, shp in LAYER_PARAMS:
        din(name, [DEPTH] + shp)
    din("ffn_wg", [2, D, 2 * DFE])
    din("ffn_wu", [2, D, 2 * DFE])
    din("ffn_wd", [2, 2 * DFE, D])
    din("moe_wr", [2, D, 8])
    din("moe_br", [2, 1, 8])
    din("moe_wg", [2, 8, D, DFE])
    din("moe_wu", [2, 8, D, DFE])
    din("moe_wd", [2, 8, DFE, D])

    y = P.dram("y", [L, D], F32, kind="ExternalOutput")
    skind = "ExternalOutput" if debug else "Internal"
    QTm = P.dram("QTm", [NH, 96, L], BF16, kind=skind)
    KTm = P.dram("KTm", [NH, 96, L], BF16, kind=skind)
    Vm = P.dram("Vm", [L, NH * 65], BF16, kind=skind)
    QTf = P.dram("QTf", [NH, 68, L], BF16, kind=skind)
    KTf = P.dram("KTf", [NH, 68, L], BF16, kind=skind)
    Vf = P.dram("Vf", [L, NH * 65], BF16, kind=skind)
    mssm = P.dram("mssm", [2, 128, L], BF16, kind=skind)
    h2T_d = P.dram("h2T", [KD, 128, L], BF16, kind=skind)
    g12 = P.dram("g12", [DEPTH, 2, 128, D], F32, kind=skind)
    ytile = [Tl(y.t, "y%d" % t) for t in range(NT)]

    ident = P.sb("ident", [128, 128], BF16)
    identf = P.sb("identf", [128, 128], F32)
    ones_f = P.sb("ones_f", [128, 512], F32)
    ones_b = P.sb("ones_b", [128, 128], BF16)
    for t_, dt_ in ((ident, BF16), (identf, F32)):
        P.op("pool", lambda e, t_=t_: e.memset(t_[:], 1.0), writes=[t_])
        P.op("pool", lambda e, t_=t_: e.affine_select(out=t_[:], in_=t_[:], pattern=[[1, 128]], compare_op=ALU.is_equal,
                                                    fill=0.0, base=0, channel_multiplier=-1), reads=[t_], writes=[t_])
    P.op("pool", lambda e: e.memset(ones_f[:], 1.0), writes=[ones_f])
    P.op("pool", lambda e: e.memset(ones_b[:], 1.0), writes=[ones_b])

    TB = [P.ps("TB%d" % i, [128, 1024], BF16) for i in range(2)]
    FB = [P.ps("FB%d" % i, [128, 512], F32) for i in range(6)]

    def rstd_chain(ss_ap, n, nfeat, tmp, out, reads, eng_r="dve"):
        (tt, tv), (ot, ov) = tmp, out
        P.op("act", lambda e: e.activation(out=tv, in_=ss_ap, func=AF.Sqrt, scale=1.0 / nfeat, bias=eps_c[:, 0:1]),
             reads=list(reads) + [eps_c], writes=[tt])
        P.op("dve", lambda e: e.reciprocal(out=ov, in_=tv), reads=[tt], writes=[ot])

    eps_c = P.sb("eps_c", [128, 1], F32)
    P.op("pool", lambda e: e.memset(eps_c[:], EPS), writes=[eps_c])

    G_bc = P.sb("G_bc", [128, D], F32)
    xs = [P.sb("xs%d" % i, [128, D], F32) for i in range(2)]
    sq_junk = P.sb("sq_junk", [128, D], F32)
    xh = [P.sb("xh%d" % i, [128, D], BF16) for i in range(2)]
    stat = [P.sb("stat%d" % i, [128, 16], F32) for i in range(4)]
    comb_all = P.sb("comb_all", [128, NT, 8], F32)

    modc = P.sb("modc", [128, DEPTH, 4, KD], F32)
    nmc = P.sb("nmc", [128, DEPTH, 2, KD], F32)
    cosT = P.sb("cosT", [128, NT, 16], F32)
    sinT = P.sb("sinT", [128, NT, 16], F32)
    P.push()
    c_col = P.sb("c_colsb", [128, KD], F32)
    P.dma("sp", c_col[:], c_in[:], writes=[c_col])
    c_e = P.sb("c_e", [128, KD], F32)
    c_act = P.sb("c_act", [128, KD], F32)
    P.op("act", lambda e: e.activation(out=c_e[:], in_=c_col[:], func=AF.Exp, scale=-1.0), reads=[c_col], writes=[c_e])
    P.op("dve", lambda e: e.tensor_scalar(out=c_e[:], in0=c_e[:], scalar1=1.0, scalar2=None, op0=ALU.add), reads=[c_e], writes=[c_e])
    P.op("dve", lambda e: e.reciprocal(out=c_e[:], in_=c_e[:]), reads=[c_e], writes=[c_e])
    P.op("dve", lambda e: e.tensor_tensor(out=c_act[:], in0=c_col[:], in1=c_e[:], op=ALU.mult), reads=[c_col, c_e], writes=[c_act])
    C_bc = P.sb("C_bc", [128, KD, 128], F32)
    for k in range(KD):
        P.op("dve", lambda e, k=k: e.tensor_scalar(out=C_bc[:, k, :], in0=ones_f[:, 0:128], scalar1=c_act[:, k:k + 1], scalar2=None,
                                                   op0=ALU.mult), reads=[ones_f, c_act], writes=[C_bc])
    for l in range(n_layers):
        P.dma("sp", nmc[:, l, 0, :], A["norm_mix_c"][l], writes=[nmc])
        P.dma("sp", nmc[:, l, 1, :], A["norm_ffn_c"][l], writes=[nmc])
    wst = [P.sb("wst%d" % i, [128, 2048], F32) for i in range(3)]
    wst_i = [0]

    def next_wst():
        t = wst[wst_i[0] % 3]
        wst_i[0] += 1
        return t
    ada_sb = P.sb("ada_sb", [128, 2048], F32)
    brow = P.sb("brow", [128, 2048], F32)
    for l in range(n_layers):
        for ng in range(3):
            P.dma("sp", brow[:], A["b_ada"][l][:, ng * 2048:(ng + 1) * 2048].to_broadcast([128, 2048]), writes=[brow])
            for k in range(KD):
                st = next_wst()
                P.dma("sp", st[:], A["w_ada"][l, k * 128:(k + 1) * 128, ng * 2048:(ng + 1) * 2048], writes=[st])
                for j in range(4):
                    P.op("pe", lambda e, st=st, j=j, k=k: e.matmul(FB[j][:], lhsT=C_bc[:, k, :], rhs=st[:, j * 512:(j + 1) * 512],
                                                                 start=(k == 0), stop=(k == KD - 1)), reads=[st, C_bc], writes=[FB[j]])
            for j in range(4):
                P.op("dve", lambda e, j=j: e.tensor_tensor(out=ada_sb[:, j * 512:(j + 1) * 512], in0=FB[j][:], in1=brow[:, j * 512:(j + 1) * 512], op=ALU.add),
                     reads=[FB[j], brow], writes=[ada_sb])
            for half in range(2):
                seg = 2 * ng + half
                src = ada_sb[:, half * 1024:(half + 1) * 1024]
                if seg in (2, 5):
                    P.dma("sp", g12[l, 0 if seg == 2 else 1], src, reads=[ada_sb], writes=[g12])
                else:
                    v = {0: 1, 1: 0, 3: 3, 4: 2}[seg]
                    for k in range(KD):
                        P.op("pe", lambda e, half=half, k=k: e.transpose(FB[4][:, 0:128], ada_sb[:, half * 1024 + k * 128: half * 1024 + (k + 1) * 128], identf[:]),
                             reads=[ada_sb, identf], writes=[FB[4]])
                        if seg in (1, 4):
                            nm = 0 if seg == 1 else 1
                            P.op("dve", lambda e, l=l, v=v, k=k, nm=nm: e.scalar_tensor_tensor(
                                out=modc[:, l, v, k:k + 1], in0=FB[4][:, 0:1], scalar=1.0, in1=nmc[:, l, nm, k:k + 1],
                                op0=ALU.add, op1=ALU.mult), reads=[FB[4], nmc], writes=[modc])
                        else:
                            P.op("dve", lambda e, l=l, v=v, k=k: e.tensor_copy(out=modc[:, l, v, k:k + 1], in_=FB[4][:, 0:1]),
                                 reads=[FB[4]], writes=[modc])

    posi = P.sb("posi", [128, NT], I32)
    posf = P.sb("posf", [128, NT], F32)
    inv_bc = P.sb("inv_bcs", [128, 16], F32)
    ang = P.sb("ang", [128, NT, 16], F32)
    kk = P.sb("kk", [128, NT, 16], F32)
    P.dma("sp", posi[:], pos_in[:], writes=[posi])
    P.dma("sp", inv_bc[:], inv_in[:], writes=[inv_bc])
    P.op("dve", lambda e: e.tensor_copy(out=posf[:], in_=posi[:]), reads=[posi], writes=[posf])
    for t in range(NT):
        P.op("dve", lambda e, t=t: e.tensor_scalar(out=ang[:, t, :], in0=inv_bc[:], scalar1=posf[:, t:t + 1], scalar2=None, op0=ALU.mult),
             reads=[inv_bc, posf], writes=[ang])

    MAGIC = 12582912.0
    C1 = 6.28125
    C2 = 2.0 * math.pi - C1

    def sin_reduced(dst, src_t, src_v, shape_v, shift, tmp_t):
        tv = tmp_t[:] if shape_v is None else shape_v(tmp_t)
        P.op("dve", lambda e: e.tensor_scalar(out=tv, in0=src_v, scalar1=shift, scalar2=1.0 / (2 * math.pi), op0=ALU.add, op1=ALU.mult),
             reads=[src_t], writes=[tmp_t])
        P.op("dve", lambda e: e.tensor_scalar(out=tv, in0=tv, scalar1=MAGIC, scalar2=None, op0=ALU.add), reads=[tmp_t], writes=[tmp_t])
        P.op("dve", lambda e: e.tensor_scalar(out=tv, in0=tv, scalar1=-MAGIC, scalar2=None, op0=ALU.add), reads=[tmp_t], writes=[tmp_t])
        P.op("dve", lambda e: e.scalar_tensor_tensor(out=dst[0][:] if shape_v is None else shape_v(dst[0]), in0=tv, scalar=-C1, in1=src_v,
                                                     op0=ALU.mult, op1=ALU.add), reads=[tmp_t, src_t], writes=[dst[0]])
        dv = dst[0][:] if shape_v is None else shape_v(dst[0])
        P.op("dve", lambda e: e.scalar_tensor_tensor(out=dv, in0=tv, scalar=-C2, in1=dv, op0=ALU.mult, op1=ALU.add),
             reads=[tmp_t, dst[0]], writes=[dst[0]])
        P.op("dve", lambda e: e.tensor_scalar(out=dv, in0=dv, scalar1=shift, scalar2=3.14159, op0=ALU.add, op1=ALU.min), reads=[dst[0]], writes=[dst[0]])
        P.op("dve", lambda e: e.tensor_scalar(out=dv, in0=dv, scalar1=-3.14159, scalar2=None, op0=ALU.max), reads=[dst[0]], writes=[dst[0]])
        P.op("act", lambda e: e.activation(out=dv, in_=dv, func=AF.Sin), reads=[dst[0]], writes=[dst[0]])

    sin_reduced((sinT,), ang, ang[:], None, 0.0, kk)
    sin_reduced((cosT,), ang, ang[:], None, math.pi / 2, kk)
    onesrow = P.sb("onesrow", [NH, L], BF16)
    P.op("pool", lambda e: e.memset(onesrow[:], 1.0), writes=[onesrow])
    for r_ in (66, 67):
        P.dma("sp", QTf[:, r_, :], onesrow[:], reads=[onesrow], writes=[QTf])
    for r_ in (64, 65):
        P.dma("sp", KTf[:, r_, :], onesrow[:], reads=[onesrow], writes=[KTf])
    P.pop()

    def load_layer(l):
        lp = lambda n: A[n][l]
        P.dma("pool", w_in_sb[:], lp("w_in").rearrange("(k p) n -> p k n", p=128), writes=[w_in_sb])
        P.dma("pool", w_uq_sb[:], lp("w_uq").rearrange("(k p) n -> p k n", p=128), writes=[w_uq_sb])
        P.dma("pool", w_ukv_sb[:], lp("w_ukv"), writes=[w_ukv_sb])
        P.dma("pool", w_glu_sb[:], lp("w_glu").rearrange("(k p) n -> p k n", p=128), writes=[w_glu_sb])
        for t_, n_ in ((qn_c, "q_norm"), (kvn_c, "kv_norm"), (gqm, "gq_m"), (gkm, "gk_m"), (gqf, "gq_f"), (gkf, "gk_f"),
                       (dcol, "ssm_d"), (bglu_c, "b_glu")):
            P.dma("sp", t_[:], lp(n_), writes=[t_])
        P.dma("sp", nbf[:], lp("fox_bf"), writes=[nbf])
        P.op("dve", lambda e: e.tensor_scalar(out=nbf[:], in0=nbf[:], scalar1=-1.0, scalar2=None, op0=ALU.mult), reads=[nbf], writes=[nbf])
        P.op("dve", lambda e: e.tensor_scalar(out=nbglu_c[:], in0=bglu_c[:], scalar1=-1.0, scalar2=None, op0=ALU.mult), reads=[bglu_c], writes=[nbglu_c])
        for m in range(2):
            P.op("dve", lambda e, m=m: e.tensor_scalar(out=Ddiag[:, m, :], in0=identf[:], scalar1=dcol[:, m:m + 1], scalar2=None, op0=ALU.mult),
                 reads=[identf, dcol], writes=[Ddiag])
        V = lambda i: s5p[:, i, :]
        LRE, LIM, LDT, DT, ZRE, ZIM, MAG, SN, CS, LBR, LBI, DEN, KR, KI, NKI, T1, T2, CK, SK, CK2, SK2 = range(21)
        P.dma("sp", V(LRE), lp("lam_re"), writes=[s5p])
        P.dma("sp", V(LIM), lp("lam_im"), writes=[s5p])
        P.dma("sp", V(LDT), lp("log_dt"), writes=[s5p])
        sop = lambda fn: P.op("dve", fn, reads=[s5p], writes=[s5p])
        P.op("act", lambda e: e.activation(out=V(DT), in_=V(LDT), func=AF.Exp), reads=[s5p], writes=[s5p])
        sop(lambda e: e.tensor_tensor(out=V(ZRE), in0=V(LRE), in1=V(DT), op=ALU.mult))
        sop(lambda e: e.tensor_tensor(out=V(ZIM), in0=V(LIM), in1=V(DT), op=ALU.mult))
        P.op("act", lambda e: e.activation(out=V(MAG), in_=V(ZRE), func=AF.Exp), reads=[s5p], writes=[s5p])
        sv = lambda i: (lambda t: t[:, i, :])
        sin_reduced((s5p,), s5p, V(ZIM), sv(SN), 0.0, s5p) if False else None
        for dst_i, shift in ((SN, 0.0), (CS, math.pi / 2)):
            sop(lambda e, shift=shift: e.tensor_scalar(out=V(T1), in0=V(ZIM), scalar1=shift, scalar2=1.0 / (2 * math.pi), op0=ALU.add, op1=ALU.mult))
            sop(lambda e: e.tensor_scalar(out=V(T1), in0=V(T1), scalar1=MAGIC, scalar2=None, op0=ALU.add))
            sop(lambda e: e.tensor_scalar(out=V(T1), in0=V(T1), scalar1=-MAGIC, scalar2=None, op0=ALU.add))
            sop(lambda e, dst_i=dst_i: e.scalar_tensor_tensor(out=V(dst_i), in0=V(T1), scalar=-C1, in1=V(ZIM), op0=ALU.mult, op1=ALU.add))
            sop(lambda e, dst_i=dst_i: e.scalar_tensor_tensor(out=V(dst_i), in0=V(T1), scalar=-C2, in1=V(dst_i), op0=ALU.mult, op1=ALU.add))
            sop(lambda e, dst_i=dst_i, shift=shift: e.tensor_scalar(out=V(dst_i), in0=V(dst_i), scalar1=shift, scalar2=3.14159, op0=ALU.add, op1=ALU.min))
            sop(lambda e, dst_i=dst_i: e.tensor_scalar(out=V(dst_i), in0=V(dst_i), scalar1=-3.14159, scalar2=None, op0=ALU.max))
            P.op("act", lambda e, dst_i=dst_i: e.activation(out=V(dst_i), in_=V(dst_i), func=AF.Sin), reads=[s5p], writes=[s5p])
        sop(lambda e: e.tensor_tensor(out=V(LBR), in0=V(MAG), in1=V(CS), op=ALU.mult))
        sop(lambda e: e.tensor_tensor(out=V(LBI), in0=V(MAG), in1=V(SN), op=ALU.mult))
        sop(lambda e: e.tensor_tensor(out=V(DEN), in0=V(LRE), in1=V(LRE), op=ALU.mult))
        sop(lambda e: e.tensor_tensor(out=V(T1), in0=V(LIM), in1=V(LIM), op=ALU.mult))
        sop(lambda e: e.tensor_tensor(out=V(DEN), in0=V(DEN), in1=V(T1), op=ALU.add))
        sop(lambda e: e.reciprocal(out=V(DEN), in_=V(DEN)))
        sop(lambda e: e.tensor_scalar(out=V(T2), in0=V(LBR), scalar1=-1.0, scalar2=None, op0=ALU.add))
        sop(lambda e: e.tensor_tensor(out=V(KR), in0=V(T2), in1=V(LRE), op=ALU.mult))
        sop(lambda e: e.tensor_tensor(out=V(T1), in0=V(LBI), in1=V(LIM), op=ALU.mult))
        sop(lambda e: e.tensor_tensor(out=V(KR), in0=V(KR), in1=V(T1), op=ALU.add))
        sop(lambda e: e.tensor_tensor(out=V(KR), in0=V(KR), in1=V(DEN), op=ALU.mult))
        sop(lambda e: e.tensor_tensor(out=V(KI), in0=V(LBI), in1=V(LRE), op=ALU.mult))
        sop(lambda e: e.tensor_tensor(out=V(T1), in0=V(T2), in1=V(LIM), op=ALU.mult))
        sop(lambda e: e.tensor_tensor(out=V(KI), in0=V(KI), in1=V(T1), op=ALU.subtract))
        sop(lambda e: e.tensor_tensor(out=V(KI), in0=V(KI), in1=V(DEN), op=ALU.mult))
        sop(lambda e: e.tensor_scalar(out=V(NKI), in0=V(KI), scalar1=-1.0, scalar2=None, op0=ALU.mult))
        for j in range(8):
            P.dma("sp", blk_f[:, 0, :], lp("b_re")[j], writes=[blk_f])
            P.dma("sp", blk_f[:, 1, :], lp("b_im")[j], writes=[blk_f])
            P.op("dve", lambda e, j=j: e.tensor_scalar(out=blk_o[:, 0, :], in0=blk_f[:, 0, :], scalar1=s5p[:, KR, j:j + 1], scalar2=None, op0=ALU.mult),
                 reads=[blk_f, s5p], writes=[blk_o])
            P.op("dve", lambda e, j=j: e.scalar_tensor_tensor(out=blk_o[:, 0, :], in0=blk_f[:, 1, :], scalar=s5p[:, NKI, j:j + 1], in1=blk_o[:, 0, :],
                                                              op0=ALU.mult, op1=ALU.add), reads=[blk_f, s5p, blk_o], writes=[blk_o])
            P.op("dve", lambda e, j=j: e.tensor_scalar(out=blk_o[:, 1, :], in0=blk_f[:, 1, :], scalar1=s5p[:, KR, j:j + 1], scalar2=None, op0=ALU.mult),
                 reads=[blk_f, s5p], writes=[blk_o])
            P.op("dve", lambda e, j=j: e.scalar_tensor_tensor(out=blk_o[:, 1, :], in0=blk_f[:, 0, :], scalar=s5p[:, KI, j:j + 1], in1=blk_o[:, 1, :],
                                                              op0=ALU.mult, op1=ALU.add), reads=[blk_f, s5p, blk_o], writes=[blk_o])
            for ri, dstT in ((0, BreT), (1, BimT)):
                P.op("pe", lambda e, ri=ri: e.transpose(FB[5][:, 0:128], blk_o[:, ri, :], identf[:]), reads=[blk_o, identf], writes=[FB[5]])
                P.op("act", lambda e, dstT=dstT, j=j: e.activation(out=dstT[:, j, :], in_=FB[5][:, 0:128], func=AF.Copy), reads=[FB[5]], writes=[dstT])
        P.dma("pool", CreT[:], lp("c_re").rearrange("j p f -> p j f"), writes=[CreT])
        P.dma("pool", nCimT[:], lp("c_im").rearrange("j p f -> p j f"), writes=[nCimT])
        P.op("dve", lambda e: e.tensor_scalar(out=nCimT[:], in0=nCimT[:], scalar1=-1.0, scalar2=None, op0=ALU.mult), reads=[nCimT], writes=[nCimT])
        P.op("dve", lambda e: e.memset(Ctab[:, :, 0:1], 1.0), writes=[Ctab])
        P.op("dve", lambda e: e.memset(Stab[:, :, 0:1], 0.0), writes=[Stab])
        sop(lambda e: e.tensor_copy(out=V(CK), in_=V(CS)))
        sop(lambda e: e.tensor_copy(out=V(SK), in_=V(SN)))
        n = 1
        while n < SUB:
            tabt_v = pt[0][:].rearrange("p a b -> p (a b)")[:, 0:8 * n].rearrange("p (j n) -> p j n", j=8)
            tabu_v = pt[1][:].rearrange("p a b -> p (a b)")[:, 0:8 * n].rearrange("p (j n) -> p j n", j=8)
            tabt = pt[0]
            tabu = pt[1]
            ckb = s5p[:, CK, :].unsqueeze(2).to_broadcast([128, 8, n])
            skb = s5p[:, SK, :].unsqueeze(2).to_broadcast([128, 8, n])
            P.op("dve", lambda e, n=n, ckb=ckb: e.tensor_tensor(out=tabt_v, in0=Ctab[:, :, 0:n], in1=ckb, op=ALU.mult), reads=[Ctab, s5p], writes=[tabt])
            P.op("dve", lambda e, n=n, skb=skb: e.tensor_tensor(out=tabu_v, in0=Stab[:, :, 0:n], in1=skb, op=ALU.mult), reads=[Stab, s5p], writes=[tabu])
            P.op("dve", lambda e, n=n: e.tensor_tensor(out=Ctab[:, :, n:2 * n], in0=tabt_v, in1=tabu_v, op=ALU.subtract), reads=[tabt, tabu], writes=[Ctab])
            P.op("dve", lambda e, n=n, skb=skb: e.tensor_tensor(out=tabt_v, in0=Ctab[:, :, 0:n], in1=skb, op=ALU.mult), reads=[Ctab, s5p], writes=[tabt])
            P.op("dve", lambda e, n=n, ckb=ckb: e.tensor_tensor(out=tabu_v, in0=Stab[:, :, 0:n], in1=ckb, op=ALU.mult), reads=[Stab, s5p], writes=[tabu])
            P.op("dve", lambda e, n=n: e.tensor_tensor(out=Stab[:, :, n:2 * n], in0=tabt_v, in1=tabu_v, op=ALU.add), reads=[tabt, tabu], writes=[Stab])
            sop(lambda e: e.tensor_tensor(out=V(T1), in0=V(CK), in1=V(CK), op=ALU.mult))
            sop(lambda e: e.tensor_tensor(out=V(T2), in0=V(SK), in1=V(SK), op=ALU.mult))
            sop(lambda e: e.tensor_tensor(out=V(SK2), in0=V(CK), in1=V(SK), op=ALU.mult))
            sop(lambda e: e.tensor_tensor(out=V(CK), in0=V(T1), in1=V(T2), op=ALU.subtract))
            sop(lambda e: e.tensor_scalar(out=V(SK), in0=V(SK2), scalar1=2.0, scalar2=None, op0=ALU.mult))
            n *= 2
        P.op("dve", lambda e: e.tensor_copy(out=Rtab[:], in_=s5p[:, MAG, :].unsqueeze(2).to_broadcast([128, 8, SUB])), reads=[s5p], writes=[Rtab])
        return dict(CK=CK, SK=SK)

    def load_c(l):
        P.dma("sp", onorm_c[:], A["out_norm"][l], writes=[onorm_c])
        P.dma("sp", G_bc[:], g12[l, 0], reads=[g12], writes=[G_bc])
        for k in range(KD):
            st = wstc[k % 2]
            P.dma("sp", st[:], A["w_out"][l][k * 128:(k + 1) * 128, :], writes=[st])
            P.op("dve", lambda e, st=st, k=k: e.scalar_tensor_tensor(out=w_out_sb[:, k, :], in0=st[:], scalar=onorm_c[:, k:k + 1], in1=G_bc[:],
                                                                     op0=ALU.mult, op1=ALU.mult), reads=[st, onorm_c, G_bc], writes=[w_out_sb])

    def norm_transpose(src_t, src_v, l, v_a, v_b, dst_t, dst_slice, si, fp32_path=None):
        st_ = stat[si % 4]
        P.op("act", lambda e: e.activation(out=sq_junk[:], in_=src_v, func=AF.Square, accum_out=st_[:, 0:1]), reads=[src_t], writes=[sq_junk, st_])
        P.op("act", lambda e: e.activation(out=st_[:, 1:2], in_=st_[:, 0:1], func=AF.Sqrt, scale=1.0 / D, bias=eps_c[:, 0:1]), reads=[st_, eps_c], writes=[st_])
        P.op("dve", lambda e: e.reciprocal(out=st_[:, 2:3], in_=st_[:, 1:2]), reads=[st_], writes=[st_])
        xh_ = xh[si % 2]
        P.op("dve", lambda e: e.tensor_scalar(out=xh_[:], in0=src_v, scalar1=st_[:, 2:3], scalar2=None, op0=ALU.mult), reads=[src_t, st_], writes=[xh_])
        tb = TB[si % 2]
        for k in range(KD):
            P.op("pe", lambda e, k=k: e.transpose(tb[:, k * 128:(k + 1) * 128], xh_[:, k * 128:(k + 1) * 128], ident[:]), reads=[xh_, ident], writes=[tb])
        for k in range(KD):
            P.op("act", lambda e, k=k: e.activation(out=dst_t[:, k, dst_slice], in_=tb[:, k * 128:(k + 1) * 128], func=AF.Identity,
                                                    scale=modc[:, l, v_a, k:k + 1], bias=modc[:, l, v_b, k:k + 1]), reads=[tb, modc], writes=[dst_t])
        if fp32_path is not None:
            xhf_, dstf = fp32_path
            P.op("dve", lambda e: e.tensor_scalar(out=xhf_[:], in0=src_v, scalar1=st_[:, 2:3], scalar2=None, op0=ALU.mult), reads=[src_t, st_], writes=[xhf_])
            for k in range(KD):
                fb = FB[k % 2]
                P.op("pe", lambda e, k=k, fb=fb: e.transpose(fb[:, 0:128], xhf_[:, k * 128:(k + 1) * 128], identf[:]), reads=[xhf_, identf], writes=[fb])
                P.op("act", lambda e, k=k, fb=fb: e.activation(out=dstf[:, k, :], in_=fb[:, 0:128], func=AF.Identity,
                                                             scale=modc[:, l, v_a, k:k + 1], bias=modc[:, l, v_b, k:k + 1]), reads=[fb, modc], writes=[dstf])

    def head_rms(src_t, src_v3, nh, hd, gain_t, dst_t, dst_v3, si, extra_ss=None):
        st_ = stat[si % 4]
        P.op("act", lambda e: e.activation(out=sq_junk[:, 0:nh * hd].rearrange("p (h d) -> p h d", h=nh), in_=src_v3, func=AF.Square), reads=[src_t], writes=[sq_junk])
        P.op("dve", lambda e: e.tensor_reduce(out=st_[:, 0:nh], in_=sq_junk[:, 0:nh * hd].rearrange("p (h d) -> p h d", h=nh), axis=AX.X, op=ALU.add),
             reads=[sq_junk], writes=[st_])
        tot = hd
        if extra_ss is not None:
            et, ev, en = extra_ss
            P.op("dve", lambda e: e.tensor_scalar(out=st_[:, 0:nh], in0=st_[:, 0:nh], scalar1=ev, scalar2=None, op0=ALU.add), reads=[st_, et], writes=[st_])
            tot = hd + en
        P.op("act", lambda e: e.activation(out=st_[:, 6:6 + nh], in_=st_[:, 0:nh], func=AF.Sqrt, scale=1.0 / tot, bias=eps_c[:, 0:1]), reads=[st_, eps_c], writes=[st_])
        P.op("dve", lambda e: e.reciprocal(out=st_[:, 6:6 + nh], in_=st_[:, 6:6 + nh]), reads=[st_], writes=[st_])
        return st_

    def rope(src_t, dst_t, cos_v, sin_v):
        x1 = src_t[:, :, 64:80]
        x2 = src_t[:, :, 80:96]
        cb = cos_v.unsqueeze(1).to_broadcast([128, NH, 16])
        sb_ = sin_v.unsqueeze(1).to_broadcast([128, NH, 16])
        P.op("dve", lambda e: e.tensor_copy(out=dst_t[:, :, 0:64], in_=src_t[:, :, 0:64]), reads=[src_t], writes=[dst_t])
        P.op("dve", lambda e: e.tensor_tensor(out=rt[0][:], in0=x1, in1=cb, op=ALU.mult), reads=[src_t, cosT], writes=[rt[0]])
        P.op("dve", lambda e: e.tensor_tensor(out=rt[1][:], in0=x2, in1=sb_, op=ALU.mult), reads=[src_t, sinT], writes=[rt[1]])
        P.op("dve", lambda e: e.tensor_tensor(out=dst_t[:, :, 64:80], in0=rt[0][:], in1=rt[1][:], op=ALU.subtract), reads=[rt[0], rt[1]], writes=[dst_t])
        P.op("dve", lambda e: e.tensor_tensor(out=rt[2][:], in0=x1, in1=sb_, op=ALU.mult), reads=[src_t, sinT], writes=[rt[2]])
        P.op("dve", lambda e: e.tensor_tensor(out=rt[3][:], in0=x2, in1=cb, op=ALU.mult), reads=[src_t, cosT], writes=[rt[3]])
        P.op("dve", lambda e: e.tensor_tensor(out=dst_t[:, :, 80:96], in0=rt[2][:], in1=rt[3][:], op=ALU.add), reads=[rt[2], rt[3]], writes=[dst_t])

    def phase_a(l, s5c):
        src = x_in if l == 0 else None
        for b in range(NB):
            hTb = hT[b % 2]
            for ti in range(4):
                t = b * 4 + ti
                xs_ = xs[t % 2]
                if l == 0:
                    P.dma("sp", xs_[:], x_in[t * 128:(t + 1) * 128, :], writes=[xs_])
                else:
                    P.dma("sp", xs_[:], y[t * 128:(t + 1) * 128, :], reads=[ytile[t]], writes=[xs_])
                norm_transpose(xs_, xs_[:], l, 0, 1, hTb, slice(ti * 128, (ti + 1) * 128), t)
            for m in range(2):
                for k in range(KD):
                    P.op("pe", lambda e, m=m, k=k: e.matmul(FB[m][:], lhsT=w_in_sb[:, k, m * 128:(m + 1) * 128], rhs=hTb[:, k, :], start=(k == 0), stop=(k == KD - 1)),
                         reads=[w_in_sb, hTb], writes=[FB[m]])
                P.op("act", lambda e, m=m: e.activation(out=uT[:, m, :], in_=FB[m][:], func=AF.Copy), reads=[FB[m]], writes=[uT])
            for k in range(KD):
                P.op("pe", lambda e, k=k: e.matmul(FB[2][0:NH, :], lhsT=w_in_sb[:, k, 1824:1830], rhs=hTb[:, k, :], start=(k == 0), stop=(k == KD - 1)),
                     reads=[w_in_sb, hTb], writes=[FB[2]])
            P.op("act", lambda e: e.activation(out=fg_e[:], in_=FB[2][0:NH, :], func=AF.Exp, scale=-1.0, bias=nbf[:, 0:1]), reads=[FB[2], nbf], writes=[fg_e])
            P.op("act", lambda e: e.activation(out=fg_sp[:], in_=fg_e[:], func=AF.Ln, bias=ones_f[0:NH, 0:1]), reads=[fg_e, ones_f], writes=[fg_sp])
            if b == 0:
                P.op("dve", lambda e: e.tensor_tensor_scan(out=fg_cum[:], data0=ones_f[0:NH, 0:512], data1=fg_sp[:], initial=0.0, op0=ALU.mult, op1=ALU.add),
                     reads=[ones_f, fg_sp], writes=[fg_cum])
            else:
                P.op("dve", lambda e: e.tensor_tensor_scan(out=fg_cum[:], data0=ones_f[0:NH, 0:512], data1=fg_sp[:], initial=fg_carry[:, 0:1], op0=ALU.mult, op1=ALU.add),
                     reads=[ones_f, fg_sp, fg_carry], writes=[fg_cum])
            P.op("dve", lambda e: e.tensor_copy(out=fg_carry[:], in_=fg_cum[:, 511:512]), reads=[fg_cum], writes=[fg_carry])
            P.op("dve", lambda e: e.tensor_scalar(out=fg_sp[:], in0=fg_cum[:], scalar1=8.0, scalar2=None, op0=ALU.mult), reads=[fg_cum], writes=[fg_sp])
            P.op("dve", lambda e: e.tensor_copy(out=fg_hi[:], in_=fg_sp[:]), reads=[fg_sp], writes=[fg_hi])
            P.op("dve", lambda e: e.tensor_tensor(out=fg_lo[:], in0=fg_sp[:], in1=fg_hi[:], op=ALU.subtract), reads=[fg_sp, fg_hi], writes=[fg_lo])
            P.op("dve", lambda e: e.tensor_scalar(out=fg_nhi[:], in0=fg_hi[:], scalar1=-1.0, scalar2=None, op0=ALU.mult), reads=[fg_hi], writes=[fg_nhi])
            P.op("dve", lambda e: e.tensor_scalar(out=fg_nlo[:], in0=fg_lo[:], scalar1=-1.0, scalar2=None, op0=ALU.mult), reads=[fg_lo], writes=[fg_nlo])
            bs = slice(b * 512, (b + 1) * 512)
            P.dma("sp", QTf[:, 64, bs], fg_nhi[:], reads=[fg_nhi], writes=[QTf])
            P.dma("sp", QTf[:, 65, bs], fg_nlo[:], reads=[fg_nlo], writes=[QTf])
            P.dma("sp", KTf[:, 66, bs], fg_hi[:], reads=[fg_hi], writes=[KTf])
            P.dma("sp", KTf[:, 67, bs], fg_lo[:], reads=[fg_lo], writes=[KTf])

            for ti in range(4):
                t = b * 4 + ti
                tsl = slice(ti * 128, (ti + 1) * 128)
                segs = ((FB[2], 256, 416), (FB[3], 672, 384), (FB[4], 1056, 384), (FB[5], 1440, 384))
                for fb, c0, w in segs:
                    for k in range(KD):
                        P.op("pe", lambda e, fb=fb, c0=c0, w=w, k=k: e.matmul(fb[:, 0:w], lhsT=hTb[:, k, tsl], rhs=w_in_sb[:, k, c0:c0 + w], start=(k == 0), stop=(k == KD - 1)),
                             reads=[hTb, w_in_sb], writes=[fb])
                st_ = stat[0]
                P.op("act", lambda e: e.activation(out=sq_junk[:, 0:256], in_=FB[2][:, 0:256], func=AF.Square, accum_out=st_[:, 12:13]), reads=[FB[2]], writes=[sq_junk, st_])
                P.op("act", lambda e: e.activation(out=st_[:, 13:14], in_=st_[:, 12:13], func=AF.Sqrt, scale=1.0 / 256, bias=eps_c[:, 0:1]), reads=[st_, eps_c], writes=[st_])
                P.op("dve", lambda e: e.reciprocal(out=st_[:, 13:14], in_=st_[:, 13:14]), reads=[st_], writes=[st_])
                P.op("dve", lambda e: e.tensor_scalar(out=cq_h[:], in0=FB[2][:, 0:256], scalar1=st_[:, 13:14], scalar2=None, op0=ALU.mult), reads=[FB[2], st_], writes=[cq_h])
                st1 = stat[1]
                P.op("act", lambda e: e.activation(out=sq_junk[:, 256:384], in_=FB[2][:, 256:384], func=AF.Square, accum_out=st1[:, 12:13]), reads=[FB[2]], writes=[sq_junk, st1])
                P.op("act", lambda e: e.activation(out=st1[:, 13:14], in_=st1[:, 12:13], func=AF.Sqrt, scale=1.0 / 128, bias=eps_c[:, 0:1]), reads=[st1, eps_c], writes=[st1])
                P.op("dve", lambda e: e.reciprocal(out=st1[:, 13:14], in_=st1[:, 13:14]), reads=[st1], writes=[st1])
                P.op("dve", lambda e: e.tensor_scalar(out=ckv_h[:], in0=FB[2][:, 256:384], scalar1=st1[:, 13:14], scalar2=None, op0=ALU.mult), reads=[FB[2], st1], writes=[ckv_h])
                P.op("act", lambda e: e.activation(out=kn[:, 0, 64:96], in_=FB[2][:, 384:416], func=AF.Copy), reads=[FB[2]], writes=[kn])
                P.op("act", lambda e: e.activation(out=sq_junk[:, 384:416], in_=FB[2][:, 384:416], func=AF.Square, accum_out=st1[:, 14:15]), reads=[FB[2]], writes=[sq_junk, st1])
                tb = TB[0]
                for j in range(2):
                    P.op("pe", lambda e, j=j: e.transpose(tb[:, j * 128:(j + 1) * 128], cq_h[:, j * 128:(j + 1) * 128], ident[:]), reads=[cq_h, ident], writes=[tb])
                P.op("pe", lambda e: e.transpose(tb[:, 256:384], ckv_h[:], ident[:]), reads=[ckv_h, ident], writes=[tb])
                for j in range(2):
                    P.op("act", lambda e, j=j: e.activation(out=cqT[:, j, :], in_=tb[:, j * 128:(j + 1) * 128], func=AF.Copy, scale=qn_c[:, j:j + 1]), reads=[tb, qn_c], writes=[cqT])
                P.op("act", lambda e: e.activation(out=ckvT[:], in_=tb[:, 256:384], func=AF.Copy, scale=kvn_c[:, 0:1]), reads=[tb, kvn_c], writes=[ckvT])
                for j in range(2):
                    P.op("pe", lambda e, j=j: e.matmul(FB[0][:], lhsT=cqT[:, j, :], rhs=w_uq_sb[:, j, 0:512], start=(j == 0), stop=(j == 1)), reads=[cqT, w_uq_sb], writes=[FB[0]])
                for j in range(2):
                    P.op("pe", lambda e, j=j: e.matmul(FB[1][:, 0:64], lhsT=cqT[:, j, :], rhs=w_uq_sb[:, j, 512:576], start=(j == 0), stop=(j == 1)), reads=[cqT, w_uq_sb], writes=[FB[1]])
                P.op("act", lambda e: e.activation(out=qn[:].rearrange("p h d -> p (h d)")[:, 0:512], in_=FB[0][:], func=AF.Copy), reads=[FB[0]], writes=[qn])
                P.op("act", lambda e: e.activation(out=qn[:].rearrange("p h d -> p (h d)")[:, 512:576], in_=FB[1][:, 0:64], func=AF.Copy), reads=[FB[1]], writes=[qn])
                sq = head_rms(qn, qn[:], NH, 96, gqm, None, None, 2)
                P.op("dve", lambda e, sq=sq: e.tensor_tensor(out=qn[:], in0=qn[:], in1=sq[:, 6:12].unsqueeze(2).to_broadcast([128, NH, 96]), op=ALU.mult), reads=[qn, sq], writes=[qn])
                P.op("dve", lambda e: e.tensor_tensor(out=qn[:], in0=qn[:], in1=gqm[:].unsqueeze(1).to_broadcast([128, NH, 96]), op=ALU.mult), reads=[qn, gqm], writes=[qn])
                rope(qn, qfin, cosT[:, t, :], sinT[:, t, :])
                P.op("pe", lambda e: e.matmul(FB[0][:], lhsT=ckvT[:], rhs=w_ukv_sb[:, 0:512], start=True, stop=True), reads=[ckvT, w_ukv_sb], writes=[FB[0]])
                P.op("pe", lambda e: e.matmul(FB[1][:, 0:256], lhsT=ckvT[:], rhs=w_ukv_sb[:, 512:768], start=True, stop=True), reads=[ckvT, w_ukv_sb], writes=[FB[1]])
                vst = Vm_st[t % 2]
                kv0 = FB[0][:].rearrange("p (h d) -> p h d", h=4)
                kv1 = FB[1][:, 0:256].rearrange("p (h d) -> p h d", h=2)
                P.op("act", lambda e: e.activation(out=kn[:, 0:4, 0:64], in_=kv0[:, :, 0:64], func=AF.Copy), reads=[FB[0]], writes=[kn])
                P.op("act", lambda e: e.activation(out=kn[:, 4:6, 0:64], in_=kv1[:, :, 0:64], func=AF.Copy), reads=[FB[1]], writes=[kn])
                P.op("dve", lambda e: e.tensor_copy(out=vst[:, 0:4, 0:64], in_=kv0[:, :, 64:128]), reads=[FB[0]], writes=[vst])
                P.op("dve", lambda e: e.tensor_copy(out=vst[:, 4:6, 0:64], in_=kv1[:, :, 64:128]), reads=[FB[1]], writes=[vst])
                P.dma("sp", Vm[t * 128:(t + 1) * 128, :], vst[:].rearrange("p h d -> p (h d)"), reads=[vst], writes=[Vm])
                P.op("dve", lambda e: e.tensor_copy(out=kn[:, 1:6, 64:96], in_=kn[:, 0:1, 64:96].to_broadcast([128, 5, 32])), reads=[kn], writes=[kn])
                st3 = stat[3]
                P.op("act", lambda e: e.activation(out=sq_junk[:, 0:384].rearrange("p (h d) -> p h d", h=NH), in_=kn[:, :, 0:64], func=AF.Square), reads=[kn], writes=[sq_junk])
                P.op("dve", lambda e: e.tensor_reduce(out=st3[:, 0:NH], in_=sq_junk[:, 0:384].rearrange("p (h d) -> p h d", h=NH), axis=AX.X, op=ALU.add), reads=[sq_junk], writes=[st3])
                P.op("dve", lambda e: e.tensor_scalar(out=st3[:, 0:NH], in0=st3[:, 0:NH], scalar1=st1[:, 14:15], scalar2=None, op0=ALU.add), reads=[st3, st1], writes=[st3])
                P.op("act", lambda e: e.activation(out=st3[:, 6:12], in_=st3[:, 0:NH], func=AF.Sqrt, scale=1.0 / 96, bias=eps_c[:, 0:1]), reads=[st3, eps_c], writes=[st3])
                P.op("dve", lambda e: e.reciprocal(out=st3[:, 6:12], in_=st3[:, 6:12]), reads=[st3], writes=[st3])
                P.op("dve", lambda e: e.tensor_tensor(out=kn[:], in0=kn[:], in1=st3[:, 6:12].unsqueeze(2).to_broadcast([128, NH, 96]), op=ALU.mult), reads=[kn, st3], writes=[kn])
                P.op("dve", lambda e: e.tensor_tensor(out=kn[:], in0=kn[:], in1=gkm[:].unsqueeze(1).to_broadcast([128, NH, 96]), op=ALU.mult), reads=[kn, gkm], writes=[kn])
                rope(kn, kfin, cosT[:, t, :], sinT[:, t, :])
                gsl = slice(t * 128, (t + 1) * 128)
                for src_, st_l, dst_d in ((qfin, QTm_st, QTm), (kfin, KTm_st, KTm)):
                    tb2 = TB[1]
                    st_t = st_l[t % 2]
                    for h in range(NH):
                        P.op("pe", lambda e, h=h, src_=src_: e.transpose(tb2[0:96, h * 128:(h + 1) * 128], src_[:, h, :], ident[:]), reads=[src_, ident], writes=[tb2])
                    P.op("act", lambda e, st_t=st_t: e.activation(out=st_t[:], in_=tb2[0:96, 0:768].rearrange("p (h t) -> p h t", h=NH), func=AF.Copy), reads=[tb2], writes=[st_t])
                    P.dma("sp", dst_d[:, :, gsl].rearrange("h p t -> p h t"), st_t[:], reads=[st_t], writes=[dst_d])
                for fb, g_t, dstb, st_l, dst_d in ((FB[3], gqf, fqb, QTf_st, QTf), (FB[4], gkf, fkb, KTf_st, KTf)):
                    st_t = st_l[t % 2]
                    P.op("act", lambda e, fb=fb: e.activation(out=fqn[:].rearrange("p h d -> p (h d)"), in_=fb[:, 0:384], func=AF.Copy), reads=[fb], writes=[fqn])
                    sq = head_rms(fqn, fqn[:], NH, 64, g_t, None, None, 2)
                    P.op("dve", lambda e, sq=sq: e.tensor_tensor(out=fqn[:], in0=fqn[:], in1=sq[:, 6:12].unsqueeze(2).to_broadcast([128, NH, 64]), op=ALU.mult), reads=[fqn, sq], writes=[fqn])
                    P.op("dve", lambda e, g_t=g_t, dstb=dstb: e.tensor_tensor(out=dstb[:], in0=fqn[:], in1=g_t[:].unsqueeze(1).to_broadcast([128, NH, 64]), op=ALU.mult), reads=[fqn, g_t], writes=[dstb])
                    tb2 = TB[1]
                    for h in range(NH):
                        P.op("pe", lambda e, h=h, dstb=dstb: e.transpose(tb2[0:64, h * 128:(h + 1) * 128], dstb[:, h, :], ident[:]), reads=[dstb, ident], writes=[tb2])
                    P.op("act", lambda e, st_t=st_t: e.activation(out=st_t[:], in_=tb2[0:64, 0:768].rearrange("p (h t) -> p h t", h=NH), func=AF.Copy), reads=[tb2], writes=[st_t])
                    P.dma("sp", dst_d[:, 0:64, gsl].rearrange("h p t -> p h t"), st_t[:], reads=[st_t], writes=[dst_d])
                vst = Vf_st[t % 2]
                P.op("dve", lambda e, vst=vst: e.tensor_copy(out=vst[:, :, 0:64], in_=FB[5][:, 0:384].rearrange("p (h d) -> p h d", h=NH)), reads=[FB[5]], writes=[vst])
                P.dma("sp", Vf[t * 128:(t + 1) * 128, :], vst[:].rearrange("p h d -> p (h d)"), reads=[vst], writes=[Vf])
            for sc in range(512 // SUB):
                first = (b == 0 and sc == 0)
                ss_ = slice(sc * SUB, (sc + 1) * SUB)
                gs = slice(b * 512 + sc * SUB, b * 512 + (sc + 1) * SUB)
                if not first:
                    wl_re = wlast[:, 0, :]
                    wl_im = wlast[:, 1, :]
                    ck = s5p[:, s5c["CK"], :]
                    sk = s5p[:, s5c["SK"], :]
                    P.op("dve", lambda e: e.tensor_tensor(out=w0t[:, 0, :], in0=wl_re, in1=ck, op=ALU.mult), reads=[wlast, s5p], writes=[w0t])
                    P.op("dve", lambda e: e.tensor_tensor(out=w0t[:, 1, :], in0=wl_im, in1=sk, op=ALU.mult), reads=[wlast, s5p], writes=[w0t])
                    P.op("dve", lambda e: e.tensor_tensor(out=w0t[:, 2, :], in0=wl_re, in1=sk, op=ALU.mult), reads=[wlast, s5p], writes=[w0t])
                    P.op("dve", lambda e: e.tensor_tensor(out=w0t[:, 3, :], in0=wl_im, in1=ck, op=ALU.mult), reads=[wlast, s5p], writes=[w0t])
                    P.op("dve", lambda e: e.tensor_tensor(out=w0[:, 0, :], in0=w0t[:, 0, :], in1=w0t[:, 1, :], op=ALU.subtract), reads=[w0t], writes=[w0])
                    P.op("dve", lambda e: e.tensor_tensor(out=w0[:, 1, :], in0=w0t[:, 2, :], in1=w0t[:, 3, :], op=ALU.add), reads=[w0t], writes=[w0])
                for m in range(2):
                    hs = slice(4 * m, 4 * m + 4)
                    for jj in range(4):
                        j = 4 * m + jj
                        fbp = FB[j % 2]
                        P.op("pe", lambda e: e.matmul(fbp[:, 0:SUB], lhsT=BreT[:, j, :], rhs=uT[:, m, ss_], start=True, stop=True), reads=[BreT, uT], writes=[fbp])
                        P.op("pe", lambda e: e.matmul(fbp[:, SUB:2 * SUB], lhsT=BimT[:, j, :], rhs=uT[:, m, ss_], start=True, stop=True), reads=[BimT, uT], writes=[fbp])
                        b2 = fbp[:, 0:2 * SUB].rearrange("p (r t) -> p r t", r=2)
                        cb = Ctab[:, j, :].unsqueeze(1).to_broadcast([128, 2, SUB])
                        sb_ = Stab[:, j, :].unsqueeze(1).to_broadcast([128, 2, SUB])
                        P.op("dve", lambda e: e.tensor_tensor(out=pre_c[:], in0=b2, in1=cb, op=ALU.mult), reads=[fbp, Ctab], writes=[pre_c])
                        P.op("dve", lambda e: e.tensor_tensor(out=pre_s[:], in0=b2, in1=sb_, op=ALU.mult), reads=[fbp, Stab], writes=[pre_s])
                        P.op("dve", lambda e: e.tensor_tensor(out=pin_re[:], in0=pre_c[:, 0, :], in1=pre_s[:, 1, :], op=ALU.add), reads=[pre_c, pre_s], writes=[pin_re])
                        P.op("dve", lambda e: e.tensor_tensor(out=pin_im[:], in0=pre_c[:, 1, :], in1=pre_s[:, 0, :], op=ALU.subtract), reads=[pre_c, pre_s], writes=[pin_im])
                        for pin, Wt, ri in ((pin_re, W_re, 0), (pin_im, W_im, 1)):
                            if first:
                                P.op("dve", lambda e: e.tensor_tensor_scan(out=Wt[:, jj, :], data0=Rtab[:, j, :], data1=pin[:], initial=0.0, op0=ALU.mult, op1=ALU.add),
                                     reads=[Rtab, pin], writes=[Wt])
                            else:
                                P.op("dve", lambda e: e.tensor_tensor_scan(out=Wt[:, jj, :], data0=Rtab[:, j, :], data1=pin[:], initial=w0[:, ri, j:j + 1],
                                                                           op0=ALU.mult, op1=ALU.add), reads=[Rtab, pin, w0], writes=[Wt])
                    P.op("dve", lambda e: e.tensor_copy(out=wlast[:, 0, hs], in_=W_re[:, :, SUB - 1]), reads=[W_re], writes=[wlast])
                    P.op("dve", lambda e: e.tensor_copy(out=wlast[:, 1, hs], in_=W_im[:, :, SUB - 1]), reads=[W_im], writes=[wlast])
                    Ch = Ctab[:, hs, :]
                    Sh = Stab[:, hs, :]
                    P.op("dve", lambda e: e.tensor_tensor(out=pt[0][:], in0=W_re[:], in1=Ch, op=ALU.mult), reads=[W_re, Ctab], writes=[pt[0]])
                    P.op("dve", lambda e: e.tensor_tensor(out=pt[1][:], in0=W_im[:], in1=Sh, op=ALU.mult), reads=[W_im, Stab], writes=[pt[1]])
                    P.op("dve", lambda e: e.tensor_tensor(out=s_re[:], in0=pt[0][:], in1=pt[1][:], op=ALU.subtract), reads=[pt[0], pt[1]], writes=[s_re])
                    P.op("dve", lambda e: e.tensor_tensor(out=pt[0][:], in0=W_re[:], in1=Sh, op=ALU.mult), reads=[W_re, Stab], writes=[pt[0]])
                    P.op("dve", lambda e: e.tensor_tensor(out=pt[1][:], in0=W_im[:], in1=Ch, op=ALU.mult), reads=[W_im, Stab], writes=[pt[1]])
                    P.op("dve", lambda e: e.tensor_tensor(out=s_im[:], in0=pt[0][:], in1=pt[1][:], op=ALU.add), reads=[pt[0], pt[1]], writes=[s_im])
                    osl = slice(m * SUB, (m + 1) * SUB)
                    for jj in range(4):
                        j = 4 * m + jj
                        P.op("pe", lambda e: e.matmul(FB[2][:, osl], lhsT=CreT[:, j, :], rhs=s_re[:, jj, :], start=(jj == 0), stop=False), reads=[CreT, s_re], writes=[FB[2]])
                        P.op("pe", lambda e: e.matmul(FB[2][:, osl], lhsT=nCimT[:, j, :], rhs=s_im[:, jj, :], start=False, stop=False), reads=[nCimT, s_im], writes=[FB[2]])
                    P.op("pe", lambda e: e.matmul(FB[2][:, osl], lhsT=Ddiag[:, m, :], rhs=uT[:, m, ss_], start=False, stop=True), reads=[Ddiag, uT], writes=[FB[2]])
                yv = FB[2][:, 0:2 * SUB].rearrange("p (m t) -> p m t", m=2)
                P.op("act", lambda e: e.activation(out=yg[:], in_=yv, func=AF.Copy), reads=[FB[2]], writes=[yg])
                P.op("dve", lambda e: e.tensor_tensor(out=yt1[:], in0=yg[:], in1=yg[:], op=ALU.mult), reads=[yg], writes=[yt1])
                P.op("dve", lambda e: e.tensor_scalar(out=yt1[:], in0=yt1[:], scalar1=0.044715, scalar2=1.0, op0=ALU.mult, op1=ALU.add), reads=[yt1], writes=[yt1])
                P.op("dve", lambda e: e.tensor_tensor(out=yt1[:], in0=yt1[:], in1=yg[:], op=ALU.mult), reads=[yt1, yg], writes=[yt1])
                P.op("dve", lambda e: e.tensor_scalar(out=yt1[:], in0=yt1[:], scalar1=-45.0, scalar2=None, op0=ALU.max), reads=[yt1], writes=[yt1])
                P.op("act", lambda e: e.activation(out=yt1[:], in_=yt1[:], func=AF.Exp, scale=-1.5957691216), reads=[yt1], writes=[yt1])
                P.op("dve", lambda e: e.tensor_scalar(out=yt1[:], in0=yt1[:], scalar1=1.0, scalar2=None, op0=ALU.add), reads=[yt1], writes=[yt1])
                P.op("dve", lambda e: e.reciprocal(out=yt1[:], in_=yt1[:]), reads=[yt1], writes=[yt1])
                P.op("dve", lambda e: e.tensor_tensor(out=yg[:], in0=yg[:], in1=yt1[:], op=ALU.mult), reads=[yg, yt1], writes=[yg])
                P.op("dve", lambda e: e.tensor_copy(out=yTb[:], in_=yg[:]), reads=[yg], writes=[yTb])
                for mo in range(2):
                    osl = slice(mo * SUB, (mo + 1) * SUB)
                    for k in range(2):
                        P.op("pe", lambda e, mo=mo, k=k, osl=osl: e.matmul(FB[3][:, osl], lhsT=w_glu_sb[:, k, mo * 128:(mo + 1) * 128], rhs=yTb[:, k, :], start=(k == 0), stop=(k == 1)),
                             reads=[w_glu_sb, yTb], writes=[FB[3]])
                    P.op("act", lambda e, mo=mo, osl=osl: e.activation(out=yt2[:, mo, :], in_=FB[3][:, osl], func=AF.Exp, scale=-1.0, bias=nbglu_c[:, mo:mo + 1]), reads=[FB[3], nbglu_c], writes=[yt2])
                P.op("dve", lambda e: e.tensor_scalar(out=yt2[:], in0=yt2[:], scalar1=1.0, scalar2=None, op0=ALU.add), reads=[yt2], writes=[yt2])
                P.op("dve", lambda e: e.reciprocal(out=yt2[:], in_=yt2[:]), reads=[yt2], writes=[yt2])
                P.op("dve", lambda e: e.tensor_tensor(out=osm[:], in0=yg[:], in1=yt2[:], op=ALU.mult), reads=[yg, yt2], writes=[osm])
                P.op("dve", lambda e: e.tensor_tensor(out=o2[:], in0=osm[:], in1=osm[:], op=ALU.mult), reads=[osm], writes=[o2])
                for m in range(2):
                    P.op("pe", lambda e, m=m: e.matmul(FB[3][:, 0:SUB], lhsT=ones_f[:, 0:128], rhs=o2[:, m, :], start=(m == 0), stop=(m == 1)), reads=[ones_f, o2], writes=[FB[3]])
                P.op("act", lambda e: e.activation(out=rs_bc[:], in_=FB[3][:, 0:SUB], func=AF.Sqrt, scale=1.0 / 256, bias=eps_c[:, 0:1]), reads=[FB[3], eps_c], writes=[rs_bc])
                P.op("dve", lambda e: e.reciprocal(out=rs_bc[:], in_=rs_bc[:]), reads=[rs_bc], writes=[rs_bc])
                P.op("dve", lambda e: e.tensor_tensor(out=msm[:], in0=osm[:], in1=rs_bc[:].unsqueeze(1).to_broadcast([128, 2, SUB]), op=ALU.mult), reads=[osm, rs_bc], writes=[msm])
                P.dma("sp", mssm[:, :, gs].rearrange("m p t -> p m t"), msm[:], reads=[msm], writes=[mssm])

    def phase_b(l):
        hi = 0
        for mixer in range(2):
            QTd, KTd, Vd, dk, scale = ((QTm, KTm, Vm, 96, 1.0 / math.sqrt(96.0)), (QTf, KTf, Vf, 68, 0.125))[mixer]
            P.dma("sp", V_sb[:], Vd[:].rearrange("(t p) c -> p t c", p=128), reads=[Vd], writes=[V_sb])
            for h in range(NH):
                Qs = QT_sb[hi % 2]
                Ks = KT_sb[hi % 2]
                hi += 1
                P.dma("sp", Qs[0:dk, :], QTd[h], reads=[QTd], writes=[Qs])
                P.dma("sp", Ks[0:dk, :], KTd[h], reads=[KTd], writes=[Ks])
                it = 0
                for b in range(NB):
                    nk = 4 * b + 4
                    for kt in range(nk):
                        j = kt - 4 * b
                        q0 = 0 if j <= 0 else 128 * j
                        Sb = FB[it % 2]
                        pt_ = PT[it % 3]
                        it += 1
                        qsl = slice(b * 512 + q0, (b + 1) * 512)
                        P.op("pe", lambda e, Sb=Sb, kt=kt, qsl=qsl, q0=q0: e.matmul(Sb[:, q0:512], lhsT=Ks[0:dk, kt * 128:(kt + 1) * 128], rhs=Qs[0:dk, qsl], start=True, stop=True),
                             reads=[Ks, Qs], writes=[Sb])
                        P.op("act", lambda e, Sb=Sb, pt_=pt_, q0=q0: e.activation(out=pt_[:, q0:512], in_=Sb[:, q0:512], func=AF.Exp, scale=scale), reads=[Sb], writes=[pt_])
                        if j >= 0:
                            if mixer == 0:
                                P.op("pool", lambda e, pt_=pt_, q0=q0: e.memset(pt_[64:128, q0:q0 + 64], 0.0), writes=[pt_])
                            else:
                                P.op("pool", lambda e, pt_=pt_, q0=q0: e.affine_select(out=pt_[:, q0:q0 + 128], in_=pt_[:, q0:q0 + 128], pattern=[[1, 128]], compare_op=ALU.is_ge,
                                                                                     fill=0.0, base=0, channel_multiplier=-1), reads=[pt_], writes=[pt_])
                        for qi in range(max(j, 0), 4):
                            Ob = FB[2 + qi]
                            P.op("pe", lambda e, Ob=Ob, pt_=pt_, qi=qi, kt=kt: e.matmul(Ob[:, 0:65], lhsT=pt_[:, qi * 128:(qi + 1) * 128], rhs=V_sb[:, kt, h * 65:(h + 1) * 65],
                                                                                       start=(kt == 0), stop=(kt == 4 * b + qi)), reads=[pt_, V_sb], writes=[Ob])
                    for qi in range(4):
                        Ob = FB[2 + qi]
                        rd = rden[qi]
                        t = 4 * b + qi
                        col = mixer * 384 + h * 64
                        P.op("dve", lambda e, Ob=Ob, rd=rd: e.reciprocal(out=rd[:], in_=Ob[:, 64:65]), reads=[Ob], writes=[rd])
                        P.op("dve", lambda e, Ob=Ob, rd=rd, t=t, col=col: e.tensor_scalar(out=o_attn[:, t, col:col + 64], in0=Ob[:, 0:64], scalar1=rd[:, 0:1], scalar2=None, op0=ALU.mult),
                             reads=[Ob, rd], writes=[o_attn])

    def phase_c(l, moe):
        j2 = l // 2
        if moe:
            P.dma("sp", wr_sb[:], A["moe_wr"][j2].rearrange("(k p) n -> p k n", p=128), writes=[wr_sb])
            P.dma("sp", br_sb[:], A["moe_br"][j2].to_broadcast([128, 8]), writes=[br_sb])
        for t in range(NT):
            xs_ = xs[t % 2]
            if l == 0:
                P.dma("sp", xs_[:], x_in[t * 128:(t + 1) * 128, :], writes=[xs_])
            else:
                P.dma("sp", xs_[:], y[t * 128:(t + 1) * 128, :], reads=[ytile[t]], writes=[xs_])
            st_ = stat[t % 4]
            for mx in range(2):
                P.op("act", lambda e, mx=mx: e.activation(out=sq_junk[:, mx * 384:(mx + 1) * 384], in_=o_attn[:, t, mx * 384:(mx + 1) * 384], func=AF.Square, accum_out=st_[:, mx:mx + 1]),
                     reads=[o_attn], writes=[sq_junk, st_])
            P.op("act", lambda e: e.activation(out=st_[:, 2:4], in_=st_[:, 0:2], func=AF.Sqrt, scale=1.0 / 384, bias=eps_c[:, 0:1]), reads=[st_, eps_c], writes=[st_])
            P.op("dve", lambda e: e.reciprocal(out=st_[:, 2:4], in_=st_[:, 2:4]), reads=[st_], writes=[st_])
            for mx in range(2):
                P.op("dve", lambda e, mx=mx: e.tensor_scalar(out=mat[:, mx * 384:(mx + 1) * 384], in0=o_attn[:, t, mx * 384:(mx + 1) * 384], scalar1=st_[:, 2 + mx:3 + mx], scalar2=None, op0=ALU.mult),
                     reads=[o_attn, st_], writes=[mat])
            tb = TB[t % 2]
            for k in range(6):
                P.op("pe", lambda e, k=k: e.transpose(tb[:, k * 128:(k + 1) * 128], mat[:, k * 128:(k + 1) * 128], ident[:]), reads=[mat, ident], writes=[tb])
            P.op("act", lambda e: e.activation(out=mT[:, 2:8, :], in_=tb[:, 0:768].rearrange("p (k t) -> p k t", k=6), func=AF.Copy), reads=[tb], writes=[mT])
            P.dma("sp", mT[:, 0:2, :], mssm[:, :, t * 128:(t + 1) * 128].rearrange("m p t -> p m t"), reads=[mssm], writes=[mT])
            xn = xnew[t % 2]
            for hf in range(2):
                fb = FB[hf]
                for k in range(KD):
                    P.op("pe", lambda e, fb=fb, k=k, hf=hf: e.matmul(fb[:], lhsT=mT[:, k, :], rhs=w_out_sb[:, k, hf * 512:(hf + 1) * 512], start=(k == 0), stop=(k == KD - 1)),
                         reads=[mT, w_out_sb], writes=[fb])
                P.op("dve", lambda e, fb=fb, hf=hf: e.tensor_tensor(out=xn[:, hf * 512:(hf + 1) * 512], in0=fb[:], in1=xs_[:, hf * 512:(hf + 1) * 512], op=ALU.add), reads=[fb, xs_], writes=[xn])
            P.dma("sp", y[t * 128:(t + 1) * 128, :], xn[:], reads=[xn], writes=[ytile[t]])
            norm_transpose(xn, xn[:], l, 2, 3, h2T_st, slice(0, 128), t, fp32_path=(xhf, h2Tf) if moe else None)
            P.dma("sp", h2T_d[:, :, t * 128:(t + 1) * 128].rearrange("k p t -> p k t"), h2T_st[:], reads=[h2T_st], writes=[h2T_d])
            if moe:
                fb = FB[2]
                for k in range(KD):
                    P.op("pe", lambda e, k=k: e.matmul(fb[:, 0:8], lhsT=h2Tf[:, k, :], rhs=wr_sb[:, k, :], start=(k == 0), stop=(k == KD - 1)), reads=[h2Tf, wr_sb], writes=[fb])
                lg, m1, m2, lg2 = rtmp
                s1, s2, s3, s4 = rsc
                P.op("dve", lambda e: e.tensor_tensor(out=lg[:], in0=fb[:, 0:8], in1=br_sb[:], op=ALU.add), reads=[fb, br_sb], writes=[lg])
                P.op("dve", lambda e: e.tensor_reduce(out=s1[:], in_=lg[:], axis=AX.X, op=ALU.max), reads=[lg], writes=[s1])
                P.op("dve", lambda e: e.tensor_scalar(out=m1[:], in0=lg[:], scalar1=s1[:, 0:1], scalar2=None, op0=ALU.is_equal), reads=[lg, s1], writes=[m1])
                P.op("dve", lambda e: e.scalar_tensor_tensor(out=lg2[:], in0=m1[:], scalar=-1e30, in1=lg[:], op0=ALU.mult, op1=ALU.add), reads=[m1, lg], writes=[lg2])
                P.op("dve", lambda e: e.tensor_reduce(out=s2[:], in_=lg2[:], axis=AX.X, op=ALU.max), reads=[lg2], writes=[s2])
                P.op("dve", lambda e: e.tensor_scalar(out=m2[:], in0=lg2[:], scalar1=s2[:, 0:1], scalar2=None, op0=ALU.is_equal), reads=[lg2, s2], writes=[m2])
                P.op("dve", lambda e: e.tensor_tensor(out=s3[:], in0=s2[:], in1=s1[:], op=ALU.subtract), reads=[s1, s2], writes=[s3])
                P.op("act", lambda e: e.activation(out=s3[:], in_=s3[:], func=AF.Exp), reads=[s3], writes=[s3])
                P.op("dve", lambda e: e.tensor_scalar(out=s3[:], in0=s3[:], scalar1=1.0, scalar2=None, op0=ALU.add), reads=[s3], writes=[s3])
                P.op("dve", lambda e: e.reciprocal(out=s3[:], in_=s3[:]), reads=[s3], writes=[s3])
                P.op("dve", lambda e: e.tensor_scalar(out=s4[:], in0=s3[:], scalar1=-1.0, scalar2=1.0, op0=ALU.mult, op1=ALU.add), reads=[s3], writes=[s4])
                P.op("dve", lambda e: e.tensor_scalar(out=m1[:], in0=m1[:], scalar1=s3[:, 0:1], scalar2=None, op0=ALU.mult), reads=[m1, s3], writes=[m1])
                P.op("dve", lambda e, t=t: e.scalar_tensor_tensor(out=comb_all[:, t, :], in0=m2[:], scalar=s4[:, 0:1], in1=m1[:], op0=ALU.mult, op1=ALU.add), reads=[m2, s4, m1], writes=[comb_all])

    def phase_d(l, moe):
        j2 = l // 2
        P.dma("sp", G_bc[:], g12[l, 1], reads=[g12], writes=[G_bc])
        ne = 8 if moe else 2
        it = 0
        oi = 0
        for ex in range(ne):
            if moe:
                wg = A["moe_wg"][j2, ex]
                wu = A["moe_wu"][j2, ex]
                wd = A["moe_wd"][j2, ex]
            else:
                wg = A["ffn_wg"][j2][:, ex * DFE:(ex + 1) * DFE]
                wu = A["ffn_wu"][j2][:, ex * DFE:(ex + 1) * DFE]
                wd = A["ffn_wd"][j2][ex * DFE:(ex + 1) * DFE, :]
            Wg_, Wu_, Wd_ = Wg_sb[ex % 2], Wu_sb[ex % 2], Wd_sb[ex % 2]
            P.dma("pool", Wg_[:], wg.rearrange("(k p) f -> p k f", p=128), writes=[Wg_])
            P.dma("pool", Wu_[:], wu.rearrange("(k p) f -> p k f", p=128), writes=[Wu_])
            P.dma("pool", Wd_[:], wd.rearrange("(c p) d -> p c d", p=128), writes=[Wd_])
            for b in range(NB):
                hb = h2T_sb[(ex * NB + b) % 2]
                P.dma("sp", hb[:], h2T_d[:, :, b * 512:(b + 1) * 512].rearrange("k p t -> p k t"), reads=[h2T_d], writes=[hb])
                for c in range(NFC):
                    gb = FB[(it % 2) * 2]
                    ub = FB[(it % 2) * 2 + 1]
                    sg_ = sg[it % 2]
                    it += 1
                    for k in range(KD):
                        P.op("pe", lambda e, gb=gb, k=k, c=c: e.matmul(gb[:], lhsT=Wg_[:, k, c * 128:(c + 1) * 128], rhs=hb[:, k, :], start=(k == 0), stop=(k == KD - 1)), reads=[Wg_, hb], writes=[gb])
                    for k in range(KD):
                        P.op("pe", lambda e, ub=ub, k=k, c=c: e.matmul(ub[:], lhsT=Wu_[:, k, c * 128:(c + 1) * 128], rhs=hb[:, k, :], start=(k == 0), stop=(k == KD - 1)), reads=[Wu_, hb], writes=[ub])
                    P.op("act", lambda e, gb=gb, sg_=sg_: e.activation(out=sg_[:], in_=gb[:], func=AF.Silu), reads=[gb], writes=[sg_])
                    P.op("dve", lambda e, ub=ub, sg_=sg_, c=c: e.tensor_tensor(out=aT[:, c, :], in0=ub[:], in1=sg_[:], op=ALU.mult), reads=[ub, sg_], writes=[aT])
                for ti in range(4):
                    t = b * 4 + ti
                    os_ = ost[oi % 2]
                    oi += 1
                    for hf in range(2):
                        fb = FB[4 + hf]
                        for c in range(NFC):
                            P.op("pe", lambda e, fb=fb, c=c, ti=ti, hf=hf: e.matmul(fb[:], lhsT=aT[:, c, ti * 128:(ti + 1) * 128], rhs=Wd_[:, c, hf * 512:(hf + 1) * 512], start=(c == 0), stop=(c == NFC - 1)),
                                 reads=[aT, Wd_], writes=[fb])
                        if moe:
                            P.op("dve", lambda e, fb=fb, hf=hf, t=t, ex=ex, os_=os_: e.scalar_tensor_tensor(out=os_[:, hf * 512:(hf + 1) * 512], in0=fb[:], scalar=comb_all[:, t, ex:ex + 1],
                                                                                                       in1=G_bc[:, hf * 512:(hf + 1) * 512], op0=ALU.mult, op1=ALU.mult), reads=[fb, comb_all, G_bc], writes=[os_])
                        else:
                            P.op("dve", lambda e, fb=fb, hf=hf, os_=os_: e.tensor_tensor(out=os_[:, hf * 512:(hf + 1) * 512], in0=fb[:], in1=G_bc[:, hf * 512:(hf + 1) * 512], op=ALU.mult),
                                 reads=[fb, G_bc], writes=[os_])
                    P.dma("pool", y[t * 128:(t + 1) * 128, :], os_[:], reads=[os_, ytile[t]], writes=[ytile[t]], accum_op=ALU.add)

    stop = build.stop_after
    G = globals()

    def use(dct):
        for k_, v_ in dct.items():
            if k_ not in ("P", "L", "NT"):
                G[k_] = v_
    dbg = P.dram("dbg_oattn", [128, NT * 768], BF16, kind="ExternalOutput") if debug else None
    for l in range(n_layers):
        moe = (l % 2 == 1)
        P.push()
        use(_alloc_a(P, L))
        s5c = load_layer(l)
        phase_a(l, s5c)
        P.pop()
        if stop == "a":
            break
        P.push()
        use(_alloc_bc(P, L))
        P.push()
        use(_alloc_b(P, L))
        phase_b(l)
        P.pop()
        if debug and (stop == "b" or l == n_layers - 1):
            P.dma("sp", dbg[:], o_attn[:].rearrange("p t c -> p (t c)"), reads=[o_attn], writes=[dbg])
        if stop == "b":
            P.pop()
            break
        P.push()
        use(_alloc_c(P, L))
        load_c(l)
        phase_c(l, moe)
        P.pop()
        P.pop()
        if stop == "c":
            break
        P.push()
        use(_alloc_d(P, L))
        phase_d(l, moe)
        P.pop()
    P.finish()
    build.n_inst = P.n_inst
    return nc


build.stop_after = None


def prep_shared(inp, n_layers=DEPTH):
    f = lambda a: np.ascontiguousarray(np.asarray(a, dtype=np.float32))
    col = lambda a, k: f(np.asarray(a).reshape(DEPTH, k, 128).transpose(0, 2, 1))
    out = {}
    out["norm_mix_c"] = col(inp["norm_mix"], KD)
    out["norm_ffn_c"] = col(inp["norm_ffn"], KD)
    out["w_ada"] = f(inp["w_ada"])
    out["b_ada"] = f(np.asarray(inp["b_ada"]).reshape(DEPTH, 1, 6 * D))
    out["w_in"] = f(inp["w_in"])
    sm = lambda a: f(np.asarray(a).reshape(DEPTH, 8, 128).transpose(0, 2, 1))
    out["lam_re"] = sm(inp["ssm_lam_re"])
    out["lam_im"] = sm(inp["ssm_lam_im"])
    out["log_dt"] = sm(np.repeat(np.asarray(inp["ssm_log_dt"])[:, :, None], 64, axis=2))
    b_re = np.asarray(inp["ssm_b_re"]); b_im = np.asarray(inp["ssm_b_im"])
    c_re = np.asarray(inp["ssm_c_re"]); c_im = np.asarray(inp["ssm_c_im"])
    blk = {k: np.zeros((DEPTH, 8, 128, 128), np.float32) for k in ("b_re", "b_im", "c_re", "c_im")}
    for g in range(16):
        j, gg = g // 2, g % 2
        fc = (g % 8) * 16
        blk["b_re"][:, j, gg * 64:(gg + 1) * 64, fc:fc + 16] = b_re[:, g]
        blk["b_im"][:, j, gg * 64:(gg + 1) * 64, fc:fc + 16] = b_im[:, g]
        blk["c_re"][:, j, gg * 64:(gg + 1) * 64, fc:fc + 16] = c_re[:, g].transpose(0, 2, 1)
        blk["c_im"][:, j, gg * 64:(gg + 1) * 64, fc:fc + 16] = c_im[:, g].transpose(0, 2, 1)
    out.update(blk)
    out["ssm_d"] = col(inp["ssm_d"], 2)
    out["w_glu"] = f(inp["ssm_w_glu"])
    out["b_glu"] = col(inp["ssm_b_glu"], 2)
    out["q_norm"] = col(inp["mla_q_norm"], 2)
    out["kv_norm"] = col(inp["mla_kv_norm"], 1)
    out["w_uq"] = f(inp["mla_w_uq"])
    out["w_ukv"] = f(inp["mla_w_ukv"])
    rep = lambda a: f(np.broadcast_to(np.asarray(a)[:, None, :], (DEPTH, 128, np.asarray(a).shape[1])))
    out["gq_m"] = rep(inp["mla_qk_gq"])
    out["gk_m"] = rep(inp["mla_qk_gk"])
    out["fox_bf"] = f(np.asarray(inp["fox_b_f"]).reshape(DEPTH, NH, 1))
    out["gq_f"] = rep(inp["fox_qk_gq"])
    out["gk_f"] = rep(inp["fox_qk_gk"])
    out["out_norm"] = col(inp["out_norm"], KD)
    out["w_out"] = f(inp["w_out"])
    out["ffn_wg"] = f(inp["ffn_w_gate"])
    out["ffn_wu"] = f(inp["ffn_w_up"])
    out["ffn_wd"] = f(inp["ffn_w_down"])
    out["moe_wr"] = f(inp["moe_w_router"])
    out["moe_br"] = f(np.asarray(inp["moe_b_router"]).reshape(2, 1, 8))
    out["moe_wg"] = f(inp["moe_w_gate"])
    out["moe_wu"] = f(inp["moe_w_up"])
    out["moe_wd"] = f(inp["moe_w_down"])
    half = 16
    inv = (10000.0 ** (-np.arange(half, dtype=np.float32) / half)).astype(np.float32)
    out["inv_bc"] = f(np.broadcast_to(inv[None, :], (128, 16)))
    return out


def prep_core(inp, b, L):
    NT = L // 128
    m = {}
    m["x"] = np.ascontiguousarray(np.asarray(inp["x"])[b, :L, :], dtype=np.float32)
    m["c_col"] = np.ascontiguousarray(np.asarray(inp["c"], dtype=np.float32)[b].reshape(KD, 128).T)
    m["pos"] = np.ascontiguousarray(np.asarray(inp["positions"])[b, :L].astype(np.int32).reshape(NT, 128).T)
    return m


_CACHE = {}


def run(inp, L, n_layers=DEPTH, debug=False, cores=8, trace=False):
    key = (L, n_layers, debug, build.stop_after)
    if key not in _CACHE:
        _CACHE[key] = build(L, n_layers, debug)
    nc = _CACHE[key]
    shared = prep_shared(inp, n_layers)
    in_maps = []
    for b in range(cores):
        m = dict(shared)
        m.update(prep_core(inp, b, L))
        in_maps.append(m)
    res = run_bass_kernel_spmd(nc, in_maps, core_ids=list(range(cores)), **({"trace": True} if trace else {}))
    return res


def kernel(**inputs):
    L = np.asarray(inputs["x"]).shape[1]
    res = run(inputs, L)
    out = np.stack([np.asarray(r["y"], dtype=np.float32) for r in res.results], axis=0)
    return out
```

```python
import contextlib
import math
import sys
import numpy as np
import concourse.bass as bass
import concourse.mybir as mybir
from concourse.bass_utils import run_bass_kernel_spmd

F32 = mybir.dt.float32
BF16 = mybir.dt.bfloat16
I32 = mybir.dt.int32
AF = mybir.ActivationFunctionType
ALU = mybir.AluOpType
AX = mybir.AxisListType

ENGS = ("pe", "act", "dve", "pool", "sp")

D = 1024
KD = 8
DEPTH = 4
EPS = 1e-6
IN_COLS = 1830
NH = 6
DFE = 1408
NFC = 11
SUB = 256


class Tl:
    __slots__ = ("t", "name", "w", "r", "excl")

    def __init__(self, t, name):
        self.t = t
        self.name = name
        self.excl = False
        self.w = {}
        self.r = {}

    def __getitem__(self, idx):
        return self.t[idx]


class Prog:
    max_ops = 10 ** 9
    log = None

    def __init__(self, nc, ring_sizes=None):
        self.nc = nc
        self.es = contextlib.ExitStack()
        self.cnt = {e: 0 for e in ENGS}
        self.sems = {}
        self.seen = {e: {} for e in ENGS}
        for e in ENGS:
            self.sems[("eng", e)] = self.es.enter_context(nc.semaphore("s_" + e))
        ring_sizes = ring_sizes or {"sp": 16, "pool": 8, "act": 2}
        self.rings = {}
        self.ring_i = {}
        for e, k in ring_sizes.items():
            self.rings[e] = []
            for i in range(k):
                key = ("ring", e, i)
                self.sems[key] = self.es.enter_context(nc.semaphore("r_%s%d" % (e, i)))
                self.rings[e].append([key, 0])
            self.ring_i[e] = 0
        self.n_inst = 0
        self.scopes = [self.es]
        self.E = {"pe": nc.tensor, "act": nc.scalar, "dve": nc.vector, "pool": nc.gpsimd, "sp": nc.sync}

    def sb(self, name, shape, dt):
        self._uid = getattr(self, "_uid", 0) + 1
        return Tl(self.scopes[-1].enter_context(self.nc.sbuf_tensor("%s_%d" % (name, self._uid), list(shape), dt)), name)

    def push(self):
        self.scopes.append(contextlib.ExitStack())

    def barrier(self):
        need = {}
        for e, ring in self.rings.items():
            for key, v in ring:
                if v > 0:
                    need[key] = v
        for e in ENGS:
            if self.cnt[e] > 0:
                need[("eng", e)] = self.cnt[e]
        for eng in ENGS:
            seen = self.seen[eng]
            for k, v in need.items():
                if k == ("eng", eng) or seen.get(k, 0) >= v:
                    continue
                seen[k] = v
                self.E[eng].wait_ge(self.sems[k], v)

    def pop(self):
        self.barrier()
        self.scopes.pop().close()

    def ps(self, name, shape, dt):
        t = Tl(self.es.enter_context(self.nc.psum_tensor(name, list(shape), dt)), name)
        t.excl = True
        return t

    def dram(self, name, shape, dt, kind="Internal"):
        return Tl(self.nc.dram_tensor(name, list(shape), dt, kind=kind).ap(), name)

    def _collect(self, eng, reads, writes):
        need = {}
        me = ("eng", eng)
        for t in reads:
            for k, v in t.w.items():
                if need.get(k, 0) < v:
                    need[k] = v
            if t.excl:
                for k, v in t.r.items():
                    if k != me and need.get(k, 0) < v:
                        need[k] = v
        for t in writes:
            for k, v in t.w.items():
                if need.get(k, 0) < v:
                    need[k] = v
            for k, v in t.r.items():
                if need.get(k, 0) < v:
                    need[k] = v
        waits = []
        seen = self.seen[eng]
        for k, v in need.items():
            if eng == "pe" and k == ("eng", "pe"):
                continue
            if seen.get(k, 0) >= v:
                continue
            seen[k] = v
            waits.append((self.sems[k], v))
        return waits

    def _commit(self, reads, writes, key, val):
        for t in reads:
            if t.r.get(key, 0) < val:
                t.r[key] = val
        for t in writes:
            t.w = {key: val}
            t.r = {}

    def op(self, eng, fn, reads=(), writes=()):
        self.n_inst += 1
        if Prog.log is not None:
            Prog.log.append((self.n_inst, eng, sys._getframe(1).f_lineno))
        if self.n_inst > Prog.max_ops:
            return
        waits = self._collect(eng, reads, writes)
        self.cnt[eng] += 1
        key = ("eng", eng)
        val = self.cnt[eng]
        self._commit(reads, writes, key, val)
        e = self.E[eng]
        for s, v in waits[1:]:
            e.wait_ge(s, v)
        ins = fn(e)
        if waits:
            ins._wait_ge(waits[0][0], waits[0][1])
        ins.then_inc(self.sems[key], 1)

    def dma(self, eng, out, in_, reads=(), writes=(), **kw):
        self.n_inst += 1
        if Prog.log is not None:
            Prog.log.append((self.n_inst, "dma-" + eng, sys._getframe(1).f_lineno))
        if self.n_inst > Prog.max_ops:
            return
        ring = self.rings[eng]
        slot = ring[self.ring_i[eng] % len(ring)]
        self.ring_i[eng] += 1
        key, pv = slot
        waits = self._collect(eng, reads, writes)
        seen = self.seen[eng]
        if pv > 0 and seen.get(key, 0) < pv:
            seen[key] = pv
            waits.append((self.sems[key], pv))
        val = pv + 16
        slot[1] = val
        self._commit(reads, writes, key, val)
        e = self.E[eng]
        for s, v in waits[1:]:
            e.wait_ge(s, v)
        ins = e.dma_start(out=out, in_=in_, **kw)
        if waits:
            ins._wait_ge(waits[0][0], waits[0][1])
        ins.then_inc(self.sems[key], 16)

    def finish(self, eng="sp"):
        need = {}
        for e, ring in self.rings.items():
            for key, v in ring:
                if v > 0:
                    need[key] = v
        for e in ENGS:
            if self.cnt[e] > 0:
                need[("eng", e)] = self.cnt[e]
        E = self.E[eng]
        for k, v in need.items():
            E.wait_ge(self.sems[k], v)
        self.es.close()


LAYER_PARAMS = [
    ("norm_mix_c", [128, KD]), ("norm_ffn_c", [128, KD]),
    ("w_ada", [D, 6 * D]), ("b_ada", [1, 6 * D]),
    ("w_in", [D, IN_COLS]),
    ("lam_re", [128, 8]), ("lam_im", [128, 8]), ("log_dt", [128, 8]),
    ("b_re", [8, 128, 128]), ("b_im", [8, 128, 128]),
    ("c_re", [8, 128, 128]), ("c_im", [8, 128, 128]),
    ("ssm_d", [128, 2]), ("w_glu", [256, 256]), ("b_glu", [128, 2]),
    ("q_norm", [128, 2]), ("kv_norm", [128, 1]),
    ("w_uq", [256, 576]), ("w_ukv", [128, 768]),
    ("gq_m", [128, 96]), ("gk_m", [128, 96]),
    ("fox_bf", [NH, 1]), ("gq_f", [128, 64]), ("gk_f", [128, 64]),
    ("out_norm", [128, KD]), ("w_out", [D, D]),
]


def _alloc_a(P, L):
    NT = L // 128
    w_in_sb = P.sb("w_in_sb", [128, KD, IN_COLS], BF16)
    w_uq_sb = P.sb("w_uq_sb", [128, 2, 576], BF16)
    w_ukv_sb = P.sb("w_ukv_sb", [128, 768], BF16)
    w_glu_sb = P.sb("w_glu_sb", [128, 2, 256], BF16)
    qn_c = P.sb("qn_c", [128, 2], F32)
    kvn_c = P.sb("kvn_c", [128, 1], F32)
    gqm = P.sb("gqm", [128, 96], F32)
    gkm = P.sb("gkm", [128, 96], F32)
    gqf = P.sb("gqf", [128, 64], F32)
    gkf = P.sb("gkf", [128, 64], F32)
    nbf = P.sb("nbf", [NH, 1], F32)
    dcol = P.sb("dcol", [128, 2], F32)
    bglu_c = P.sb("bglu_c", [128, 2], F32)
    nbglu_c = P.sb("nbglu_c", [128, 2], F32)
    s5p = P.sb("s5p", [128, 24, 8], F32)
    BreT = P.sb("BreT", [128, 8, 128], BF16)
    BimT = P.sb("BimT", [128, 8, 128], BF16)
    CreT = P.sb("CreT", [128, 8, 128], BF16)
    nCimT = P.sb("nCimT", [128, 8, 128], BF16)
    Ddiag = P.sb("Ddiag", [128, 2, 128], BF16)
    Ctab = P.sb("Ctab", [128, 8, SUB], F32)
    Stab = P.sb("Stab", [128, 8, SUB], F32)
    Rtab = P.sb("Rtab", [128, 8, SUB], F32)
    blk_f = P.sb("blk_f", [128, 2, 128], F32)
    blk_o = P.sb("blk_o", [128, 2, 128], F32)
    hT = [P.sb("hT%d" % i, [128, KD, 512], BF16) for i in range(2)]
    uT = P.sb("uT", [128, 2, 512], BF16)
    cq_h = P.sb("cq_h", [128, 256], BF16)
    ckv_h = P.sb("ckv_h", [128, 128], BF16)
    cqT = P.sb("cqT", [128, 2, 128], BF16)
    ckvT = P.sb("ckvT", [128, 128], BF16)
    qn = P.sb("qn", [128, NH, 96], F32)
    kn = P.sb("kn", [128, NH, 96], F32)
    rt = [P.sb("rt%d" % i, [128, NH, 16], F32) for i in range(4)]
    qfin = P.sb("qfin", [128, NH, 96], BF16)
    kfin = P.sb("kfin", [128, NH, 96], BF16)
    fqn = P.sb("fqn", [128, NH, 64], F32)
    fqb = P.sb("fqb", [128, NH, 64], BF16)
    fkb = P.sb("fkb", [128, NH, 64], BF16)
    QTm_st = [P.sb("QTm_st%d" % i, [96, NH, 128], BF16) for i in range(2)]
    KTm_st = [P.sb("KTm_st%d" % i, [96, NH, 128], BF16) for i in range(2)]
    QTf_st = [P.sb("QTf_st%d" % i, [64, NH, 128], BF16) for i in range(2)]
    KTf_st = [P.sb("KTf_st%d" % i, [64, NH, 128], BF16) for i in range(2)]
    Vm_st = [P.sb("Vm_st%d" % i, [128, NH, 65], BF16) for i in range(2)]
    Vf_st = [P.sb("Vf_st%d" % i, [128, NH, 65], BF16) for i in range(2)]
    for t_ in Vm_st + Vf_st:
        P.op("pool", lambda e, t_=t_: e.memset(t_[:], 1.0), writes=[t_])
    fg_e = P.sb("fg_e", [NH, 512], F32)
    fg_sp = P.sb("fg_sp", [NH, 512], F32)
    fg_cum = P.sb("fg_cum", [NH, 512], F32)
    fg_carry = P.sb("fg_carry", [NH, 1], F32)
    fg_hi = P.sb("fg_hi", [NH, 512], BF16)
    fg_lo = P.sb("fg_lo", [NH, 512], BF16)
    fg_nhi = P.sb("fg_nhi", [NH, 512], BF16)
    fg_nlo = P.sb("fg_nlo", [NH, 512], BF16)
    W_re = P.sb("W_re", [128, 4, SUB], F32)
    W_im = P.sb("W_im", [128, 4, SUB], F32)
    wlast = P.sb("wlast", [128, 2, 8], F32)
    pre_c = P.sb("pre_c", [128, 2, SUB], F32)
    pre_s = P.sb("pre_s", [128, 2, SUB], F32)
    pin_re = P.sb("pin_re", [128, SUB], F32)
    pin_im = P.sb("pin_im", [128, SUB], F32)
    w0 = P.sb("w0", [128, 2, 8], F32)
    w0t = P.sb("w0t", [128, 4, 8], F32)
    pt = [P.sb("pt%d" % i, [128, 4, SUB], F32) for i in range(2)]
    s_re = P.sb("s_re", [128, 4, SUB], BF16)
    s_im = P.sb("s_im", [128, 4, SUB], BF16)
    yg = P.sb("yg", [128, 2, SUB], F32)
    yt1 = P.sb("yt1", [128, 2, SUB], F32)
    yt2 = P.sb("yt2", [128, 2, SUB], F32)
    yTb = P.sb("yTb", [128, 2, SUB], BF16)
    o2 = P.sb("o2", [128, 2, SUB], F32)
    osm = P.sb("osm", [128, 2, SUB], F32)
    rs_bc = P.sb("rs_bc", [128, SUB], F32)
    msm = P.sb("msm", [128, 2, SUB], BF16)
    return locals()


def _alloc_bc(P, L):
    NT = L // 128
    o_attn = P.sb("o_attn", [128, NT, 768], BF16)
    return locals()


def _alloc_b(P, L):
    NT = L // 128
    QT_sb = [P.sb("QT_sb%d" % i, [96, L], BF16) for i in range(2)]
    KT_sb = [P.sb("KT_sb%d" % i, [96, L], BF16) for i in range(2)]
    V_sb = P.sb("V_sb", [128, NT, NH * 65], BF16)
    PT = [P.sb("PT%d" % i, [128, 512], BF16) for i in range(3)]
    rden = [P.sb("rden%d" % i, [128, 4], F32) for i in range(2)]
    osb = [P.sb("osb%d" % i, [65, 512], F32) for i in range(2)]
    return locals()


def _alloc_c(P, L):
    NT = L // 128
    w_out_sb = P.sb("w_out_sb", [128, KD, D], BF16)
    wstc = [P.sb("wstc%d" % i, [128, D], F32) for i in range(2)]
    onorm_c = P.sb("onorm_c", [128, KD], F32)
    mat = P.sb("mat", [128, 768], BF16)
    mT = P.sb("mT", [128, KD, 128], BF16)
    xnew = [P.sb("xnew%d" % i, [128, D], F32) for i in range(2)]
    h2T_st = P.sb("h2T_st", [128, KD, 128], BF16)
    xhf = P.sb("xhf", [128, D], F32)
    h2Tf = P.sb("h2Tf", [128, KD, 128], F32)
    wr_sb = P.sb("wr_sb", [128, KD, 8], F32)
    br_sb = P.sb("br_sb", [128, 8], F32)
    rtmp = [P.sb("rtmp%d" % i, [128, 8], F32) for i in range(4)]
    rsc = [P.sb("rsc%d" % i, [128, 1], F32) for i in range(4)]
    return locals()


def _alloc_d(P, L):
    Wg_sb = [P.sb("Wg_sb%d" % i, [128, KD, DFE], BF16) for i in range(2)]
    Wu_sb = [P.sb("Wu_sb%d" % i, [128, KD, DFE], BF16) for i in range(2)]
    Wd_sb = [P.sb("Wd_sb%d" % i, [128, NFC, D], BF16) for i in range(2)]
    h2T_sb = [P.sb("h2T_sb%d" % i, [128, KD, 512], BF16) for i in range(2)]
    sg = [P.sb("sg%d" % i, [128, 512], BF16) for i in range(2)]
    aT = P.sb("aT", [128, NFC, 512], BF16)
    ost = [P.sb("ost%d" % i, [128, D], F32) for i in range(2)]
    return locals()


def build(L, n_layers=DEPTH, debug=False):
    NT = L // 128
    NB = L // 512
    nc = bass.Bass("TRN2", target_bir_lowering=False)
    P = Prog(nc)
    A = {}

    def din(name, shape, dt=F32):
        A[name] = P.dram(name, shape, dt, kind="ExternalInput")
        return A[name]

    x_in = din("x", [L, D])
    c_in = din("c_col", [128, KD])
    pos_in = din("pos", [128, NT], I32)
    inv_in = din("inv_bc", [128, 16])
    for name, shp in LAYER_PARAMS:
        din(name, [DEPTH] + shp)
    din("ffn_wg", [2, D, 2 * DFE])
    din("ffn_wu", [2, D, 2 * DFE])
    din("ffn_wd", [2, 2 * DFE, D])
    din("moe_wr", [2, D, 8])
    din("moe_br", [2, 1, 8])
    din("moe_wg", [2, 8, D, DFE])
    din("moe_wu", [2, 8, D, DFE])
    din("moe_wd", [2, 8, DFE, D])

    y = P.dram("y", [L, D], F32, kind="ExternalOutput")
    skind = "ExternalOutput" if debug else "Internal"
    QTm = P.dram("QTm", [NH, 96, L], BF16, kind=skind)
    KTm = P.dram("KTm", [NH, 96, L], BF16, kind=skind)
    Vm = P.dram("Vm", [L, NH * 65], BF16, kind=skind)
    QTf = P.dram("QTf", [NH, 68, L], BF16, kind=skind)
    KTf = P.dram("KTf", [NH, 68, L], BF16, kind=skind)
    Vf = P.dram("Vf", [L, NH * 65], BF16, kind=skind)
    mssm = P.dram("mssm", [2, 128, L], BF16, kind=skind)
    h2T_d = P.dram("h2T", [KD, 128, L], BF16, kind=skind)
    g12 = P.dram("g12", [DEPTH, 2, 128, D], F32, kind=skind)
    ytile = [Tl(y.t, "y%d" % t) for t in range(NT)]

    ident = P.sb("ident", [128, 128], BF16)
    identf = P.sb("identf", [128, 128], F32)
    ones_f = P.sb("ones_f", [128, 512], F32)
    ones_b = P.sb("ones_b", [128, 128], BF16)
    for t_, dt_ in ((ident, BF16), (identf, F32)):
        P.op("pool", lambda e, t_=t_: e.memset(t_[:], 1.0), writes=[t_])
        P.op("pool", lambda e, t_=t_: e.affine_select(out=t_[:], in_=t_[:], pattern=[[1, 128]], compare_op=ALU.is_equal,
                                                    fill=0.0, base=0, channel_multiplier=-1), reads=[t_], writes=[t_])
    P.op("pool", lambda e: e.memset(ones_f[:], 1.0), writes=[ones_f])
    P.op("pool", lambda e: e.memset(ones_b[:], 1.0), writes=[ones_b])

    TB = [P.ps("TB%d" % i, [128, 1024], BF16) for i in range(2)]
    FB = [P.ps("FB%d" % i, [128, 512], F32) for i in range(6)]

    def rstd_chain(ss_ap, n, nfeat, tmp, out, reads, eng_r="dve"):
        (tt, tv), (ot, ov) = tmp, out
        P.op("act", lambda e: e.activation(out=tv, in_=ss_ap, func=AF.Sqrt, scale=1.0 / nfeat, bias=eps_c[:, 0:1]),
             reads=list(reads) + [eps_c], writes=[tt])
        P.op("dve", lambda e: e.reciprocal(out=ov, in_=tv), reads=[tt], writes=[ot])

    eps_c = P.sb("eps_c", [128, 1], F32)
    P.op("pool", lambda e: e.memset(eps_c[:], EPS), writes=[eps_c])

    G_bc = P.sb("G_bc", [128, D], F32)
    xs = [P.sb("xs%d" % i, [128, D], F32) for i in range(2)]
    sq_junk = P.sb("sq_junk", [128, D], F32)
    xh = [P.sb("xh%d" % i, [128, D], BF16) for i in range(2)]
    stat = [P.sb("stat%d" % i, [128, 16], F32) for i in range(4)]
    comb_all = P.sb("comb_all", [128, NT, 8], F32)

    modc = P.sb("modc", [128, DEPTH, 4, KD], F32)
    nmc = P.sb("nmc", [128, DEPTH, 2, KD], F32)
    cosT = P.sb("cosT", [128, NT, 16], F32)
    sinT = P.sb("sinT", [128, NT, 16], F32)
    P.push()
    c_col = P.sb("c_colsb", [128, KD], F32)
    P.dma("sp", c_col[:], c_in[:], writes=[c_col])
    c_e = P.sb("c_e", [128, KD], F32)
    c_act = P.sb("c_act", [128, KD], F32)
    P.op("act", lambda e: e.activation(out=c_e[:], in_=c_col[:], func=AF.Exp, scale=-1.0), reads=[c_col], writes=[c_e])
    P.op("dve", lambda e: e.tensor_scalar(out=c_e[:], in0=c_e[:], scalar1=1.0, scalar2=None, op0=ALU.add), reads=[c_e], writes=[c_e])
    P.op("dve", lambda e: e.reciprocal(out=c_e[:], in_=c_e[:]), reads=[c_e], writes=[c_e])
    P.op("dve", lambda e: e.tensor_tensor(out=c_act[:], in0=c_col[:], in1=c_e[:], op=ALU.mult), reads=[c_col, c_e], writes=[c_act])
    C_bc = P.sb("C_bc", [128, KD, 128], F32)
    for k in range(KD):
        P.op("dve", lambda e, k=k: e.tensor_scalar(out=C_bc[:, k, :], in0=ones_f[:, 0:128], scalar1=c_act[:, k:k + 1], scalar2=None,
                                                   op0=ALU.mult), reads=[ones_f, c_act], writes=[C_bc])
    for l in range(n_layers):
        P.dma("sp", nmc[:, l, 0, :], A["norm_mix_c"][l], writes=[nmc])
        P.dma("sp", nmc[:, l, 1, :], A["norm_ffn_c"][l], writes=[nmc])
    wst = [P.sb("wst%d" % i, [128, 2048], F32) for i in range(3)]
    wst_i = [0]

    def next_wst():
        t = wst[wst_i[0] % 3]
        wst_i[0] += 1
        return t
    ada_sb = P.sb("ada_sb", [128, 2048], F32)
    brow = P.sb("brow", [128, 2048], F32)
    for l in range(n_layers):
        for ng in range(3):
            P.dma("sp", brow[:], A["b_ada"][l][:, ng * 2048:(ng + 1) * 2048].to_broadcast([128, 2048]), writes=[brow])
            for k in range(KD):
                st = next_wst()
                P.dma("sp", st[:], A["w_ada"][l, k * 128:(k + 1) * 128, ng * 2048:(ng + 1) * 2048], writes=[st])
                for j in range(4):
                    P.op("pe", lambda e, st=st, j=j, k=k: e.matmul(FB[j][:], lhsT=C_bc[:, k, :], rhs=st[:, j * 512:(j + 1) * 512],
                                                                 start=(k == 0), stop=(k == KD - 1)), reads=[st, C_bc], writes=[FB[j]])
            for j in range(4):
                P.op("dve", lambda e, j=j: e.tensor_tensor(out=ada_sb[:, j * 512:(j + 1) * 512], in0=FB[j][:], in1=brow[:, j * 512:(j + 1) * 512], op=ALU.add),
                     reads=[FB[j], brow], writes=[ada_sb])
            for half in range(2):
                seg = 2 * ng + half
                src = ada_sb[:, half * 1024:(half + 1) * 1024]
                if seg in (2, 5):
                    P.dma("sp", g12[l, 0 if seg == 2 else 1], src, reads=[ada_sb], writes=[g12])
                else:
                    v = {0: 1, 1: 0, 3: 3, 4: 2}[seg]
                    for k in range(KD):
                        P.op("pe", lambda e, half=half, k=k: e.transpose(FB[4][:, 0:128], ada_sb[:, half * 1024 + k * 128: half * 1024 + (k + 1) * 128], identf[:]),
                             reads=[ada_sb, identf], writes=[FB[4]])
                        if seg in (1, 4):
                            nm = 0 if seg == 1 else 1
                            P.op("dve", lambda e, l=l, v=v, k=k, nm=nm: e.scalar_tensor_tensor(
                                out=modc[:, l, v, k:k + 1], in0=FB[4][:, 0:1], scalar=1.0, in1=nmc[:, l, nm, k:k + 1],
                                op0=ALU.add, op1=ALU.mult), reads=[FB[4], nmc], writes=[modc])
                        else:
                            P.op("dve", lambda e, l=l, v=v, k=k: e.tensor_copy(out=modc[:, l, v, k:k + 1], in_=FB[4][:, 0:1]),
                                 reads=[FB[4]], writes=[modc])

    posi = P.sb("posi", [128, NT], I32)
    posf = P.sb("posf", [128, NT], F32)
    inv_bc = P.sb("inv_bcs", [128, 16], F32)
    ang = P.sb("ang", [128, NT, 16], F32)
    kk = P.sb("kk", [128, NT, 16], F32)
    P.dma("sp", posi[:], pos_in[:], writes=[posi])
    P.dma("sp", inv_bc[:], inv_in[:], writes=[inv_bc])
    P.op("dve", lambda e: e.tensor_copy(out=posf[:], in_=posi[:]), reads=[posi], writes=[posf])
    for t in range(NT):
        P.op("dve", lambda e, t=t: e.tensor_scalar(out=ang[:, t, :], in0=inv_bc[:], scalar1=posf[:, t:t + 1], scalar2=None, op0=ALU.mult),
             reads=[inv_bc, posf], writes=[ang])

    MAGIC = 12582912.0
    C1 = 6.28125
    C2 = 2.0 * math.pi - C1

    def sin_reduced(dst, src_t, src_v, shape_v, shift, tmp_t):
        tv = tmp_t[:] if shape_v is None else shape_v(tmp_t)
        P.op("dve", lambda e: e.tensor_scalar(out=tv, in0=src_v, scalar1=shift, scalar2=1.0 / (2 * math.pi), op0=ALU.add, op1=ALU.mult),
             reads=[src_t], writes=[tmp_t])
        P.op("dve", lambda e: e.tensor_scalar(out=tv, in0=tv, scalar1=MAGIC, scalar2=None, op0=ALU.add), reads=[tmp_t], writes=[tmp_t])
        P.op("dve", lambda e: e.tensor_scalar(out=tv, in0=tv, scalar1=-MAGIC, scalar2=None, op0=ALU.add), reads=[tmp_t], writes=[tmp_t])
        P.op("dve", lambda e: e.scalar_tensor_tensor(out=dst[0][:] if shape_v is None else shape_v(dst[0]), in0=tv, scalar=-C1, in1=src_v,
                                                     op0=ALU.mult, op1=ALU.add), reads=[tmp_t, src_t], writes=[dst[0]])
        dv = dst[0][:] if shape_v is None else shape_v(dst[0])
        P.op("dve", lambda e: e.scalar_tensor_tensor(out=dv, in0=tv, scalar=-C2, in1=dv, op0=ALU.mult, op1=ALU.add),
             reads=[tmp_t, dst[0]], writes=[dst[0]])
        P.op("dve", lambda e: e.tensor_scalar(out=dv, in0=dv, scalar1=shift, scalar2=3.14159, op0=ALU.add, op1=ALU.min), reads=[dst[0]], writes=[dst[0]])
        P.op("dve", lambda e: e.tensor_scalar(out=dv, in0=dv, scalar1=-3.14159, scalar2=None, op0=ALU.max), reads=[dst[0]], writes=[dst[0]])
        P.op("act", lambda e: e.activation(out=dv, in_=dv, func=AF.Sin), reads=[dst[0]], writes=[dst[0]])

    sin_reduced((sinT,), ang, ang[:], None, 0.0, kk)
    sin_reduced((cosT,), ang, ang[:], None, math.pi / 2, kk)
    onesrow = P.sb("onesrow", [NH, L], BF16)
    P.op("pool", lambda e: e.memset(onesrow[:], 1.0), writes=[onesrow])
    for r_ in (66, 67):
        P.dma("sp", QTf[:, r_, :], onesrow[:], reads=[onesrow], writes=[QTf])
    for r_ in (64, 65):
        P.dma("sp", KTf[:, r_, :], onesrow[:], reads=[onesrow], writes=[KTf])
    P.pop()

    def load_layer(l):
        lp = lambda n: A[n][l]
        P.dma("pool", w_in_sb[:], lp("w_in").rearrange("(k p) n -> p k n", p=128), writes=[w_in_sb])
        P.dma("pool", w_uq_sb[:], lp("w_uq").rearrange("(k p) n -> p k n", p=128), writes=[w_uq_sb])
        P.dma("pool", w_ukv_sb[:], lp("w_ukv"), writes=[w_ukv_sb])
        P.dma("pool", w_glu_sb[:], lp("w_glu").rearrange("(k p) n -> p k n", p=128), writes=[w_glu_sb])
        for t_, n_ in ((qn_c, "q_norm"), (kvn_c, "kv_norm"), (gqm, "gq_m"), (gkm, "gk_m"), (gqf, "gq_f"), (gkf, "gk_f"),
                       (dcol, "ssm_d"), (bglu_c, "b_glu")):
            P.dma("sp", t_[:], lp(n_), writes=[t_])
        P.dma("sp", nbf[:], lp("fox_bf"), writes=[nbf])
        P.op("dve", lambda e: e.tensor_scalar(out=nbf[:], in0=nbf[:], scalar1=-1.0, scalar2=None, op0=ALU.mult), reads=[nbf], writes=[nbf])
        P.op("dve", lambda e: e.tensor_scalar(out=nbglu_c[:], in0=bglu_c[:], scalar1=-1.0, scalar2=None, op0=ALU.mult), reads=[bglu_c], writes=[nbglu_c])
        for m in range(2):
            P.op("dve", lambda e, m=m: e.tensor_scalar(out=Ddiag[:, m, :], in0=identf[:], scalar1=dcol[:, m:m + 1], scalar2=None, op0=ALU.mult),
                 reads=[identf, dcol], writes=[Ddiag])
        V = lambda i: s5p[:, i, :]
        LRE, LIM, LDT, DT, ZRE, ZIM, MAG, SN, CS, LBR, LBI, DEN, KR, KI, NKI, T1, T2, CK, SK, CK2, SK2 = range(21)
        P.dma("sp", V(LRE), lp("lam_re"), writes=[s5p])
        P.dma("sp", V(LIM), lp("lam_im"), writes=[s5p])
        P.dma("sp", V(LDT), lp("log_dt"), writes=[s5p])
        sop = lambda fn: P.op("dve", fn, reads=[s5p], writes=[s5p])
        P.op("act", lambda e: e.activation(out=V(DT), in_=V(LDT), func=AF.Exp), reads=[s5p], writes=[s5p])
        sop(lambda e: e.tensor_tensor(out=V(ZRE), in0=V(LRE), in1=V(DT), op=ALU.mult))
        sop(lambda e: e.tensor_tensor(out=V(ZIM), in0=V(LIM), in1=V(DT), op=ALU.mult))
        P.op("act", lambda e: e.activation(out=V(MAG), in_=V(ZRE), func=AF.Exp), reads=[s5p], writes=[s5p])
        sv = lambda i: (lambda t: t[:, i, :])
        sin_reduced((s5p,), s5p, V(ZIM), sv(SN), 0.0, s5p) if False else None
        for dst_i, shift in ((SN, 0.0), (CS, math.pi / 2)):
            sop(lambda e, shift=shift: e.tensor_scalar(out=V(T1), in0=V(ZIM), scalar1=shift, scalar2=1.0 / (2 * math.pi), op0=ALU.add, op1=ALU.mult))
            sop(lambda e: e.tensor_scalar(out=V(T1), in0=V(T1), scalar1=MAGIC, scalar2=None, op0=ALU.add))
            sop(lambda e: e.tensor_scalar(out=V(T1), in0=V(T1), scalar1=-MAGIC, scalar2=None, op0=ALU.add))
            sop(lambda e, dst_i=dst_i: e.scalar_tensor_tensor(out=V(dst_i), in0=V(T1), scalar=-C1, in1=V(ZIM), op0=ALU.mult, op1=ALU.add))
            sop(lambda e, dst_i=dst_i: e.scalar_tensor_tensor(out=V(dst_i), in0=V(T1), scalar=-C2, in1=V(dst_i), op0=ALU.mult, op1=ALU.add))
            sop(lambda e, dst_i=dst_i, shift=shift: e.tensor_scalar(out=V(dst_i), in0=V(dst_i), scalar1=shift, scalar2=3.14159, op0=ALU.add, op1=ALU.min))
            sop(lambda e, dst_i=dst_i: e.tensor_scalar(out=V(dst_i), in0=V(dst_i), scalar1=-3.14159, scalar2=None, op0=ALU.max))
            P.op("act", lambda e, dst_i=dst_i: e.activation(out=V(dst_i), in_=V(dst_i), func=AF.Sin), reads=[s5p], writes=[s5p])
        sop(lambda e: e.tensor_tensor(out=V(LBR), in0=V(MAG), in1=V(CS), op=ALU.mult))
        sop(lambda e: e.tensor_tensor(out=V(LBI), in0=V(MAG), in1=V(SN), op=ALU.mult))
        sop(lambda e: e.tensor_tensor(out=V(DEN), in0=V(LRE), in1=V(LRE), op=ALU.mult))
        sop(lambda e: e.tensor_tensor(out=V(T1), in0=V(LIM), in1=V(LIM), op=ALU.mult))
        sop(lambda e: e.tensor_tensor(out=V(DEN), in0=V(DEN), in1=V(T1), op=ALU.add))
        sop(lambda e: e.reciprocal(out=V(DEN), in_=V(DEN)))
        sop(lambda e: e.tensor_scalar(out=V(T2), in0=V(LBR), scalar1=-1.0, scalar2=None, op0=ALU.add))
        sop(lambda e: e.tensor_tensor(out=V(KR), in0=V(T2), in1=V(LRE), op=ALU.mult))
        sop(lambda e: e.tensor_tensor(out=V(T1), in0=V(LBI), in1=V(LIM), op=ALU.mult))
        sop(lambda e: e.tensor_tensor(out=V(KR), in0=V(KR), in1=V(T1), op=ALU.add))
        sop(lambda e: e.tensor_tensor(out=V(KR), in0=V(KR), in1=V(DEN), op=ALU.mult))
        sop(lambda e: e.tensor_tensor(out=V(KI), in0=V(LBI), in1=V(LRE), op=ALU.mult))
        sop(lambda e: e.tensor_tensor(out=V(T1), in0=V(T2), in1=V(LIM), op=ALU.mult))
        sop(lambda e: e.tensor_tensor(out=V(KI), in0=V(KI), in1=V(T1), op=ALU.subtract))
        sop(lambda e: e.tensor_tensor(out=V(KI), in0=V(KI), in1=V(DEN), op=ALU.mult))
        sop(lambda e: e.tensor_scalar(out=V(NKI), in0=V(KI), scalar1=-1.0, scalar2=None, op0=ALU.mult))
        for j in range(8):
            P.dma("sp", blk_f[:, 0, :], lp("b_re")[j], writes=[blk_f])
            P.dma("sp", blk_f[:, 1, :], lp("b_im")[j], writes=[blk_f])
            P.op("dve", lambda e, j=j: e.tensor_scalar(out=blk_o[:, 0, :], in0=blk_f[:, 0, :], scalar1=s5p[:, KR, j:j + 1], scalar2=None, op0=ALU.mult),
                 reads=[blk_f, s5p], writes=[blk_o])
            P.op("dve", lambda e, j=j: e.scalar_tensor_tensor(out=blk_o[:, 0, :], in0=blk_f[:, 1, :], scalar=s5p[:, NKI, j:j + 1], in1=blk_o[:, 0, :],
                                                              op0=ALU.mult, op1=ALU.add), reads=[blk_f, s5p, blk_o], writes=[blk_o])
            P.op("dve", lambda e, j=j: e.tensor_scalar(out=blk_o[:, 1, :], in0=blk_f[:, 1, :], scalar1=s5p[:, KR, j:j + 1], scalar2=None, op0=ALU.mult),
                 reads=[blk_f, s5p], writes=[blk_o])
            P.op("dve", lambda e, j=j: e.scalar_tensor_tensor(out=blk_o[:, 1, :], in0=blk_f[:, 0, :], scalar=s5p[:, KI, j:j + 1], in1=blk_o[:, 1, :],
                                                              op0=ALU.mult, op1=ALU.add), reads=[blk_f, s5p, blk_o], writes=[blk_o])
            for ri, dstT in ((0, BreT), (1, BimT)):
                P.op("pe", lambda e, ri=ri: e.transpose(FB[5][:, 0:128], blk_o[:, ri, :], identf[:]), reads=[blk_o, identf], writes=[FB[5]])
                P.op("act", lambda e, dstT=dstT, j=j: e.activation(out=dstT[:, j, :], in_=FB[5][:, 0:128], func=AF.Copy), reads=[FB[5]], writes=[dstT])
        P.dma("pool", CreT[:], lp("c_re").rearrange("j p f -> p j f"), writes=[CreT])
        P.dma("pool", nCimT[:], lp("c_im").rearrange("j p f -> p j f"), writes=[nCimT])
        P.op("dve", lambda e: e.tensor_scalar(out=nCimT[:], in0=nCimT[:], scalar1=-1.0, scalar2=None, op0=ALU.mult), reads=[nCimT], writes=[nCimT])
        P.op("dve", lambda e: e.memset(Ctab[:, :, 0:1], 1.0), writes=[Ctab])
        P.op("dve", lambda e: e.memset(Stab[:, :, 0:1], 0.0), writes=[Stab])
        sop(lambda e: e.tensor_copy(out=V(CK), in_=V(CS)))
        sop(lambda e: e.tensor_copy(out=V(SK), in_=V(SN)))
        n = 1
        while n < SUB:
            tabt_v = pt[0][:].rearrange("p a b -> p (a b)")[:, 0:8 * n].rearrange("p (j n) -> p j n", j=8)
            tabu_v = pt[1][:].rearrange("p a b -> p (a b)")[:, 0:8 * n].rearrange("p (j n) -> p j n", j=8)
            tabt = pt[0]
            tabu = pt[1]
            ckb = s5p[:, CK, :].unsqueeze(2).to_broadcast([128, 8, n])
            skb = s5p[:, SK, :].unsqueeze(2).to_broadcast([128, 8, n])
            P.op("dve", lambda e, n=n, ckb=ckb: e.tensor_tensor(out=tabt_v, in0=Ctab[:, :, 0:n], in1=ckb, op=ALU.mult), reads=[Ctab, s5p], writes=[tabt])
            P.op("dve", lambda e, n=n, skb=skb: e.tensor_tensor(out=tabu_v, in0=Stab[:, :, 0:n], in1=skb, op=ALU.mult), reads=[Stab, s5p], writes=[tabu])
            P.op("dve", lambda e, n=n: e.tensor_tensor(out=Ctab[:, :, n:2 * n], in0=tabt_v, in1=tabu_v, op=ALU.subtract), reads=[tabt, tabu], writes=[Ctab])
            P.op("dve", lambda e, n=n, skb=skb: e.tensor_tensor(out=tabt_v, in0=Ctab[:, :, 0:n], in1=skb, op=ALU.mult), reads=[Ctab, s5p], writes=[tabt])
            P.op("dve", lambda e, n=n, ckb=ckb: e.tensor_tensor(out=tabu_v, in0=Stab[:, :, 0:n], in1=ckb, op=ALU.mult), reads=[Stab, s5p], writes=[tabu])
            P.op("dve", lambda e, n=n: e.tensor_tensor(out=Stab[:, :, n:2 * n], in0=tabt_v, in1=tabu_v, op=ALU.add), reads=[tabt, tabu], writes=[Stab])
            sop(lambda e: e.tensor_tensor(out=V(T1), in0=V(CK), in1=V(CK), op=ALU.mult))
            sop(lambda e: e.tensor_tensor(out=V(T2), in0=V(SK), in1=V(SK), op=ALU.mult))
            sop(lambda e: e.tensor_tensor(out=V(SK2), in0=V(CK), in1=V(SK), op=ALU.mult))
            sop(lambda e: e.tensor_tensor(out=V(CK), in0=V(T1), in1=V(T2), op=ALU.subtract))
            sop(lambda e: e.tensor_scalar(out=V(SK), in0=V(SK2), scalar1=2.0, scalar2=None, op0=ALU.mult))
            n *= 2
        P.op("dve", lambda e: e.tensor_copy(out=Rtab[:], in_=s5p[:, MAG, :].unsqueeze(2).to_broadcast([128, 8, SUB])), reads=[s5p], writes=[Rtab])
        return dict(CK=CK, SK=SK)

    def load_c(l):
        P.dma("sp", onorm_c[:], A["out_norm"][l], writes=[onorm_c])
        P.dma("sp", G_bc[:], g12[l, 0], reads=[g12], writes=[G_bc])
        for k in range(KD):
            st = wstc[k % 2]
            P.dma("sp", st[:], A["w_out"][l][k * 128:(k + 1) * 128, :], writes=[st])
            P.op("dve", lambda e, st=st, k=k: e.scalar_tensor_tensor(out=w_out_sb[:, k, :], in0=st[:], scalar=onorm_c[:, k:k + 1], in1=G_bc[:],
                                                                     op0=ALU.mult, op1=ALU.mult), reads=[st, onorm_c, G_bc], writes=[w_out_sb])

    def norm_transpose(src_t, src_v, l, v_a, v_b, dst_t, dst_slice, si, fp32_path=None):
        st_ = stat[si % 4]
        P.op("act", lambda e: e.activation(out=sq_junk[:], in_=src_v, func=AF.Square, accum_out=st_[:, 0:1]), reads=[src_t], writes=[sq_junk, st_])
        P.op("act", lambda e: e.activation(out=st_[:, 1:2], in_=st_[:, 0:1], func=AF.Sqrt, scale=1.0 / D, bias=eps_c[:, 0:1]), reads=[st_, eps_c], writes=[st_])
        P.op("dve", lambda e: e.reciprocal(out=st_[:, 2:3], in_=st_[:, 1:2]), reads=[st_], writes=[st_])
        xh_ = xh[si % 2]
        P.op("dve", lambda e: e.tensor_scalar(out=xh_[:], in0=src_v, scalar1=st_[:, 2:3], scalar2=None, op0=ALU.mult), reads=[src_t, st_], writes=[xh_])
        tb = TB[si % 2]
        for k in range(KD):
            P.op("pe", lambda e, k=k: e.transpose(tb[:, k * 128:(k + 1) * 128], xh_[:, k * 128:(k + 1) * 128], ident[:]), reads=[xh_, ident], writes=[tb])
        for k in range(KD):
            P.op("act", lambda e, k=k: e.activation(out=dst_t[:, k, dst_slice], in_=tb[:, k * 128:(k + 1) * 128], func=AF.Identity,
                                                    scale=modc[:, l, v_a, k:k + 1], bias=modc[:, l, v_b, k:k + 1]), reads=[tb, modc], writes=[dst_t])
        if fp32_path is not None:
            xhf_, dstf = fp32_path
            P.op("dve", lambda e: e.tensor_scalar(out=xhf_[:], in0=src_v, scalar1=st_[:, 2:3], scalar2=None, op0=ALU.mult), reads=[src_t, st_], writes=[xhf_])
            for k in range(KD):
                fb = FB[k % 2]
                P.op("pe", lambda e, k=k, fb=fb: e.transpose(fb[:, 0:128], xhf_[:, k * 128:(k + 1) * 128], identf[:]), reads=[xhf_, identf], writes=[fb])
                P.op("act", lambda e, k=k, fb=fb: e.activation(out=dstf[:, k, :], in_=fb[:, 0:128], func=AF.Identity,
                                                             scale=modc[:, l, v_a, k:k + 1], bias=modc[:, l, v_b, k:k + 1]), reads=[fb, modc], writes=[dstf])

    def head_rms(src_t, src_v3, nh, hd, gain_t, dst_t, dst_v3, si, extra_ss=None):
        st_ = stat[si % 4]
        P.op("act", lambda e: e.activation(out=sq_junk[:, 0:nh * hd].rearrange("p (h d) -> p h d", h=nh), in_=src_v3, func=AF.Square), reads=[src_t], writes=[sq_junk])
        P.op("dve", lambda e: e.tensor_reduce(out=st_[:, 0:nh], in_=sq_junk[:, 0:nh * hd].rearrange("p (h d) -> p h d", h=nh), axis=AX.X, op=ALU.add),
             reads=[sq_junk], writes=[st_])
        tot = hd
        if extra_ss is not None:
            et, ev, en = extra_ss
            P.op("dve", lambda e: e.tensor_scalar(out=st_[:, 0:nh], in0=st_[:, 0:nh], scalar1=ev, scalar2=None, op0=ALU.add), reads=[st_, et], writes=[st_])
            tot = hd + en
        P.op("act", lambda e: e.activation(out=st_[:, 6:6 + nh], in_=st_[:, 0:nh], func=AF.Sqrt, scale=1.0 / tot, bias=eps_c[:, 0:1]), reads=[st_, eps_c], writes=[st_])
        P.op("dve", lambda e: e.reciprocal(out=st_[:, 6:6 + nh], in_=st_[:, 6:6 + nh]), reads=[st_], writes=[st_])
        return st_

    def rope(src_t, dst_t, cos_v, sin_v):
        x1 = src_t[:, :, 64:80]
        x2 = src_t[:, :, 80:96]
        cb = cos_v.unsqueeze(1).to_broadcast([128, NH, 16])
        sb_ = sin_v.unsqueeze(1).to_broadcast([128, NH, 16])
        P.op("dve", lambda e: e.tensor_copy(out=dst_t[:, :, 0:64], in_=src_t[:, :, 0:64]), reads=[src_t], writes=[dst_t])
        P.op("dve", lambda e: e.tensor_tensor(out=rt[0][:], in0=x1, in1=cb, op=ALU.mult), reads=[src_t, cosT], writes=[rt[0]])
        P.op("dve", lambda e: e.tensor_tensor(out=rt[1][:], in0=x2, in1=sb_, op=ALU.mult), reads=[src_t, sinT], writes=[rt[1]])
        P.op("dve", lambda e: e.tensor_tensor(out=dst_t[:, :, 64:80], in0=rt[0][:], in1=rt[1][:], op=ALU.subtract), reads=[rt[0], rt[1]], writes=[dst_t])
        P.op("dve", lambda e: e.tensor_tensor(out=rt[2][:], in0=x1, in1=sb_, op=ALU.mult), reads=[src_t, sinT], writes=[rt[2]])
        P.op("dve", lambda e: e.tensor_tensor(out=rt[3][:], in0=x2, in1=cb, op=ALU.mult), reads=[src_t, cosT], writes=[rt[3]])
        P.op("dve", lambda e: e.tensor_tensor(out=dst_t[:, :, 80:96], in0=rt[2][:], in1=rt[3][:], op=ALU.add), reads=[rt[2], rt[3]], writes=[dst_t])

    def phase_a(l, s5c):
        src = x_in if l == 0 else None
        for b in range(NB):
            hTb = hT[b % 2]
            for ti in range(4):
                t = b * 4 + ti
                xs_ = xs[t % 2]
                if l == 0:
                    P.dma("sp", xs_[:], x_in[t * 128:(t + 1) * 128, :], writes=[xs_])
                else:
                    P.dma("sp", xs_[:], y[t * 128:(t + 1) * 128, :], reads=[ytile[t]], writes=[xs_])
                norm_transpose(xs_, xs_[:], l, 0, 1, hTb, slice(ti * 128, (ti + 1) * 128), t)
            for m in range(2):
                for k in range(KD):
                    P.op("pe", lambda e, m=m, k=k: e.matmul(FB[m][:], lhsT=w_in_sb[:, k, m * 128:(m + 1) * 128], rhs=hTb[:, k, :], start=(k == 0), stop=(k == KD - 1)),
                         reads=[w_in_sb, hTb], writes=[FB[m]])
                P.op("act", lambda e, m=m: e.activation(out=uT[:, m, :], in_=FB[m][:], func=AF.Copy), reads=[FB[m]], writes=[uT])
            for k in range(KD):
                P.op("pe", lambda e, k=k: e.matmul(FB[2][0:NH, :], lhsT=w_in_sb[:, k, 1824:1830], rhs=hTb[:, k, :], start=(k == 0), stop=(k == KD - 1)),
                     reads=[w_in_sb, hTb], writes=[FB[2]])
            P.op("act", lambda e: e.activation(out=fg_e[:], in_=FB[2][0:NH, :], func=AF.Exp, scale=-1.0, bias=nbf[:, 0:1]), reads=[FB[2], nbf], writes=[fg_e])
            P.op("act", lambda e: e.activation(out=fg_sp[:], in_=fg_e[:], func=AF.Ln, bias=ones_f[0:NH, 0:1]), reads=[fg_e, ones_f], writes=[fg_sp])
            if b == 0:
                P.op("dve", lambda e: e.tensor_tensor_scan(out=fg_cum[:], data0=ones_f[0:NH, 0:512], data1=fg_sp[:], initial=0.0, op0=ALU.mult, op1=ALU.add),
                     reads=[ones_f, fg_sp], writes=[fg_cum])
            else:
                P.op("dve", lambda e: e.tensor_tensor_scan(out=fg_cum[:], data0=ones_f[0:NH, 0:512], data1=fg_sp[:], initial=fg_carry[:, 0:1], op0=ALU.mult, op1=ALU.add),
                     reads=[ones_f, fg_sp, fg_carry], writes=[fg_cum])
            P.op("dve", lambda e: e.tensor_copy(out=fg_carry[:], in_=fg_cum[:, 511:512]), reads=[fg_cum], writes=[fg_carry])
            P.op("dve", lambda e: e.tensor_scalar(out=fg_sp[:], in0=fg_cum[:], scalar1=8.0, scalar2=None, op0=ALU.mult), reads=[fg_cum], writes=[fg_sp])
            P.op("dve", lambda e: e.tensor_copy(out=fg_hi[:], in_=fg_sp[:]), reads=[fg_sp], writes=[fg_hi])
            P.op("dve", lambda e: e.tensor_tensor(out=fg_lo[:], in0=fg_sp[:], in1=fg_hi[:], op=ALU.subtract), reads=[fg_sp, fg_hi], writes=[fg_lo])
            P.op("dve", lambda e: e.tensor_scalar(out=fg_nhi[:], in0=fg_hi[:], scalar1=-1.0, scalar2=None, op0=ALU.mult), reads=[fg_hi], writes=[fg_nhi])
            P.op("dve", lambda e: e.tensor_scalar(out=fg_nlo[:], in0=fg_lo[:], scalar1=-1.0, scalar2=None, op0=ALU.mult), reads=[fg_lo], writes=[fg_nlo])
            bs = slice(b * 512, (b + 1) * 512)
            P.dma("sp", QTf[:, 64, bs], fg_nhi[:], reads=[fg_nhi], writes=[QTf])
            P.dma("sp", QTf[:, 65, bs], fg_nlo[:], reads=[fg_nlo], writes=[QTf])
            P.dma("sp", KTf[:, 66, bs], fg_hi[:], reads=[fg_hi], writes=[KTf])
            P.dma("sp", KTf[:, 67, bs], fg_lo[:], reads=[fg_lo], writes=[KTf])

            for ti in range(4):
                t = b * 4 + ti
                tsl = slice(ti * 128, (ti + 1) * 128)
                segs = ((FB[2], 256, 416), (FB[3], 672, 384), (FB[4], 1056, 384), (FB[5], 1440, 384))
                for fb, c0, w in segs:
                    for k in range(KD):
                        P.op("pe", lambda e, fb=fb, c0=c0, w=w, k=k: e.matmul(fb[:, 0:w], lhsT=hTb[:, k, tsl], rhs=w_in_sb[:, k, c0:c0 + w], start=(k == 0), stop=(k == KD - 1)),
                             reads=[hTb, w_in_sb], writes=[fb])
                st_ = stat[0]
                P.op("act", lambda e: e.activation(out=sq_junk[:, 0:256], in_=FB[2][:, 0:256], func=AF.Square, accum_out=st_[:, 12:13]), reads=[FB[2]], writes=[sq_junk, st_])
                P.op("act", lambda e: e.activation(out=st_[:, 13:14], in_=st_[:, 12:13], func=AF.Sqrt, scale=1.0 / 256, bias=eps_c[:, 0:1]), reads=[st_, eps_c], writes=[st_])
                P.op("dve", lambda e: e.reciprocal(out=st_[:, 13:14], in_=st_[:, 13:14]), reads=[st_], writes=[st_])
                P.op("dve", lambda e: e.tensor_scalar(out=cq_h[:], in0=FB[2][:, 0:256], scalar1=st_[:, 13:14], scalar2=None, op0=ALU.mult), reads=[FB[2], st_], writes=[cq_h])
                st1 = stat[1]
                P.op("act", lambda e: e.activation(out=sq_junk[:, 256:384], in_=FB[2][:, 256:384], func=AF.Square, accum_out=st1[:, 12:13]), reads=[FB[2]], writes=[sq_junk, st1])
                P.op("act", lambda e: e.activation(out=st1[:, 13:14], in_=st1[:, 12:13], func=AF.Sqrt, scale=1.0 / 128, bias=eps_c[:, 0:1]), reads=[st1, eps_c], writes=[st1])
                P.op("dve", lambda e: e.reciprocal(out=st1[:, 13:14], in_=st1[:, 13:14]), reads=[st1], writes=[st1])
                P.op("dve", lambda e: e.tensor_scalar(out=ckv_h[:], in0=FB[2][:, 256:384], scalar1=st1[:, 13:14], scalar2=None, op0=ALU.mult), reads=[FB[2], st1], writes=[ckv_h])
                P.op("act", lambda e: e.activation(out=kn[:, 0, 64:96], in_=FB[2][:, 384:416], func=AF.Copy), reads=[FB[2]], writes=[kn])
                P.op("act", lambda e: e.activation(out=sq_junk[:, 384:416], in_=FB[2][:, 384:416], func=AF.Square, accum_out=st1[:, 14:15]), reads=[FB[2]], writes=[sq_junk, st1])
                tb = TB[0]
                for j in range(2):
                    P.op("pe", lambda e, j=j: e.transpose(tb[:, j * 128:(j + 1) * 128], cq_h[:, j * 128:(j + 1) * 128], ident[:]), reads=[cq_h, ident], writes=[tb])
                P.op("pe", lambda e: e.transpose(tb[:, 256:384], ckv_h[:], ident[:]), reads=[ckv_h, ident], writes=[tb])
                for j in range(2):
                    P.op("act", lambda e, j=j: e.activation(out=cqT[:, j, :], in_=tb[:, j * 128:(j + 1) * 128], func=AF.Copy, scale=qn_c[:, j:j + 1]), reads=[tb, qn_c], writes=[cqT])
                P.op("act", lambda e: e.activation(out=ckvT[:], in_=tb[:, 256:384], func=AF.Copy, scale=kvn_c[:, 0:1]), reads=[tb, kvn_c], writes=[ckvT])
                for j in range(2):
                    P.op("pe", lambda e, j=j: e.matmul(FB[0][:], lhsT=cqT[:, j, :], rhs=w_uq_sb[:, j, 0:512], start=(j == 0), stop=(j == 1)), reads=[cqT, w_uq_sb], writes=[FB[0]])
                for j in range(2):
                    P.op("pe", lambda e, j=j: e.matmul(FB[1][:, 0:64], lhsT=cqT[:, j, :], rhs=w_uq_sb[:, j, 512:576], start=(j == 0), stop=(j == 1)), reads=[cqT, w_uq_sb], writes=[FB[1]])
                P.op("act", lambda e: e.activation(out=qn[:].rearrange("p h d -> p (h d)")[:, 0:512], in_=FB[0][:], func=AF.Copy), reads=[FB[0]], writes=[qn])
                P.op("act", lambda e: e.activation(out=qn[:].rearrange("p h d -> p (h d)")[:, 512:576], in_=FB[1][:, 0:64], func=AF.Copy), reads=[FB[1]], writes=[qn])
                sq = head_rms(qn, qn[:], NH, 96, gqm, None, None, 2)
                P.op("dve", lambda e, sq=sq: e.tensor_tensor(out=qn[:], in0=qn[:], in1=sq[:, 6:12].unsqueeze(2).to_broadcast([128, NH, 96]), op=ALU.mult), reads=[qn, sq], writes=[qn])
                P.op("dve", lambda e: e.tensor_tensor(out=qn[:], in0=qn[:], in1=gqm[:].unsqueeze(1).to_broadcast([128, NH, 96]), op=ALU.mult), reads=[qn, gqm], writes=[qn])
                rope(qn, qfin, cosT[:, t, :], sinT[:, t, :])
                P.op("pe", lambda e: e.matmul(FB[0][:], lhsT=ckvT[:], rhs=w_ukv_sb[:, 0:512], start=True, stop=True), reads=[ckvT, w_ukv_sb], writes=[FB[0]])
                P.op("pe", lambda e: e.matmul(FB[1][:, 0:256], lhsT=ckvT[:], rhs=w_ukv_sb[:, 512:768], start=True, stop=True), reads=[ckvT, w_ukv_sb], writes=[FB[1]])
                vst = Vm_st[t % 2]
                kv0 = FB[0][:].rearrange("p (h d) -> p h d", h=4)
                kv1 = FB[1][:, 0:256].rearrange("p (h d) -> p h d", h=2)
                P.op("act", lambda e: e.activation(out=kn[:, 0:4, 0:64], in_=kv0[:, :, 0:64], func=AF.Copy), reads=[FB[0]], writes=[kn])
                P.op("act", lambda e: e.activation(out=kn[:, 4:6, 0:64], in_=kv1[:, :, 0:64], func=AF.Copy), reads=[FB[1]], writes=[kn])
                P.op("dve", lambda e: e.tensor_copy(out=vst[:, 0:4, 0:64], in_=kv0[:, :, 64:128]), reads=[FB[0]], writes=[vst])
                P.op("dve", lambda e: e.tensor_copy(out=vst[:, 4:6, 0:64], in_=kv1[:, :, 64:128]), reads=[FB[1]], writes=[vst])
                P.dma("sp", Vm[t * 128:(t + 1) * 128, :], vst[:].rearrange("p h d -> p (h d)"), reads=[vst], writes=[Vm])
                P.op("dve", lambda e: e.tensor_copy(out=kn[:, 1:6, 64:96], in_=kn[:, 0:1, 64:96].to_broadcast([128, 5, 32])), reads=[kn], writes=[kn])
                st3 = stat[3]
                P.op("act", lambda e: e.activation(out=sq_junk[:, 0:384].rearrange("p (h d) -> p h d", h=NH), in_=kn[:, :, 0:64], func=AF.Square), reads=[kn], writes=[sq_junk])
                P.op("dve", lambda e: e.tensor_reduce(out=st3[:, 0:NH], in_=sq_junk[:, 0:384].rearrange("p (h d) -> p h d", h=NH), axis=AX.X, op=ALU.add), reads=[sq_junk], writes=[st3])
                P.op("dve", lambda e: e.tensor_scalar(out=st3[:, 0:NH], in0=st3[:, 0:NH], scalar1=st1[:, 14:15], scalar2=None, op0=ALU.add), reads=[st3, st1], writes=[st3])
                P.op("act", lambda e: e.activation(out=st3[:, 6:12], in_=st3[:, 0:NH], func=AF.Sqrt, scale=1.0 / 96, bias=eps_c[:, 0:1]), reads=[st3, eps_c], writes=[st3])
                P.op("dve", lambda e: e.reciprocal(out=st3[:, 6:12], in_=st3[:, 6:12]), reads=[st3], writes=[st3])
                P.op("dve", lambda e: e.tensor_tensor(out=kn[:], in0=kn[:], in1=st3[:, 6:12].unsqueeze(2).to_broadcast([128, NH, 96]), op=ALU.mult), reads=[kn, st3], writes=[kn])
                P.op("dve", lambda e: e.tensor_tensor(out=kn[:], in0=kn[:], in1=gkm[:].unsqueeze(1).to_broadcast([128, NH, 96]), op=ALU.mult), reads=[kn, gkm], writes=[kn])
                rope(kn, kfin, cosT[:, t, :], sinT[:, t, :])
                gsl = slice(t * 128, (t + 1) * 128)
                for src_, st_l, dst_d in ((qfin, QTm_st, QTm), (kfin, KTm_st, KTm)):
                    tb2 = TB[1]
                    st_t = st_l[t % 2]
                    for h in range(NH):
                        P.op("pe", lambda e, h=h, src_=src_: e.transpose(tb2[0:96, h * 128:(h + 1) * 128], src_[:, h, :], ident[:]), reads=[src_, ident], writes=[tb2])
                    P.op("act", lambda e, st_t=st_t: e.activation(out=st_t[:], in_=tb2[0:96, 0:768].rearrange("p (h t) -> p h t", h=NH), func=AF.Copy), reads=[tb2], writes=[st_t])
                    P.dma("sp", dst_d[:, :, gsl].rearrange("h p t -> p h t"), st_t[:], reads=[st_t], writes=[dst_d])
                for fb, g_t, dstb, st_l, dst_d in ((FB[3], gqf, fqb, QTf_st, QTf), (FB[4], gkf, fkb, KTf_st, KTf)):
                    st_t = st_l[t % 2]
                    P.op("act", lambda e, fb=fb: e.activation(out=fqn[:].rearrange("p h d -> p (h d)"), in_=fb[:, 0:384], func=AF.Copy), reads=[fb], writes=[fqn])
                    sq = head_rms(fqn, fqn[:], NH, 64, g_t, None, None, 2)
                    P.op("dve", lambda e, sq=sq: e.tensor_tensor(out=fqn[:], in0=fqn[:], in1=sq[:, 6:12].unsqueeze(2).to_broadcast([128, NH, 64]), op=ALU.mult), reads=[fqn, sq], writes=[fqn])
                    P.op("dve", lambda e, g_t=g_t, dstb=dstb: e.tensor_tensor(out=dstb[:], in0=fqn[:], in1=g_t[:].unsqueeze(1).to_broadcast([128, NH, 64]), op=ALU.mult), reads=[fqn, g_t], writes=[dstb])
                    tb2 = TB[1]
                    for h in range(NH):
                        P.op("pe", lambda e, h=h, dstb=dstb: e.transpose(tb2[0:64, h * 128:(h + 1) * 128], dstb[:, h, :], ident[:]), reads=[dstb, ident], writes=[tb2])
                    P.op("act", lambda e, st_t=st_t: e.activation(out=st_t[:], in_=tb2[0:64, 0:768].rearrange("p (h t) -> p h t", h=NH), func=AF.Copy), reads=[tb2], writes=[st_t])
                    P.dma("sp", dst_d[:, 0:64, gsl].rearrange("h p t -> p h t"), st_t[:], reads=[st_t], writes=[dst_d])
                vst = Vf_st[t % 2]
                P.op("dve", lambda e, vst=vst: e.tensor_copy(out=vst[:, :, 0:64], in_=FB[5][:, 0:384].rearrange("p (h d) -> p h d", h=NH)), reads=[FB[5]], writes=[vst])
                P.dma("sp", Vf[t * 128:(t + 1) * 128, :], vst[:].rearrange("p h d -> p (h d)"), reads=[vst], writes=[Vf])
            for sc in range(512 // SUB):
                first = (b == 0 and sc == 0)
                ss_ = slice(sc * SUB, (sc + 1) * SUB)
                gs = slice(b * 512 + sc * SUB, b * 512 + (sc + 1) * SUB)
                if not first:
                    wl_re = wlast[:, 0, :]
                    wl_im = wlast[:, 1, :]
                    ck = s5p[:, s5c["CK"], :]
                    sk = s5p[:, s5c["SK"], :]
                    P.op("dve", lambda e: e.tensor_tensor(out=w0t[:, 0, :], in0=wl_re, in1=ck, op=ALU.mult), reads=[wlast, s5p], writes=[w0t])
                    P.op("dve", lambda e: e.tensor_tensor(out=w0t[:, 1, :], in0=wl_im, in1=sk, op=ALU.mult), reads=[wlast, s5p], writes=[w0t])
                    P.op("dve", lambda e: e.tensor_tensor(out=w0t[:, 2, :], in0=wl_re, in1=sk, op=ALU.mult), reads=[wlast, s5p], writes=[w0t])
                    P.op("dve", lambda e: e.tensor_tensor(out=w0t[:, 3, :], in0=wl_im, in1=ck, op=ALU.mult), reads=[wlast, s5p], writes=[w0t])
                    P.op("dve", lambda e: e.tensor_tensor(out=w0[:, 0, :], in0=w0t[:, 0, :], in1=w0t[:, 1, :], op=ALU.subtract), reads=[w0t], writes=[w0])
                    P.op("dve", lambda e: e.tensor_tensor(out=w0[:, 1, :], in0=w0t[:, 2, :], in1=w0t[:, 3, :], op=ALU.add), reads=[w0t], writes=[w0])
                for m in range(2):
                    hs = slice(4 * m, 4 * m + 4)
                    for jj in range(4):
                        j = 4 * m + jj
                        fbp = FB[j % 2]
                        P.op("pe", lambda e: e.matmul(fbp[:, 0:SUB], lhsT=BreT[:, j, :], rhs=uT[:, m, ss_], start=True, stop=True), reads=[BreT, uT], writes=[fbp])
                        P.op("pe", lambda e: e.matmul(fbp[:, SUB:2 * SUB], lhsT=BimT[:, j, :], rhs=uT[:, m, ss_], start=True, stop=True), reads=[BimT, uT], writes=[fbp])
                        b2 = fbp[:, 0:2 * SUB].rearrange("p (r t) -> p r t", r=2)
                        cb = Ctab[:, j, :].unsqueeze(1).to_broadcast([128, 2, SUB])
                        sb_ = Stab[:, j, :].unsqueeze(1).to_broadcast([128, 2, SUB])
                        P.op("dve", lambda e: e.tensor_tensor(out=pre_c[:], in0=b2, in1=cb, op=ALU.mult), reads=[fbp, Ctab], writes=[pre_c])
                        P.op("dve", lambda e: e.tensor_tensor(out=pre_s[:], in0=b2, in1=sb_, op=ALU.mult), reads=[fbp, Stab], writes=[pre_s])
                        P.op("dve", lambda e: e.tensor_tensor(out=pin_re[:], in0=pre_c[:, 0, :], in1=pre_s[:, 1, :], op=ALU.add), reads=[pre_c, pre_s], writes=[pin_re])
                        P.op("dve", lambda e: e.tensor_tensor(out=pin_im[:], in0=pre_c[:, 1, :], in1=pre_s[:, 0, :], op=ALU.subtract), reads=[pre_c, pre_s], writes=[pin_im])
                        for pin, Wt, ri in ((pin_re, W_re, 0), (pin_im, W_im, 1)):
                            if first:
                                P.op("dve", lambda e: e.tensor_tensor_scan(out=Wt[:, jj, :], data0=Rtab[:, j, :], data1=pin[:], initial=0.0, op0=ALU.mult, op1=ALU.add),
                                     reads=[Rtab, pin], writes=[Wt])
                            else:
                                P.op("dve", lambda e: e.tensor_tensor_scan(out=Wt[:, jj, :], data0=Rtab[:, j, :], data1=pin[:], initial=w0[:, ri, j:j + 1],
                                                                           op0=ALU.mult, op1=ALU.add), reads=[Rtab, pin, w0], writes=[Wt])
                    P.op("dve", lambda e: e.tensor_copy(out=wlast[:, 0, hs], in_=W_re[:, :, SUB - 1]), reads=[W_re], writes=[wlast])
                    P.op("dve", lambda e: e.tensor_copy(out=wlast[:, 1, hs], in_=W_im[:, :, SUB - 1]), reads=[W_im], writes=[wlast])
                    Ch = Ctab[:, hs, :]
                    Sh = Stab[:, hs, :]
                    P.op("dve", lambda e: e.tensor_tensor(out=pt[0][:], in0=W_re[:], in1=Ch, op=ALU.mult), reads=[W_re, Ctab], writes=[pt[0]])
                    P.op("dve", lambda e: e.tensor_tensor(out=pt[1][:], in0=W_im[:], in1=Sh, op=ALU.mult), reads=[W_im, Stab], writes=[pt[1]])
                    P.op("dve", lambda e: e.tensor_tensor(out=s_re[:], in0=pt[0][:], in1=pt[1][:], op=ALU.subtract), reads=[pt[0], pt[1]], writes=[s_re])
                    P.op("dve", lambda e: e.tensor_tensor(out=pt[0][:], in0=W_re[:], in1=Sh, op=ALU.mult), reads=[W_re, Stab], writes=[pt[0]])
                    P.op("dve", lambda e: e.tensor_tensor(out=pt[1][:], in0=W_im[:], in1=Ch, op=ALU.mult), reads=[W_im, Stab], writes=[pt[1]])
                    P.op("dve", lambda e: e.tensor_tensor(out=s_im[:], in0=pt[0][:], in1=pt[1][:], op=ALU.add), reads=[pt[0], pt[1]], writes=[s_im])
                    osl = slice(m * SUB, (m + 1) * SUB)
                    for jj in range(4):
                        j = 4 * m + jj
                        P.op("pe", lambda e: e.matmul(FB[2][:, osl], lhsT=CreT[:, j, :], rhs=s_re[:, jj, :], start=(jj == 0), stop=False), reads=[CreT, s_re], writes=[FB[2]])
                        P.op("pe", lambda e: e.matmul(FB[2][:, osl], lhsT=nCimT[:, j, :], rhs=s_im[:, jj, :], start=False, stop=False), reads=[nCimT, s_im], writes=[FB[2]])
                    P.op("pe", lambda e: e.matmul(FB[2][:, osl], lhsT=Ddiag[:, m, :], rhs=uT[:, m, ss_], start=False, stop=True), reads=[Ddiag, uT], writes=[FB[2]])
                yv = FB[2][:, 0:2 * SUB].rearrange("p (m t) -> p m t", m=2)
                P.op("act", lambda e: e.activation(out=yg[:], in_=yv, func=AF.Copy), reads=[FB[2]], writes=[yg])
                P.op("dve", lambda e: e.tensor_tensor(out=yt1[:], in0=yg[:], in1=yg[:], op=ALU.mult), reads=[yg], writes=[yt1])
                P.op("dve", lambda e: e.tensor_scalar(out=yt1[:], in0=yt1[:], scalar1=0.044715, scalar2=1.0, op0=ALU.mult, op1=ALU.add), reads=[yt1], writes=[yt1])
                P.op("dve", lambda e: e.tensor_tensor(out=yt1[:], in0=yt1[:], in1=yg[:], op=ALU.mult), reads=[yt1, yg], writes=[yt1])
                P.op("dve", lambda e: e.tensor_scalar(out=yt1[:], in0=yt1[:], scalar1=-45.0, scalar2=None, op0=ALU.max), reads=[yt1], writes=[yt1])
                P.op("act", lambda e: e.activation(out=yt1[:], in_=yt1[:], func=AF.Exp, scale=-1.5957691216), reads=[yt1], writes=[yt1])
                P.op("dve", lambda e: e.tensor_scalar(out=yt1[:], in0=yt1[:], scalar1=1.0, scalar2=None, op0=ALU.add), reads=[yt1], writes=[yt1])
                P.op("dve", lambda e: e.reciprocal(out=yt1[:], in_=yt1[:]), reads=[yt1], writes=[yt1])
                P.op("dve", lambda e: e.tensor_tensor(out=yg[:], in0=yg[:], in1=yt1[:], op=ALU.mult), reads=[yg, yt1], writes=[yg])
                P.op("dve", lambda e: e.tensor_copy(out=yTb[:], in_=yg[:]), reads=[yg], writes=[yTb])
                for mo in range(2):
                    osl = slice(mo * SUB, (mo + 1) * SUB)
                    for k in range(2):
                        P.op("pe", lambda e, mo=mo, k=k, osl=osl: e.matmul(FB[3][:, osl], lhsT=w_glu_sb[:, k, mo * 128:(mo + 1) * 128], rhs=yTb[:, k, :], start=(k == 0), stop=(k == 1)),
                             reads=[w_glu_sb, yTb], writes=[FB[3]])
                    P.op("act", lambda e, mo=mo, osl=osl: e.activation(out=yt2[:, mo, :], in_=FB[3][:, osl], func=AF.Exp, scale=-1.0, bias=nbglu_c[:, mo:mo + 1]), reads=[FB[3], nbglu_c], writes=[yt2])
                P.op("dve", lambda e: e.tensor_scalar(out=yt2[:], in0=yt2[:], scalar1=1.0, scalar2=None, op0=ALU.add), reads=[yt2], writes=[yt2])
                P.op("dve", lambda e: e.reciprocal(out=yt2[:], in_=yt2[:]), reads=[yt2], writes=[yt2])
                P.op("dve", lambda e: e.tensor_tensor(out=osm[:], in0=yg[:], in1=yt2[:], op=ALU.mult), reads=[yg, yt2], writes=[osm])
                P.op("dve", lambda e: e.tensor_tensor(out=o2[:], in0=osm[:], in1=osm[:], op=ALU.mult), reads=[osm], writes=[o2])
                for m in range(2):
                    P.op("pe", lambda e, m=m: e.matmul(FB[3][:, 0:SUB], lhsT=ones_f[:, 0:128], rhs=o2[:, m, :], start=(m == 0), stop=(m == 1)), reads=[ones_f, o2], writes=[FB[3]])
                P.op("act", lambda e: e.activation(out=rs_bc[:], in_=FB[3][:, 0:SUB], func=AF.Sqrt, scale=1.0 / 256, bias=eps_c[:, 0:1]), reads=[FB[3], eps_c], writes=[rs_bc])
                P.op("dve", lambda e: e.reciprocal(out=rs_bc[:], in_=rs_bc[:]), reads=[rs_bc], writes=[rs_bc])
                P.op("dve", lambda e: e.tensor_tensor(out=msm[:], in0=osm[:], in1=rs_bc[:].unsqueeze(1).to_broadcast([128, 2, SUB]), op=ALU.mult), reads=[osm, rs_bc], writes=[msm])
                P.dma("sp", mssm[:, :, gs].rearrange("m p t -> p m t"), msm[:], reads=[msm], writes=[mssm])

    def phase_b(l):
        hi = 0
        ob_i = 0
        for mixer in range(2):
            QTd, KTd, Vd, dk, scale = ((QTm, KTm, Vm, 96, 1.0 / math.sqrt(96.0)), (QTf, KTf, Vf, 68, 0.125))[mixer]
            P.dma("sp", V_sb[:], Vd[:].rearrange("(t p) c -> p t c", p=128), reads=[Vd], writes=[V_sb])
            for h in range(NH):
                Qs = QT_sb[hi % 2]
                Ks = KT_sb[hi % 2]
                hi += 1
                P.dma("sp", Qs[0:dk, :], QTd[h], reads=[QTd], writes=[Qs])
                P.dma("sp", Ks[0:dk, :], KTd[h], reads=[KTd], writes=[Ks])
                it = 0
                for b in range(NB):
                    nk = 4 * b + 4
                    for kt in range(nk):
                        j = kt - 4 * b
                        q0 = 0 if j <= 0 else 128 * j
                        Sb = FB[it % 2]
                        pt_ = PT[it % 3]
                        it += 1
                        qsl = slice(b * 512 + q0, (b + 1) * 512)
                        P.op("pe", lambda e, Sb=Sb, kt=kt, qsl=qsl, q0=q0: e.matmul(Sb[:, q0:512], lhsT=Ks[0:dk, kt * 128:(kt + 1) * 128], rhs=Qs[0:dk, qsl], start=True, stop=True),
                             reads=[Ks, Qs], writes=[Sb])
                        P.op("act", lambda e, Sb=Sb, pt_=pt_, q0=q0: e.activation(out=pt_[:, q0:512], in_=Sb[:, q0:512], func=AF.Exp, scale=scale), reads=[Sb], writes=[pt_])
                        if j >= 0:
                            if mixer == 0:
                                P.op("pool", lambda e, pt_=pt_, q0=q0: e.memset(pt_[64:128, q0:q0 + 64], 0.0), writes=[pt_])
                            else:
                                P.op("pool", lambda e, pt_=pt_, q0=q0: e.affine_select(out=pt_[:, q0:q0 + 128], in_=pt_[:, q0:q0 + 128], pattern=[[1, 128]], compare_op=ALU.is_ge,
                                                                                     fill=0.0, base=0, channel_multiplier=-1), reads=[pt_], writes=[pt_])
                        OT = FB[2 + (ob_i % 2)]
                        P.op("pe", lambda e: e.matmul(OT[0:65, q0:512], lhsT=V_sb[:, kt, h * 65:(h + 1) * 65], rhs=pt_[:, q0:512], start=(kt == 0), stop=(kt == nk - 1)),
                             reads=[pt_, V_sb], writes=[OT])
                    OT = FB[2 + (ob_i % 2)]
                    Otr = FB[4 + (ob_i % 2)]
                    osb_ = osb[ob_i % 2]
                    rd = rden[ob_i % 2]
                    ob_i += 1
                    col = mixer * 384 + h * 64
                    P.op("act", lambda e: e.activation(out=osb_[:], in_=OT[0:65, :], func=AF.Copy), reads=[OT], writes=[osb_])
                    for qi in range(4):
                        P.op("pe", lambda e: e.transpose(Otr[:, qi * 65:(qi + 1) * 65], osb_[:, qi * 128:(qi + 1) * 128], identf[0:65, 0:65]), reads=[osb_, identf], writes=[Otr])
                    o3 = Otr[:, 0:260].rearrange("p (q c) -> p q c", q=4)
                    P.op("dve", lambda e: e.reciprocal(out=rd[:], in_=o3[:, :, 64]), reads=[Otr], writes=[rd])
                    P.op("dve", lambda e: e.tensor_tensor(out=o_attn[:, 4 * b:4 * b + 4, col:col + 64], in0=o3[:, :, 0:64], in1=rd[:].unsqueeze(2).to_broadcast([128, 4, 64]), op=ALU.mult),
                         reads=[Otr, rd], writes=[o_attn])

    def phase_c(l, moe):
        j2 = l // 2
        if moe:
            P.dma("sp", wr_sb[:], A["moe_wr"][j2].rearrange("(k p) n -> p k n", p=128), writes=[wr_sb])
            P.dma("sp", br_sb[:], A["moe_br"][j2].to_broadcast([128, 8]), writes=[br_sb])
        for t in range(NT):
            xs_ = xs[t % 2]
            if l == 0:
                P.dma("sp", xs_[:], x_in[t * 128:(t + 1) * 128, :], writes=[xs_])
            else:
                P.dma("sp", xs_[:], y[t * 128:(t + 1) * 128, :], reads=[ytile[t]], writes=[xs_])
            st_ = stat[t % 4]
            for mx in range(2):
                P.op("act", lambda e, mx=mx: e.activation(out=sq_junk[:, mx * 384:(mx + 1) * 384], in_=o_attn[:, t, mx * 384:(mx + 1) * 384], func=AF.Square, accum_out=st_[:, mx:mx + 1]),
                     reads=[o_attn], writes=[sq_junk, st_])
            P.op("act", lambda e: e.activation(out=st_[:, 2:4], in_=st_[:, 0:2], func=AF.Sqrt, scale=1.0 / 384, bias=eps_c[:, 0:1]), reads=[st_, eps_c], writes=[st_])
            P.op("dve", lambda e: e.reciprocal(out=st_[:, 2:4], in_=st_[:, 2:4]), reads=[st_], writes=[st_])
            for mx in range(2):
                P.op("dve", lambda e, mx=mx: e.tensor_scalar(out=mat[:, mx * 384:(mx + 1) * 384], in0=o_attn[:, t, mx * 384:(mx + 1) * 384], scalar1=st_[:, 2 + mx:3 + mx], scalar2=None, op0=ALU.mult),
                     reads=[o_attn, st_], writes=[mat])
            tb = TB[t % 2]
            for k in range(6):
                P.op("pe", lambda e, k=k: e.transpose(tb[:, k * 128:(k + 1) * 128], mat[:, k * 128:(k + 1) * 128], ident[:]), reads=[mat, ident], writes=[tb])
            P.op("act", lambda e: e.activation(out=mT[:, 2:8, :], in_=tb[:, 0:768].rearrange("p (k t) -> p k t", k=6), func=AF.Copy), reads=[tb], writes=[mT])
            P.dma("sp", mT[:, 0:2, :], mssm[:, :, t * 128:(t + 1) * 128].rearrange("m p t -> p m t"), reads=[mssm], writes=[mT])
            xn = xnew[t % 2]
            for hf in range(2):
                fb = FB[hf]
                for k in range(KD):
                    P.op("pe", lambda e, fb=fb, k=k, hf=hf: e.matmul(fb[:], lhsT=mT[:, k, :], rhs=w_out_sb[:, k, hf * 512:(hf + 1) * 512], start=(k == 0), stop=(k == KD - 1)),
                         reads=[mT, w_out_sb], writes=[fb])
                P.op("dve", lambda e, fb=fb, hf=hf: e.tensor_tensor(out=xn[:, hf * 512:(hf + 1) * 512], in0=fb[:], in1=xs_[:, hf * 512:(hf + 1) * 512], op=ALU.add), reads=[fb, xs_], writes=[xn])
            P.dma("sp", y[t * 128:(t + 1) * 128, :], xn[:], reads=[xn], writes=[ytile[t]])
            norm_transpose(xn, xn[:], l, 2, 3, h2T_st, slice(0, 128), t, fp32_path=(xhf, h2Tf) if moe else None)
            P.dma("sp", h2T_d[:, :, t * 128:(t + 1) * 128].rearrange("k p t -> p k t"), h2T_st[:], reads=[h2T_st], writes=[h2T_d])
            if moe:
                fb = FB[2]
                for k in range(KD):
                    P.op("pe", lambda e, k=k: e.matmul(fb[:, 0:8], lhsT=h2Tf[:, k, :], rhs=wr_sb[:, k, :], start=(k == 0), stop=(k == KD - 1)), reads=[h2Tf, wr_sb], writes=[fb])
                lg, m1, m2, lg2 = rtmp
                s1, s2, s3, s4 = rsc
                P.op("dve", lambda e: e.tensor_tensor(out=lg[:], in0=fb[:, 0:8], in1=br_sb[:], op=ALU.add), reads=[fb, br_sb], writes=[lg])
                P.op("dve", lambda e: e.tensor_reduce(out=s1[:], in_=lg[:], axis=AX.X, op=ALU.max), reads=[lg], writes=[s1])
                P.op("dve", lambda e: e.tensor_scalar(out=m1[:], in0=lg[:], scalar1=s1[:, 0:1], scalar2=None, op0=ALU.is_equal), reads=[lg, s1], writes=[m1])
                P.op("dve", lambda e: e.scalar_tensor_tensor(out=lg2[:], in0=m1[:], scalar=-1e30, in1=lg[:], op0=ALU.mult, op1=ALU.add), reads=[m1, lg], writes=[lg2])
                P.op("dve", lambda e: e.tensor_reduce(out=s2[:], in_=lg2[:], axis=AX.X, op=ALU.max), reads=[lg2], writes=[s2])
                P.op("dve", lambda e: e.tensor_scalar(out=m2[:], in0=lg2[:], scalar1=s2[:, 0:1], scalar2=None, op0=ALU.is_equal), reads=[lg2, s2], writes=[m2])
                P.op("dve", lambda e: e.tensor_tensor(out=s3[:], in0=s2[:], in1=s1[:], op=ALU.subtract), reads=[s1, s2], writes=[s3])
                P.op("act", lambda e: e.activation(out=s3[:], in_=s3[:], func=AF.Exp), reads=[s3], writes=[s3])
                P.op("dve", lambda e: e.tensor_scalar(out=s3[:], in0=s3[:], scalar1=1.0, scalar2=None, op0=ALU.add), reads=[s3], writes=[s3])
                P.op("dve", lambda e: e.reciprocal(out=s3[:], in_=s3[:]), reads=[s3], writes=[s3])
                P.op("dve", lambda e: e.tensor_scalar(out=s4[:], in0=s3[:], scalar1=-1.0, scalar2=1.0, op0=ALU.mult, op1=ALU.add), reads=[s3], writes=[s4])
                P.op("dve", lambda e: e.tensor_scalar(out=m1[:], in0=m1[:], scalar1=s3[:, 0:1], scalar2=None, op0=ALU.mult), reads=[m1, s3], writes=[m1])
                P.op("dve", lambda e, t=t: e.scalar_tensor_tensor(out=comb_all[:, t, :], in0=m2[:], scalar=s4[:, 0:1], in1=m1[:], op0=ALU.mult, op1=ALU.add), reads=[m2, s4, m1], writes=[comb_all])

    def phase_d(l, moe):
        j2 = l // 2
        P.dma("sp", G_bc[:], g12[l, 1], reads=[g12], writes=[G_bc])
        ne = 8 if moe else 2
        it = 0
        oi = 0
        for ex in range(ne):
            if moe:
                wg = A["moe_wg"][j2, ex]
                wu = A["moe_wu"][j2, ex]
                wd = A["moe_wd"][j2, ex]
            else:
                wg = A["ffn_wg"][j2][:, ex * DFE:(ex + 1) * DFE]
                wu = A["ffn_wu"][j2][:, ex * DFE:(ex + 1) * DFE]
                wd = A["ffn_wd"][j2][ex * DFE:(ex + 1) * DFE, :]
            Wg_, Wu_, Wd_ = Wg_sb[ex % 2], Wu_sb[ex % 2], Wd_sb[ex % 2]
            P.dma("pool", Wg_[:], wg.rearrange("(k p) f -> p k f", p=128), writes=[Wg_])
            P.dma("pool", Wu_[:], wu.rearrange("(k p) f -> p k f", p=128), writes=[Wu_])
            P.dma("pool", Wd_[:], wd.rearrange("(c p) d -> p c d", p=128), writes=[Wd_])
            for b in range(NB):
                hb = h2T_sb[(ex * NB + b) % 2]
                P.dma("sp", hb[:], h2T_d[:, :, b * 512:(b + 1) * 512].rearrange("k p t -> p k t"), reads=[h2T_d], writes=[hb])
                for c in range(NFC):
                    gb = FB[(it % 2) * 2]
                    ub = FB[(it % 2) * 2 + 1]
                    sg_ = sg[it % 2]
                    it += 1
                    for k in range(KD):
                        P.op("pe", lambda e, gb=gb, k=k, c=c: e.matmul(gb[:], lhsT=Wg_[:, k, c * 128:(c + 1) * 128], rhs=hb[:, k, :], start=(k == 0), stop=(k == KD - 1)), reads=[Wg_, hb], writes=[gb])
                    for k in range(KD):
                        P.op("pe", lambda e, ub=ub, k=k, c=c: e.matmul(ub[:], lhsT=Wu_[:, k, c * 128:(c + 1) * 128], rhs=hb[:, k, :], start=(k == 0), stop=(k == KD - 1)), reads=[Wu_, hb], writes=[ub])
                    P.op("act", lambda e, gb=gb, sg_=sg_: e.activation(out=sg_[:], in_=gb[:], func=AF.Silu), reads=[gb], writes=[sg_])
                    P.op("dve", lambda e, ub=ub, sg_=sg_, c=c: e.tensor_tensor(out=aT[:, c, :], in0=ub[:], in1=sg_[:], op=ALU.mult), reads=[ub, sg_], writes=[aT])
                for ti in range(4):
                    t = b * 4 + ti
                    os_ = ost[oi % 2]
                    oi += 1
                    for hf in range(2):
                        fb = FB[4 + hf]
                        for c in range(NFC):
                            P.op("pe", lambda e, fb=fb, c=c, ti=ti, hf=hf: e.matmul(fb[:], lhsT=aT[:, c, ti * 128:(ti + 1) * 128], rhs=Wd_[:, c, hf * 512:(hf + 1) * 512], start=(c == 0), stop=(c == NFC - 1)),
                                 reads=[aT, Wd_], writes=[fb])
                        if moe:
                            P.op("dve", lambda e, fb=fb, hf=hf, t=t, ex=ex, os_=os_: e.scalar_tensor_tensor(out=os_[:, hf * 512:(hf + 1) * 512], in0=fb[:], scalar=comb_all[:, t, ex:ex + 1],
                                                                                                       in1=G_bc[:, hf * 512:(hf + 1) * 512], op0=ALU.mult, op1=ALU.mult), reads=[fb, comb_all, G_bc], writes=[os_])
                        else:
                            P.op("dve", lambda e, fb=fb, hf=hf, os_=os_: e.tensor_tensor(out=os_[:, hf * 512:(hf + 1) * 512], in0=fb[:], in1=G_bc[:, hf * 512:(hf + 1) * 512], op=ALU.mult),
                                 reads=[fb, G_bc], writes=[os_])
                    P.dma("pool", y[t * 128:(t + 1) * 128, :], os_[:], reads=[os_, ytile[t]], writes=[ytile[t]], accum_op=ALU.add)

    stop = build.stop_after
    G = globals()

    def use(dct):
        for k_, v_ in dct.items():
            if k_ not in ("P", "L", "NT"):
                G[k_] = v_
    dbg = P.dram("dbg_oattn", [128, NT * 768], BF16, kind="ExternalOutput") if debug else None
    for l in range(n_layers):
        moe = (l % 2 == 1)
        P.push()
        use(_alloc_a(P, L))
        s5c = load_layer(l)
        phase_a(l, s5c)
        P.pop()
        if stop == "a":
            break
        P.push()
        use(_alloc_bc(P, L))
        P.push()
        use(_alloc_b(P, L))
        phase_b(l)
        P.pop()
        if debug and (stop == "b" or l == n_layers - 1):
            P.dma("sp", dbg[:], o_attn[:].rearrange("p t c -> p (t c)"), reads=[o_attn], writes=[dbg])
        if stop == "b":
            P.pop()
            break
        P.push()
        use(_alloc_c(P, L))
        load_c(l)
        phase_c(l, moe)
        P.pop()
        P.pop()
        if stop == "c":
            break
        P.push()
        use(_alloc_d(P, L))
        phase_d(l, moe)
        P.pop()
    P.finish()
    build.n_inst = P.n_inst
    return nc


build.stop_after = None


def prep_shared(inp, n_layers=DEPTH):
    f = lambda a: np.ascontiguousarray(np.asarray(a, dtype=np.float32))
    col = lambda a, k: f(np.asarray(a).reshape(DEPTH, k, 128).transpose(0, 2, 1))
    out = {}
    out["norm_mix_c"] = col(inp["norm_mix"], KD)
    out["norm_ffn_c"] = col(inp["norm_ffn"], KD)
    out["w_ada"] = f(inp["w_ada"])
    out["b_ada"] = f(np.asarray(inp["b_ada"]).reshape(DEPTH, 1, 6 * D))
    out["w_in"] = f(inp["w_in"])
    sm = lambda a: f(np.asarray(a).reshape(DEPTH, 8, 128).transpose(0, 2, 1))
    out["lam_re"] = sm(inp["ssm_lam_re"])
    out["lam_im"] = sm(inp["ssm_lam_im"])
    out["log_dt"] = sm(np.repeat(np.asarray(inp["ssm_log_dt"])[:, :, None], 64, axis=2))
    b_re = np.asarray(inp["ssm_b_re"]); b_im = np.asarray(inp["ssm_b_im"])
    c_re = np.asarray(inp["ssm_c_re"]); c_im = np.asarray(inp["ssm_c_im"])
    blk = {k: np.zeros((DEPTH, 8, 128, 128), np.float32) for k in ("b_re", "b_im", "c_re", "c_im")}
    for g in range(16):
        j, gg = g // 2, g % 2
        fc = (g % 8) * 16
        blk["b_re"][:, j, gg * 64:(gg + 1) * 64, fc:fc + 16] = b_re[:, g]
        blk["b_im"][:, j, gg * 64:(gg + 1) * 64, fc:fc + 16] = b_im[:, g]
        blk["c_re"][:, j, gg * 64:(gg + 1) * 64, fc:fc + 16] = c_re[:, g].transpose(0, 2, 1)
        blk["c_im"][:, j, gg * 64:(gg + 1) * 64, fc:fc + 16] = c_im[:, g].transpose(0, 2, 1)
    out.update(blk)
    out["ssm_d"] = col(inp["ssm_d"], 2)
    out["w_glu"] = f(inp["ssm_w_glu"])
    out["b_glu"] = col(inp["ssm_b_glu"], 2)
    out["q_norm"] = col(inp["mla_q_norm"], 2)
    out["kv_norm"] = col(inp["mla_kv_norm"], 1)
    out["w_uq"] = f(inp["mla_w_uq"])
    out["w_ukv"] = f(inp["mla_w_ukv"])
    rep = lambda a: f(np.broadcast_to(np.asarray(a)[:, None, :], (DEPTH, 128, np.asarray(a).shape[1])))
    out["gq_m"] = rep(inp["mla_qk_gq"])
    out["gk_m"] = rep(inp["mla_qk_gk"])
    out["fox_bf"] = f(np.asarray(inp["fox_b_f"]).reshape(DEPTH, NH, 1))
    out["gq_f"] = rep(inp["fox_qk_gq"])
    out["gk_f"] = rep(inp["fox_qk_gk"])
    out["out_norm"] = col(inp["out_norm"], KD)
    out["w_out"] = f(inp["w_out"])
    out["ffn_wg"] = f(inp["ffn_w_gate"])
    out["ffn_wu"] = f(inp["ffn_w_up"])
    out["ffn_wd"] = f(inp["ffn_w_down"])
    out["moe_wr"] = f(inp["moe_w_router"])
    out["moe_br"] = f(np.asarray(inp["moe_b_router"]).reshape(2, 1, 8))
    out["moe_wg"] = f(inp["moe_w_gate"])
    out["moe_wu"] = f(inp["moe_w_up"])
    out["moe_wd"] = f(inp["moe_w_down"])
    half = 16
    inv = (10000.0 ** (-np.arange(half, dtype=np.float32) / half)).astype(np.float32)
    out["inv_bc"] = f(np.broadcast_to(inv[None, :], (128, 16)))
    return out


def prep_core(inp, b, L):
    NT = L // 128
    m = {}
    m["x"] = np.ascontiguousarray(np.asarray(inp["x"])[b, :L, :], dtype=np.float32)
    m["c_col"] = np.ascontiguousarray(np.asarray(inp["c"], dtype=np.float32)[b].reshape(KD, 128).T)
    m["pos"] = np.ascontiguousarray(np.asarray(inp["positions"])[b, :L].astype(np.int32).reshape(NT, 128).T)
    return m


_CACHE = {}


def run(inp, L, n_layers=DEPTH, debug=False, cores=8, trace=False):
    key = (L, n_layers, debug, build.stop_after)
    if key not in _CACHE:
        _CACHE[key] = build(L, n_layers, debug)
    nc = _CACHE[key]
    shared = prep_shared(inp, n_layers)
    in_maps = []
    for b in range(cores):
        m = dict(shared)
        m.update(prep_core(inp, b, L))
        in_maps.append(m)
    res = run_bass_kernel_spmd(nc, in_maps, core_ids=list(range(cores)), **({"trace": True} if trace else {}))
    return res


def kernel(**inputs):
    L = np.asarray(inputs["x"]).shape[1]
    res = run(inputs, L)
    out = np.stack([np.asarray(r["y"], dtype=np.float32) for r in res.results], axis=0)
    return out
```

```python
import contextlib
import math
import sys
import numpy as np
import concourse.bass as bass
import concourse.mybir as mybir
from concourse.bass_utils import run_bass_kernel_spmd

F32 = mybir.dt.float32
BF16 = mybir.dt.bfloat16
I32 = mybir.dt.int32
AF = mybir.ActivationFunctionType
ALU = mybir.AluOpType
AX = mybir.AxisListType

ENGS = ("pe", "act", "dve", "pool", "sp")

D = 1024
KD = 8
DEPTH = 4
EPS = 1e-6
IN_COLS = 1830
NH = 6
DFE = 1408
NFC = 11
SUB = 256


class Tl:
    __slots__ = ("t", "name", "w", "r", "excl")

    def __init__(self, t, name):
        self.t = t
        self.name = name
        self.excl = False
        self.w = {}
        self.r = {}

    def __getitem__(self, idx):
        return self.t[idx]


class Prog:
    max_ops = 10 ** 9
    log = None

    def __init__(self, nc, ring_sizes=None):
        self.nc = nc
        self.es = contextlib.ExitStack()
        self.cnt = {e: 0 for e in ENGS}
        self.sems = {}
        self.seen = {e: {} for e in ENGS}
        for e in ENGS:
            self.sems[("eng", e)] = self.es.enter_context(nc.semaphore("s_" + e))
        ring_sizes = ring_sizes or {"sp": 16, "pool": 8, "act": 2}
        self.rings = {}
        self.ring_i = {}
        for e, k in ring_sizes.items():
            self.rings[e] = []
            for i in range(k):
                key = ("ring", e, i)
                self.sems[key] = self.es.enter_context(nc.semaphore("r_%s%d" % (e, i)))
                self.rings[e].append([key, 0])
            self.ring_i[e] = 0
        self.n_inst = 0
        self.scopes = [self.es]
        self.E = {"pe": nc.tensor, "act": nc.scalar, "dve": nc.vector, "pool": nc.gpsimd, "sp": nc.sync}

    def sb(self, name, shape, dt):
        self._uid = getattr(self, "_uid", 0) + 1
        return Tl(self.scopes[-1].enter_context(self.nc.sbuf_tensor("%s_%d" % (name, self._uid), list(shape), dt)), name)

    def push(self):
        self.scopes.append(contextlib.ExitStack())

    def barrier(self):
        need = {}
        for e, ring in self.rings.items():
            for key, v in ring:
                if v > 0:
                    need[key] = v
        for e in ENGS:
            if self.cnt[e] > 0:
                need[("eng", e)] = self.cnt[e]
        for eng in ENGS:
            seen = self.seen[eng]
            for k, v in need.items():
                if k == ("eng", eng) or seen.get(k, 0) >= v:
                    continue
                seen[k] = v
                self.E[eng].wait_ge(self.sems[k], v)

    def pop(self):
        self.barrier()
        self.scopes.pop().close()

    def ps(self, name, shape, dt):
        t = Tl(self.es.enter_context(self.nc.psum_tensor(name, list(shape), dt)), name)
        t.excl = True
        return t

    def dram(self, name, shape, dt, kind="Internal"):
        return Tl(self.nc.dram_tensor(name, list(shape), dt, kind=kind).ap(), name)

    def _collect(self, eng, reads, writes):
        need = {}
        me = ("eng", eng)
        for t in reads:
            for k, v in t.w.items():
                if need.get(k, 0) < v:
                    need[k] = v
            if t.excl:
                for k, v in t.r.items():
                    if k != me and need.get(k, 0) < v:
                        need[k] = v
        for t in writes:
            for k, v in t.w.items():
                if need.get(k, 0) < v:
                    need[k] = v
            for k, v in t.r.items():
                if need.get(k, 0) < v:
                    need[k] = v
        waits = []
        seen = self.seen[eng]
        for k, v in need.items():
            if eng == "pe" and k == ("eng", "pe"):
                continue
            if seen.get(k, 0) >= v:
                continue
            seen[k] = v
            waits.append((self.sems[k], v))
        return waits

    def _commit(self, reads, writes, key, val):
        for t in reads:
            if t.r.get(key, 0) < val:
                t.r[key] = val
        for t in writes:
            t.w = {key: val}
            t.r = {}

    def op(self, eng, fn, reads=(), writes=()):
        self.n_inst += 1
        if Prog.log is not None:
            Prog.log.append((self.n_inst, eng, sys._getframe(1).f_lineno))
        if self.n_inst > Prog.max_ops:
            return
        waits = self._collect(eng, reads, writes)
        self.cnt[eng] += 1
        key = ("eng", eng)
        val = self.cnt[eng]
        self._commit(reads, writes, key, val)
        e = self.E[eng]
        for s, v in waits[1:]:
            e.wait_ge(s, v)
        ins = fn(e)
        if waits:
            ins._wait_ge(waits[0][0], waits[0][1])
        ins.then_inc(self.sems[key], 1)

    def dma(self, eng, out, in_, reads=(), writes=(), **kw):
        self.n_inst += 1
        if Prog.log is not None:
            Prog.log.append((self.n_inst, "dma-" + eng, sys._getframe(1).f_lineno))
        if self.n_inst > Prog.max_ops:
            return
        ring = self.rings[eng]
        slot = ring[self.ring_i[eng] % len(ring)]
        self.ring_i[eng] += 1
        key, pv = slot
        waits = self._collect(eng, reads, writes)
        seen = self.seen[eng]
        if pv > 0 and seen.get(key, 0) < pv:
            seen[key] = pv
            waits.append((self.sems[key], pv))
        val = pv + 16
        slot[1] = val
        self._commit(reads, writes, key, val)
        e = self.E[eng]
        for s, v in waits[1:]:
            e.wait_ge(s, v)
        ins = e.dma_start(out=out, in_=in_, **kw)
        if waits:
            ins._wait_ge(waits[0][0], waits[0][1])
        ins.then_inc(self.sems[key], 16)

    def finish(self, eng="sp"):
        need = {}
        for e, ring in self.rings.items():
            for key, v in ring:
                if v > 0:
                    need[key] = v
        for e in ENGS:
            if self.cnt[e] > 0:
                need[("eng", e)] = self.cnt[e]
        E = self.E[eng]
        for k, v in need.items():
            E.wait_ge(self.sems[k], v)
        self.es.close()


LAYER_PARAMS = [
    ("norm_mix_c", [128, KD]), ("norm_ffn_c", [128, KD]),
    ("w_ada", [D, 6 * D]), ("b_ada", [1, 6 * D]),
    ("w_in", [D, IN_COLS]),
    ("lam_re", [128, 8]), ("lam_im", [128, 8]), ("log_dt", [128, 8]),
    ("b_re", [8, 128, 128]), ("b_im", [8, 128, 128]),
    ("c_re", [8, 128, 128]), ("c_im", [8, 128, 128]),
    ("ssm_d", [128, 2]), ("w_glu", [256, 256]), ("b_glu", [128, 2]),
    ("q_norm", [128, 2]), ("kv_norm", [128, 1]),
    ("w_uq", [256, 576]), ("w_ukv", [128, 768]),
    ("gq_m", [128, 96]), ("gk_m", [128, 96]),
    ("fox_bf", [NH, 1]), ("gq_f", [128, 64]), ("gk_f", [128, 64]),
    ("out_norm", [128, KD]), ("w_out", [D, D]),
]


PADDED = ("w_ada", "w_in", "b_re", "b_im", "c_re", "c_im", "w_glu", "w_uq", "w_ukv", "w_out",
          "ffn_wg", "ffn_wu", "ffn_wd", "moe_wg", "moe_wu", "moe_wd")


def _alloc_a(P, L):
    NT = L // 128
    w_in_sb = P.sb("w_in_sb", [128, KD, IN_COLS], BF16)
    w_uq_sb = P.sb("w_uq_sb", [128, 2, 576], BF16)
    w_ukv_sb = P.sb("w_ukv_sb", [128, 768], BF16)
    w_glu_sb = P.sb("w_glu_sb", [128, 2, 256], BF16)
    qn_c = P.sb("qn_c", [128, 2], F32)
    kvn_c = P.sb("kvn_c", [128, 1], F32)
    gqm = P.sb("gqm", [128, 96], F32)
    gkm = P.sb("gkm", [128, 96], F32)
    gqf = P.sb("gqf", [128, 64], F32)
    gkf = P.sb("gkf", [128, 64], F32)
    nbf = P.sb("nbf", [NH, 1], F32)
    dcol = P.sb("dcol", [128, 2], F32)
    bglu_c = P.sb("bglu_c", [128, 2], F32)
    nbglu_c = P.sb("nbglu_c", [128, 2], F32)
    s5p = P.sb("s5p", [128, 24, 8], F32)
    BreT = P.sb("BreT", [128, 8, 128], BF16)
    BimT = P.sb("BimT", [128, 8, 128], BF16)
    CreT = P.sb("CreT", [128, 8, 128], BF16)
    nCimT = P.sb("nCimT", [128, 8, 128], BF16)
    Ddiag = P.sb("Ddiag", [128, 2, 128], BF16)
    Ctab = P.sb("Ctab", [128, 8, SUB], F32)
    Stab = P.sb("Stab", [128, 8, SUB], F32)
    Rtab = P.sb("Rtab", [128, 8, SUB], F32)
    blk_f = P.sb("blk_f", [128, 2, 128], F32)
    blk_o = P.sb("blk_o", [128, 2, 128], F32)
    hT = [P.sb("hT%d" % i, [128, KD, 512], BF16) for i in range(2)]
    uT = P.sb("uT", [128, 2, 512], BF16)
    cq_h = P.sb("cq_h", [128, 256], BF16)
    ckv_h = P.sb("ckv_h", [128, 128], BF16)
    cqT = P.sb("cqT", [128, 2, 128], BF16)
    ckvT = P.sb("ckvT", [128, 128], BF16)
    qn = P.sb("qn", [128, NH, 96], F32)
    kn = P.sb("kn", [128, NH, 96], F32)
    rt = [P.sb("rt%d" % i, [128, NH, 16], F32) for i in range(4)]
    qfin = P.sb("qfin", [128, NH, 96], BF16)
    kfin = P.sb("kfin", [128, NH, 96], BF16)
    fqn = P.sb("fqn", [128, NH, 64], F32)
    fqb = P.sb("fqb", [128, NH, 64], BF16)
    fkb = P.sb("fkb", [128, NH, 64], BF16)
    QTm_st = [P.sb("QTm_st%d" % i, [96, NH, 128], BF16) for i in range(2)]
    KTm_st = [P.sb("KTm_st%d" % i, [96, NH, 128], BF16) for i in range(2)]
    QTf_st = [P.sb("QTf_st%d" % i, [64, NH, 128], BF16) for i in range(2)]
    KTf_st = [P.sb("KTf_st%d" % i, [64, NH, 128], BF16) for i in range(2)]
    Vm_st = [P.sb("Vm_st%d" % i, [128, NH, 65], BF16) for i in range(2)]
    Vf_st = [P.sb("Vf_st%d" % i, [128, NH, 65], BF16) for i in range(2)]
    for t_ in Vm_st + Vf_st:
        P.op("pool", lambda e, t_=t_: e.memset(t_[:], 1.0), writes=[t_])
    fg_e = P.sb("fg_e", [NH, 512], F32)
    fg_sp = P.sb("fg_sp", [NH, 512], F32)
    fg_cum = P.sb("fg_cum", [NH, 512], F32)
    fg_carry = P.sb("fg_carry", [NH, 1], F32)
    fg_hi = P.sb("fg_hi", [NH, 512], BF16)
    fg_lo = P.sb("fg_lo", [NH, 512], BF16)
    fg_nhi = P.sb("fg_nhi", [NH, 512], BF16)
    fg_nlo = P.sb("fg_nlo", [NH, 512], BF16)
    W_re = P.sb("W_re", [128, 4, SUB], F32)
    W_im = P.sb("W_im", [128, 4, SUB], F32)
    wlast = P.sb("wlast", [128, 2, 8], F32)
    pre_c = P.sb("pre_c", [128, 2, SUB], F32)
    pre_s = P.sb("pre_s", [128, 2, SUB], F32)
    pin_re = P.sb("pin_re", [128, SUB], F32)
    pin_im = P.sb("pin_im", [128, SUB], F32)
    w0 = P.sb("w0", [128, 2, 8], F32)
    w0t = P.sb("w0t", [128, 4, 8], F32)
    pt = [P.sb("pt%d" % i, [128, 4, SUB], F32) for i in range(2)]
    s_re = P.sb("s_re", [128, 4, SUB], BF16)
    s_im = P.sb("s_im", [128, 4, SUB], BF16)
    yg = P.sb("yg", [128, 2, SUB], F32)
    yt1 = P.sb("yt1", [128, 2, SUB], F32)
    yt2 = P.sb("yt2", [128, 2, SUB], F32)
    yTb = P.sb("yTb", [128, 2, SUB], BF16)
    o2 = P.sb("o2", [128, 2, SUB], F32)
    osm = P.sb("osm", [128, 2, SUB], F32)
    rs_bc = P.sb("rs_bc", [128, SUB], F32)
    msm = P.sb("msm", [128, 2, SUB], BF16)
    return locals()


def _alloc_bc(P, L):
    NT = L // 128
    o_attn = P.sb("o_attn", [128, NT, 768], BF16)
    return locals()


def _alloc_b(P, L):
    NT = L // 128
    QT_sb = [P.sb("QT_sb%d" % i, [96, L], BF16) for i in range(2)]
    KT_sb = [P.sb("KT_sb%d" % i, [96, L], BF16) for i in range(2)]
    V_sb = P.sb("V_sb", [128, NT, NH * 65], BF16)
    PT = [P.sb("PT%d" % i, [128, 512], BF16) for i in range(3)]
    rden = [P.sb("rden%d" % i, [128, 4], F32) for i in range(2)]
    osb = [P.sb("osb%d" % i, [65, 512], F32) for i in range(2)]
    return locals()


def _alloc_c(P, L):
    NT = L // 128
    w_out_sb = P.sb("w_out_sb", [128, KD, D], BF16)
    wstc = [P.sb("wstc%d" % i, [128, D], F32) for i in range(2)]
    onorm_c = P.sb("onorm_c", [128, KD], F32)
    mat = P.sb("mat", [128, 768], BF16)
    mT = P.sb("mT", [128, KD, 128], BF16)
    xnew = [P.sb("xnew%d" % i, [128, D], F32) for i in range(2)]
    h2T_st = P.sb("h2T_st", [128, KD, 128], BF16)
    xhf = P.sb("xhf", [128, D], F32)
    h2Tf = P.sb("h2Tf", [128, KD, 128], F32)
    wr_sb = P.sb("wr_sb", [128, KD, 8], F32)
    br_sb = P.sb("br_sb", [128, 8], F32)
    rtmp = [P.sb("rtmp%d" % i, [128, 8], F32) for i in range(4)]
    rsc = [P.sb("rsc%d" % i, [128, 1], F32) for i in range(4)]
    return locals()


def _alloc_d(P, L):
    Wg_sb = [P.sb("Wg_sb%d" % i, [128, KD, DFE], BF16) for i in range(2)]
    Wu_sb = [P.sb("Wu_sb%d" % i, [128, KD, DFE], BF16) for i in range(2)]
    Wd_sb = [P.sb("Wd_sb%d" % i, [128, NFC, D], BF16) for i in range(2)]
    h2T_sb = [P.sb("h2T_sb%d" % i, [128, KD, 512], BF16) for i in range(2)]
    sg = [P.sb("sg%d" % i, [128, 512], BF16) for i in range(2)]
    aT = P.sb("aT", [128, NFC, 512], BF16)
    ost = [P.sb("ost%d" % i, [128, D], F32) for i in range(2)]
    return locals()


def build(L, n_layers=DEPTH, debug=False):
    NT = L // 128
    NB = L // 512
    nc = bass.Bass("TRN2", target_bir_lowering=False)
    P = Prog(nc)
    A = {}

    def din(name, shape, dt=F32):
        if name in PADDED:
            rows = int(np.prod(shape[:-1]))
            t = P.dram(name, [rows + 1, shape[-1]], dt, kind="ExternalInput")
            names = "abcdefg"[:len(shape) - 1]
            pat = "(%s) z -> %s z" % (" ".join(names), " ".join(names))
            A[name] = Tl(t.t[0:rows, :].rearrange(pat, **{n_: int(v_) for n_, v_ in zip(names, shape[:-1])}), name)
            return A[name]
        A[name] = P.dram(name, shape, dt, kind="ExternalInput")
        return A[name]

    x_in = din("x", [L, D])
    c_in = din("c_col", [128, KD])
    pos_in = din("pos", [128, NT], I32)
    inv_in = din("inv_bc", [128, 16])
    for name, shp in LAYER_PARAMS:
        din(name, [DEPTH] + shp)
    din("ffn_wg", [2, D, 2 * DFE])
    din("ffn_wu", [2, D, 2 * DFE])
    din("ffn_wd", [2, 2 * DFE, D])
    din("moe_wr", [2, D, 8])
    din("moe_br", [2, 1, 8])
    din("moe_wg", [2, 8, D, DFE])
    din("moe_wu", [2, 8, D, DFE])
    din("moe_wd", [2, 8, DFE, D])

    y = P.dram("y", [L, D], F32, kind="ExternalOutput")
    skind = "ExternalOutput" if debug else "Internal"
    QTm = P.dram("QTm", [NH, 96, L], BF16, kind=skind)
    KTm = P.dram("KTm", [NH, 96, L], BF16, kind=skind)
    Vm = P.dram("Vm", [L, NH * 65], BF16, kind=skind)
    QTf = P.dram("QTf", [NH, 68, L], BF16, kind=skind)
    KTf = P.dram("KTf", [NH, 68, L], BF16, kind=skind)
    Vf = P.dram("Vf", [L, NH * 65], BF16, kind=skind)
    mssm = P.dram("mssm", [2, 128, L], BF16, kind=skind)
    h2T_d = P.dram("h2T", [KD, 128, L], BF16, kind=skind)
    g12 = P.dram("g12", [DEPTH, 2, 128, D], F32, kind=skind)
    ytile = [Tl(y.t, "y%d" % t) for t in range(NT)]

    ident = P.sb("ident", [128, 128], BF16)
    identf = P.sb("identf", [128, 128], F32)
    ones_f = P.sb("ones_f", [128, 512], F32)
    ones_b = P.sb("ones_b", [128, 128], BF16)
    for t_, dt_ in ((ident, BF16), (identf, F32)):
        P.op("pool", lambda e, t_=t_: e.memset(t_[:], 1.0), writes=[t_])
        P.op("pool", lambda e, t_=t_: e.affine_select(out=t_[:], in_=t_[:], pattern=[[1, 128]], compare_op=ALU.is_equal,
                                                    fill=0.0, base=0, channel_multiplier=-1), reads=[t_], writes=[t_])
    P.op("pool", lambda e: e.memset(ones_f[:], 1.0), writes=[ones_f])
    P.op("pool", lambda e: e.memset(ones_b[:], 1.0), writes=[ones_b])

    TB = [P.ps("TB%d" % i, [128, 1024], BF16) for i in range(2)]
    FB = [P.ps("FB%d" % i, [128, 512], F32) for i in range(6)]

    def rstd_chain(ss_ap, n, nfeat, tmp, out, reads, eng_r="dve"):
        (tt, tv), (ot, ov) = tmp, out
        P.op("act", lambda e: e.activation(out=tv, in_=ss_ap, func=AF.Sqrt, scale=1.0 / nfeat, bias=eps_c[:, 0:1]),
             reads=list(reads) + [eps_c], writes=[tt])
        P.op("dve", lambda e: e.reciprocal(out=ov, in_=tv), reads=[tt], writes=[ot])

    eps_c = P.sb("eps_c", [128, 1], F32)
    P.op("pool", lambda e: e.memset(eps_c[:], EPS), writes=[eps_c])

    G_bc = P.sb("G_bc", [128, D], F32)
    xs = [P.sb("xs%d" % i, [128, D], F32) for i in range(2)]
    sq_junk = P.sb("sq_junk", [128, D], F32)
    xh = [P.sb("xh%d" % i, [128, D], BF16) for i in range(2)]
    stat = [P.sb("stat%d" % i, [128, 16], F32) for i in range(4)]
    comb_all = P.sb("comb_all", [128, NT, 8], F32)

    modc = P.sb("modc", [128, DEPTH, 4, KD], F32)
    nmc = P.sb("nmc", [128, DEPTH, 2, KD], F32)
    cosT = P.sb("cosT", [128, NT, 16], F32)
    sinT = P.sb("sinT", [128, NT, 16], F32)
    P.push()
    c_col = P.sb("c_colsb", [128, KD], F32)
    P.dma("sp", c_col[:], c_in[:], writes=[c_col])
    c_e = P.sb("c_e", [128, KD], F32)
    c_act = P.sb("c_act", [128, KD], F32)
    P.op("act", lambda e: e.activation(out=c_e[:], in_=c_col[:], func=AF.Exp, scale=-1.0), reads=[c_col], writes=[c_e])
    P.op("dve", lambda e: e.tensor_scalar(out=c_e[:], in0=c_e[:], scalar1=1.0, scalar2=None, op0=ALU.add), reads=[c_e], writes=[c_e])
    P.op("dve", lambda e: e.reciprocal(out=c_e[:], in_=c_e[:]), reads=[c_e], writes=[c_e])
    P.op("dve", lambda e: e.tensor_tensor(out=c_act[:], in0=c_col[:], in1=c_e[:], op=ALU.mult), reads=[c_col, c_e], writes=[c_act])
    C_bc = P.sb("C_bc", [128, KD, 128], F32)
    for k in range(KD):
        P.op("dve", lambda e, k=k: e.tensor_scalar(out=C_bc[:, k, :], in0=ones_f[:, 0:128], scalar1=c_act[:, k:k + 1], scalar2=None,
                                                   op0=ALU.mult), reads=[ones_f, c_act], writes=[C_bc])
    for l in range(n_layers):
        P.dma("sp", nmc[:, l, 0, :], A["norm_mix_c"][l], writes=[nmc])
        P.dma("sp", nmc[:, l, 1, :], A["norm_ffn_c"][l], writes=[nmc])
    wst = [P.sb("wst%d" % i, [128, 2048], F32) for i in range(3)]
    wst_i = [0]

    def next_wst():
        t = wst[wst_i[0] % 3]
        wst_i[0] += 1
        return t
    ada_sb = P.sb("ada_sb", [128, 2048], F32)
    brow = P.sb("brow", [128, 2048], F32)
    for l in range(n_layers):
        for ng in range(3):
            P.dma("sp", brow[:], A["b_ada"][l][:, ng * 2048:(ng + 1) * 2048].to_broadcast([128, 2048]), writes=[brow])
            for k in range(KD):
                st = next_wst()
                P.dma("sp", st[:], A["w_ada"][l, k * 128:(k + 1) * 128, ng * 2048:(ng + 1) * 2048], writes=[st])
                for j in range(4):
                    P.op("pe", lambda e, st=st, j=j, k=k: e.matmul(FB[j][:], lhsT=C_bc[:, k, :], rhs=st[:, j * 512:(j + 1) * 512],
                                                                 start=(k == 0), stop=(k == KD - 1)), reads=[st, C_bc], writes=[FB[j]])
            for j in range(4):
                P.op("dve", lambda e, j=j: e.tensor_tensor(out=ada_sb[:, j * 512:(j + 1) * 512], in0=FB[j][:], in1=brow[:, j * 512:(j + 1) * 512], op=ALU.add),
                     reads=[FB[j], brow], writes=[ada_sb])
            for half in range(2):
                seg = 2 * ng + half
                src = ada_sb[:, half * 1024:(half + 1) * 1024]
                if seg in (2, 5):
                    P.dma("sp", g12[l, 0 if seg == 2 else 1], src, reads=[ada_sb], writes=[g12])
                else:
                    v = {0: 1, 1: 0, 3: 3, 4: 2}[seg]
                    for k in range(KD):
                        P.op("pe", lambda e, half=half, k=k: e.transpose(FB[4][:, 0:128], ada_sb[:, half * 1024 + k * 128: half * 1024 + (k + 1) * 128], identf[:]),
                             reads=[ada_sb, identf], writes=[FB[4]])
                        if seg in (1, 4):
                            nm = 0 if seg == 1 else 1
                            P.op("dve", lambda e, l=l, v=v, k=k, nm=nm: e.scalar_tensor_tensor(
                                out=modc[:, l, v, k:k + 1], in0=FB[4][:, 0:1], scalar=1.0, in1=nmc[:, l, nm, k:k + 1],
                                op0=ALU.add, op1=ALU.mult), reads=[FB[4], nmc], writes=[modc])
                        else:
                            P.op("dve", lambda e, l=l, v=v, k=k: e.tensor_copy(out=modc[:, l, v, k:k + 1], in_=FB[4][:, 0:1]),
                                 reads=[FB[4]], writes=[modc])

    posi = P.sb("posi", [128, NT], I32)
    posf = P.sb("posf", [128, NT], F32)
    inv_bc = P.sb("inv_bcs", [128, 16], F32)
    ang = P.sb("ang", [128, NT, 16], F32)
    kk = P.sb("kk", [128, NT, 16], F32)
    P.dma("sp", posi[:], pos_in[:], writes=[posi])
    P.dma("sp", inv_bc[:], inv_in[:], writes=[inv_bc])
    P.op("dve", lambda e: e.tensor_copy(out=posf[:], in_=posi[:]), reads=[posi], writes=[posf])
    for t in range(NT):
        P.op("dve", lambda e, t=t: e.tensor_scalar(out=ang[:, t, :], in0=inv_bc[:], scalar1=posf[:, t:t + 1], scalar2=None, op0=ALU.mult),
             reads=[inv_bc, posf], writes=[ang])

    MAGIC = 12582912.0
    C1 = 6.28125
    C2 = 2.0 * math.pi - C1

    def sin_reduced(dst, src_t, src_v, shape_v, shift, tmp_t):
        tv = tmp_t[:] if shape_v is None else shape_v(tmp_t)
        P.op("dve", lambda e: e.tensor_scalar(out=tv, in0=src_v, scalar1=shift, scalar2=1.0 / (2 * math.pi), op0=ALU.add, op1=ALU.mult),
             reads=[src_t], writes=[tmp_t])
        P.op("dve", lambda e: e.tensor_scalar(out=tv, in0=tv, scalar1=MAGIC, scalar2=None, op0=ALU.add), reads=[tmp_t], writes=[tmp_t])
        P.op("dve", lambda e: e.tensor_scalar(out=tv, in0=tv, scalar1=-MAGIC, scalar2=None, op0=ALU.add), reads=[tmp_t], writes=[tmp_t])
        P.op("dve", lambda e: e.scalar_tensor_tensor(out=dst[0][:] if shape_v is None else shape_v(dst[0]), in0=tv, scalar=-C1, in1=src_v,
                                                     op0=ALU.mult, op1=ALU.add), reads=[tmp_t, src_t], writes=[dst[0]])
        dv = dst[0][:] if shape_v is None else shape_v(dst[0])
        P.op("dve", lambda e: e.scalar_tensor_tensor(out=dv, in0=tv, scalar=-C2, in1=dv, op0=ALU.mult, op1=ALU.add),
             reads=[tmp_t, dst[0]], writes=[dst[0]])
        P.op("dve", lambda e: e.tensor_scalar(out=dv, in0=dv, scalar1=shift, scalar2=3.14159, op0=ALU.add, op1=ALU.min), reads=[dst[0]], writes=[dst[0]])
        P.op("dve", lambda e: e.tensor_scalar(out=dv, in0=dv, scalar1=-3.14159, scalar2=None, op0=ALU.max), reads=[dst[0]], writes=[dst[0]])
        P.op("act", lambda e: e.activation(out=dv, in_=dv, func=AF.Sin), reads=[dst[0]], writes=[dst[0]])

    sin_reduced((sinT,), ang, ang[:], None, 0.0, kk)
    sin_reduced((cosT,), ang, ang[:], None, math.pi / 2, kk)
    onesrow = P.sb("onesrow", [NH, L], BF16)
    P.op("pool", lambda e: e.memset(onesrow[:], 1.0), writes=[onesrow])
    for r_ in (66, 67):
        P.dma("sp", QTf[:, r_, :], onesrow[:], reads=[onesrow], writes=[QTf])
    for r_ in (64, 65):
        P.dma("sp", KTf[:, r_, :], onesrow[:], reads=[onesrow], writes=[KTf])
    P.pop()

    def load_layer(l):
        lp = lambda n: A[n][l]
        P.dma("pool", w_in_sb[:], lp("w_in").rearrange("(k p) n -> p k n", p=128), writes=[w_in_sb])
        P.dma("pool", w_uq_sb[:], lp("w_uq").rearrange("(k p) n -> p k n", p=128), writes=[w_uq_sb])
        P.dma("pool", w_ukv_sb[:], lp("w_ukv"), writes=[w_ukv_sb])
        P.dma("pool", w_glu_sb[:], lp("w_glu").rearrange("(k p) n -> p k n", p=128), writes=[w_glu_sb])
        for t_, n_ in ((qn_c, "q_norm"), (kvn_c, "kv_norm"), (gqm, "gq_m"), (gkm, "gk_m"), (gqf, "gq_f"), (gkf, "gk_f"),
                       (dcol, "ssm_d"), (bglu_c, "b_glu")):
            P.dma("sp", t_[:], lp(n_), writes=[t_])
        P.dma("sp", nbf[:], lp("fox_bf"), writes=[nbf])
        P.op("dve", lambda e: e.tensor_scalar(out=nbf[:], in0=nbf[:], scalar1=-1.0, scalar2=None, op0=ALU.mult), reads=[nbf], writes=[nbf])
        P.op("dve", lambda e: e.tensor_scalar(out=nbglu_c[:], in0=bglu_c[:], scalar1=-1.0, scalar2=None, op0=ALU.mult), reads=[bglu_c], writes=[nbglu_c])
        for m in range(2):
            P.op("dve", lambda e, m=m: e.tensor_scalar(out=Ddiag[:, m, :], in0=identf[:], scalar1=dcol[:, m:m + 1], scalar2=None, op0=ALU.mult),
                 reads=[identf, dcol], writes=[Ddiag])
        V = lambda i: s5p[:, i, :]
        LRE, LIM, LDT, DT, ZRE, ZIM, MAG, SN, CS, LBR, LBI, DEN, KR, KI, NKI, T1, T2, CK, SK, CK2, SK2 = range(21)
        P.dma("sp", V(LRE), lp("lam_re"), writes=[s5p])
        P.dma("sp", V(LIM), lp("lam_im"), writes=[s5p])
        P.dma("sp", V(LDT), lp("log_dt"), writes=[s5p])
        sop = lambda fn: P.op("dve", fn, reads=[s5p], writes=[s5p])
        P.op("act", lambda e: e.activation(out=V(DT), in_=V(LDT), func=AF.Exp), reads=[s5p], writes=[s5p])
        sop(lambda e: e.tensor_tensor(out=V(ZRE), in0=V(LRE), in1=V(DT), op=ALU.mult))
        sop(lambda e: e.tensor_tensor(out=V(ZIM), in0=V(LIM), in1=V(DT), op=ALU.mult))
        P.op("act", lambda e: e.activation(out=V(MAG), in_=V(ZRE), func=AF.Exp), reads=[s5p], writes=[s5p])
        sv = lambda i: (lambda t: t[:, i, :])
        sin_reduced((s5p,), s5p, V(ZIM), sv(SN), 0.0, s5p) if False else None
        for dst_i, shift in ((SN, 0.0), (CS, math.pi / 2)):
            sop(lambda e, shift=shift: e.tensor_scalar(out=V(T1), in0=V(ZIM), scalar1=shift, scalar2=1.0 / (2 * math.pi), op0=ALU.add, op1=ALU.mult))
            sop(lambda e: e.tensor_scalar(out=V(T1), in0=V(T1), scalar1=MAGIC, scalar2=None, op0=ALU.add))
            sop(lambda e: e.tensor_scalar(out=V(T1), in0=V(T1), scalar1=-MAGIC, scalar2=None, op0=ALU.add))
            sop(lambda e, dst_i=dst_i: e.scalar_tensor_tensor(out=V(dst_i), in0=V(T1), scalar=-C1, in1=V(ZIM), op0=ALU.mult, op1=ALU.add))
            sop(lambda e, dst_i=dst_i: e.scalar_tensor_tensor(out=V(dst_i), in0=V(T1), scalar=-C2, in1=V(dst_i), op0=ALU.mult, op1=ALU.add))
            sop(lambda e, dst_i=dst_i, shift=shift: e.tensor_scalar(out=V(dst_i), in0=V(dst_i), scalar1=shift, scalar2=3.14159, op0=ALU.add, op1=ALU.min))
            sop(lambda e, dst_i=dst_i: e.tensor_scalar(out=V(dst_i), in0=V(dst_i), scalar1=-3.14159, scalar2=None, op0=ALU.max))
            P.op("act", lambda e, dst_i=dst_i: e.activation(out=V(dst_i), in_=V(dst_i), func=AF.Sin), reads=[s5p], writes=[s5p])
        sop(lambda e: e.tensor_tensor(out=V(LBR), in0=V(MAG), in1=V(CS), op=ALU.mult))
        sop(lambda e: e.tensor_tensor(out=V(LBI), in0=V(MAG), in1=V(SN), op=ALU.mult))
        sop(lambda e: e.tensor_tensor(out=V(DEN), in0=V(LRE), in1=V(LRE), op=ALU.mult))
        sop(lambda e: e.tensor_tensor(out=V(T1), in0=V(LIM), in1=V(LIM), op=ALU.mult))
        sop(lambda e: e.tensor_tensor(out=V(DEN), in0=V(DEN), in1=V(T1), op=ALU.add))
        sop(lambda e: e.reciprocal(out=V(DEN), in_=V(DEN)))
        sop(lambda e: e.tensor_scalar(out=V(T2), in0=V(LBR), scalar1=-1.0, scalar2=None, op0=ALU.add))
        sop(lambda e: e.tensor_tensor(out=V(KR), in0=V(T2), in1=V(LRE), op=ALU.mult))
        sop(lambda e: e.tensor_tensor(out=V(T1), in0=V(LBI), in1=V(LIM), op=ALU.mult))
        sop(lambda e: e.tensor_tensor(out=V(KR), in0=V(KR), in1=V(T1), op=ALU.add))
        sop(lambda e: e.tensor_tensor(out=V(KR), in0=V(KR), in1=V(DEN), op=ALU.mult))
        sop(lambda e: e.tensor_tensor(out=V(KI), in0=V(LBI), in1=V(LRE), op=ALU.mult))
        sop(lambda e: e.tensor_tensor(out=V(T1), in0=V(T2), in1=V(LIM), op=ALU.mult))
        sop(lambda e: e.tensor_tensor(out=V(KI), in0=V(KI), in1=V(T1), op=ALU.subtract))
        sop(lambda e: e.tensor_tensor(out=V(KI), in0=V(KI), in1=V(DEN), op=ALU.mult))
        sop(lambda e: e.tensor_scalar(out=V(NKI), in0=V(KI), scalar1=-1.0, scalar2=None, op0=ALU.mult))
        for j in range(8):
            P.dma("sp", blk_f[:, 0, :], lp("b_re")[j], writes=[blk_f])
            P.dma("sp", blk_f[:, 1, :], lp("b_im")[j], writes=[blk_f])
            P.op("dve", lambda e, j=j: e.tensor_scalar(out=blk_o[:, 0, :], in0=blk_f[:, 0, :], scalar1=s5p[:, KR, j:j + 1], scalar2=None, op0=ALU.mult),
                 reads=[blk_f, s5p], writes=[blk_o])
            P.op("dve", lambda e, j=j: e.scalar_tensor_tensor(out=blk_o[:, 0, :], in0=blk_f[:, 1, :], scalar=s5p[:, NKI, j:j + 1], in1=blk_o[:, 0, :],
                                                              op0=ALU.mult, op1=ALU.add), reads=[blk_f, s5p, blk_o], writes=[blk_o])
            P.op("dve", lambda e, j=j: e.tensor_scalar(out=blk_o[:, 1, :], in0=blk_f[:, 1, :], scalar1=s5p[:, KR, j:j + 1], scalar2=None, op0=ALU.mult),
                 reads=[blk_f, s5p], writes=[blk_o])
            P.op("dve", lambda e, j=j: e.scalar_tensor_tensor(out=blk_o[:, 1, :], in0=blk_f[:, 0, :], scalar=s5p[:, KI, j:j + 1], in1=blk_o[:, 1, :],
                                                              op0=ALU.mult, op1=ALU.add), reads=[blk_f, s5p, blk_o], writes=[blk_o])
            for ri, dstT in ((0, BreT), (1, BimT)):
                P.op("pe", lambda e, ri=ri: e.transpose(FB[5][:, 0:128], blk_o[:, ri, :], identf[:]), reads=[blk_o, identf], writes=[FB[5]])
                P.op("act", lambda e, dstT=dstT, j=j: e.activation(out=dstT[:, j, :], in_=FB[5][:, 0:128], func=AF.Copy), reads=[FB[5]], writes=[dstT])
        P.dma("pool", CreT[:], lp("c_re").rearrange("j p f -> p j f"), writes=[CreT])
        P.dma("pool", nCimT[:], lp("c_im").rearrange("j p f -> p j f"), writes=[nCimT])
        P.op("dve", lambda e: e.tensor_scalar(out=nCimT[:], in0=nCimT[:], scalar1=-1.0, scalar2=None, op0=ALU.mult), reads=[nCimT], writes=[nCimT])
        P.op("dve", lambda e: e.memset(Ctab[:, :, 0:1], 1.0), writes=[Ctab])
        P.op("dve", lambda e: e.memset(Stab[:, :, 0:1], 0.0), writes=[Stab])
        sop(lambda e: e.tensor_copy(out=V(CK), in_=V(CS)))
        sop(lambda e: e.tensor_copy(out=V(SK), in_=V(SN)))
        n = 1
        while n < SUB:
            tabt_v = pt[0][:].rearrange("p a b -> p (a b)")[:, 0:8 * n].rearrange("p (j n) -> p j n", j=8)
            tabu_v = pt[1][:].rearrange("p a b -> p (a b)")[:, 0:8 * n].rearrange("p (j n) -> p j n", j=8)
            tabt = pt[0]
            tabu = pt[1]
            ckb = s5p[:, CK, :].unsqueeze(2).to_broadcast([128, 8, n])
            skb = s5p[:, SK, :].unsqueeze(2).to_broadcast([128, 8, n])
            P.op("dve", lambda e, n=n, ckb=ckb: e.tensor_tensor(out=tabt_v, in0=Ctab[:, :, 0:n], in1=ckb, op=ALU.mult), reads=[Ctab, s5p], writes=[tabt])
            P.op("dve", lambda e, n=n, skb=skb: e.tensor_tensor(out=tabu_v, in0=Stab[:, :, 0:n], in1=skb, op=ALU.mult), reads=[Stab, s5p], writes=[tabu])
            P.op("dve", lambda e, n=n: e.tensor_tensor(out=Ctab[:, :, n:2 * n], in0=tabt_v, in1=tabu_v, op=ALU.subtract), reads=[tabt, tabu], writes=[Ctab])
            P.op("dve", lambda e, n=n, skb=skb: e.tensor_tensor(out=tabt_v, in0=Ctab[:, :, 0:n], in1=skb, op=ALU.mult), reads=[Ctab, s5p], writes=[tabt])
            P.op("dve", lambda e, n=n, ckb=ckb: e.tensor_tensor(out=tabu_v, in0=Stab[:, :, 0:n], in1=ckb, op=ALU.mult), reads=[Stab, s5p], writes=[tabu])
            P.op("dve", lambda e, n=n: e.tensor_tensor(out=Stab[:, :, n:2 * n], in0=tabt_v, in1=tabu_v, op=ALU.add), reads=[tabt, tabu], writes=[Stab])
            sop(lambda e: e.tensor_tensor(out=V(T1), in0=V(CK), in1=V(CK), op=ALU.mult))
            sop(lambda e: e.tensor_tensor(out=V(T2), in0=V(SK), in1=V(SK), op=ALU.mult))
            sop(lambda e: e.tensor_tensor(out=V(SK2), in0=V(CK), in1=V(SK), op=ALU.mult))
            sop(lambda e: e.tensor_tensor(out=V(CK), in0=V(T1), in1=V(T2), op=ALU.subtract))
            sop(lambda e: e.tensor_scalar(out=V(SK), in0=V(SK2), scalar1=2.0, scalar2=None, op0=ALU.mult))
            n *= 2
        P.op("dve", lambda e: e.tensor_copy(out=Rtab[:], in_=s5p[:, MAG, :].unsqueeze(2).to_broadcast([128, 8, SUB])), reads=[s5p], writes=[Rtab])
        return dict(CK=CK, SK=SK)

    def load_c(l):
        P.dma("sp", onorm_c[:], A["out_norm"][l], writes=[onorm_c])
        P.dma("sp", G_bc[:], g12[l, 0], reads=[g12], writes=[G_bc])
        for k in range(KD):
            st = wstc[k % 2]
            P.dma("sp", st[:], A["w_out"][l][k * 128:(k + 1) * 128, :], writes=[st])
            P.op("dve", lambda e, st=st, k=k: e.scalar_tensor_tensor(out=w_out_sb[:, k, :], in0=st[:], scalar=onorm_c[:, k:k + 1], in1=G_bc[:],
                                                                     op0=ALU.mult, op1=ALU.mult), reads=[st, onorm_c, G_bc], writes=[w_out_sb])

    def norm_transpose(src_t, src_v, l, v_a, v_b, dst_t, dst_slice, si, fp32_path=None):
        st_ = stat[si % 4]
        P.op("act", lambda e: e.activation(out=sq_junk[:], in_=src_v, func=AF.Square, accum_out=st_[:, 0:1]), reads=[src_t], writes=[sq_junk, st_])
        P.op("act", lambda e: e.activation(out=st_[:, 1:2], in_=st_[:, 0:1], func=AF.Sqrt, scale=1.0 / D, bias=eps_c[:, 0:1]), reads=[st_, eps_c], writes=[st_])
        P.op("dve", lambda e: e.reciprocal(out=st_[:, 2:3], in_=st_[:, 1:2]), reads=[st_], writes=[st_])
        xh_ = xh[si % 2]
        P.op("dve", lambda e: e.tensor_scalar(out=xh_[:], in0=src_v, scalar1=st_[:, 2:3], scalar2=None, op0=ALU.mult), reads=[src_t, st_], writes=[xh_])
        tb = TB[si % 2]
        for k in range(KD):
            P.op("pe", lambda e, k=k: e.transpose(tb[:, k * 128:(k + 1) * 128], xh_[:, k * 128:(k + 1) * 128], ident[:]), reads=[xh_, ident], writes=[tb])
        for k in range(KD):
            P.op("act", lambda e, k=k: e.activation(out=dst_t[:, k, dst_slice], in_=tb[:, k * 128:(k + 1) * 128], func=AF.Identity,
                                                    scale=modc[:, l, v_a, k:k + 1], bias=modc[:, l, v_b, k:k + 1]), reads=[tb, modc], writes=[dst_t])
        if fp32_path is not None:
            xhf_, dstf = fp32_path
            P.op("dve", lambda e: e.tensor_scalar(out=xhf_[:], in0=src_v, scalar1=st_[:, 2:3], scalar2=None, op0=ALU.mult), reads=[src_t, st_], writes=[xhf_])
            for k in range(KD):
                fb = FB[k % 2]
                P.op("pe", lambda e, k=k, fb=fb: e.transpose(fb[:, 0:128], xhf_[:, k * 128:(k + 1) * 128], identf[:]), reads=[xhf_, identf], writes=[fb])
                P.op("act", lambda e, k=k, fb=fb: e.activation(out=dstf[:, k, :], in_=fb[:, 0:128], func=AF.Identity,
                                                             scale=modc[:, l, v_a, k:k + 1], bias=modc[:, l, v_b, k:k + 1]), reads=[fb, modc], writes=[dstf])

    def head_rms(src_t, src_v3, nh, hd, gain_t, dst_t, dst_v3, si, extra_ss=None):
        st_ = stat[si % 4]
        P.op("act", lambda e: e.activation(out=sq_junk[:, 0:nh * hd].rearrange("p (h d) -> p h d", h=nh), in_=src_v3, func=AF.Square), reads=[src_t], writes=[sq_junk])
        P.op("dve", lambda e: e.tensor_reduce(out=st_[:, 0:nh], in_=sq_junk[:, 0:nh * hd].rearrange("p (h d) -> p h d", h=nh), axis=AX.X, op=ALU.add),
             reads=[sq_junk], writes=[st_])
        tot = hd
        if extra_ss is not None:
            et, ev, en = extra_ss
            P.op("dve", lambda e: e.tensor_scalar(out=st_[:, 0:nh], in0=st_[:, 0:nh], scalar1=ev, scalar2=None, op0=ALU.add), reads=[st_, et], writes=[st_])
            tot = hd + en
        P.op("act", lambda e: e.activation(out=st_[:, 6:6 + nh], in_=st_[:, 0:nh], func=AF.Sqrt, scale=1.0 / tot, bias=eps_c[:, 0:1]), reads=[st_, eps_c], writes=[st_])
        P.op("dve", lambda e: e.reciprocal(out=st_[:, 6:6 + nh], in_=st_[:, 6:6 + nh]), reads=[st_], writes=[st_])
        return st_

    def rope(src_t, dst_t, cos_v, sin_v):
        x1 = src_t[:, :, 64:80]
        x2 = src_t[:, :, 80:96]
        cb = cos_v.unsqueeze(1).to_broadcast([128, NH, 16])
        sb_ = sin_v.unsqueeze(1).to_broadcast([128, NH, 16])
        P.op("dve", lambda e: e.tensor_copy(out=dst_t[:, :, 0:64], in_=src_t[:, :, 0:64]), reads=[src_t], writes=[dst_t])
        P.op("dve", lambda e: e.tensor_tensor(out=rt[0][:], in0=x1, in1=cb, op=ALU.mult), reads=[src_t, cosT], writes=[rt[0]])
        P.op("dve", lambda e: e.tensor_tensor(out=rt[1][:], in0=x2, in1=sb_, op=ALU.mult), reads=[src_t, sinT], writes=[rt[1]])
        P.op("dve", lambda e: e.tensor_tensor(out=dst_t[:, :, 64:80], in0=rt[0][:], in1=rt[1][:], op=ALU.subtract), reads=[rt[0], rt[1]], writes=[dst_t])
        P.op("dve", lambda e: e.tensor_tensor(out=rt[2][:], in0=x1, in1=sb_, op=ALU.mult), reads=[src_t, sinT], writes=[rt[2]])
        P.op("dve", lambda e: e.tensor_tensor(out=rt[3][:], in0=x2, in1=cb, op=ALU.mult), reads=[src_t, cosT], writes=[rt[3]])
        P.op("dve", lambda e: e.tensor_tensor(out=dst_t[:, :, 80:96], in0=rt[2][:], in1=rt[3][:], op=ALU.add), reads=[rt[2], rt[3]], writes=[dst_t])

    def phase_a(l, s5c):
        src = x_in if l == 0 else None
        for b in range(NB):
            hTb = hT[b % 2]
            for ti in range(4):
                t = b * 4 + ti
                xs_ = xs[t % 2]
                if l == 0:
                    P.dma("sp", xs_[:], x_in[t * 128:(t + 1) * 128, :], writes=[xs_])
                else:
                    P.dma("sp", xs_[:], y[t * 128:(t + 1) * 128, :], reads=[ytile[t]], writes=[xs_])
                norm_transpose(xs_, xs_[:], l, 0, 1, hTb, slice(ti * 128, (ti + 1) * 128), t)
            for m in range(2):
                for k in range(KD):
                    P.op("pe", lambda e, m=m, k=k: e.matmul(FB[m][:], lhsT=w_in_sb[:, k, m * 128:(m + 1) * 128], rhs=hTb[:, k, :], start=(k == 0), stop=(k == KD - 1)),
                         reads=[w_in_sb, hTb], writes=[FB[m]])
                P.op("act", lambda e, m=m: e.activation(out=uT[:, m, :], in_=FB[m][:], func=AF.Copy), reads=[FB[m]], writes=[uT])
            for k in range(KD):
                P.op("pe", lambda e, k=k: e.matmul(FB[2][0:NH, :], lhsT=w_in_sb[:, k, 1824:1830], rhs=hTb[:, k, :], start=(k == 0), stop=(k == KD - 1)),
                     reads=[w_in_sb, hTb], writes=[FB[2]])
            P.op("act", lambda e: e.activation(out=fg_e[:], in_=FB[2][0:NH, :], func=AF.Exp, scale=-1.0, bias=nbf[:, 0:1]), reads=[FB[2], nbf], writes=[fg_e])
            P.op("act", lambda e: e.activation(out=fg_sp[:], in_=fg_e[:], func=AF.Ln, bias=ones_f[0:NH, 0:1]), reads=[fg_e, ones_f], writes=[fg_sp])
            if b == 0:
                P.op("dve", lambda e: e.tensor_tensor_scan(out=fg_cum[:], data0=ones_f[0:NH, 0:512], data1=fg_sp[:], initial=0.0, op0=ALU.mult, op1=ALU.add),
                     reads=[ones_f, fg_sp], writes=[fg_cum])
            else:
                P.op("dve", lambda e: e.tensor_tensor_scan(out=fg_cum[:], data0=ones_f[0:NH, 0:512], data1=fg_sp[:], initial=fg_carry[:, 0:1], op0=ALU.mult, op1=ALU.add),
                     reads=[ones_f, fg_sp, fg_carry], writes=[fg_cum])
            P.op("dve", lambda e: e.tensor_copy(out=fg_carry[:], in_=fg_cum[:, 511:512]), reads=[fg_cum], writes=[fg_carry])
            P.op("dve", lambda e: e.tensor_scalar(out=fg_sp[:], in0=fg_cum[:], scalar1=8.0, scalar2=None, op0=ALU.mult), reads=[fg_cum], writes=[fg_sp])
            P.op("dve", lambda e: e.tensor_copy(out=fg_hi[:], in_=fg_sp[:]), reads=[fg_sp], writes=[fg_hi])
            P.op("dve", lambda e: e.tensor_tensor(out=fg_lo[:], in0=fg_sp[:], in1=fg_hi[:], op=ALU.subtract), reads=[fg_sp, fg_hi], writes=[fg_lo])
            P.op("dve", lambda e: e.tensor_scalar(out=fg_nhi[:], in0=fg_hi[:], scalar1=-1.0, scalar2=None, op0=ALU.mult), reads=[fg_hi], writes=[fg_nhi])
            P.op("dve", lambda e: e.tensor_scalar(out=fg_nlo[:], in0=fg_lo[:], scalar1=-1.0, scalar2=None, op0=ALU.mult), reads=[fg_lo], writes=[fg_nlo])
            bs = slice(b * 512, (b + 1) * 512)
            P.dma("sp", QTf[:, 64, bs], fg_nhi[:], reads=[fg_nhi], writes=[QTf])
            P.dma("sp", QTf[:, 65, bs], fg_nlo[:], reads=[fg_nlo], writes=[QTf])
            P.dma("sp", KTf[:, 66, bs], fg_hi[:], reads=[fg_hi], writes=[KTf])
            P.dma("sp", KTf[:, 67, bs], fg_lo[:], reads=[fg_lo], writes=[KTf])

            for ti in range(4):
                t = b * 4 + ti
                tsl = slice(ti * 128, (ti + 1) * 128)
                segs = ((FB[2], 256, 416), (FB[3], 672, 384), (FB[4], 1056, 384), (FB[5], 1440, 384))
                for fb, c0, w in segs:
                    for k in range(KD):
                        P.op("pe", lambda e, fb=fb, c0=c0, w=w, k=k: e.matmul(fb[:, 0:w], lhsT=hTb[:, k, tsl], rhs=w_in_sb[:, k, c0:c0 + w], start=(k == 0), stop=(k == KD - 1)),
                             reads=[hTb, w_in_sb], writes=[fb])
                st_ = stat[0]
                P.op("act", lambda e: e.activation(out=sq_junk[:, 0:256], in_=FB[2][:, 0:256], func=AF.Square, accum_out=st_[:, 12:13]), reads=[FB[2]], writes=[sq_junk, st_])
                P.op("act", lambda e: e.activation(out=st_[:, 13:14], in_=st_[:, 12:13], func=AF.Sqrt, scale=1.0 / 256, bias=eps_c[:, 0:1]), reads=[st_, eps_c], writes=[st_])
                P.op("dve", lambda e: e.reciprocal(out=st_[:, 13:14], in_=st_[:, 13:14]), reads=[st_], writes=[st_])
                P.op("dve", lambda e: e.tensor_scalar(out=cq_h[:], in0=FB[2][:, 0:256], scalar1=st_[:, 13:14], scalar2=None, op0=ALU.mult), reads=[FB[2], st_], writes=[cq_h])
                st1 = stat[1]
                P.op("act", lambda e: e.activation(out=sq_junk[:, 256:384], in_=FB[2][:, 256:384], func=AF.Square, accum_out=st1[:, 12:13]), reads=[FB[2]], writes=[sq_junk, st1])
                P.op("act", lambda e: e.activation(out=st1[:, 13:14], in_=st1[:, 12:13], func=AF.Sqrt, scale=1.0 / 128, bias=eps_c[:, 0:1]), reads=[st1, eps_c], writes=[st1])
                P.op("dve", lambda e: e.reciprocal(out=st1[:, 13:14], in_=st1[:, 13:14]), reads=[st1], writes=[st1])
                P.op("dve", lambda e: e.tensor_scalar(out=ckv_h[:], in0=FB[2][:, 256:384], scalar1=st1[:, 13:14], scalar2=None, op0=ALU.mult), reads=[FB[2], st1], writes=[ckv_h])
                P.op("act", lambda e: e.activation(out=kn[:, 0, 64:96], in_=FB[2][:, 384:416], func=AF.Copy), reads=[FB[2]], writes=[kn])
                P.op("act", lambda e: e.activation(out=sq_junk[:, 384:416], in_=FB[2][:, 384:416], func=AF.Square, accum_out=st1[:, 14:15]), reads=[FB[2]], writes=[sq_junk, st1])
                tb = TB[0]
                for j in range(2):
                    P.op("pe", lambda e, j=j: e.transpose(tb[:, j * 128:(j + 1) * 128], cq_h[:, j * 128:(j + 1) * 128], ident[:]), reads=[cq_h, ident], writes=[tb])
                P.op("pe", lambda e: e.transpose(tb[:, 256:384], ckv_h[:], ident[:]), reads=[ckv_h, ident], writes=[tb])
                for j in range(2):
                    P.op("act", lambda e, j=j: e.activation(out=cqT[:, j, :], in_=tb[:, j * 128:(j + 1) * 128], func=AF.Copy, scale=qn_c[:, j:j + 1]), reads=[tb, qn_c], writes=[cqT])
                P.op("act", lambda e: e.activation(out=ckvT[:], in_=tb[:, 256:384], func=AF.Copy, scale=kvn_c[:, 0:1]), reads=[tb, kvn_c], writes=[ckvT])
                for j in range(2):
                    P.op("pe", lambda e, j=j: e.matmul(FB[0][:], lhsT=cqT[:, j, :], rhs=w_uq_sb[:, j, 0:512], start=(j == 0), stop=(j == 1)), reads=[cqT, w_uq_sb], writes=[FB[0]])
                for j in range(2):
                    P.op("pe", lambda e, j=j: e.matmul(FB[1][:, 0:64], lhsT=cqT[:, j, :], rhs=w_uq_sb[:, j, 512:576], start=(j == 0), stop=(j == 1)), reads=[cqT, w_uq_sb], writes=[FB[1]])
                P.op("act", lambda e: e.activation(out=qn[:].rearrange("p h d -> p (h d)")[:, 0:512], in_=FB[0][:], func=AF.Copy), reads=[FB[0]], writes=[qn])
                P.op("act", lambda e: e.activation(out=qn[:].rearrange("p h d -> p (h d)")[:, 512:576], in_=FB[1][:, 0:64], func=AF.Copy), reads=[FB[1]], writes=[qn])
                sq = head_rms(qn, qn[:], NH, 96, gqm, None, None, 2)
                P.op("dve", lambda e, sq=sq: e.tensor_tensor(out=qn[:], in0=qn[:], in1=sq[:, 6:12].unsqueeze(2).to_broadcast([128, NH, 96]), op=ALU.mult), reads=[qn, sq], writes=[qn])
                P.op("dve", lambda e: e.tensor_tensor(out=qn[:], in0=qn[:], in1=gqm[:].unsqueeze(1).to_broadcast([128, NH, 96]), op=ALU.mult), reads=[qn, gqm], writes=[qn])
                rope(qn, qfin, cosT[:, t, :], sinT[:, t, :])
                P.op("pe", lambda e: e.matmul(FB[0][:], lhsT=ckvT[:], rhs=w_ukv_sb[:, 0:512], start=True, stop=True), reads=[ckvT, w_ukv_sb], writes=[FB[0]])
                P.op("pe", lambda e: e.matmul(FB[1][:, 0:256], lhsT=ckvT[:], rhs=w_ukv_sb[:, 512:768], start=True, stop=True), reads=[ckvT, w_ukv_sb], writes=[FB[1]])
                vst = Vm_st[t % 2]
                kv0 = FB[0][:].rearrange("p (h d) -> p h d", h=4)
                kv1 = FB[1][:, 0:256].rearrange("p (h d) -> p h d", h=2)
                P.op("act", lambda e: e.activation(out=kn[:, 0:4, 0:64], in_=kv0[:, :, 0:64], func=AF.Copy), reads=[FB[0]], writes=[kn])
                P.op("act", lambda e: e.activation(out=kn[:, 4:6, 0:64], in_=kv1[:, :, 0:64], func=AF.Copy), reads=[FB[1]], writes=[kn])
                P.op("dve", lambda e: e.tensor_copy(out=vst[:, 0:4, 0:64], in_=kv0[:, :, 64:128]), reads=[FB[0]], writes=[vst])
                P.op("dve", lambda e: e.tensor_copy(out=vst[:, 4:6, 0:64], in_=kv1[:, :, 64:128]), reads=[FB[1]], writes=[vst])
                P.dma("sp", Vm[t * 128:(t + 1) * 128, :], vst[:].rearrange("p h d -> p (h d)"), reads=[vst], writes=[Vm])
                P.op("dve", lambda e: e.tensor_copy(out=kn[:, 1:6, 64:96], in_=kn[:, 0:1, 64:96].to_broadcast([128, 5, 32])), reads=[kn], writes=[kn])
                st3 = stat[3]
                P.op("act", lambda e: e.activation(out=sq_junk[:, 0:384].rearrange("p (h d) -> p h d", h=NH), in_=kn[:, :, 0:64], func=AF.Square), reads=[kn], writes=[sq_junk])
                P.op("dve", lambda e: e.tensor_reduce(out=st3[:, 0:NH], in_=sq_junk[:, 0:384].rearrange("p (h d) -> p h d", h=NH), axis=AX.X, op=ALU.add), reads=[sq_junk], writes=[st3])
                P.op("dve", lambda e: e.tensor_scalar(out=st3[:, 0:NH], in0=st3[:, 0:NH], scalar1=st1[:, 14:15], scalar2=None, op0=ALU.add), reads=[st3, st1], writes=[st3])
                P.op("act", lambda e: e.activation(out=st3[:, 6:12], in_=st3[:, 0:NH], func=AF.Sqrt, scale=1.0 / 96, bias=eps_c[:, 0:1]), reads=[st3, eps_c], writes=[st3])
                P.op("dve", lambda e: e.reciprocal(out=st3[:, 6:12], in_=st3[:, 6:12]), reads=[st3], writes=[st3])
                P.op("dve", lambda e: e.tensor_tensor(out=kn[:], in0=kn[:], in1=st3[:, 6:12].unsqueeze(2).to_broadcast([128, NH, 96]), op=ALU.mult), reads=[kn, st3], writes=[kn])
                P.op("dve", lambda e: e.tensor_tensor(out=kn[:], in0=kn[:], in1=gkm[:].unsqueeze(1).to_broadcast([128, NH, 96]), op=ALU.mult), reads=[kn, gkm], writes=[kn])
                rope(kn, kfin, cosT[:, t, :], sinT[:, t, :])
                gsl = slice(t * 128, (t + 1) * 128)
                for src_, st_l, dst_d in ((qfin, QTm_st, QTm), (kfin, KTm_st, KTm)):
                    tb2 = TB[1]
                    st_t = st_l[t % 2]
                    for h in range(NH):
                        P.op("pe", lambda e, h=h, src_=src_: e.transpose(tb2[0:96, h * 128:(h + 1) * 128], src_[:, h, :], ident[:]), reads=[src_, ident], writes=[tb2])
                    P.op("act", lambda e, st_t=st_t: e.activation(out=st_t[:], in_=tb2[0:96, 0:768].rearrange("p (h t) -> p h t", h=NH), func=AF.Copy), reads=[tb2], writes=[st_t])
                    P.dma("sp", dst_d[:, :, gsl].rearrange("h p t -> p h t"), st_t[:], reads=[st_t], writes=[dst_d])
                for fb, g_t, dstb, st_l, dst_d in ((FB[3], gqf, fqb, QTf_st, QTf), (FB[4], gkf, fkb, KTf_st, KTf)):
                    st_t = st_l[t % 2]
                    P.op("act", lambda e, fb=fb: e.activation(out=fqn[:].rearrange("p h d -> p (h d)"), in_=fb[:, 0:384], func=AF.Copy), reads=[fb], writes=[fqn])
                    sq = head_rms(fqn, fqn[:], NH, 64, g_t, None, None, 2)
                    P.op("dve", lambda e, sq=sq: e.tensor_tensor(out=fqn[:], in0=fqn[:], in1=sq[:, 6:12].unsqueeze(2).to_broadcast([128, NH, 64]), op=ALU.mult), reads=[fqn, sq], writes=[fqn])
                    P.op("dve", lambda e, g_t=g_t, dstb=dstb: e.tensor_tensor(out=dstb[:], in0=fqn[:], in1=g_t[:].unsqueeze(1).to_broadcast([128, NH, 64]), op=ALU.mult), reads=[fqn, g_t], writes=[dstb])
                    tb2 = TB[1]
                    for h in range(NH):
                        P.op("pe", lambda e, h=h, dstb=dstb: e.transpose(tb2[0:64, h * 128:(h + 1) * 128], dstb[:, h, :], ident[:]), reads=[dstb, ident], writes=[tb2])
                    P.op("act", lambda e, st_t=st_t: e.activation(out=st_t[:], in_=tb2[0:64, 0:768].rearrange("p (h t) -> p h t", h=NH), func=AF.Copy), reads=[tb2], writes=[st_t])
                    P.dma("sp", dst_d[:, 0:64, gsl].rearrange("h p t -> p h t"), st_t[:], reads=[st_t], writes=[dst_d])
                vst = Vf_st[t % 2]
                P.op("dve", lambda e, vst=vst: e.tensor_copy(out=vst[:, :, 0:64], in_=FB[5][:, 0:384].rearrange("p (h d) -> p h d", h=NH)), reads=[FB[5]], writes=[vst])
                P.dma("sp", Vf[t * 128:(t + 1) * 128, :], vst[:].rearrange("p h d -> p (h d)"), reads=[vst], writes=[Vf])
            for sc in range(512 // SUB):
                first = (b == 0 and sc == 0)
                ss_ = slice(sc * SUB, (sc + 1) * SUB)
                gs = slice(b * 512 + sc * SUB, b * 512 + (sc + 1) * SUB)
                if not first:
                    wl_re = wlast[:, 0, :]
                    wl_im = wlast[:, 1, :]
                    ck = s5p[:, s5c["CK"], :]
                    sk = s5p[:, s5c["SK"], :]
                    P.op("dve", lambda e: e.tensor_tensor(out=w0t[:, 0, :], in0=wl_re, in1=ck, op=ALU.mult), reads=[wlast, s5p], writes=[w0t])
                    P.op("dve", lambda e: e.tensor_tensor(out=w0t[:, 1, :], in0=wl_im, in1=sk, op=ALU.mult), reads=[wlast, s5p], writes=[w0t])
                    P.op("dve", lambda e: e.tensor_tensor(out=w0t[:, 2, :], in0=wl_re, in1=sk, op=ALU.mult), reads=[wlast, s5p], writes=[w0t])
                    P.op("dve", lambda e: e.tensor_tensor(out=w0t[:, 3, :], in0=wl_im, in1=ck, op=ALU.mult), reads=[wlast, s5p], writes=[w0t])
                    P.op("dve", lambda e: e.tensor_tensor(out=w0[:, 0, :], in0=w0t[:, 0, :], in1=w0t[:, 1, :], op=ALU.subtract), reads=[w0t], writes=[w0])
                    P.op("dve", lambda e: e.tensor_tensor(out=w0[:, 1, :], in0=w0t[:, 2, :], in1=w0t[:, 3, :], op=ALU.add), reads=[w0t], writes=[w0])
                for m in range(2):
                    hs = slice(4 * m, 4 * m + 4)
                    for jj in range(4):
                        j = 4 * m + jj
                        fbp = FB[j % 2]
                        P.op("pe", lambda e: e.matmul(fbp[:, 0:SUB], lhsT=BreT[:, j, :], rhs=uT[:, m, ss_], start=True, stop=True), reads=[BreT, uT], writes=[fbp])
                        P.op("pe", lambda e: e.matmul(fbp[:, SUB:2 * SUB], lhsT=BimT[:, j, :], rhs=uT[:, m, ss_], start=True, stop=True), reads=[BimT, uT], writes=[fbp])
                        b2 = fbp[:, 0:2 * SUB].rearrange("p (r t) -> p r t", r=2)
                        cb = Ctab[:, j, :].unsqueeze(1).to_broadcast([128, 2, SUB])
                        sb_ = Stab[:, j, :].unsqueeze(1).to_broadcast([128, 2, SUB])
                        P.op("dve", lambda e: e.tensor_tensor(out=pre_c[:], in0=b2, in1=cb, op=ALU.mult), reads=[fbp, Ctab], writes=[pre_c])
                        P.op("dve", lambda e: e.tensor_tensor(out=pre_s[:], in0=b2, in1=sb_, op=ALU.mult), reads=[fbp, Stab], writes=[pre_s])
                        P.op("dve", lambda e: e.tensor_tensor(out=pin_re[:], in0=pre_c[:, 0, :], in1=pre_s[:, 1, :], op=ALU.add), reads=[pre_c, pre_s], writes=[pin_re])
                        P.op("dve", lambda e: e.tensor_tensor(out=pin_im[:], in0=pre_c[:, 1, :], in1=pre_s[:, 0, :], op=ALU.subtract), reads=[pre_c, pre_s], writes=[pin_im])
                        for pin, Wt, ri in ((pin_re, W_re, 0), (pin_im, W_im, 1)):
                            if first:
                                P.op("dve", lambda e: e.tensor_tensor_scan(out=Wt[:, jj, :], data0=Rtab[:, j, :], data1=pin[:], initial=0.0, op0=ALU.mult, op1=ALU.add),
                                     reads=[Rtab, pin], writes=[Wt])
                            else:
                                P.op("dve", lambda e: e.tensor_tensor_scan(out=Wt[:, jj, :], data0=Rtab[:, j, :], data1=pin[:], initial=w0[:, ri, j:j + 1],
                                                                           op0=ALU.mult, op1=ALU.add), reads=[Rtab, pin, w0], writes=[Wt])
                    P.op("dve", lambda e: e.tensor_copy(out=wlast[:, 0, hs], in_=W_re[:, :, SUB - 1]), reads=[W_re], writes=[wlast])
                    P.op("dve", lambda e: e.tensor_copy(out=wlast[:, 1, hs], in_=W_im[:, :, SUB - 1]), reads=[W_im], writes=[wlast])
                    Ch = Ctab[:, hs, :]
                    Sh = Stab[:, hs, :]
                    P.op("dve", lambda e: e.tensor_tensor(out=pt[0][:], in0=W_re[:], in1=Ch, op=ALU.mult), reads=[W_re, Ctab], writes=[pt[0]])
                    P.op("dve", lambda e: e.tensor_tensor(out=pt[1][:], in0=W_im[:], in1=Sh, op=ALU.mult), reads=[W_im, Stab], writes=[pt[1]])
                    P.op("dve", lambda e: e.tensor_tensor(out=s_re[:], in0=pt[0][:], in1=pt[1][:], op=ALU.subtract), reads=[pt[0], pt[1]], writes=[s_re])
                    P.op("dve", lambda e: e.tensor_tensor(out=pt[0][:], in0=W_re[:], in1=Sh, op=ALU.mult), reads=[W_re, Stab], writes=[pt[0]])
                    P.op("dve", lambda e: e.tensor_tensor(out=pt[1][:], in0=W_im[:], in1=Ch, op=ALU.mult), reads=[W_im, Stab], writes=[pt[1]])
                    P.op("dve", lambda e: e.tensor_tensor(out=s_im[:], in0=pt[0][:], in1=pt[1][:], op=ALU.add), reads=[pt[0], pt[1]], writes=[s_im])
                    osl = slice(m * SUB, (m + 1) * SUB)
                    for jj in range(4):
                        j = 4 * m + jj
                        P.op("pe", lambda e: e.matmul(FB[2][:, osl], lhsT=CreT[:, j, :], rhs=s_re[:, jj, :], start=(jj == 0), stop=False), reads=[CreT, s_re], writes=[FB[2]])
                        P.op("pe", lambda e: e.matmul(FB[2][:, osl], lhsT=nCimT[:, j, :], rhs=s_im[:, jj, :], start=False, stop=False), reads=[nCimT, s_im], writes=[FB[2]])
                    P.op("pe", lambda e: e.matmul(FB[2][:, osl], lhsT=Ddiag[:, m, :], rhs=uT[:, m, ss_], start=False, stop=True), reads=[Ddiag, uT], writes=[FB[2]])
                yv = FB[2][:, 0:2 * SUB].rearrange("p (m t) -> p m t", m=2)
                P.op("act", lambda e: e.activation(out=yg[:], in_=yv, func=AF.Copy), reads=[FB[2]], writes=[yg])
                P.op("dve", lambda e: e.tensor_tensor(out=yt1[:], in0=yg[:], in1=yg[:], op=ALU.mult), reads=[yg], writes=[yt1])
                P.op("dve", lambda e: e.tensor_scalar(out=yt1[:], in0=yt1[:], scalar1=0.044715, scalar2=1.0, op0=ALU.mult, op1=ALU.add), reads=[yt1], writes=[yt1])
                P.op("dve", lambda e: e.tensor_tensor(out=yt1[:], in0=yt1[:], in1=yg[:], op=ALU.mult), reads=[yt1, yg], writes=[yt1])
                P.op("dve", lambda e: e.tensor_scalar(out=yt1[:], in0=yt1[:], scalar1=-45.0, scalar2=None, op0=ALU.max), reads=[yt1], writes=[yt1])
                P.op("act", lambda e: e.activation(out=yt1[:], in_=yt1[:], func=AF.Exp, scale=-1.5957691216), reads=[yt1], writes=[yt1])
                P.op("dve", lambda e: e.tensor_scalar(out=yt1[:], in0=yt1[:], scalar1=1.0, scalar2=None, op0=ALU.add), reads=[yt1], writes=[yt1])
                P.op("dve", lambda e: e.reciprocal(out=yt1[:], in_=yt1[:]), reads=[yt1], writes=[yt1])
                P.op("dve", lambda e: e.tensor_tensor(out=yg[:], in0=yg[:], in1=yt1[:], op=ALU.mult), reads=[yg, yt1], writes=[yg])
                P.op("dve", lambda e: e.tensor_copy(out=yTb[:], in_=yg[:]), reads=[yg], writes=[yTb])
                for mo in range(2):
                    osl = slice(mo * SUB, (mo + 1) * SUB)
                    for k in range(2):
                        P.op("pe", lambda e, mo=mo, k=k, osl=osl: e.matmul(FB[3][:, osl], lhsT=w_glu_sb[:, k, mo * 128:(mo + 1) * 128], rhs=yTb[:, k, :], start=(k == 0), stop=(k == 1)),
                             reads=[w_glu_sb, yTb], writes=[FB[3]])
                    P.op("act", lambda e, mo=mo, osl=osl: e.activation(out=yt2[:, mo, :], in_=FB[3][:, osl], func=AF.Exp, scale=-1.0, bias=nbglu_c[:, mo:mo + 1]), reads=[FB[3], nbglu_c], writes=[yt2])
                P.op("dve", lambda e: e.tensor_scalar(out=yt2[:], in0=yt2[:], scalar1=1.0, scalar2=None, op0=ALU.add), reads=[yt2], writes=[yt2])
                P.op("dve", lambda e: e.reciprocal(out=yt2[:], in_=yt2[:]), reads=[yt2], writes=[yt2])
                P.op("dve", lambda e: e.tensor_tensor(out=osm[:], in0=yg[:], in1=yt2[:], op=ALU.mult), reads=[yg, yt2], writes=[osm])
                P.op("dve", lambda e: e.tensor_tensor(out=o2[:], in0=osm[:], in1=osm[:], op=ALU.mult), reads=[osm], writes=[o2])
                for m in range(2):
                    P.op("pe", lambda e, m=m: e.matmul(FB[3][:, 0:SUB], lhsT=ones_f[:, 0:128], rhs=o2[:, m, :], start=(m == 0), stop=(m == 1)), reads=[ones_f, o2], writes=[FB[3]])
                P.op("act", lambda e: e.activation(out=rs_bc[:], in_=FB[3][:, 0:SUB], func=AF.Sqrt, scale=1.0 / 256, bias=eps_c[:, 0:1]), reads=[FB[3], eps_c], writes=[rs_bc])
                P.op("dve", lambda e: e.reciprocal(out=rs_bc[:], in_=rs_bc[:]), reads=[rs_bc], writes=[rs_bc])
                P.op("dve", lambda e: e.tensor_tensor(out=msm[:], in0=osm[:], in1=rs_bc[:].unsqueeze(1).to_broadcast([128, 2, SUB]), op=ALU.mult), reads=[osm, rs_bc], writes=[msm])
                P.dma("sp", mssm[:, :, gs].rearrange("m p t -> p m t"), msm[:], reads=[msm], writes=[mssm])

    def phase_b(l):
        hi = 0
        ob_i = 0
        for mixer in range(2):
            QTd, KTd, Vd, dk, scale = ((QTm, KTm, Vm, 96, 1.0 / math.sqrt(96.0)), (QTf, KTf, Vf, 68, 0.125))[mixer]
            P.dma("sp", V_sb[:], Vd[:].rearrange("(t p) c -> p t c", p=128), reads=[Vd], writes=[V_sb])
            for h in range(NH):
                Qs = QT_sb[hi % 2]
                Ks = KT_sb[hi % 2]
                hi += 1
                P.dma("sp", Qs[0:dk, :], QTd[h], reads=[QTd], writes=[Qs])
                P.dma("sp", Ks[0:dk, :], KTd[h], reads=[KTd], writes=[Ks])
                it = 0
                for b in range(NB):
                    nk = 4 * b + 4
                    for kt in range(nk):
                        j = kt - 4 * b
                        q0 = 0 if j <= 0 else 128 * j
                        Sb = FB[it % 2]
                        pt_ = PT[it % 3]
                        it += 1
                        qsl = slice(b * 512 + q0, (b + 1) * 512)
                        P.op("pe", lambda e, Sb=Sb, kt=kt, qsl=qsl, q0=q0: e.matmul(Sb[:, q0:512], lhsT=Ks[0:dk, kt * 128:(kt + 1) * 128], rhs=Qs[0:dk, qsl], start=True, stop=True),
                             reads=[Ks, Qs], writes=[Sb])
                        P.op("act", lambda e, Sb=Sb, pt_=pt_, q0=q0: e.activation(out=pt_[:, q0:512], in_=Sb[:, q0:512], func=AF.Exp, scale=scale), reads=[Sb], writes=[pt_])
                        if j >= 0:
                            if mixer == 0:
                                P.op("pool", lambda e, pt_=pt_, q0=q0: e.memset(pt_[64:128, q0:q0 + 64], 0.0), writes=[pt_])
                            else:
                                P.op("pool", lambda e, pt_=pt_, q0=q0: e.affine_select(out=pt_[:, q0:q0 + 128], in_=pt_[:, q0:q0 + 128], pattern=[[1, 128]], compare_op=ALU.is_ge,
                                                                                     fill=0.0, base=0, channel_multiplier=-1), reads=[pt_], writes=[pt_])
                        OT = FB[2 + (ob_i % 2)]
                        P.op("pe", lambda e: e.matmul(OT[0:65, q0:512], lhsT=V_sb[:, kt, h * 65:(h + 1) * 65], rhs=pt_[:, q0:512], start=(kt == 0), stop=(kt == nk - 1)),
                             reads=[pt_, V_sb], writes=[OT])
                    OT = FB[2 + (ob_i % 2)]
                    Otr = FB[4 + (ob_i % 2)]
                    osb_ = osb[ob_i % 2]
                    rd = rden[ob_i % 2]
                    ob_i += 1
                    col = mixer * 384 + h * 64
                    P.op("act", lambda e: e.activation(out=osb_[:], in_=OT[0:65, :], func=AF.Copy), reads=[OT], writes=[osb_])
                    for qi in range(4):
                        P.op("pe", lambda e: e.transpose(Otr[:, qi * 65:(qi + 1) * 65], osb_[:, qi * 128:(qi + 1) * 128], identf[0:65, 0:65]), reads=[osb_, identf], writes=[Otr])
                    o3 = Otr[:, 0:260].rearrange("p (q c) -> p q c", q=4)
                    P.op("dve", lambda e: e.reciprocal(out=rd[:], in_=o3[:, :, 64]), reads=[Otr], writes=[rd])
                    P.op("dve", lambda e: e.tensor_tensor(out=o_attn[:, 4 * b:4 * b + 4, col:col + 64], in0=o3[:, :, 0:64], in1=rd[:].unsqueeze(2).to_broadcast([128, 4, 64]), op=ALU.mult),
                         reads=[Otr, rd], writes=[o_attn])

    def phase_c(l, moe):
        j2 = l // 2
        if moe:
            P.dma("sp", wr_sb[:], A["moe_wr"][j2].rearrange("(k p) n -> p k n", p=128), writes=[wr_sb])
            P.dma("sp", br_sb[:], A["moe_br"][j2].to_broadcast([128, 8]), writes=[br_sb])
        for t in range(NT):
            xs_ = xs[t % 2]
            if l == 0:
                P.dma("sp", xs_[:], x_in[t * 128:(t + 1) * 128, :], writes=[xs_])
            else:
                P.dma("sp", xs_[:], y[t * 128:(t + 1) * 128, :], reads=[ytile[t]], writes=[xs_])
            st_ = stat[t % 4]
            for mx in range(2):
                P.op("act", lambda e, mx=mx: e.activation(out=sq_junk[:, mx * 384:(mx + 1) * 384], in_=o_attn[:, t, mx * 384:(mx + 1) * 384], func=AF.Square, accum_out=st_[:, mx:mx + 1]),
                     reads=[o_attn], writes=[sq_junk, st_])
            P.op("act", lambda e: e.activation(out=st_[:, 2:4], in_=st_[:, 0:2], func=AF.Sqrt, scale=1.0 / 384, bias=eps_c[:, 0:1]), reads=[st_, eps_c], writes=[st_])
            P.op("dve", lambda e: e.reciprocal(out=st_[:, 2:4], in_=st_[:, 2:4]), reads=[st_], writes=[st_])
            for mx in range(2):
                P.op("dve", lambda e, mx=mx: e.tensor_scalar(out=mat[:, mx * 384:(mx + 1) * 384], in0=o_attn[:, t, mx * 384:(mx + 1) * 384], scalar1=st_[:, 2 + mx:3 + mx], scalar2=None, op0=ALU.mult),
                     reads=[o_attn, st_], writes=[mat])
            tb = TB[t % 2]
            for k in range(6):
                P.op("pe", lambda e, k=k: e.transpose(tb[:, k * 128:(k + 1) * 128], mat[:, k * 128:(k + 1) * 128], ident[:]), reads=[mat, ident], writes=[tb])
            P.op("act", lambda e: e.activation(out=mT[:, 2:8, :], in_=tb[:, 0:768].rearrange("p (k t) -> p k t", k=6), func=AF.Copy), reads=[tb], writes=[mT])
            P.dma("sp", mT[:, 0:2, :], mssm[:, :, t * 128:(t + 1) * 128].rearrange("m p t -> p m t"), reads=[mssm], writes=[mT])
            xn = xnew[t % 2]
            for hf in range(2):
                fb = FB[hf]
                for k in range(KD):
                    P.op("pe", lambda e, fb=fb, k=k, hf=hf: e.matmul(fb[:], lhsT=mT[:, k, :], rhs=w_out_sb[:, k, hf * 512:(hf + 1) * 512], start=(k == 0), stop=(k == KD - 1)),
                         reads=[mT, w_out_sb], writes=[fb])
                P.op("dve", lambda e, fb=fb, hf=hf: e.tensor_tensor(out=xn[:, hf * 512:(hf + 1) * 512], in0=fb[:], in1=xs_[:, hf * 512:(hf + 1) * 512], op=ALU.add), reads=[fb, xs_], writes=[xn])
            P.dma("sp", y[t * 128:(t + 1) * 128, :], xn[:], reads=[xn], writes=[ytile[t]])
            norm_transpose(xn, xn[:], l, 2, 3, h2T_st, slice(0, 128), t, fp32_path=(xhf, h2Tf) if moe else None)
            P.dma("sp", h2T_d[:, :, t * 128:(t + 1) * 128].rearrange("k p t -> p k t"), h2T_st[:], reads=[h2T_st], writes=[h2T_d])
            if moe:
                fb = FB[2]
                for k in range(KD):
                    P.op("pe", lambda e, k=k: e.matmul(fb[:, 0:8], lhsT=h2Tf[:, k, :], rhs=wr_sb[:, k, :], start=(k == 0), stop=(k == KD - 1)), reads=[h2Tf, wr_sb], writes=[fb])
                lg, m1, m2, lg2 = rtmp
                s1, s2, s3, s4 = rsc
                P.op("dve", lambda e: e.tensor_tensor(out=lg[:], in0=fb[:, 0:8], in1=br_sb[:], op=ALU.add), reads=[fb, br_sb], writes=[lg])
                P.op("dve", lambda e: e.tensor_reduce(out=s1[:], in_=lg[:], axis=AX.X, op=ALU.max), reads=[lg], writes=[s1])
                P.op("dve", lambda e: e.tensor_scalar(out=m1[:], in0=lg[:], scalar1=s1[:, 0:1], scalar2=None, op0=ALU.is_equal), reads=[lg, s1], writes=[m1])
                P.op("dve", lambda e: e.scalar_tensor_tensor(out=lg2[:], in0=m1[:], scalar=-1e30, in1=lg[:], op0=ALU.mult, op1=ALU.add), reads=[m1, lg], writes=[lg2])
                P.op("dve", lambda e: e.tensor_reduce(out=s2[:], in_=lg2[:], axis=AX.X, op=ALU.max), reads=[lg2], writes=[s2])
                P.op("dve", lambda e: e.tensor_scalar(out=m2[:], in0=lg2[:], scalar1=s2[:, 0:1], scalar2=None, op0=ALU.is_equal), reads=[lg2, s2], writes=[m2])
                P.op("dve", lambda e: e.tensor_tensor(out=s3[:], in0=s2[:], in1=s1[:], op=ALU.subtract), reads=[s1, s2], writes=[s3])
                P.op("act", lambda e: e.activation(out=s3[:], in_=s3[:], func=AF.Exp), reads=[s3], writes=[s3])
                P.op("dve", lambda e: e.tensor_scalar(out=s3[:], in0=s3[:], scalar1=1.0, scalar2=None, op0=ALU.add), reads=[s3], writes=[s3])
                P.op("dve", lambda e: e.reciprocal(out=s3[:], in_=s3[:]), reads=[s3], writes=[s3])
                P.op("dve", lambda e: e.tensor_scalar(out=s4[:], in0=s3[:], scalar1=-1.0, scalar2=1.0, op0=ALU.mult, op1=ALU.add), reads=[s3], writes=[s4])
                P.op("dve", lambda e: e.tensor_scalar(out=m1[:], in0=m1[:], scalar1=s3[:, 0:1], scalar2=None, op0=ALU.mult), reads=[m1, s3], writes=[m1])
                P.op("dve", lambda e, t=t: e.scalar_tensor_tensor(out=comb_all[:, t, :], in0=m2[:], scalar=s4[:, 0:1], in1=m1[:], op0=ALU.mult, op1=ALU.add), reads=[m2, s4, m1], writes=[comb_all])

    def phase_d(l, moe):
        j2 = l // 2
        P.dma("sp", G_bc[:], g12[l, 1], reads=[g12], writes=[G_bc])
        ne = 8 if moe else 2
        it = 0
        oi = 0
        for ex in range(ne):
            if moe:
                wg = A["moe_wg"][j2, ex]
                wu = A["moe_wu"][j2, ex]
                wd = A["moe_wd"][j2, ex]
            else:
                wg = A["ffn_wg"][j2][:, ex * DFE:(ex + 1) * DFE]
                wu = A["ffn_wu"][j2][:, ex * DFE:(ex + 1) * DFE]
                wd = A["ffn_wd"][j2][ex * DFE:(ex + 1) * DFE, :]
            Wg_, Wu_, Wd_ = Wg_sb[ex % 2], Wu_sb[ex % 2], Wd_sb[ex % 2]
            P.dma("pool", Wg_[:], wg.rearrange("(k p) f -> p k f", p=128), writes=[Wg_])
            P.dma("pool", Wu_[:], wu.rearrange("(k p) f -> p k f", p=128), writes=[Wu_])
            P.dma("pool", Wd_[:], wd.rearrange("(c p) d -> p c d", p=128), writes=[Wd_])
            for b in range(NB):
                hb = h2T_sb[(ex * NB + b) % 2]
                P.dma("sp", hb[:], h2T_d[:, :, b * 512:(b + 1) * 512].rearrange("k p t -> p k t"), reads=[h2T_d], writes=[hb])
                for c in range(NFC):
                    gb = FB[(it % 2) * 2]
                    ub = FB[(it % 2) * 2 + 1]
                    sg_ = sg[it % 2]
                    it += 1
                    for k in range(KD):
                        P.op("pe", lambda e, gb=gb, k=k, c=c: e.matmul(gb[:], lhsT=Wg_[:, k, c * 128:(c + 1) * 128], rhs=hb[:, k, :], start=(k == 0), stop=(k == KD - 1)), reads=[Wg_, hb], writes=[gb])
                    for k in range(KD):
                        P.op("pe", lambda e, ub=ub, k=k, c=c: e.matmul(ub[:], lhsT=Wu_[:, k, c * 128:(c + 1) * 128], rhs=hb[:, k, :], start=(k == 0), stop=(k == KD - 1)), reads=[Wu_, hb], writes=[ub])
                    P.op("act", lambda e, gb=gb, sg_=sg_: e.activation(out=sg_[:], in_=gb[:], func=AF.Silu), reads=[gb], writes=[sg_])
                    P.op("dve", lambda e, ub=ub, sg_=sg_, c=c: e.tensor_tensor(out=aT[:, c, :], in0=ub[:], in1=sg_[:], op=ALU.mult), reads=[ub, sg_], writes=[aT])
                for ti in range(4):
                    t = b * 4 + ti
                    os_ = ost[oi % 2]
                    oi += 1
                    for hf in range(2):
                        fb = FB[4 + hf]
                        for c in range(NFC):
                            P.op("pe", lambda e, fb=fb, c=c, ti=ti, hf=hf: e.matmul(fb[:], lhsT=aT[:, c, ti * 128:(ti + 1) * 128], rhs=Wd_[:, c, hf * 512:(hf + 1) * 512], start=(c == 0), stop=(c == NFC - 1)),
                                 reads=[aT, Wd_], writes=[fb])
                        if moe:
                            P.op("dve", lambda e, fb=fb, hf=hf, t=t, ex=ex, os_=os_: e.scalar_tensor_tensor(out=os_[:, hf * 512:(hf + 1) * 512], in0=fb[:], scalar=comb_all[:, t, ex:ex + 1],
                                                                                                       in1=G_bc[:, hf * 512:(hf + 1) * 512], op0=ALU.mult, op1=ALU.mult), reads=[fb, comb_all, G_bc], writes=[os_])
                        else:
                            P.op("dve", lambda e, fb=fb, hf=hf, os_=os_: e.tensor_tensor(out=os_[:, hf * 512:(hf + 1) * 512], in0=fb[:], in1=G_bc[:, hf * 512:(hf + 1) * 512], op=ALU.mult),
                                 reads=[fb, G_bc], writes=[os_])
                    P.dma("pool", y[t * 128:(t + 1) * 128, :], os_[:], reads=[os_, ytile[t]], writes=[ytile[t]], accum_op=ALU.add)

    stop = build.stop_after
    G = globals()

    def use(dct):
        for k_, v_ in dct.items():
            if k_ not in ("P", "L", "NT"):
                G[k_] = v_
    dbg = P.dram("dbg_oattn", [128, NT * 768], BF16, kind="ExternalOutput") if debug else None
    for l in range(n_layers):
        moe = (l % 2 == 1)
        P.push()
        use(_alloc_a(P, L))
        s5c = load_layer(l)
        phase_a(l, s5c)
        P.pop()
        if stop == "a":
            break
        P.push()
        use(_alloc_bc(P, L))
        P.push()
        use(_alloc_b(P, L))
        phase_b(l)
        P.pop()
        if debug and (stop == "b" or l == n_layers - 1):
            P.dma("sp", dbg[:], o_attn[:].rearrange("p t c -> p (t c)"), reads=[o_attn], writes=[dbg])
        if stop == "b":
            P.pop()
            break
        P.push()
        use(_alloc_c(P, L))
        load_c(l)
        phase_c(l, moe)
        P.pop()
        P.pop()
        if stop == "c":
            break
        P.push()
        use(_alloc_d(P, L))
        phase_d(l, moe)
        P.pop()
    P.finish()
    build.n_inst = P.n_inst
    return nc


build.stop_after = None


def prep_shared(inp, n_layers=DEPTH):
    f = lambda a: np.ascontiguousarray(np.asarray(a, dtype=np.float32))
    col = lambda a, k: f(np.asarray(a).reshape(DEPTH, k, 128).transpose(0, 2, 1))
    out = {}
    out["norm_mix_c"] = col(inp["norm_mix"], KD)
    out["norm_ffn_c"] = col(inp["norm_ffn"], KD)
    out["w_ada"] = f(inp["w_ada"])
    out["b_ada"] = f(np.asarray(inp["b_ada"]).reshape(DEPTH, 1, 6 * D))
    out["w_in"] = f(inp["w_in"])
    sm = lambda a: f(np.asarray(a).reshape(DEPTH, 8, 128).transpose(0, 2, 1))
    out["lam_re"] = sm(inp["ssm_lam_re"])
    out["lam_im"] = sm(inp["ssm_lam_im"])
    out["log_dt"] = sm(np.repeat(np.asarray(inp["ssm_log_dt"])[:, :, None], 64, axis=2))
    b_re = np.asarray(inp["ssm_b_re"]); b_im = np.asarray(inp["ssm_b_im"])
    c_re = np.asarray(inp["ssm_c_re"]); c_im = np.asarray(inp["ssm_c_im"])
    blk = {k: np.zeros((DEPTH, 8, 128, 128), np.float32) for k in ("b_re", "b_im", "c_re", "c_im")}
    for g in range(16):
        j, gg = g // 2, g % 2
        fc = (g % 8) * 16
        blk["b_re"][:, j, gg * 64:(gg + 1) * 64, fc:fc + 16] = b_re[:, g]
        blk["b_im"][:, j, gg * 64:(gg + 1) * 64, fc:fc + 16] = b_im[:, g]
        blk["c_re"][:, j, gg * 64:(gg + 1) * 64, fc:fc + 16] = c_re[:, g].transpose(0, 2, 1)
        blk["c_im"][:, j, gg * 64:(gg + 1) * 64, fc:fc + 16] = c_im[:, g].transpose(0, 2, 1)
    out.update(blk)
    out["ssm_d"] = col(inp["ssm_d"], 2)
    out["w_glu"] = f(inp["ssm_w_glu"])
    out["b_glu"] = col(inp["ssm_b_glu"], 2)
    out["q_norm"] = col(inp["mla_q_norm"], 2)
    out["kv_norm"] = col(inp["mla_kv_norm"], 1)
    out["w_uq"] = f(inp["mla_w_uq"])
    out["w_ukv"] = f(inp["mla_w_ukv"])
    rep = lambda a: f(np.broadcast_to(np.asarray(a)[:, None, :], (DEPTH, 128, np.asarray(a).shape[1])))
    out["gq_m"] = rep(inp["mla_qk_gq"])
    out["gk_m"] = rep(inp["mla_qk_gk"])
    out["fox_bf"] = f(np.asarray(inp["fox_b_f"]).reshape(DEPTH, NH, 1))
    out["gq_f"] = rep(inp["fox_qk_gq"])
    out["gk_f"] = rep(inp["fox_qk_gk"])
    out["out_norm"] = col(inp["out_norm"], KD)
    out["w_out"] = f(inp["w_out"])
    out["ffn_wg"] = f(inp["ffn_w_gate"])
    out["ffn_wu"] = f(inp["ffn_w_up"])
    out["ffn_wd"] = f(inp["ffn_w_down"])
    out["moe_wr"] = f(inp["moe_w_router"])
    out["moe_br"] = f(np.asarray(inp["moe_b_router"]).reshape(2, 1, 8))
    out["moe_wg"] = f(inp["moe_w_gate"])
    out["moe_wu"] = f(inp["moe_w_up"])
    out["moe_wd"] = f(inp["moe_w_down"])
    half = 16
    inv = (10000.0 ** (-np.arange(half, dtype=np.float32) / half)).astype(np.float32)
    out["inv_bc"] = f(np.broadcast_to(inv[None, :], (128, 16)))
    return out


def prep_core(inp, b, L):
    NT = L // 128
    m = {}
    m["x"] = np.ascontiguousarray(np.asarray(inp["x"])[b, :L, :], dtype=np.float32)
    m["c_col"] = np.ascontiguousarray(np.asarray(inp["c"], dtype=np.float32)[b].reshape(KD, 128).T)
    m["pos"] = np.ascontiguousarray(np.asarray(inp["positions"])[b, :L].astype(np.int32).reshape(NT, 128).T)
    return m


_CACHE = {}


def run(inp, L, n_layers=DEPTH, debug=False, cores=8, trace=False):
    key = (L, n_layers, debug, build.stop_after)
    if key not in _CACHE:
        _CACHE[key] = build(L, n_layers, debug)
    nc = _CACHE[key]
    shared = prep_shared(inp, n_layers)
    in_maps = []
    for b in range(cores):
        m = dict(shared)
        for n_ in PADDED:
            a = shared[n_]
            a2 = a.reshape(-1, a.shape[-1])
            m[n_] = np.concatenate([a2, np.full((1, a2.shape[1]), float(b), np.float32)], axis=0)
        m.update(prep_core(inp, b, L))
        in_maps.append(m)
    res = run_bass_kernel_spmd(nc, in_maps, core_ids=list(range(cores)), **({"trace": True} if trace else {}))
    return res


def kernel(**inputs):
    L = np.asarray(inputs["x"]).shape[1]
    res = run(inputs, L)
    out = np.stack([np.asarray(r["y"], dtype=np.float32) for r in res.results], axis=0)
    return out
```

```python
import contextlib
import math
import sys
import numpy as np
import concourse.bass as bass
import concourse.mybir as mybir
from concourse.bass_utils import run_bass_kernel_spmd

F32 = mybir.dt.float32
BF16 = mybir.dt.bfloat16
I32 = mybir.dt.int32
AF = mybir.ActivationFunctionType
ALU = mybir.AluOpType
AX = mybir.AxisListType

ENGS = ("pe", "act", "dve", "pool", "sp")

D = 1024
KD = 8
DEPTH = 4
EPS = 1e-6
IN_COLS = 1830
NH = 6
DFE = 1408
NFC = 11
SUB = 256


class Tl:
    __slots__ = ("t", "name", "w", "r", "excl")

    def __init__(self, t, name):
        self.t = t
        self.name = name
        self.excl = False
        self.w = {}
        self.r = {}

    def __getitem__(self, idx):
        return self.t[idx]


class Prog:
    max_ops = 10 ** 9
    log = None

    def __init__(self, nc, ring_sizes=None):
        self.nc = nc
        self.es = contextlib.ExitStack()
        self.cnt = {e: 0 for e in ENGS}
        self.sems = {}
        self.seen = {e: {} for e in ENGS}
        for e in ENGS:
            self.sems[("eng", e)] = self.es.enter_context(nc.semaphore("s_" + e))
        ring_sizes = ring_sizes or {"sp": 16, "pool": 8, "act": 2}
        self.rings = {}
        self.ring_i = {}
        for e, k in ring_sizes.items():
            self.rings[e] = []
            for i in range(k):
                key = ("ring", e, i)
                self.sems[key] = self.es.enter_context(nc.semaphore("r_%s%d" % (e, i)))
                self.rings[e].append([key, 0])
            self.ring_i[e] = 0
        self.n_inst = 0
        self.scopes = [self.es]
        self.E = {"pe": nc.tensor, "act": nc.scalar, "dve": nc.vector, "pool": nc.gpsimd, "sp": nc.sync}

    def sb(self, name, shape, dt):
        self._uid = getattr(self, "_uid", 0) + 1
        return Tl(self.scopes[-1].enter_context(self.nc.sbuf_tensor("%s_%d" % (name, self._uid), list(shape), dt)), name)

    def push(self):
        self.scopes.append(contextlib.ExitStack())

    def barrier(self):
        need = {}
        for e, ring in self.rings.items():
            for key, v in ring:
                if v > 0:
                    need[key] = v
        for e in ENGS:
            if self.cnt[e] > 0:
                need[("eng", e)] = self.cnt[e]
        for eng in ENGS:
            seen = self.seen[eng]
            for k, v in need.items():
                if k == ("eng", eng) or seen.get(k, 0) >= v:
                    continue
                seen[k] = v
                self.E[eng].wait_ge(self.sems[k], v)

    def pop(self):
        self.barrier()
        self.scopes.pop().close()

    def ps(self, name, shape, dt):
        t = Tl(self.es.enter_context(self.nc.psum_tensor(name, list(shape), dt)), name)
        t.excl = True
        return t

    def dram(self, name, shape, dt, kind="Internal"):
        return Tl(self.nc.dram_tensor(name, list(shape), dt, kind=kind).ap(), name)

    def _collect(self, eng, reads, writes):
        need = {}
        me = ("eng", eng)
        for t in reads:
            for k, v in t.w.items():
                if need.get(k, 0) < v:
                    need[k] = v
            if t.excl:
                for k, v in t.r.items():
                    if k != me and need.get(k, 0) < v:
                        need[k] = v
        for t in writes:
            for k, v in t.w.items():
                if need.get(k, 0) < v:
                    need[k] = v
            for k, v in t.r.items():
                if need.get(k, 0) < v:
                    need[k] = v
        waits = []
        seen = self.seen[eng]
        for k, v in need.items():
            if eng == "pe" and k == ("eng", "pe"):
                continue
            if seen.get(k, 0) >= v:
                continue
            seen[k] = v
            waits.append((self.sems[k], v))
        return waits

    def _commit(self, reads, writes, key, val):
        for t in reads:
            if t.r.get(key, 0) < val:
                t.r[key] = val
        for t in writes:
            t.w = {key: val}
            t.r = {}

    def op(self, eng, fn, reads=(), writes=()):
        self.n_inst += 1
        if Prog.log is not None:
            Prog.log.append((self.n_inst, eng, sys._getframe(1).f_lineno))
        if self.n_inst > Prog.max_ops:
            return
        waits = self._collect(eng, reads, writes)
        self.cnt[eng] += 1
        key = ("eng", eng)
        val = self.cnt[eng]
        self._commit(reads, writes, key, val)
        e = self.E[eng]
        for s, v in waits[1:]:
            e.wait_ge(s, v)
        ins = fn(e)
        if waits:
            ins._wait_ge(waits[0][0], waits[0][1])
        ins.then_inc(self.sems[key], 1)

    def dma(self, eng, out, in_, reads=(), writes=(), **kw):
        self.n_inst += 1
        if Prog.log is not None:
            Prog.log.append((self.n_inst, "dma-" + eng, sys._getframe(1).f_lineno))
        if self.n_inst > Prog.max_ops:
            return
        ring = self.rings[eng]
        slot = ring[self.ring_i[eng] % len(ring)]
        self.ring_i[eng] += 1
        key, pv = slot
        waits = self._collect(eng, reads, writes)
        seen = self.seen[eng]
        if pv > 0 and seen.get(key, 0) < pv:
            seen[key] = pv
            waits.append((self.sems[key], pv))
        val = pv + 16
        slot[1] = val
        self._commit(reads, writes, key, val)
        e = self.E[eng]
        for s, v in waits[1:]:
            e.wait_ge(s, v)
        ins = e.dma_start(out=out, in_=in_, **kw)
        if waits:
            ins._wait_ge(waits[0][0], waits[0][1])
        ins.then_inc(self.sems[key], 16)

    def finish(self, eng="sp"):
        need = {}
        for e, ring in self.rings.items():
            for key, v in ring:
                if v > 0:
                    need[key] = v
        for e in ENGS:
            if self.cnt[e] > 0:
                need[("eng", e)] = self.cnt[e]
        E = self.E[eng]
        for k, v in need.items():
            E.wait_ge(self.sems[k], v)
        self.es.close()


LAYER_PARAMS = [
    ("norm_mix_c", [128, KD]), ("norm_ffn_c", [128, KD]),
    ("w_ada", [D, 6 * D]), ("b_ada", [1, 6 * D]),
    ("w_in", [D, IN_COLS]),
    ("lam_re", [128, 8]), ("lam_im", [128, 8]), ("log_dt", [128, 8]),
    ("b_re", [8, 128, 128]), ("b_im", [8, 128, 128]),
    ("c_re", [8, 128, 128]), ("c_im", [8, 128, 128]),
    ("ssm_d", [128, 2]), ("w_glu", [256, 256]), ("b_glu", [128, 2]),
    ("q_norm", [128, 2]), ("kv_norm", [128, 1]),
    ("w_uq", [256, 576]), ("w_ukv", [128, 768]),
    ("gq_m", [128, 96]), ("gk_m", [128, 96]),
    ("fox_bf", [NH, 1]), ("gq_f", [128, 64]), ("gk_f", [128, 64]),
    ("out_norm", [128, KD]), ("w_out", [D, D]),
]


PADDED = ("w_ada", "w_in", "b_re", "b_im", "c_re", "c_im", "w_glu", "w_uq", "w_ukv", "w_out",
          "ffn_wg", "ffn_wu", "ffn_wd", "moe_wg", "moe_wu", "moe_wd")


def _alloc_a(P, L):
    NT = L // 128
    w_in_sb = P.sb("w_in_sb", [128, KD, IN_COLS], BF16)
    w_uq_sb = P.sb("w_uq_sb", [128, 2, 576], BF16)
    w_ukv_sb = P.sb("w_ukv_sb", [128, 768], BF16)
    w_glu_sb = P.sb("w_glu_sb", [128, 2, 256], BF16)
    qn_c = P.sb("qn_c", [128, 2], F32)
    kvn_c = P.sb("kvn_c", [128, 1], F32)
    gqm = P.sb("gqm", [128, 96], F32)
    gkm = P.sb("gkm", [128, 96], F32)
    gqf = P.sb("gqf", [128, 64], F32)
    gkf = P.sb("gkf", [128, 64], F32)
    nbf = P.sb("nbf", [NH, 1], F32)
    dcol = P.sb("dcol", [128, 2], F32)
    bglu_c = P.sb("bglu_c", [128, 2], F32)
    nbglu_c = P.sb("nbglu_c", [128, 2], F32)
    s5p = P.sb("s5p", [128, 24, 8], F32)
    BreT = P.sb("BreT", [128, 8, 128], BF16)
    BimT = P.sb("BimT", [128, 8, 128], BF16)
    CreT = P.sb("CreT", [128, 8, 128], BF16)
    nCimT = P.sb("nCimT", [128, 8, 128], BF16)
    Ddiag = P.sb("Ddiag", [128, 2, 128], BF16)
    Ctab = P.sb("Ctab", [128, 8, SUB], F32)
    Stab = P.sb("Stab", [128, 8, SUB], F32)
    Rtab = P.sb("Rtab", [128, 8, SUB], F32)
    blk_f = P.sb("blk_f", [128, 2, 128], F32)
    blk_o = P.sb("blk_o", [128, 2, 128], F32)
    hT = [P.sb("hT%d" % i, [128, KD, 512], BF16) for i in range(2)]
    uT = P.sb("uT", [128, 2, 512], BF16)
    cq_h = P.sb("cq_h", [128, 256], BF16)
    ckv_h = P.sb("ckv_h", [128, 128], BF16)
    cqT = P.sb("cqT", [128, 2, 128], BF16)
    ckvT = P.sb("ckvT", [128, 128], BF16)
    qn = P.sb("qn", [128, NH, 96], F32)
    kn = P.sb("kn", [128, NH, 96], F32)
    rt = [P.sb("rt%d" % i, [128, NH, 16], F32) for i in range(4)]
    qfin = P.sb("qfin", [128, NH, 96], BF16)
    kfin = P.sb("kfin", [128, NH, 96], BF16)
    fqn = P.sb("fqn", [128, NH, 64], F32)
    fqb = P.sb("fqb", [128, NH, 64], BF16)
    fkb = P.sb("fkb", [128, NH, 64], BF16)
    QTm_st = [P.sb("QTm_st%d" % i, [96, NH, 128], BF16) for i in range(2)]
    KTm_st = [P.sb("KTm_st%d" % i, [96, NH, 128], BF16) for i in range(2)]
    QTf_st = [P.sb("QTf_st%d" % i, [64, NH, 128], BF16) for i in range(2)]
    KTf_st = [P.sb("KTf_st%d" % i, [64, NH, 128], BF16) for i in range(2)]
    Vm_st = [P.sb("Vm_st%d" % i, [128, NH, 65], BF16) for i in range(2)]
    Vf_st = [P.sb("Vf_st%d" % i, [128, NH, 65], BF16) for i in range(2)]
    for t_ in Vm_st + Vf_st:
        P.op("pool", lambda e, t_=t_: e.memset(t_[:], 1.0), writes=[t_])
    fg_e = P.sb("fg_e", [NH, 512], F32)
    fg_sp = P.sb("fg_sp", [NH, 512], F32)
    fg_cum = P.sb("fg_cum", [NH, 512], F32)
    fg_carry = P.sb("fg_carry", [NH, 1], F32)
    fg_hi = P.sb("fg_hi", [NH, 512], BF16)
    fg_lo = P.sb("fg_lo", [NH, 512], BF16)
    fg_nhi = P.sb("fg_nhi", [NH, 512], BF16)
    fg_nlo = P.sb("fg_nlo", [NH, 512], BF16)
    W_re = P.sb("W_re", [128, 4, SUB], F32)
    W_im = P.sb("W_im", [128, 4, SUB], F32)
    wlast = P.sb("wlast", [128, 2, 8], F32)
    pre_c = P.sb("pre_c", [128, 2, SUB], F32)
    pre_s = P.sb("pre_s", [128, 2, SUB], F32)
    pin_re = P.sb("pin_re", [128, SUB], F32)
    pin_im = P.sb("pin_im", [128, SUB], F32)
    w0 = P.sb("w0", [128, 2, 8], F32)
    w0t = P.sb("w0t", [128, 4, 8], F32)
    pt = [P.sb("pt%d" % i, [128, 4, SUB], F32) for i in range(2)]
    s_re = P.sb("s_re", [128, 4, SUB], BF16)
    s_im = P.sb("s_im", [128, 4, SUB], BF16)
    yg = P.sb("yg", [128, 2, SUB], F32)
    yt1 = P.sb("yt1", [128, 2, SUB], F32)
    yt2 = P.sb("yt2", [128, 2, SUB], F32)
    yTb = P.sb("yTb", [128, 2, SUB], BF16)
    o2 = P.sb("o2", [128, 2, SUB], F32)
    osm = P.sb("osm", [128, 2, SUB], F32)
    rs_bc = P.sb("rs_bc", [128, SUB], F32)
    msm = P.sb("msm", [128, 2, SUB], BF16)
    return locals()


def _alloc_bc(P, L):
    NT = L // 128
    o_attn = P.sb("o_attn", [128, NT, 768], BF16)
    return locals()


def _alloc_b(P, L):
    NT = L // 128
    QT_sb = [P.sb("QT_sb%d" % i, [96, L], BF16) for i in range(2)]
    KT_sb = [P.sb("KT_sb%d" % i, [96, L], BF16) for i in range(2)]
    V_sb = P.sb("V_sb", [128, NT, NH * 65], BF16)
    PT = [P.sb("PT%d" % i, [128, 512], BF16) for i in range(3)]
    rden = [P.sb("rden%d" % i, [128, 4], F32) for i in range(2)]
    osb = [P.sb("osb%d" % i, [65, 512], F32) for i in range(2)]
    return locals()


def _alloc_c(P, L):
    NT = L // 128
    w_out_sb = P.sb("w_out_sb", [128, KD, D], BF16)
    wstc = [P.sb("wstc%d" % i, [128, D], F32) for i in range(2)]
    onorm_c = P.sb("onorm_c", [128, KD], F32)
    mat_l = [P.sb("mat%d" % i, [128, 768], BF16) for i in range(2)]
    mT_l = [P.sb("mT%d" % i, [128, KD, 128], BF16) for i in range(2)]
    xnew = [P.sb("xnew%d" % i, [128, D], F32) for i in range(2)]
    h2T_st_l = [P.sb("h2T_st%d" % i, [128, KD, 128], BF16) for i in range(2)]
    xhf = P.sb("xhf", [128, D], F32)
    h2Tf = P.sb("h2Tf", [128, KD, 128], F32)
    wr_sb = P.sb("wr_sb", [128, KD, 8], F32)
    br_sb = P.sb("br_sb", [128, 8], F32)
    rtmp = [P.sb("rtmp%d" % i, [128, 8], F32) for i in range(4)]
    rsc = [P.sb("rsc%d" % i, [128, 1], F32) for i in range(4)]
    return locals()


def _alloc_d(P, L):
    Wg_sb = [P.sb("Wg_sb%d" % i, [128, KD, DFE], BF16) for i in range(2)]
    Wu_sb = [P.sb("Wu_sb%d" % i, [128, KD, DFE], BF16) for i in range(2)]
    Wd_sb = [P.sb("Wd_sb%d" % i, [128, NFC, D], BF16) for i in range(2)]
    h2T_sb = [P.sb("h2T_sb%d" % i, [128, KD, 512], BF16) for i in range(2)]
    sg = [P.sb("sg%d" % i, [128, 512], BF16) for i in range(2)]
    aT = P.sb("aT", [128, NFC, 512], BF16)
    ost = [P.sb("ost%d" % i, [128, D], F32) for i in range(2)]
    return locals()


def build(L, n_layers=DEPTH, debug=False):
    NT = L // 128
    NB = L // 512
    nc = bass.Bass("TRN2", target_bir_lowering=False)
    P = Prog(nc)
    A = {}

    def din(name, shape, dt=F32):
        if name in PADDED:
            rows = int(np.prod(shape[:-1]))
            t = P.dram(name, [rows + 1, shape[-1]], dt, kind="ExternalInput")
            names = "abcdefg"[:len(shape) - 1]
            pat = "(%s) z -> %s z" % (" ".join(names), " ".join(names))
            A[name] = Tl(t.t[0:rows, :].rearrange(pat, **{n_: int(v_) for n_, v_ in zip(names, shape[:-1])}), name)
            return A[name]
        A[name] = P.dram(name, shape, dt, kind="ExternalInput")
        return A[name]

    x_in = din("x", [L, D])
    c_in = din("c_col", [128, KD])
    pos_in = din("pos", [128, NT], I32)
    inv_in = din("inv_bc", [128, 16])
    for name, shp in LAYER_PARAMS:
        din(name, [DEPTH] + shp)
    din("ffn_wg", [2, D, 2 * DFE])
    din("ffn_wu", [2, D, 2 * DFE])
    din("ffn_wd", [2, 2 * DFE, D])
    din("moe_wr", [2, D, 8])
    din("moe_br", [2, 1, 8])
    din("moe_wg", [2, 8, D, DFE])
    din("moe_wu", [2, 8, D, DFE])
    din("moe_wd", [2, 8, DFE, D])

    y = P.dram("y", [L, D], F32, kind="ExternalOutput")
    skind = "ExternalOutput" if debug else "Internal"
    QTm = P.dram("QTm", [NH, 96, L], BF16, kind=skind)
    KTm = P.dram("KTm", [NH, 96, L], BF16, kind=skind)
    Vm = P.dram("Vm", [L, NH * 65], BF16, kind=skind)
    QTf = P.dram("QTf", [NH, 68, L], BF16, kind=skind)
    KTf = P.dram("KTf", [NH, 68, L], BF16, kind=skind)
    Vf = P.dram("Vf", [L, NH * 65], BF16, kind=skind)
    mssm = P.dram("mssm", [2, 128, L], BF16, kind=skind)
    h2T_d = P.dram("h2T", [KD, 128, L], BF16, kind=skind)
    g12 = P.dram("g12", [DEPTH, 2, 128, D], F32, kind=skind)
    ytile = [Tl(y.t, "y%d" % t) for t in range(NT)]

    ident = P.sb("ident", [128, 128], BF16)
    identf = P.sb("identf", [128, 128], F32)
    ones_f = P.sb("ones_f", [128, 512], F32)
    ones_b = P.sb("ones_b", [128, 128], BF16)
    for t_, dt_ in ((ident, BF16), (identf, F32)):
        P.op("pool", lambda e, t_=t_: e.memset(t_[:], 1.0), writes=[t_])
        P.op("pool", lambda e, t_=t_: e.affine_select(out=t_[:], in_=t_[:], pattern=[[1, 128]], compare_op=ALU.is_equal,
                                                    fill=0.0, base=0, channel_multiplier=-1), reads=[t_], writes=[t_])
    P.op("pool", lambda e: e.memset(ones_f[:], 1.0), writes=[ones_f])
    P.op("pool", lambda e: e.memset(ones_b[:], 1.0), writes=[ones_b])

    TB = [P.ps("TB%d" % i, [128, 1024], BF16) for i in range(2)]
    FB = [P.ps("FB%d" % i, [128, 512], F32) for i in range(6)]

    def rstd_chain(ss_ap, n, nfeat, tmp, out, reads, eng_r="dve"):
        (tt, tv), (ot, ov) = tmp, out
        P.op("act", lambda e: e.activation(out=tv, in_=ss_ap, func=AF.Sqrt, scale=1.0 / nfeat, bias=eps_c[:, 0:1]),
             reads=list(reads) + [eps_c], writes=[tt])
        P.op("dve", lambda e: e.reciprocal(out=ov, in_=tv), reads=[tt], writes=[ot])

    eps_c = P.sb("eps_c", [128, 1], F32)
    P.op("pool", lambda e: e.memset(eps_c[:], EPS), writes=[eps_c])

    G_bc = P.sb("G_bc", [128, D], F32)
    xs = [P.sb("xs%d" % i, [128, D], F32) for i in range(2)]
    sq_junk = P.sb("sq_junk", [128, D], F32)
    xh = [P.sb("xh%d" % i, [128, D], BF16) for i in range(2)]
    stat = [P.sb("stat%d" % i, [128, 16], F32) for i in range(4)]
    comb_all = P.sb("comb_all", [128, NT, 8], F32)

    modc = P.sb("modc", [128, DEPTH, 4, KD], F32)
    nmc = P.sb("nmc", [128, DEPTH, 2, KD], F32)
    cosT = P.sb("cosT", [128, NT, 16], F32)
    sinT = P.sb("sinT", [128, NT, 16], F32)
    P.push()
    c_col = P.sb("c_colsb", [128, KD], F32)
    P.dma("sp", c_col[:], c_in[:], writes=[c_col])
    c_e = P.sb("c_e", [128, KD], F32)
    c_act = P.sb("c_act", [128, KD], F32)
    P.op("act", lambda e: e.activation(out=c_e[:], in_=c_col[:], func=AF.Exp, scale=-1.0), reads=[c_col], writes=[c_e])
    P.op("dve", lambda e: e.tensor_scalar(out=c_e[:], in0=c_e[:], scalar1=1.0, scalar2=None, op0=ALU.add), reads=[c_e], writes=[c_e])
    P.op("dve", lambda e: e.reciprocal(out=c_e[:], in_=c_e[:]), reads=[c_e], writes=[c_e])
    P.op("dve", lambda e: e.tensor_tensor(out=c_act[:], in0=c_col[:], in1=c_e[:], op=ALU.mult), reads=[c_col, c_e], writes=[c_act])
    C_bc = P.sb("C_bc", [128, KD, 128], F32)
    for k in range(KD):
        P.op("dve", lambda e, k=k: e.tensor_scalar(out=C_bc[:, k, :], in0=ones_f[:, 0:128], scalar1=c_act[:, k:k + 1], scalar2=None,
                                                   op0=ALU.mult), reads=[ones_f, c_act], writes=[C_bc])
    for l in range(n_layers):
        P.dma("sp", nmc[:, l, 0, :], A["norm_mix_c"][l], writes=[nmc])
        P.dma("sp", nmc[:, l, 1, :], A["norm_ffn_c"][l], writes=[nmc])
    wst = [P.sb("wst%d" % i, [128, 2048], F32) for i in range(3)]
    wst_i = [0]

    def next_wst():
        t = wst[wst_i[0] % 3]
        wst_i[0] += 1
        return t
    ada_sb = P.sb("ada_sb", [128, 2048], F32)
    brow = P.sb("brow", [128, 2048], F32)
    for l in range(n_layers):
        for ng in range(3):
            P.dma("sp", brow[:], A["b_ada"][l][:, ng * 2048:(ng + 1) * 2048].to_broadcast([128, 2048]), writes=[brow])
            for k in range(KD):
                st = next_wst()
                P.dma("sp", st[:], A["w_ada"][l, k * 128:(k + 1) * 128, ng * 2048:(ng + 1) * 2048], writes=[st])
                for j in range(4):
                    P.op("pe", lambda e, st=st, j=j, k=k: e.matmul(FB[j][:], lhsT=C_bc[:, k, :], rhs=st[:, j * 512:(j + 1) * 512],
                                                                 start=(k == 0), stop=(k == KD - 1)), reads=[st, C_bc], writes=[FB[j]])
            for j in range(4):
                P.op("dve", lambda e, j=j: e.tensor_tensor(out=ada_sb[:, j * 512:(j + 1) * 512], in0=FB[j][:], in1=brow[:, j * 512:(j + 1) * 512], op=ALU.add),
                     reads=[FB[j], brow], writes=[ada_sb])
            for half in range(2):
                seg = 2 * ng + half
                src = ada_sb[:, half * 1024:(half + 1) * 1024]
                if seg in (2, 5):
                    P.dma("sp", g12[l, 0 if seg == 2 else 1], src, reads=[ada_sb], writes=[g12])
                else:
                    v = {0: 1, 1: 0, 3: 3, 4: 2}[seg]
                    for k in range(KD):
                        P.op("pe", lambda e, half=half, k=k: e.transpose(FB[4][:, 0:128], ada_sb[:, half * 1024 + k * 128: half * 1024 + (k + 1) * 128], identf[:]),
                             reads=[ada_sb, identf], writes=[FB[4]])
                        if seg in (1, 4):
                            nm = 0 if seg == 1 else 1
                            P.op("dve", lambda e, l=l, v=v, k=k, nm=nm: e.scalar_tensor_tensor(
                                out=modc[:, l, v, k:k + 1], in0=FB[4][:, 0:1], scalar=1.0, in1=nmc[:, l, nm, k:k + 1],
                                op0=ALU.add, op1=ALU.mult), reads=[FB[4], nmc], writes=[modc])
                        else:
                            P.op("dve", lambda e, l=l, v=v, k=k: e.tensor_copy(out=modc[:, l, v, k:k + 1], in_=FB[4][:, 0:1]),
                                 reads=[FB[4]], writes=[modc])

    posi = P.sb("posi", [128, NT], I32)
    posf = P.sb("posf", [128, NT], F32)
    inv_bc = P.sb("inv_bcs", [128, 16], F32)
    ang = P.sb("ang", [128, NT, 16], F32)
    kk = P.sb("kk", [128, NT, 16], F32)
    P.dma("sp", posi[:], pos_in[:], writes=[posi])
    P.dma("sp", inv_bc[:], inv_in[:], writes=[inv_bc])
    P.op("dve", lambda e: e.tensor_copy(out=posf[:], in_=posi[:]), reads=[posi], writes=[posf])
    for t in range(NT):
        P.op("dve", lambda e, t=t: e.tensor_scalar(out=ang[:, t, :], in0=inv_bc[:], scalar1=posf[:, t:t + 1], scalar2=None, op0=ALU.mult),
             reads=[inv_bc, posf], writes=[ang])

    MAGIC = 12582912.0
    C1 = 6.28125
    C2 = 2.0 * math.pi - C1

    def sin_reduced(dst, src_t, src_v, shape_v, shift, tmp_t):
        tv = tmp_t[:] if shape_v is None else shape_v(tmp_t)
        P.op("dve", lambda e: e.tensor_scalar(out=tv, in0=src_v, scalar1=shift, scalar2=1.0 / (2 * math.pi), op0=ALU.add, op1=ALU.mult),
             reads=[src_t], writes=[tmp_t])
        P.op("dve", lambda e: e.tensor_scalar(out=tv, in0=tv, scalar1=MAGIC, scalar2=None, op0=ALU.add), reads=[tmp_t], writes=[tmp_t])
        P.op("dve", lambda e: e.tensor_scalar(out=tv, in0=tv, scalar1=-MAGIC, scalar2=None, op0=ALU.add), reads=[tmp_t], writes=[tmp_t])
        P.op("dve", lambda e: e.scalar_tensor_tensor(out=dst[0][:] if shape_v is None else shape_v(dst[0]), in0=tv, scalar=-C1, in1=src_v,
                                                     op0=ALU.mult, op1=ALU.add), reads=[tmp_t, src_t], writes=[dst[0]])
        dv = dst[0][:] if shape_v is None else shape_v(dst[0])
        P.op("dve", lambda e: e.scalar_tensor_tensor(out=dv, in0=tv, scalar=-C2, in1=dv, op0=ALU.mult, op1=ALU.add),
             reads=[tmp_t, dst[0]], writes=[dst[0]])
        P.op("dve", lambda e: e.tensor_scalar(out=dv, in0=dv, scalar1=shift, scalar2=3.14159, op0=ALU.add, op1=ALU.min), reads=[dst[0]], writes=[dst[0]])
        P.op("dve", lambda e: e.tensor_scalar(out=dv, in0=dv, scalar1=-3.14159, scalar2=None, op0=ALU.max), reads=[dst[0]], writes=[dst[0]])
        P.op("act", lambda e: e.activation(out=dv, in_=dv, func=AF.Sin), reads=[dst[0]], writes=[dst[0]])

    sin_reduced((sinT,), ang, ang[:], None, 0.0, kk)
    sin_reduced((cosT,), ang, ang[:], None, math.pi / 2, kk)
    onesrow = P.sb("onesrow", [NH, L], BF16)
    P.op("pool", lambda e: e.memset(onesrow[:], 1.0), writes=[onesrow])
    for r_ in (66, 67):
        P.dma("sp", QTf[:, r_, :], onesrow[:], reads=[onesrow], writes=[QTf])
    for r_ in (64, 65):
        P.dma("sp", KTf[:, r_, :], onesrow[:], reads=[onesrow], writes=[KTf])
    P.pop()

    def load_layer(l):
        lp = lambda n: A[n][l]
        P.dma("pool", w_in_sb[:], lp("w_in").rearrange("(k p) n -> p k n", p=128), writes=[w_in_sb])
        P.dma("pool", w_uq_sb[:], lp("w_uq").rearrange("(k p) n -> p k n", p=128), writes=[w_uq_sb])
        P.dma("pool", w_ukv_sb[:], lp("w_ukv"), writes=[w_ukv_sb])
        P.dma("pool", w_glu_sb[:], lp("w_glu").rearrange("(k p) n -> p k n", p=128), writes=[w_glu_sb])
        for t_, n_ in ((qn_c, "q_norm"), (kvn_c, "kv_norm"), (gqm, "gq_m"), (gkm, "gk_m"), (gqf, "gq_f"), (gkf, "gk_f"),
                       (dcol, "ssm_d"), (bglu_c, "b_glu")):
            P.dma("sp", t_[:], lp(n_), writes=[t_])
        P.dma("sp", nbf[:], lp("fox_bf"), writes=[nbf])
        P.op("dve", lambda e: e.tensor_scalar(out=nbf[:], in0=nbf[:], scalar1=-1.0, scalar2=None, op0=ALU.mult), reads=[nbf], writes=[nbf])
        P.op("dve", lambda e: e.tensor_scalar(out=nbglu_c[:], in0=bglu_c[:], scalar1=-1.0, scalar2=None, op0=ALU.mult), reads=[bglu_c], writes=[nbglu_c])
        for m in range(2):
            P.op("dve", lambda e, m=m: e.tensor_scalar(out=Ddiag[:, m, :], in0=identf[:], scalar1=dcol[:, m:m + 1], scalar2=None, op0=ALU.mult),
                 reads=[identf, dcol], writes=[Ddiag])
        V = lambda i: s5p[:, i, :]
        LRE, LIM, LDT, DT, ZRE, ZIM, MAG, SN, CS, LBR, LBI, DEN, KR, KI, NKI, T1, T2, CK, SK, CK2, SK2 = range(21)
        P.dma("sp", V(LRE), lp("lam_re"), writes=[s5p])
        P.dma("sp", V(LIM), lp("lam_im"), writes=[s5p])
        P.dma("sp", V(LDT), lp("log_dt"), writes=[s5p])
        sop = lambda fn: P.op("dve", fn, reads=[s5p], writes=[s5p])
        P.op("act", lambda e: e.activation(out=V(DT), in_=V(LDT), func=AF.Exp), reads=[s5p], writes=[s5p])
        sop(lambda e: e.tensor_tensor(out=V(ZRE), in0=V(LRE), in1=V(DT), op=ALU.mult))
        sop(lambda e: e.tensor_tensor(out=V(ZIM), in0=V(LIM), in1=V(DT), op=ALU.mult))
        P.op("act", lambda e: e.activation(out=V(MAG), in_=V(ZRE), func=AF.Exp), reads=[s5p], writes=[s5p])
        sv = lambda i: (lambda t: t[:, i, :])
        sin_reduced((s5p,), s5p, V(ZIM), sv(SN), 0.0, s5p) if False else None
        for dst_i, shift in ((SN, 0.0), (CS, math.pi / 2)):
            sop(lambda e, shift=shift: e.tensor_scalar(out=V(T1), in0=V(ZIM), scalar1=shift, scalar2=1.0 / (2 * math.pi), op0=ALU.add, op1=ALU.mult))
            sop(lambda e: e.tensor_scalar(out=V(T1), in0=V(T1), scalar1=MAGIC, scalar2=None, op0=ALU.add))
            sop(lambda e: e.tensor_scalar(out=V(T1), in0=V(T1), scalar1=-MAGIC, scalar2=None, op0=ALU.add))
            sop(lambda e, dst_i=dst_i: e.scalar_tensor_tensor(out=V(dst_i), in0=V(T1), scalar=-C1, in1=V(ZIM), op0=ALU.mult, op1=ALU.add))
            sop(lambda e, dst_i=dst_i: e.scalar_tensor_tensor(out=V(dst_i), in0=V(T1), scalar=-C2, in1=V(dst_i), op0=ALU.mult, op1=ALU.add))
            sop(lambda e, dst_i=dst_i, shift=shift: e.tensor_scalar(out=V(dst_i), in0=V(dst_i), scalar1=shift, scalar2=3.14159, op0=ALU.add, op1=ALU.min))
            sop(lambda e, dst_i=dst_i: e.tensor_scalar(out=V(dst_i), in0=V(dst_i), scalar1=-3.14159, scalar2=None, op0=ALU.max))
            P.op("act", lambda e, dst_i=dst_i: e.activation(out=V(dst_i), in_=V(dst_i), func=AF.Sin), reads=[s5p], writes=[s5p])
        sop(lambda e: e.tensor_tensor(out=V(LBR), in0=V(MAG), in1=V(CS), op=ALU.mult))
        sop(lambda e: e.tensor_tensor(out=V(LBI), in0=V(MAG), in1=V(SN), op=ALU.mult))
        sop(lambda e: e.tensor_tensor(out=V(DEN), in0=V(LRE), in1=V(LRE), op=ALU.mult))
        sop(lambda e: e.tensor_tensor(out=V(T1), in0=V(LIM), in1=V(LIM), op=ALU.mult))
        sop(lambda e: e.tensor_tensor(out=V(DEN), in0=V(DEN), in1=V(T1), op=ALU.add))
        sop(lambda e: e.reciprocal(out=V(DEN), in_=V(DEN)))
        sop(lambda e: e.tensor_scalar(out=V(T2), in0=V(LBR), scalar1=-1.0, scalar2=None, op0=ALU.add))
        sop(lambda e: e.tensor_tensor(out=V(KR), in0=V(T2), in1=V(LRE), op=ALU.mult))
        sop(lambda e: e.tensor_tensor(out=V(T1), in0=V(LBI), in1=V(LIM), op=ALU.mult))
        sop(lambda e: e.tensor_tensor(out=V(KR), in0=V(KR), in1=V(T1), op=ALU.add))
        sop(lambda e: e.tensor_tensor(out=V(KR), in0=V(KR), in1=V(DEN), op=ALU.mult))
        sop(lambda e: e.tensor_tensor(out=V(KI), in0=V(LBI), in1=V(LRE), op=ALU.mult))
        sop(lambda e: e.tensor_tensor(out=V(T1), in0=V(T2), in1=V(LIM), op=ALU.mult))
        sop(lambda e: e.tensor_tensor(out=V(KI), in0=V(KI), in1=V(T1), op=ALU.subtract))
        sop(lambda e: e.tensor_tensor(out=V(KI), in0=V(KI), in1=V(DEN), op=ALU.mult))
        sop(lambda e: e.tensor_scalar(out=V(NKI), in0=V(KI), scalar1=-1.0, scalar2=None, op0=ALU.mult))
        for j in range(8):
            P.dma("sp", blk_f[:, 0, :], lp("b_re")[j], writes=[blk_f])
            P.dma("sp", blk_f[:, 1, :], lp("b_im")[j], writes=[blk_f])
            P.op("dve", lambda e, j=j: e.tensor_scalar(out=blk_o[:, 0, :], in0=blk_f[:, 0, :], scalar1=s5p[:, KR, j:j + 1], scalar2=None, op0=ALU.mult),
                 reads=[blk_f, s5p], writes=[blk_o])
            P.op("dve", lambda e, j=j: e.scalar_tensor_tensor(out=blk_o[:, 0, :], in0=blk_f[:, 1, :], scalar=s5p[:, NKI, j:j + 1], in1=blk_o[:, 0, :],
                                                              op0=ALU.mult, op1=ALU.add), reads=[blk_f, s5p, blk_o], writes=[blk_o])
            P.op("dve", lambda e, j=j: e.tensor_scalar(out=blk_o[:, 1, :], in0=blk_f[:, 1, :], scalar1=s5p[:, KR, j:j + 1], scalar2=None, op0=ALU.mult),
                 reads=[blk_f, s5p], writes=[blk_o])
            P.op("dve", lambda e, j=j: e.scalar_tensor_tensor(out=blk_o[:, 1, :], in0=blk_f[:, 0, :], scalar=s5p[:, KI, j:j + 1], in1=blk_o[:, 1, :],
                                                              op0=ALU.mult, op1=ALU.add), reads=[blk_f, s5p, blk_o], writes=[blk_o])
            for ri, dstT in ((0, BreT), (1, BimT)):
                P.op("pe", lambda e, ri=ri: e.transpose(FB[5][:, 0:128], blk_o[:, ri, :], identf[:]), reads=[blk_o, identf], writes=[FB[5]])
                P.op("act", lambda e, dstT=dstT, j=j: e.activation(out=dstT[:, j, :], in_=FB[5][:, 0:128], func=AF.Copy), reads=[FB[5]], writes=[dstT])
        P.dma("pool", CreT[:], lp("c_re").rearrange("j p f -> p j f"), writes=[CreT])
        P.dma("pool", nCimT[:], lp("c_im").rearrange("j p f -> p j f"), writes=[nCimT])
        P.op("dve", lambda e: e.tensor_scalar(out=nCimT[:], in0=nCimT[:], scalar1=-1.0, scalar2=None, op0=ALU.mult), reads=[nCimT], writes=[nCimT])
        P.op("dve", lambda e: e.memset(Ctab[:, :, 0:1], 1.0), writes=[Ctab])
        P.op("dve", lambda e: e.memset(Stab[:, :, 0:1], 0.0), writes=[Stab])
        sop(lambda e: e.tensor_copy(out=V(CK), in_=V(CS)))
        sop(lambda e: e.tensor_copy(out=V(SK), in_=V(SN)))
        n = 1
        while n < SUB:
            tabt_v = pt[0][:].rearrange("p a b -> p (a b)")[:, 0:8 * n].rearrange("p (j n) -> p j n", j=8)
            tabu_v = pt[1][:].rearrange("p a b -> p (a b)")[:, 0:8 * n].rearrange("p (j n) -> p j n", j=8)
            tabt = pt[0]
            tabu = pt[1]
            ckb = s5p[:, CK, :].unsqueeze(2).to_broadcast([128, 8, n])
            skb = s5p[:, SK, :].unsqueeze(2).to_broadcast([128, 8, n])
            P.op("dve", lambda e, n=n, ckb=ckb: e.tensor_tensor(out=tabt_v, in0=Ctab[:, :, 0:n], in1=ckb, op=ALU.mult), reads=[Ctab, s5p], writes=[tabt])
            P.op("dve", lambda e, n=n, skb=skb: e.tensor_tensor(out=tabu_v, in0=Stab[:, :, 0:n], in1=skb, op=ALU.mult), reads=[Stab, s5p], writes=[tabu])
            P.op("dve", lambda e, n=n: e.tensor_tensor(out=Ctab[:, :, n:2 * n], in0=tabt_v, in1=tabu_v, op=ALU.subtract), reads=[tabt, tabu], writes=[Ctab])
            P.op("dve", lambda e, n=n, skb=skb: e.tensor_tensor(out=tabt_v, in0=Ctab[:, :, 0:n], in1=skb, op=ALU.mult), reads=[Ctab, s5p], writes=[tabt])
            P.op("dve", lambda e, n=n, ckb=ckb: e.tensor_tensor(out=tabu_v, in0=Stab[:, :, 0:n], in1=ckb, op=ALU.mult), reads=[Stab, s5p], writes=[tabu])
            P.op("dve", lambda e, n=n: e.tensor_tensor(out=Stab[:, :, n:2 * n], in0=tabt_v, in1=tabu_v, op=ALU.add), reads=[tabt, tabu], writes=[Stab])
            sop(lambda e: e.tensor_tensor(out=V(T1), in0=V(CK), in1=V(CK), op=ALU.mult))
            sop(lambda e: e.tensor_tensor(out=V(T2), in0=V(SK), in1=V(SK), op=ALU.mult))
            sop(lambda e: e.tensor_tensor(out=V(SK2), in0=V(CK), in1=V(SK), op=ALU.mult))
            sop(lambda e: e.tensor_tensor(out=V(CK), in0=V(T1), in1=V(T2), op=ALU.subtract))
            sop(lambda e: e.tensor_scalar(out=V(SK), in0=V(SK2), scalar1=2.0, scalar2=None, op0=ALU.mult))
            n *= 2
        P.op("dve", lambda e: e.tensor_copy(out=Rtab[:], in_=s5p[:, MAG, :].unsqueeze(2).to_broadcast([128, 8, SUB])), reads=[s5p], writes=[Rtab])
        return dict(CK=CK, SK=SK)

    def load_c(l):
        P.dma("sp", onorm_c[:], A["out_norm"][l], writes=[onorm_c])
        P.dma("sp", G_bc[:], g12[l, 0], reads=[g12], writes=[G_bc])
        for k in range(KD):
            st = wstc[k % 2]
            P.dma("sp", st[:], A["w_out"][l][k * 128:(k + 1) * 128, :], writes=[st])
            P.op("dve", lambda e, st=st, k=k: e.scalar_tensor_tensor(out=w_out_sb[:, k, :], in0=st[:], scalar=onorm_c[:, k:k + 1], in1=G_bc[:],
                                                                     op0=ALU.mult, op1=ALU.mult), reads=[st, onorm_c, G_bc], writes=[w_out_sb])

    def norm_transpose(src_t, src_v, l, v_a, v_b, dst_t, dst_slice, si, fp32_path=None):
        st_ = stat[si % 4]
        P.op("act", lambda e: e.activation(out=sq_junk[:], in_=src_v, func=AF.Square, accum_out=st_[:, 0:1]), reads=[src_t], writes=[sq_junk, st_])
        P.op("act", lambda e: e.activation(out=st_[:, 1:2], in_=st_[:, 0:1], func=AF.Sqrt, scale=1.0 / D, bias=eps_c[:, 0:1]), reads=[st_, eps_c], writes=[st_])
        P.op("dve", lambda e: e.reciprocal(out=st_[:, 2:3], in_=st_[:, 1:2]), reads=[st_], writes=[st_])
        xh_ = xh[si % 2]
        P.op("dve", lambda e: e.tensor_scalar(out=xh_[:], in0=src_v, scalar1=st_[:, 2:3], scalar2=None, op0=ALU.mult), reads=[src_t, st_], writes=[xh_])
        tb = TB[si % 2]
        for k in range(KD):
            P.op("pe", lambda e, k=k: e.transpose(tb[:, k * 128:(k + 1) * 128], xh_[:, k * 128:(k + 1) * 128], ident[:]), reads=[xh_, ident], writes=[tb])
        for k in range(KD):
            P.op("act", lambda e, k=k: e.activation(out=dst_t[:, k, dst_slice], in_=tb[:, k * 128:(k + 1) * 128], func=AF.Identity,
                                                    scale=modc[:, l, v_a, k:k + 1], bias=modc[:, l, v_b, k:k + 1]), reads=[tb, modc], writes=[dst_t])
        if fp32_path is not None:
            xhf_, dstf = fp32_path
            P.op("dve", lambda e: e.tensor_scalar(out=xhf_[:], in0=src_v, scalar1=st_[:, 2:3], scalar2=None, op0=ALU.mult), reads=[src_t, st_], writes=[xhf_])
            for k in range(KD):
                fb = FB[k % 2]
                P.op("pe", lambda e, k=k, fb=fb: e.transpose(fb[:, 0:128], xhf_[:, k * 128:(k + 1) * 128], identf[:]), reads=[xhf_, identf], writes=[fb])
                P.op("act", lambda e, k=k, fb=fb: e.activation(out=dstf[:, k, :], in_=fb[:, 0:128], func=AF.Identity,
                                                             scale=modc[:, l, v_a, k:k + 1], bias=modc[:, l, v_b, k:k + 1]), reads=[fb, modc], writes=[dstf])

    def head_rms(src_t, src_v3, nh, hd, gain_t, dst_t, dst_v3, si, extra_ss=None):
        st_ = stat[si % 4]
        P.op("act", lambda e: e.activation(out=sq_junk[:, 0:nh * hd].rearrange("p (h d) -> p h d", h=nh), in_=src_v3, func=AF.Square), reads=[src_t], writes=[sq_junk])
        P.op("dve", lambda e: e.tensor_reduce(out=st_[:, 0:nh], in_=sq_junk[:, 0:nh * hd].rearrange("p (h d) -> p h d", h=nh), axis=AX.X, op=ALU.add),
             reads=[sq_junk], writes=[st_])
        tot = hd
        if extra_ss is not None:
            et, ev, en = extra_ss
            P.op("dve", lambda e: e.tensor_scalar(out=st_[:, 0:nh], in0=st_[:, 0:nh], scalar1=ev, scalar2=None, op0=ALU.add), reads=[st_, et], writes=[st_])
            tot = hd + en
        P.op("act", lambda e: e.activation(out=st_[:, 6:6 + nh], in_=st_[:, 0:nh], func=AF.Sqrt, scale=1.0 / tot, bias=eps_c[:, 0:1]), reads=[st_, eps_c], writes=[st_])
        P.op("dve", lambda e: e.reciprocal(out=st_[:, 6:6 + nh], in_=st_[:, 6:6 + nh]), reads=[st_], writes=[st_])
        return st_

    def rope(src_t, dst_t, cos_v, sin_v):
        x1 = src_t[:, :, 64:80]
        x2 = src_t[:, :, 80:96]
        cb = cos_v.unsqueeze(1).to_broadcast([128, NH, 16])
        sb_ = sin_v.unsqueeze(1).to_broadcast([128, NH, 16])
        P.op("dve", lambda e: e.tensor_copy(out=dst_t[:, :, 0:64], in_=src_t[:, :, 0:64]), reads=[src_t], writes=[dst_t])
        P.op("dve", lambda e: e.tensor_tensor(out=rt[0][:], in0=x1, in1=cb, op=ALU.mult), reads=[src_t, cosT], writes=[rt[0]])
        P.op("dve", lambda e: e.tensor_tensor(out=rt[1][:], in0=x2, in1=sb_, op=ALU.mult), reads=[src_t, sinT], writes=[rt[1]])
        P.op("dve", lambda e: e.tensor_tensor(out=dst_t[:, :, 64:80], in0=rt[0][:], in1=rt[1][:], op=ALU.subtract), reads=[rt[0], rt[1]], writes=[dst_t])
        P.op("dve", lambda e: e.tensor_tensor(out=rt[2][:], in0=x1, in1=sb_, op=ALU.mult), reads=[src_t, sinT], writes=[rt[2]])
        P.op("dve", lambda e: e.tensor_tensor(out=rt[3][:], in0=x2, in1=cb, op=ALU.mult), reads=[src_t, cosT], writes=[rt[3]])
        P.op("dve", lambda e: e.tensor_tensor(out=dst_t[:, :, 80:96], in0=rt[2][:], in1=rt[3][:], op=ALU.add), reads=[rt[2], rt[3]], writes=[dst_t])

    def phase_a(l, s5c):
        src = x_in if l == 0 else None
        for b in range(NB):
            hTb = hT[b % 2]
            for ti in range(4):
                t = b * 4 + ti
                xs_ = xs[t % 2]
                if l == 0:
                    P.dma("sp", xs_[:], x_in[t * 128:(t + 1) * 128, :], writes=[xs_])
                else:
                    P.dma("sp", xs_[:], y[t * 128:(t + 1) * 128, :], reads=[ytile[t]], writes=[xs_])
                norm_transpose(xs_, xs_[:], l, 0, 1, hTb, slice(ti * 128, (ti + 1) * 128), t)
            for m in range(2):
                for k in range(KD):
                    P.op("pe", lambda e, m=m, k=k: e.matmul(FB[m][:], lhsT=w_in_sb[:, k, m * 128:(m + 1) * 128], rhs=hTb[:, k, :], start=(k == 0), stop=(k == KD - 1)),
                         reads=[w_in_sb, hTb], writes=[FB[m]])
                P.op("act", lambda e, m=m: e.activation(out=uT[:, m, :], in_=FB[m][:], func=AF.Copy), reads=[FB[m]], writes=[uT])
            for k in range(KD):
                P.op("pe", lambda e, k=k: e.matmul(FB[2][0:NH, :], lhsT=w_in_sb[:, k, 1824:1830], rhs=hTb[:, k, :], start=(k == 0), stop=(k == KD - 1)),
                     reads=[w_in_sb, hTb], writes=[FB[2]])
            P.op("act", lambda e: e.activation(out=fg_e[:], in_=FB[2][0:NH, :], func=AF.Exp, scale=-1.0, bias=nbf[:, 0:1]), reads=[FB[2], nbf], writes=[fg_e])
            P.op("act", lambda e: e.activation(out=fg_sp[:], in_=fg_e[:], func=AF.Ln, bias=ones_f[0:NH, 0:1]), reads=[fg_e, ones_f], writes=[fg_sp])
            if b == 0:
                P.op("dve", lambda e: e.tensor_tensor_scan(out=fg_cum[:], data0=ones_f[0:NH, 0:512], data1=fg_sp[:], initial=0.0, op0=ALU.mult, op1=ALU.add),
                     reads=[ones_f, fg_sp], writes=[fg_cum])
            else:
                P.op("dve", lambda e: e.tensor_tensor_scan(out=fg_cum[:], data0=ones_f[0:NH, 0:512], data1=fg_sp[:], initial=fg_carry[:, 0:1], op0=ALU.mult, op1=ALU.add),
                     reads=[ones_f, fg_sp, fg_carry], writes=[fg_cum])
            P.op("dve", lambda e: e.tensor_copy(out=fg_carry[:], in_=fg_cum[:, 511:512]), reads=[fg_cum], writes=[fg_carry])
            P.op("dve", lambda e: e.tensor_scalar(out=fg_sp[:], in0=fg_cum[:], scalar1=8.0, scalar2=None, op0=ALU.mult), reads=[fg_cum], writes=[fg_sp])
            P.op("dve", lambda e: e.tensor_copy(out=fg_hi[:], in_=fg_sp[:]), reads=[fg_sp], writes=[fg_hi])
            P.op("dve", lambda e: e.tensor_tensor(out=fg_lo[:], in0=fg_sp[:], in1=fg_hi[:], op=ALU.subtract), reads=[fg_sp, fg_hi], writes=[fg_lo])
            P.op("dve", lambda e: e.tensor_scalar(out=fg_nhi[:], in0=fg_hi[:], scalar1=-1.0, scalar2=None, op0=ALU.mult), reads=[fg_hi], writes=[fg_nhi])
            P.op("dve", lambda e: e.tensor_scalar(out=fg_nlo[:], in0=fg_lo[:], scalar1=-1.0, scalar2=None, op0=ALU.mult), reads=[fg_lo], writes=[fg_nlo])
            bs = slice(b * 512, (b + 1) * 512)
            P.dma("sp", QTf[:, 64, bs], fg_nhi[:], reads=[fg_nhi], writes=[QTf])
            P.dma("sp", QTf[:, 65, bs], fg_nlo[:], reads=[fg_nlo], writes=[QTf])
            P.dma("sp", KTf[:, 66, bs], fg_hi[:], reads=[fg_hi], writes=[KTf])
            P.dma("sp", KTf[:, 67, bs], fg_lo[:], reads=[fg_lo], writes=[KTf])

            for ti in range(4):
                t = b * 4 + ti
                tsl = slice(ti * 128, (ti + 1) * 128)
                segs = ((FB[2], 256, 416), (FB[3], 672, 384), (FB[4], 1056, 384), (FB[5], 1440, 384))
                for fb, c0, w in segs:
                    for k in range(KD):
                        P.op("pe", lambda e, fb=fb, c0=c0, w=w, k=k: e.matmul(fb[:, 0:w], lhsT=hTb[:, k, tsl], rhs=w_in_sb[:, k, c0:c0 + w], start=(k == 0), stop=(k == KD - 1)),
                             reads=[hTb, w_in_sb], writes=[fb])
                st_ = stat[0]
                P.op("act", lambda e: e.activation(out=sq_junk[:, 0:256], in_=FB[2][:, 0:256], func=AF.Square, accum_out=st_[:, 12:13]), reads=[FB[2]], writes=[sq_junk, st_])
                P.op("act", lambda e: e.activation(out=st_[:, 13:14], in_=st_[:, 12:13], func=AF.Sqrt, scale=1.0 / 256, bias=eps_c[:, 0:1]), reads=[st_, eps_c], writes=[st_])
                P.op("dve", lambda e: e.reciprocal(out=st_[:, 13:14], in_=st_[:, 13:14]), reads=[st_], writes=[st_])
                P.op("dve", lambda e: e.tensor_scalar(out=cq_h[:], in0=FB[2][:, 0:256], scalar1=st_[:, 13:14], scalar2=None, op0=ALU.mult), reads=[FB[2], st_], writes=[cq_h])
                st1 = stat[1]
                P.op("act", lambda e: e.activation(out=sq_junk[:, 256:384], in_=FB[2][:, 256:384], func=AF.Square, accum_out=st1[:, 12:13]), reads=[FB[2]], writes=[sq_junk, st1])
                P.op("act", lambda e: e.activation(out=st1[:, 13:14], in_=st1[:, 12:13], func=AF.Sqrt, scale=1.0 / 128, bias=eps_c[:, 0:1]), reads=[st1, eps_c], writes=[st1])
                P.op("dve", lambda e: e.reciprocal(out=st1[:, 13:14], in_=st1[:, 13:14]), reads=[st1], writes=[st1])
                P.op("dve", lambda e: e.tensor_scalar(out=ckv_h[:], in0=FB[2][:, 256:384], scalar1=st1[:, 13:14], scalar2=None, op0=ALU.mult), reads=[FB[2], st1], writes=[ckv_h])
                P.op("act", lambda e: e.activation(out=kn[:, 0, 64:96], in_=FB[2][:, 384:416], func=AF.Copy), reads=[FB[2]], writes=[kn])
                P.op("act", lambda e: e.activation(out=sq_junk[:, 384:416], in_=FB[2][:, 384:416], func=AF.Square, accum_out=st1[:, 14:15]), reads=[FB[2]], writes=[sq_junk, st1])
                tb = TB[0]
                for j in range(2):
                    P.op("pe", lambda e, j=j: e.transpose(tb[:, j * 128:(j + 1) * 128], cq_h[:, j * 128:(j + 1) * 128], ident[:]), reads=[cq_h, ident], writes=[tb])
                P.op("pe", lambda e: e.transpose(tb[:, 256:384], ckv_h[:], ident[:]), reads=[ckv_h, ident], writes=[tb])
                for j in range(2):
                    P.op("act", lambda e, j=j: e.activation(out=cqT[:, j, :], in_=tb[:, j * 128:(j + 1) * 128], func=AF.Copy, scale=qn_c[:, j:j + 1]), reads=[tb, qn_c], writes=[cqT])
                P.op("act", lambda e: e.activation(out=ckvT[:], in_=tb[:, 256:384], func=AF.Copy, scale=kvn_c[:, 0:1]), reads=[tb, kvn_c], writes=[ckvT])
                for j in range(2):
                    P.op("pe", lambda e, j=j: e.matmul(FB[0][:], lhsT=cqT[:, j, :], rhs=w_uq_sb[:, j, 0:512], start=(j == 0), stop=(j == 1)), reads=[cqT, w_uq_sb], writes=[FB[0]])
                for j in range(2):
                    P.op("pe", lambda e, j=j: e.matmul(FB[1][:, 0:64], lhsT=cqT[:, j, :], rhs=w_uq_sb[:, j, 512:576], start=(j == 0), stop=(j == 1)), reads=[cqT, w_uq_sb], writes=[FB[1]])
                P.op("act", lambda e: e.activation(out=qn[:].rearrange("p h d -> p (h d)")[:, 0:512], in_=FB[0][:], func=AF.Copy), reads=[FB[0]], writes=[qn])
                P.op("act", lambda e: e.activation(out=qn[:].rearrange("p h d -> p (h d)")[:, 512:576], in_=FB[1][:, 0:64], func=AF.Copy), reads=[FB[1]], writes=[qn])
                sq = head_rms(qn, qn[:], NH, 96, gqm, None, None, 2)
                P.op("dve", lambda e, sq=sq: e.tensor_tensor(out=qn[:], in0=qn[:], in1=sq[:, 6:12].unsqueeze(2).to_broadcast([128, NH, 96]), op=ALU.mult), reads=[qn, sq], writes=[qn])
                P.op("dve", lambda e: e.tensor_tensor(out=qn[:], in0=qn[:], in1=gqm[:].unsqueeze(1).to_broadcast([128, NH, 96]), op=ALU.mult), reads=[qn, gqm], writes=[qn])
                rope(qn, qfin, cosT[:, t, :], sinT[:, t, :])
                P.op("pe", lambda e: e.matmul(FB[0][:], lhsT=ckvT[:], rhs=w_ukv_sb[:, 0:512], start=True, stop=True), reads=[ckvT, w_ukv_sb], writes=[FB[0]])
                P.op("pe", lambda e: e.matmul(FB[1][:, 0:256], lhsT=ckvT[:], rhs=w_ukv_sb[:, 512:768], start=True, stop=True), reads=[ckvT, w_ukv_sb], writes=[FB[1]])
                vst = Vm_st[t % 2]
                kv0 = FB[0][:].rearrange("p (h d) -> p h d", h=4)
                kv1 = FB[1][:, 0:256].rearrange("p (h d) -> p h d", h=2)
                P.op("act", lambda e: e.activation(out=kn[:, 0:4, 0:64], in_=kv0[:, :, 0:64], func=AF.Copy), reads=[FB[0]], writes=[kn])
                P.op("act", lambda e: e.activation(out=kn[:, 4:6, 0:64], in_=kv1[:, :, 0:64], func=AF.Copy), reads=[FB[1]], writes=[kn])
                P.op("dve", lambda e: e.tensor_copy(out=vst[:, 0:4, 0:64], in_=kv0[:, :, 64:128]), reads=[FB[0]], writes=[vst])
                P.op("dve", lambda e: e.tensor_copy(out=vst[:, 4:6, 0:64], in_=kv1[:, :, 64:128]), reads=[FB[1]], writes=[vst])
                P.dma("sp", Vm[t * 128:(t + 1) * 128, :], vst[:].rearrange("p h d -> p (h d)"), reads=[vst], writes=[Vm])
                P.op("dve", lambda e: e.tensor_copy(out=kn[:, 1:6, 64:96], in_=kn[:, 0:1, 64:96].to_broadcast([128, 5, 32])), reads=[kn], writes=[kn])
                st3 = stat[3]
                P.op("act", lambda e: e.activation(out=sq_junk[:, 0:384].rearrange("p (h d) -> p h d", h=NH), in_=kn[:, :, 0:64], func=AF.Square), reads=[kn], writes=[sq_junk])
                P.op("dve", lambda e: e.tensor_reduce(out=st3[:, 0:NH], in_=sq_junk[:, 0:384].rearrange("p (h d) -> p h d", h=NH), axis=AX.X, op=ALU.add), reads=[sq_junk], writes=[st3])
                P.op("dve", lambda e: e.tensor_scalar(out=st3[:, 0:NH], in0=st3[:, 0:NH], scalar1=st1[:, 14:15], scalar2=None, op0=ALU.add), reads=[st3, st1], writes=[st3])
                P.op("act", lambda e: e.activation(out=st3[:, 6:12], in_=st3[:, 0:NH], func=AF.Sqrt, scale=1.0 / 96, bias=eps_c[:, 0:1]), reads=[st3, eps_c], writes=[st3])
                P.op("dve", lambda e: e.reciprocal(out=st3[:, 6:12], in_=st3[:, 6:12]), reads=[st3], writes=[st3])
                P.op("dve", lambda e: e.tensor_tensor(out=kn[:], in0=kn[:], in1=st3[:, 6:12].unsqueeze(2).to_broadcast([128, NH, 96]), op=ALU.mult), reads=[kn, st3], writes=[kn])
                P.op("dve", lambda e: e.tensor_tensor(out=kn[:], in0=kn[:], in1=gkm[:].unsqueeze(1).to_broadcast([128, NH, 96]), op=ALU.mult), reads=[kn, gkm], writes=[kn])
                rope(kn, kfin, cosT[:, t, :], sinT[:, t, :])
                gsl = slice(t * 128, (t + 1) * 128)
                for src_, st_l, dst_d in ((qfin, QTm_st, QTm), (kfin, KTm_st, KTm)):
                    tb2 = TB[1]
                    st_t = st_l[t % 2]
                    for h in range(NH):
                        P.op("pe", lambda e, h=h, src_=src_: e.transpose(tb2[0:96, h * 128:(h + 1) * 128], src_[:, h, :], ident[:]), reads=[src_, ident], writes=[tb2])
                    P.op("act", lambda e, st_t=st_t: e.activation(out=st_t[:], in_=tb2[0:96, 0:768].rearrange("p (h t) -> p h t", h=NH), func=AF.Copy), reads=[tb2], writes=[st_t])
                    P.dma("sp", dst_d[:, :, gsl].rearrange("h p t -> p h t"), st_t[:], reads=[st_t], writes=[dst_d])
                for fb, g_t, dstb, st_l, dst_d in ((FB[3], gqf, fqb, QTf_st, QTf), (FB[4], gkf, fkb, KTf_st, KTf)):
                    st_t = st_l[t % 2]
                    P.op("act", lambda e, fb=fb: e.activation(out=fqn[:].rearrange("p h d -> p (h d)"), in_=fb[:, 0:384], func=AF.Copy), reads=[fb], writes=[fqn])
                    sq = head_rms(fqn, fqn[:], NH, 64, g_t, None, None, 2)
                    P.op("dve", lambda e, sq=sq: e.tensor_tensor(out=fqn[:], in0=fqn[:], in1=sq[:, 6:12].unsqueeze(2).to_broadcast([128, NH, 64]), op=ALU.mult), reads=[fqn, sq], writes=[fqn])
                    P.op("dve", lambda e, g_t=g_t, dstb=dstb: e.tensor_tensor(out=dstb[:], in0=fqn[:], in1=g_t[:].unsqueeze(1).to_broadcast([128, NH, 64]), op=ALU.mult), reads=[fqn, g_t], writes=[dstb])
                    tb2 = TB[1]
                    for h in range(NH):
                        P.op("pe", lambda e, h=h, dstb=dstb: e.transpose(tb2[0:64, h * 128:(h + 1) * 128], dstb[:, h, :], ident[:]), reads=[dstb, ident], writes=[tb2])
                    P.op("act", lambda e, st_t=st_t: e.activation(out=st_t[:], in_=tb2[0:64, 0:768].rearrange("p (h t) -> p h t", h=NH), func=AF.Copy), reads=[tb2], writes=[st_t])
                    P.dma("sp", dst_d[:, 0:64, gsl].rearrange("h p t -> p h t"), st_t[:], reads=[st_t], writes=[dst_d])
                vst = Vf_st[t % 2]
                P.op("dve", lambda e, vst=vst: e.tensor_copy(out=vst[:, :, 0:64], in_=FB[5][:, 0:384].rearrange("p (h d) -> p h d", h=NH)), reads=[FB[5]], writes=[vst])
                P.dma("sp", Vf[t * 128:(t + 1) * 128, :], vst[:].rearrange("p h d -> p (h d)"), reads=[vst], writes=[Vf])
            for sc in range(512 // SUB):
                first = (b == 0 and sc == 0)
                ss_ = slice(sc * SUB, (sc + 1) * SUB)
                gs = slice(b * 512 + sc * SUB, b * 512 + (sc + 1) * SUB)
                if not first:
                    wl_re = wlast[:, 0, :]
                    wl_im = wlast[:, 1, :]
                    ck = s5p[:, s5c["CK"], :]
                    sk = s5p[:, s5c["SK"], :]
                    P.op("dve", lambda e: e.tensor_tensor(out=w0t[:, 0, :], in0=wl_re, in1=ck, op=ALU.mult), reads=[wlast, s5p], writes=[w0t])
                    P.op("dve", lambda e: e.tensor_tensor(out=w0t[:, 1, :], in0=wl_im, in1=sk, op=ALU.mult), reads=[wlast, s5p], writes=[w0t])
                    P.op("dve", lambda e: e.tensor_tensor(out=w0t[:, 2, :], in0=wl_re, in1=sk, op=ALU.mult), reads=[wlast, s5p], writes=[w0t])
                    P.op("dve", lambda e: e.tensor_tensor(out=w0t[:, 3, :], in0=wl_im, in1=ck, op=ALU.mult), reads=[wlast, s5p], writes=[w0t])
                    P.op("dve", lambda e: e.tensor_tensor(out=w0[:, 0, :], in0=w0t[:, 0, :], in1=w0t[:, 1, :], op=ALU.subtract), reads=[w0t], writes=[w0])
                    P.op("dve", lambda e: e.tensor_tensor(out=w0[:, 1, :], in0=w0t[:, 2, :], in1=w0t[:, 3, :], op=ALU.add), reads=[w0t], writes=[w0])
                for m in range(2):
                    hs = slice(4 * m, 4 * m + 4)
                    for jj in range(4):
                        j = 4 * m + jj
                        fbp = FB[j % 2]
                        P.op("pe", lambda e: e.matmul(fbp[:, 0:SUB], lhsT=BreT[:, j, :], rhs=uT[:, m, ss_], start=True, stop=True), reads=[BreT, uT], writes=[fbp])
                        P.op("pe", lambda e: e.matmul(fbp[:, SUB:2 * SUB], lhsT=BimT[:, j, :], rhs=uT[:, m, ss_], start=True, stop=True), reads=[BimT, uT], writes=[fbp])
                        b2 = fbp[:, 0:2 * SUB].rearrange("p (r t) -> p r t", r=2)
                        cb = Ctab[:, j, :].unsqueeze(1).to_broadcast([128, 2, SUB])
                        sb_ = Stab[:, j, :].unsqueeze(1).to_broadcast([128, 2, SUB])
                        P.op("dve", lambda e: e.tensor_tensor(out=pre_c[:], in0=b2, in1=cb, op=ALU.mult), reads=[fbp, Ctab], writes=[pre_c])
                        P.op("dve", lambda e: e.tensor_tensor(out=pre_s[:], in0=b2, in1=sb_, op=ALU.mult), reads=[fbp, Stab], writes=[pre_s])
                        P.op("dve", lambda e: e.tensor_tensor(out=pin_re[:], in0=pre_c[:, 0, :], in1=pre_s[:, 1, :], op=ALU.add), reads=[pre_c, pre_s], writes=[pin_re])
                        P.op("dve", lambda e: e.tensor_tensor(out=pin_im[:], in0=pre_c[:, 1, :], in1=pre_s[:, 0, :], op=ALU.subtract), reads=[pre_c, pre_s], writes=[pin_im])
                        for pin, Wt, ri in ((pin_re, W_re, 0), (pin_im, W_im, 1)):
                            if first:
                                P.op("dve", lambda e: e.tensor_tensor_scan(out=Wt[:, jj, :], data0=Rtab[:, j, :], data1=pin[:], initial=0.0, op0=ALU.mult, op1=ALU.add),
                                     reads=[Rtab, pin], writes=[Wt])
                            else:
                                P.op("dve", lambda e: e.tensor_tensor_scan(out=Wt[:, jj, :], data0=Rtab[:, j, :], data1=pin[:], initial=w0[:, ri, j:j + 1],
                                                                           op0=ALU.mult, op1=ALU.add), reads=[Rtab, pin, w0], writes=[Wt])
                    P.op("dve", lambda e: e.tensor_copy(out=wlast[:, 0, hs], in_=W_re[:, :, SUB - 1]), reads=[W_re], writes=[wlast])
                    P.op("dve", lambda e: e.tensor_copy(out=wlast[:, 1, hs], in_=W_im[:, :, SUB - 1]), reads=[W_im], writes=[wlast])
                    Ch = Ctab[:, hs, :]
                    Sh = Stab[:, hs, :]
                    P.op("dve", lambda e: e.tensor_tensor(out=pt[0][:], in0=W_re[:], in1=Ch, op=ALU.mult), reads=[W_re, Ctab], writes=[pt[0]])
                    P.op("dve", lambda e: e.tensor_tensor(out=pt[1][:], in0=W_im[:], in1=Sh, op=ALU.mult), reads=[W_im, Stab], writes=[pt[1]])
                    P.op("dve", lambda e: e.tensor_tensor(out=s_re[:], in0=pt[0][:], in1=pt[1][:], op=ALU.subtract), reads=[pt[0], pt[1]], writes=[s_re])
                    P.op("dve", lambda e: e.tensor_tensor(out=pt[0][:], in0=W_re[:], in1=Sh, op=ALU.mult), reads=[W_re, Stab], writes=[pt[0]])
                    P.op("dve", lambda e: e.tensor_tensor(out=pt[1][:], in0=W_im[:], in1=Ch, op=ALU.mult), reads=[W_im, Stab], writes=[pt[1]])
                    P.op("dve", lambda e: e.tensor_tensor(out=s_im[:], in0=pt[0][:], in1=pt[1][:], op=ALU.add), reads=[pt[0], pt[1]], writes=[s_im])
                    osl = slice(m * SUB, (m + 1) * SUB)
                    for jj in range(4):
                        j = 4 * m + jj
                        P.op("pe", lambda e: e.matmul(FB[2][:, osl], lhsT=CreT[:, j, :], rhs=s_re[:, jj, :], start=(jj == 0), stop=False), reads=[CreT, s_re], writes=[FB[2]])
                        P.op("pe", lambda e: e.matmul(FB[2][:, osl], lhsT=nCimT[:, j, :], rhs=s_im[:, jj, :], start=False, stop=False), reads=[nCimT, s_im], writes=[FB[2]])
                    P.op("pe", lambda e: e.matmul(FB[2][:, osl], lhsT=Ddiag[:, m, :], rhs=uT[:, m, ss_], start=False, stop=True), reads=[Ddiag, uT], writes=[FB[2]])
                yv = FB[2][:, 0:2 * SUB].rearrange("p (m t) -> p m t", m=2)
                P.op("act", lambda e: e.activation(out=yg[:], in_=yv, func=AF.Copy), reads=[FB[2]], writes=[yg])
                P.op("dve", lambda e: e.tensor_tensor(out=yt1[:], in0=yg[:], in1=yg[:], op=ALU.mult), reads=[yg], writes=[yt1])
                P.op("dve", lambda e: e.tensor_scalar(out=yt1[:], in0=yt1[:], scalar1=0.044715, scalar2=1.0, op0=ALU.mult, op1=ALU.add), reads=[yt1], writes=[yt1])
                P.op("dve", lambda e: e.tensor_tensor(out=yt1[:], in0=yt1[:], in1=yg[:], op=ALU.mult), reads=[yt1, yg], writes=[yt1])
                P.op("dve", lambda e: e.tensor_scalar(out=yt1[:], in0=yt1[:], scalar1=-45.0, scalar2=None, op0=ALU.max), reads=[yt1], writes=[yt1])
                P.op("act", lambda e: e.activation(out=yt1[:], in_=yt1[:], func=AF.Exp, scale=-1.5957691216), reads=[yt1], writes=[yt1])
                P.op("dve", lambda e: e.tensor_scalar(out=yt1[:], in0=yt1[:], scalar1=1.0, scalar2=None, op0=ALU.add), reads=[yt1], writes=[yt1])
                P.op("dve", lambda e: e.reciprocal(out=yt1[:], in_=yt1[:]), reads=[yt1], writes=[yt1])
                P.op("dve", lambda e: e.tensor_tensor(out=yg[:], in0=yg[:], in1=yt1[:], op=ALU.mult), reads=[yg, yt1], writes=[yg])
                P.op("dve", lambda e: e.tensor_copy(out=yTb[:], in_=yg[:]), reads=[yg], writes=[yTb])
                for mo in range(2):
                    osl = slice(mo * SUB, (mo + 1) * SUB)
                    for k in range(2):
                        P.op("pe", lambda e, mo=mo, k=k, osl=osl: e.matmul(FB[3][:, osl], lhsT=w_glu_sb[:, k, mo * 128:(mo + 1) * 128], rhs=yTb[:, k, :], start=(k == 0), stop=(k == 1)),
                             reads=[w_glu_sb, yTb], writes=[FB[3]])
                    P.op("act", lambda e, mo=mo, osl=osl: e.activation(out=yt2[:, mo, :], in_=FB[3][:, osl], func=AF.Exp, scale=-1.0, bias=nbglu_c[:, mo:mo + 1]), reads=[FB[3], nbglu_c], writes=[yt2])
                P.op("dve", lambda e: e.tensor_scalar(out=yt2[:], in0=yt2[:], scalar1=1.0, scalar2=None, op0=ALU.add), reads=[yt2], writes=[yt2])
                P.op("dve", lambda e: e.reciprocal(out=yt2[:], in_=yt2[:]), reads=[yt2], writes=[yt2])
                P.op("dve", lambda e: e.tensor_tensor(out=osm[:], in0=yg[:], in1=yt2[:], op=ALU.mult), reads=[yg, yt2], writes=[osm])
                P.op("dve", lambda e: e.tensor_tensor(out=o2[:], in0=osm[:], in1=osm[:], op=ALU.mult), reads=[osm], writes=[o2])
                for m in range(2):
                    P.op("pe", lambda e, m=m: e.matmul(FB[3][:, 0:SUB], lhsT=ones_f[:, 0:128], rhs=o2[:, m, :], start=(m == 0), stop=(m == 1)), reads=[ones_f, o2], writes=[FB[3]])
                P.op("act", lambda e: e.activation(out=rs_bc[:], in_=FB[3][:, 0:SUB], func=AF.Sqrt, scale=1.0 / 256, bias=eps_c[:, 0:1]), reads=[FB[3], eps_c], writes=[rs_bc])
                P.op("dve", lambda e: e.reciprocal(out=rs_bc[:], in_=rs_bc[:]), reads=[rs_bc], writes=[rs_bc])
                P.op("dve", lambda e: e.tensor_tensor(out=msm[:], in0=osm[:], in1=rs_bc[:].unsqueeze(1).to_broadcast([128, 2, SUB]), op=ALU.mult), reads=[osm, rs_bc], writes=[msm])
                P.dma("sp", mssm[:, :, gs].rearrange("m p t -> p m t"), msm[:], reads=[msm], writes=[mssm])

    def phase_b(l):
        hi = 0
        ob_i = 0
        it = 0
        for mixer in range(2):
            QTd, KTd, Vd, dk, scale = ((QTm, KTm, Vm, 96, 1.0 / math.sqrt(96.0)), (QTf, KTf, Vf, 68, 0.125))[mixer]
            P.dma("sp", V_sb[:], Vd[:].rearrange("(t p) c -> p t c", p=128), reads=[Vd], writes=[V_sb])
            for h in range(NH):
                Qs = QT_sb[hi % 2]
                Ks = KT_sb[hi % 2]
                hi += 1
                P.dma("sp", Qs[0:dk, :], QTd[h], reads=[QTd], writes=[Qs])
                P.dma("sp", Ks[0:dk, :], KTd[h], reads=[KTd], writes=[Ks])
                units = [(b, kt) for b in range(NB) for kt in range(4 * b + 4)]

                def front(u):
                    nonlocal it
                    b, kt = u
                    j = kt - 4 * b
                    q0 = 0 if j <= 0 else 128 * j
                    Sb = FB[it % 2]
                    pt_ = PT[it % 3]
                    it += 1
                    qsl = slice(b * 512 + q0, (b + 1) * 512)
                    P.op("pe", lambda e: e.matmul(Sb[:, q0:512], lhsT=Ks[0:dk, kt * 128:(kt + 1) * 128], rhs=Qs[0:dk, qsl], start=True, stop=True),
                         reads=[Ks, Qs], writes=[Sb])
                    P.op("act", lambda e: e.activation(out=pt_[:, q0:512], in_=Sb[:, q0:512], func=AF.Exp, scale=scale), reads=[Sb], writes=[pt_])
                    if j >= 0:
                        if mixer == 0:
                            P.op("pool", lambda e: e.memset(pt_[64:128, q0:q0 + 64], 0.0), writes=[pt_])
                        else:
                            P.op("pool", lambda e: e.affine_select(out=pt_[:, q0:q0 + 128], in_=pt_[:, q0:q0 + 128], pattern=[[1, 128]], compare_op=ALU.is_ge,
                                                                 fill=0.0, base=0, channel_multiplier=-1), reads=[pt_], writes=[pt_])
                    return pt_, q0

                def evac(b, OT, ob):
                    Otr = FB[4 + (ob % 2)]
                    osb_ = osb[ob % 2]
                    rd = rden[ob % 2]
                    col = mixer * 384 + h * 64
                    P.op("act", lambda e: e.activation(out=osb_[:], in_=OT[0:65, :], func=AF.Copy), reads=[OT], writes=[osb_])
                    for qi in range(4):
                        P.op("pe", lambda e: e.transpose(Otr[:, qi * 65:(qi + 1) * 65], osb_[:, qi * 128:(qi + 1) * 128], identf[0:65, 0:65]), reads=[osb_, identf], writes=[Otr])
                    o3 = Otr[:, 0:260].rearrange("p (q c) -> p q c", q=4)
                    P.op("dve", lambda e: e.reciprocal(out=rd[:], in_=o3[:, :, 64]), reads=[Otr], writes=[rd])
                    P.op("dve", lambda e: e.tensor_tensor(out=o_attn[:, 4 * b:4 * b + 4, col:col + 64], in0=o3[:, :, 0:64], in1=rd[:].unsqueeze(2).to_broadcast([128, 4, 64]), op=ALU.mult),
                         reads=[Otr, rd], writes=[o_attn])

                cur = front(units[0])
                pending = None
                for i, (b, kt) in enumerate(units):
                    nxt = front(units[i + 1]) if i + 1 < len(units) else None
                    pt_, q0 = cur
                    nk = 4 * b + 4
                    OT = FB[2 + (ob_i % 2)]
                    P.op("pe", lambda e: e.matmul(OT[0:65, q0:512], lhsT=V_sb[:, kt, h * 65:(h + 1) * 65], rhs=pt_[:, q0:512], start=(kt == 0), stop=(kt == nk - 1)),
                         reads=[pt_, V_sb], writes=[OT])
                    if pending is not None:
                        evac(*pending)
                        pending = None
                    if kt == nk - 1:
                        pending = (b, OT, ob_i)
                        ob_i += 1
                    cur = nxt
                if pending is not None:
                    evac(*pending)

    def phase_c(l, moe):
        j2 = l // 2
        if moe:
            P.dma("sp", wr_sb[:], A["moe_wr"][j2].rearrange("(k p) n -> p k n", p=128), writes=[wr_sb])
            P.dma("sp", br_sb[:], A["moe_br"][j2].to_broadcast([128, 8]), writes=[br_sb])
        for t in range(NT):
            xs_ = xs[t % 2]
            mat, mT, h2T_st = mat_l[t % 2], mT_l[t % 2], h2T_st_l[t % 2]
            if l == 0:
                P.dma("sp", xs_[:], x_in[t * 128:(t + 1) * 128, :], writes=[xs_])
            else:
                P.dma("sp", xs_[:], y[t * 128:(t + 1) * 128, :], reads=[ytile[t]], writes=[xs_])
            st_ = stat[t % 4]
            for mx in range(2):
                P.op("act", lambda e, mx=mx: e.activation(out=sq_junk[:, mx * 384:(mx + 1) * 384], in_=o_attn[:, t, mx * 384:(mx + 1) * 384], func=AF.Square, accum_out=st_[:, mx:mx + 1]),
                     reads=[o_attn], writes=[sq_junk, st_])
            P.op("act", lambda e: e.activation(out=st_[:, 2:4], in_=st_[:, 0:2], func=AF.Sqrt, scale=1.0 / 384, bias=eps_c[:, 0:1]), reads=[st_, eps_c], writes=[st_])
            P.op("dve", lambda e: e.reciprocal(out=st_[:, 2:4], in_=st_[:, 2:4]), reads=[st_], writes=[st_])
            for mx in range(2):
                P.op("dve", lambda e, mx=mx: e.tensor_scalar(out=mat[:, mx * 384:(mx + 1) * 384], in0=o_attn[:, t, mx * 384:(mx + 1) * 384], scalar1=st_[:, 2 + mx:3 + mx], scalar2=None, op0=ALU.mult),
                     reads=[o_attn, st_], writes=[mat])
            tb = TB[t % 2]
            for k in range(6):
                P.op("pe", lambda e, k=k: e.transpose(tb[:, k * 128:(k + 1) * 128], mat[:, k * 128:(k + 1) * 128], ident[:]), reads=[mat, ident], writes=[tb])
            P.op("act", lambda e: e.activation(out=mT[:, 2:8, :], in_=tb[:, 0:768].rearrange("p (k t) -> p k t", k=6), func=AF.Copy), reads=[tb], writes=[mT])
            P.dma("sp", mT[:, 0:2, :], mssm[:, :, t * 128:(t + 1) * 128].rearrange("m p t -> p m t"), reads=[mssm], writes=[mT])
            xn = xnew[t % 2]
            for hf in range(2):
                fb = FB[hf]
                for k in range(KD):
                    P.op("pe", lambda e, fb=fb, k=k, hf=hf: e.matmul(fb[:], lhsT=mT[:, k, :], rhs=w_out_sb[:, k, hf * 512:(hf + 1) * 512], start=(k == 0), stop=(k == KD - 1)),
                         reads=[mT, w_out_sb], writes=[fb])
                P.op("dve", lambda e, fb=fb, hf=hf: e.tensor_tensor(out=xn[:, hf * 512:(hf + 1) * 512], in0=fb[:], in1=xs_[:, hf * 512:(hf + 1) * 512], op=ALU.add), reads=[fb, xs_], writes=[xn])
            P.dma("sp", y[t * 128:(t + 1) * 128, :], xn[:], reads=[xn], writes=[ytile[t]])
            norm_transpose(xn, xn[:], l, 2, 3, h2T_st, slice(0, 128), t, fp32_path=(xhf, h2Tf) if moe else None)
            P.dma("sp", h2T_d[:, :, t * 128:(t + 1) * 128].rearrange("k p t -> p k t"), h2T_st[:], reads=[h2T_st], writes=[h2T_d])
            if moe:
                fb = FB[2]
                for k in range(KD):
                    P.op("pe", lambda e, k=k: e.matmul(fb[:, 0:8], lhsT=h2Tf[:, k, :], rhs=wr_sb[:, k, :], start=(k == 0), stop=(k == KD - 1)), reads=[h2Tf, wr_sb], writes=[fb])
                lg, m1, m2, lg2 = rtmp
                s1, s2, s3, s4 = rsc
                P.op("dve", lambda e: e.tensor_tensor(out=lg[:], in0=fb[:, 0:8], in1=br_sb[:], op=ALU.add), reads=[fb, br_sb], writes=[lg])
                P.op("dve", lambda e: e.tensor_reduce(out=s1[:], in_=lg[:], axis=AX.X, op=ALU.max), reads=[lg], writes=[s1])
                P.op("dve", lambda e: e.tensor_scalar(out=m1[:], in0=lg[:], scalar1=s1[:, 0:1], scalar2=None, op0=ALU.is_equal), reads=[lg, s1], writes=[m1])
                P.op("dve", lambda e: e.scalar_tensor_tensor(out=lg2[:], in0=m1[:], scalar=-1e30, in1=lg[:], op0=ALU.mult, op1=ALU.add), reads=[m1, lg], writes=[lg2])
                P.op("dve", lambda e: e.tensor_reduce(out=s2[:], in_=lg2[:], axis=AX.X, op=ALU.max), reads=[lg2], writes=[s2])
                P.op("dve", lambda e: e.tensor_scalar(out=m2[:], in0=lg2[:], scalar1=s2[:, 0:1], scalar2=None, op0=ALU.is_equal), reads=[lg2, s2], writes=[m2])
                P.op("dve", lambda e: e.tensor_tensor(out=s3[:], in0=s2[:], in1=s1[:], op=ALU.subtract), reads=[s1, s2], writes=[s3])
                P.op("act", lambda e: e.activation(out=s3[:], in_=s3[:], func=AF.Exp), reads=[s3], writes=[s3])
                P.op("dve", lambda e: e.tensor_scalar(out=s3[:], in0=s3[:], scalar1=1.0, scalar2=None, op0=ALU.add), reads=[s3], writes=[s3])
                P.op("dve", lambda e: e.reciprocal(out=s3[:], in_=s3[:]), reads=[s3], writes=[s3])
                P.op("dve", lambda e: e.tensor_scalar(out=s4[:], in0=s3[:], scalar1=-1.0, scalar2=1.0, op0=ALU.mult, op1=ALU.add), reads=[s3], writes=[s4])
                P.op("dve", lambda e: e.tensor_scalar(out=m1[:], in0=m1[:], scalar1=s3[:, 0:1], scalar2=None, op0=ALU.mult), reads=[m1, s3], writes=[m1])
                P.op("dve", lambda e, t=t: e.scalar_tensor_tensor(out=comb_all[:, t, :], in0=m2[:], scalar=s4[:, 0:1], in1=m1[:], op0=ALU.mult, op1=ALU.add), reads=[m2, s4, m1], writes=[comb_all])

    def phase_d(l, moe):
        j2 = l // 2
        P.dma("sp", G_bc[:], g12[l, 1], reads=[g12], writes=[G_bc])
        ne = 8 if moe else 2
        it = 0
        oi = 0
        for ex in range(ne):
            if moe:
                wg = A["moe_wg"][j2, ex]
                wu = A["moe_wu"][j2, ex]
                wd = A["moe_wd"][j2, ex]
            else:
                wg = A["ffn_wg"][j2][:, ex * DFE:(ex + 1) * DFE]
                wu = A["ffn_wu"][j2][:, ex * DFE:(ex + 1) * DFE]
                wd = A["ffn_wd"][j2][ex * DFE:(ex + 1) * DFE, :]
            Wg_, Wu_, Wd_ = Wg_sb[ex % 2], Wu_sb[ex % 2], Wd_sb[ex % 2]
            P.dma("pool", Wg_[:], wg.rearrange("(k p) f -> p k f", p=128), writes=[Wg_])
            P.dma("pool", Wu_[:], wu.rearrange("(k p) f -> p k f", p=128), writes=[Wu_])
            P.dma("pool", Wd_[:], wd.rearrange("(c p) d -> p c d", p=128), writes=[Wd_])
            for b in range(NB):
                hb = h2T_sb[(ex * NB + b) % 2]
                P.dma("sp", hb[:], h2T_d[:, :, b * 512:(b + 1) * 512].rearrange("k p t -> p k t"), reads=[h2T_d], writes=[hb])
                for c in range(NFC):
                    gb = FB[(it % 2) * 2]
                    ub = FB[(it % 2) * 2 + 1]
                    sg_ = sg[it % 2]
                    it += 1
                    for k in range(KD):
                        P.op("pe", lambda e, gb=gb, k=k, c=c: e.matmul(gb[:], lhsT=Wg_[:, k, c * 128:(c + 1) * 128], rhs=hb[:, k, :], start=(k == 0), stop=(k == KD - 1)), reads=[Wg_, hb], writes=[gb])
                    for k in range(KD):
                        P.op("pe", lambda e, ub=ub, k=k, c=c: e.matmul(ub[:], lhsT=Wu_[:, k, c * 128:(c + 1) * 128], rhs=hb[:, k, :], start=(k == 0), stop=(k == KD - 1)), reads=[Wu_, hb], writes=[ub])
                    P.op("act", lambda e, gb=gb, sg_=sg_: e.activation(out=sg_[:], in_=gb[:], func=AF.Silu), reads=[gb], writes=[sg_])
                    P.op("dve", lambda e, ub=ub, sg_=sg_, c=c: e.tensor_tensor(out=aT[:, c, :], in0=ub[:], in1=sg_[:], op=ALU.mult), reads=[ub, sg_], writes=[aT])
                for ti in range(4):
                    t = b * 4 + ti
                    os_ = ost[oi % 2]
                    oi += 1
                    for hf in range(2):
                        fb = FB[4 + hf]
                        for c in range(NFC):
                            P.op("pe", lambda e, fb=fb, c=c, ti=ti, hf=hf: e.matmul(fb[:], lhsT=aT[:, c, ti * 128:(ti + 1) * 128], rhs=Wd_[:, c, hf * 512:(hf + 1) * 512], start=(c == 0), stop=(c == NFC - 1)),
                                 reads=[aT, Wd_], writes=[fb])
                        if moe:
                            P.op("dve", lambda e, fb=fb, hf=hf, t=t, ex=ex, os_=os_: e.scalar_tensor_tensor(out=os_[:, hf * 512:(hf + 1) * 512], in0=fb[:], scalar=comb_all[:, t, ex:ex + 1],
                                                                                                       in1=G_bc[:, hf * 512:(hf + 1) * 512], op0=ALU.mult, op1=ALU.mult), reads=[fb, comb_all, G_bc], writes=[os_])
                        else:
                            P.op("dve", lambda e, fb=fb, hf=hf, os_=os_: e.tensor_tensor(out=os_[:, hf * 512:(hf + 1) * 512], in0=fb[:], in1=G_bc[:, hf * 512:(hf + 1) * 512], op=ALU.mult),
                                 reads=[fb, G_bc], writes=[os_])
                    P.dma("pool", y[t * 128:(t + 1) * 128, :], os_[:], reads=[os_, ytile[t]], writes=[ytile[t]], accum_op=ALU.add)

    stop = build.stop_after
    G = globals()

    def use(dct):
        for k_, v_ in dct.items():
            if k_ not in ("P", "L", "NT"):
                G[k_] = v_
    dbg = P.dram("dbg_oattn", [128, NT * 768], BF16, kind="ExternalOutput") if debug else None
    for l in range(n_layers):
        moe = (l % 2 == 1)
        P.push()
        use(_alloc_a(P, L))
        s5c = load_layer(l)
        phase_a(l, s5c)
        P.pop()
        if stop == "a":
            break
        P.push()
        use(_alloc_bc(P, L))
        P.push()
        use(_alloc_b(P, L))
        phase_b(l)
        P.pop()
        if debug and (stop == "b" or l == n_layers - 1):
            P.dma("sp", dbg[:], o_attn[:].rearrange("p t c -> p (t c)"), reads=[o_attn], writes=[dbg])
        if stop == "b":
            P.pop()
            break
        P.push()
        use(_alloc_c(P, L))
        load_c(l)
        phase_c(l, moe)
        P.pop()
        P.pop()
        if stop == "c":
            break
        P.push()
        use(_alloc_d(P, L))
        phase_d(l, moe)
        P.pop()
    P.finish()
    build.n_inst = P.n_inst
    return nc


build.stop_after = None


def prep_shared(inp, n_layers=DEPTH):
    f = lambda a: np.ascontiguousarray(np.asarray(a, dtype=np.float32))
    col = lambda a, k: f(np.asarray(a).reshape(DEPTH, k, 128).transpose(0, 2, 1))
    out = {}
    out["norm_mix_c"] = col(inp["norm_mix"], KD)
    out["norm_ffn_c"] = col(inp["norm_ffn"], KD)
    out["w_ada"] = f(inp["w_ada"])
    out["b_ada"] = f(np.asarray(inp["b_ada"]).reshape(DEPTH, 1, 6 * D))
    out["w_in"] = f(inp["w_in"])
    sm = lambda a: f(np.asarray(a).reshape(DEPTH, 8, 128).transpose(0, 2, 1))
    out["lam_re"] = sm(inp["ssm_lam_re"])
    out["lam_im"] = sm(inp["ssm_lam_im"])
    out["log_dt"] = sm(np.repeat(np.asarray(inp["ssm_log_dt"])[:, :, None], 64, axis=2))
    b_re = np.asarray(inp["ssm_b_re"]); b_im = np.asarray(inp["ssm_b_im"])
    c_re = np.asarray(inp["ssm_c_re"]); c_im = np.asarray(inp["ssm_c_im"])
    blk = {k: np.zeros((DEPTH, 8, 128, 128), np.float32) for k in ("b_re", "b_im", "c_re", "c_im")}
    for g in range(16):
        j, gg = g // 2, g % 2
        fc = (g % 8) * 16
        blk["b_re"][:, j, gg * 64:(gg + 1) * 64, fc:fc + 16] = b_re[:, g]
        blk["b_im"][:, j, gg * 64:(gg + 1) * 64, fc:fc + 16] = b_im[:, g]
        blk["c_re"][:, j, gg * 64:(gg + 1) * 64, fc:fc + 16] = c_re[:, g].transpose(0, 2, 1)
        blk["c_im"][:, j, gg * 64:(gg + 1) * 64, fc:fc + 16] = c_im[:, g].transpose(0, 2, 1)
    out.update(blk)
    out["ssm_d"] = col(inp["ssm_d"], 2)
    out["w_glu"] = f(inp["ssm_w_glu"])
    out["b_glu"] = col(inp["ssm_b_glu"], 2)
    out["q_norm"] = col(inp["mla_q_norm"], 2)
    out["kv_norm"] = col(inp["mla_kv_norm"], 1)
    out["w_uq"] = f(inp["mla_w_uq"])
    out["w_ukv"] = f(inp["mla_w_ukv"])
    rep = lambda a: f(np.broadcast_to(np.asarray(a)[:, None, :], (DEPTH, 128, np.asarray(a).shape[1])))
    out["gq_m"] = rep(inp["mla_qk_gq"])
    out["gk_m"] = rep(inp["mla_qk_gk"])
    out["fox_bf"] = f(np.asarray(inp["fox_b_f"]).reshape(DEPTH, NH, 1))
    out["gq_f"] = rep(inp["fox_qk_gq"])
    out["gk_f"] = rep(inp["fox_qk_gk"])
    out["out_norm"] = col(inp["out_norm"], KD)
    out["w_out"] = f(inp["w_out"])
    out["ffn_wg"] = f(inp["ffn_w_gate"])
    out["ffn_wu"] = f(inp["ffn_w_up"])
    out["ffn_wd"] = f(inp["ffn_w_down"])
    out["moe_wr"] = f(inp["moe_w_router"])
    out["moe_br"] = f(np.asarray(inp["moe_b_router"]).reshape(2, 1, 8))
    out["moe_wg"] = f(inp["moe_w_gate"])
    out["moe_wu"] = f(inp["moe_w_up"])
    out["moe_wd"] = f(inp["moe_w_down"])
    half = 16
    inv = (10000.0 ** (-np.arange(half, dtype=np.float32) / half)).astype(np.float32)
    out["inv_bc"] = f(np.broadcast_to(inv[None, :], (128, 16)))
    return out


def prep_core(inp, b, L):
    NT = L // 128
    m = {}
    m["x"] = np.ascontiguousarray(np.asarray(inp["x"])[b, :L, :], dtype=np.float32)
    m["c_col"] = np.ascontiguousarray(np.asarray(inp["c"], dtype=np.float32)[b].reshape(KD, 128).T)
    m["pos"] = np.ascontiguousarray(np.asarray(inp["positions"])[b, :L].astype(np.int32).reshape(NT, 128).T)
    return m


_CACHE = {}


def run(inp, L, n_layers=DEPTH, debug=False, cores=8, trace=False):
    key = (L, n_layers, debug, build.stop_after)
    if key not in _CACHE:
        _CACHE[key] = build(L, n_layers, debug)
    nc = _CACHE[key]
    shared = prep_shared(inp, n_layers)
    in_maps = []
    for b in range(cores):
        m = dict(shared)
        for n_ in PADDED:
            a = shared[n_]
            a2 = a.reshape(-1, a.shape[-1])
            m[n_] = np.concatenate([a2, np.full((1, a2.shape[1]), float(b), np.float32)], axis=0)
        m.update(prep_core(inp, b, L))
        in_maps.append(m)
    res = run_bass_kernel_spmd(nc, in_maps, core_ids=list(range(cores)), **({"trace": True} if trace else {}))
    return res


def kernel(**inputs):
    L = np.asarray(inputs["x"]).shape[1]
    res = run(inputs, L)
    out = np.stack([np.asarray(r["y"], dtype=np.float32) for r in res.results], axis=0)
    return out
```

```python
import contextlib
import math
import sys
import threading
import numpy as np
import concourse.bass as bass
import concourse.mybir as mybir
from concourse.bass_utils import run_bass_kernel_spmd

F32 = mybir.dt.float32
BF16 = mybir.dt.bfloat16
I32 = mybir.dt.int32
AF = mybir.ActivationFunctionType
ALU = mybir.AluOpType
AX = mybir.AxisListType

ENGS = ("pe", "act", "dve", "pool", "sp")

D = 1024
KD = 8
DEPTH = 4
EPS = 1e-6
IN_COLS = 1830
NH = 6
DFE = 1408
NFC = 11
SUB = 256


class Tl:
    __slots__ = ("t", "name", "w", "r", "excl")

    def __init__(self, t, name):
        self.t = t
        self.name = name
        self.excl = False
        self.w = {}
        self.r = {}

    def __getitem__(self, idx):
        return self.t[idx]


class Prog:
    max_ops = 10 ** 9
    log = None

    def __init__(self, nc, ring_sizes=None):
        self.nc = nc
        self.es = contextlib.ExitStack()
        self.cnt = {e: 0 for e in ENGS}
        self.sems = {}
        self.seen = {e: {} for e in ENGS}
        for e in ENGS:
            self.sems[("eng", e)] = self.es.enter_context(nc.semaphore("s_" + e))
        ring_sizes = ring_sizes or {"sp": 16, "pool": 8, "act": 2}
        self.rings = {}
        self.ring_i = {}
        for e, k in ring_sizes.items():
            self.rings[e] = []
            for i in range(k):
                key = ("ring", e, i)
                self.sems[key] = self.es.enter_context(nc.semaphore("r_%s%d" % (e, i)))
                self.rings[e].append([key, 0])
            self.ring_i[e] = 0
        self.n_inst = 0
        self.scopes = [self.es]
        self.hooks = {}
        self.E = {"pe": nc.tensor, "act": nc.scalar, "dve": nc.vector, "pool": nc.gpsimd, "sp": nc.sync}

    def sb(self, name, shape, dt):
        self._uid = getattr(self, "_uid", 0) + 1
        return Tl(self.scopes[-1].enter_context(self.nc.sbuf_tensor("%s_%d" % (name, self._uid), list(shape), dt)), name)

    def push(self):
        self.scopes.append(contextlib.ExitStack())

    def barrier(self):
        need = {}
        for e, ring in self.rings.items():
            for key, v in ring:
                if v > 0:
                    need[key] = v
        for e in ENGS:
            if self.cnt[e] > 0:
                need[("eng", e)] = self.cnt[e]
        for eng in ENGS:
            seen = self.seen[eng]
            for k, v in need.items():
                if k == ("eng", eng) or seen.get(k, 0) >= v:
                    continue
                seen[k] = v
                self.E[eng].wait_ge(self.sems[k], v)

    def pop(self):
        self.barrier()
        self.scopes.pop().close()

    def ps(self, name, shape, dt):
        t = Tl(self.es.enter_context(self.nc.psum_tensor(name, list(shape), dt)), name)
        t.excl = True
        return t

    def dram(self, name, shape, dt, kind="Internal"):
        return Tl(self.nc.dram_tensor(name, list(shape), dt, kind=kind).ap(), name)

    def _collect(self, eng, reads, writes):
        need = {}
        me = ("eng", eng)
        for t in reads:
            for k, v in t.w.items():
                if need.get(k, 0) < v:
                    need[k] = v
            if t.excl:
                for k, v in t.r.items():
                    if k != me and need.get(k, 0) < v:
                        need[k] = v
        for t in writes:
            for k, v in t.w.items():
                if need.get(k, 0) < v:
                    need[k] = v
            for k, v in t.r.items():
                if need.get(k, 0) < v:
                    need[k] = v
        waits = []
        seen = self.seen[eng]
        for k, v in need.items():
            if eng == "pe" and k == ("eng", "pe"):
                continue
            if seen.get(k, 0) >= v:
                continue
            seen[k] = v
            waits.append((self.sems[k], v))
        return waits

    def _commit(self, reads, writes, key, val):
        for t in reads:
            if t.r.get(key, 0) < val:
                t.r[key] = val
        for t in writes:
            t.w = {key: val}
            t.r = {}

    def op(self, eng, fn, reads=(), writes=()):
        self.n_inst += 1
        if Prog.log is not None:
            Prog.log.append((self.n_inst, eng, sys._getframe(1).f_lineno))
        if self.n_inst > Prog.max_ops:
            return
        waits = self._collect(eng, reads, writes)
        self.cnt[eng] += 1
        key = ("eng", eng)
        val = self.cnt[eng]
        self._commit(reads, writes, key, val)
        e = self.E[eng]
        for s, v in waits[1:]:
            e.wait_ge(s, v)
        ins = fn(e)
        if waits:
            ins._wait_ge(waits[0][0], waits[0][1])
        ins.then_inc(self.sems[key], 1)
        if self.hooks:
            self.hooks[threading.get_ident()]()

    def dma(self, eng, out, in_, reads=(), writes=(), **kw):
        self.n_inst += 1
        if Prog.log is not None:
            Prog.log.append((self.n_inst, "dma-" + eng, sys._getframe(1).f_lineno))
        if self.n_inst > Prog.max_ops:
            return
        ring = self.rings[eng]
        slot = ring[self.ring_i[eng] % len(ring)]
        self.ring_i[eng] += 1
        key, pv = slot
        waits = self._collect(eng, reads, writes)
        seen = self.seen[eng]
        if pv > 0 and seen.get(key, 0) < pv:
            seen[key] = pv
            waits.append((self.sems[key], pv))
        val = pv + 16
        slot[1] = val
        self._commit(reads, writes, key, val)
        e = self.E[eng]
        for s, v in waits[1:]:
            e.wait_ge(s, v)
        ins = e.dma_start(out=out, in_=in_, **kw)
        if waits:
            ins._wait_ge(waits[0][0], waits[0][1])
        ins.then_inc(self.sems[key], 16)
        if self.hooks:
            self.hooks[threading.get_ident()]()

    def finish(self, eng="sp"):
        need = {}
        for e, ring in self.rings.items():
            for key, v in ring:
                if v > 0:
                    need[key] = v
        for e in ENGS:
            if self.cnt[e] > 0:
                need[("eng", e)] = self.cnt[e]
        E = self.E[eng]
        for k, v in need.items():
            E.wait_ge(self.sems[k], v)
        self.es.close()


LAYER_PARAMS = [
    ("norm_mix_c", [128, KD]), ("norm_ffn_c", [128, KD]),
    ("w_ada", [D, 6 * D]), ("b_ada", [1, 6 * D]),
    ("w_in", [D, IN_COLS]),
    ("lam_re", [128, 8]), ("lam_im", [128, 8]), ("log_dt", [128, 8]),
    ("b_re", [8, 128, 128]), ("b_im", [8, 128, 128]),
    ("c_re", [8, 128, 128]), ("c_im", [8, 128, 128]),
    ("ssm_d", [128, 2]), ("w_glu", [256, 256]), ("b_glu", [128, 2]),
    ("q_norm", [128, 2]), ("kv_norm", [128, 1]),
    ("w_uq", [256, 576]), ("w_ukv", [128, 768]),
    ("gq_m", [128, 96]), ("gk_m", [128, 96]),
    ("fox_bf", [NH, 1]), ("gq_f", [128, 64]), ("gk_f", [128, 64]),
    ("out_norm", [128, KD]), ("w_out", [D, D]),
]


def run_pair(P, f1, f2):
    cv = threading.Condition()
    st = {"turn": 0, "alive": [True, True], "err": None}

    def make_hook(i):
        def h():
            with cv:
                if st["alive"][1 - i]:
                    st["turn"] = 1 - i
                    cv.notify_all()
                    while st["turn"] != i:
                        cv.wait()
        return h

    def worker(i, f):
        with cv:
            while st["turn"] != i:
                cv.wait()
        try:
            P.hooks[threading.get_ident()] = make_hook(i)
            f()
        except BaseException as ex:
            st["err"] = ex
        finally:
            with cv:
                st["alive"][i] = False
                st["turn"] = 1 - i
                cv.notify_all()
    ts = [threading.Thread(target=worker, args=(i, f)) for i, f in enumerate((f1, f2))]
    for t in ts:
        t.start()
    for t in ts:
        t.join()
    P.hooks.clear()
    if st["err"] is not None:
        raise st["err"]


PADDED = ("w_ada", "w_in", "b_re", "b_im", "c_re", "c_im", "w_glu", "w_uq", "w_ukv", "w_out",
          "ffn_wg", "ffn_wu", "ffn_wd", "moe_wg", "moe_wu", "moe_wd")


def _alloc_a(P, L):
    NT = L // 128
    w_in_sb = P.sb("w_in_sb", [128, KD, IN_COLS], BF16)
    w_uq_sb = P.sb("w_uq_sb", [128, 2, 576], BF16)
    w_ukv_sb = P.sb("w_ukv_sb", [128, 768], BF16)
    w_glu_sb = P.sb("w_glu_sb", [128, 2, 256], BF16)
    qn_c = P.sb("qn_c", [128, 2], F32)
    kvn_c = P.sb("kvn_c", [128, 1], F32)
    gqm = P.sb("gqm", [128, 96], F32)
    gkm = P.sb("gkm", [128, 96], F32)
    gqf = P.sb("gqf", [128, 64], F32)
    gkf = P.sb("gkf", [128, 64], F32)
    nbf = P.sb("nbf", [NH, 1], F32)
    dcol = P.sb("dcol", [128, 2], F32)
    bglu_c = P.sb("bglu_c", [128, 2], F32)
    nbglu_c = P.sb("nbglu_c", [128, 2], F32)
    s5p = P.sb("s5p", [128, 24, 8], F32)
    BreT = P.sb("BreT", [128, 8, 128], BF16)
    BimT = P.sb("BimT", [128, 8, 128], BF16)
    CreT = P.sb("CreT", [128, 8, 128], BF16)
    nCimT = P.sb("nCimT", [128, 8, 128], BF16)
    Ddiag = P.sb("Ddiag", [128, 2, 128], BF16)
    Ctab = P.sb("Ctab", [128, 8, SUB], F32)
    Stab = P.sb("Stab", [128, 8, SUB], F32)
    Rtab = P.sb("Rtab", [128, 8, SUB], F32)
    blk_f = P.sb("blk_f", [128, 2, 128], F32)
    blk_o = P.sb("blk_o", [128, 2, 128], F32)
    hT = [P.sb("hT%d" % i, [128, KD, 512], BF16) for i in range(2)]
    uT_l = [P.sb("uT%d" % i, [128, 2, 512], BF16) for i in range(2)]
    cq_h = P.sb("cq_h", [128, 256], BF16)
    ckv_h = P.sb("ckv_h", [128, 128], BF16)
    cqT = P.sb("cqT", [128, 2, 128], BF16)
    ckvT = P.sb("ckvT", [128, 128], BF16)
    qn = P.sb("qn", [128, NH, 96], F32)
    kn = P.sb("kn", [128, NH, 96], F32)
    rt = [P.sb("rt%d" % i, [128, NH, 16], F32) for i in range(4)]
    qfin = P.sb("qfin", [128, NH, 96], BF16)
    kfin = P.sb("kfin", [128, NH, 96], BF16)
    fqn = P.sb("fqn", [128, NH, 64], F32)
    fqb = P.sb("fqb", [128, NH, 64], BF16)
    fkb = P.sb("fkb", [128, NH, 64], BF16)
    QTm_st = [P.sb("QTm_st%d" % i, [96, NH, 128], BF16) for i in range(2)]
    KTm_st = [P.sb("KTm_st%d" % i, [96, NH, 128], BF16) for i in range(2)]
    QTf_st = [P.sb("QTf_st%d" % i, [64, NH, 128], BF16) for i in range(2)]
    KTf_st = [P.sb("KTf_st%d" % i, [64, NH, 128], BF16) for i in range(2)]
    Vm_st = [P.sb("Vm_st%d" % i, [128, NH, 65], BF16) for i in range(2)]
    Vf_st = [P.sb("Vf_st%d" % i, [128, NH, 65], BF16) for i in range(2)]
    for t_ in Vm_st + Vf_st:
        P.op("pool", lambda e, t_=t_: e.memset(t_[:], 1.0), writes=[t_])
    fg_e = P.sb("fg_e", [NH, 512], F32)
    fg_sp = P.sb("fg_sp", [NH, 512], F32)
    fg_cum = P.sb("fg_cum", [NH, 512], F32)
    fg_carry = P.sb("fg_carry", [NH, 1], F32)
    fg_hi = P.sb("fg_hi", [NH, 512], BF16)
    fg_lo = P.sb("fg_lo", [NH, 512], BF16)
    fg_nhi = P.sb("fg_nhi", [NH, 512], BF16)
    fg_nlo = P.sb("fg_nlo", [NH, 512], BF16)
    W_re = P.sb("W_re", [128, 4, SUB], F32)
    W_im = P.sb("W_im", [128, 4, SUB], F32)
    wlast = P.sb("wlast", [128, 2, 8], F32)
    pre_c = P.sb("pre_c", [128, 2, SUB], F32)
    pre_s = P.sb("pre_s", [128, 2, SUB], F32)
    pin_re = P.sb("pin_re", [128, SUB], F32)
    pin_im = P.sb("pin_im", [128, SUB], F32)
    w0 = P.sb("w0", [128, 2, 8], F32)
    w0t = P.sb("w0t", [128, 4, 8], F32)
    pt = [P.sb("pt%d" % i, [128, 4, SUB], F32) for i in range(2)]
    s_re = P.sb("s_re", [128, 4, SUB], BF16)
    s_im = P.sb("s_im", [128, 4, SUB], BF16)
    yg = P.sb("yg", [128, 2, SUB], F32)
    yt1 = P.sb("yt1", [128, 2, SUB], F32)
    yt2 = P.sb("yt2", [128, 2, SUB], F32)
    yTb = P.sb("yTb", [128, 2, SUB], BF16)
    o2 = P.sb("o2", [128, 2, SUB], F32)
    osm = P.sb("osm", [128, 2, SUB], F32)
    rs_bc = P.sb("rs_bc", [128, SUB], F32)
    msm = P.sb("msm", [128, 2, SUB], BF16)
    return locals()


def _alloc_bc(P, L):
    NT = L // 128
    o_attn = P.sb("o_attn", [128, NT, 768], BF16)
    return locals()


def _alloc_b(P, L):
    NT = L // 128
    QT_sb = [P.sb("QT_sb%d" % i, [96, L], BF16) for i in range(2)]
    KT_sb = [P.sb("KT_sb%d" % i, [96, L], BF16) for i in range(2)]
    V_sb = P.sb("V_sb", [128, NT, NH * 65], BF16)
    PT = [P.sb("PT%d" % i, [128, 512], BF16) for i in range(3)]
    rden = [P.sb("rden%d" % i, [128, 4], F32) for i in range(2)]
    osb = [P.sb("osb%d" % i, [65, 512], F32) for i in range(2)]
    return locals()


def _alloc_c(P, L):
    NT = L // 128
    w_out_sb = P.sb("w_out_sb", [128, KD, D], BF16)
    wstc = [P.sb("wstc%d" % i, [128, D], F32) for i in range(2)]
    onorm_c = P.sb("onorm_c", [128, KD], F32)
    mat_l = [P.sb("mat%d" % i, [128, 768], BF16) for i in range(2)]
    mT_l = [P.sb("mT%d" % i, [128, KD, 128], BF16) for i in range(2)]
    xnew = [P.sb("xnew%d" % i, [128, D], F32) for i in range(2)]
    h2T_st_l = [P.sb("h2T_st%d" % i, [128, KD, 128], BF16) for i in range(2)]
    xhf = P.sb("xhf", [128, D], F32)
    h2Tf = P.sb("h2Tf", [128, KD, 128], F32)
    wr_sb = P.sb("wr_sb", [128, KD, 8], F32)
    br_sb = P.sb("br_sb", [128, 8], F32)
    rtmp = [P.sb("rtmp%d" % i, [128, 8], F32) for i in range(4)]
    rsc = [P.sb("rsc%d" % i, [128, 1], F32) for i in range(4)]
    return locals()


def _alloc_d(P, L):
    Wg_sb = [P.sb("Wg_sb%d" % i, [128, KD, DFE], BF16) for i in range(2)]
    Wu_sb = [P.sb("Wu_sb%d" % i, [128, KD, DFE], BF16) for i in range(2)]
    Wd_sb = [P.sb("Wd_sb%d" % i, [128, NFC, D], BF16) for i in range(2)]
    h2T_sb = [P.sb("h2T_sb%d" % i, [128, KD, 512], BF16) for i in range(2)]
    sg = [P.sb("sg%d" % i, [128, 512], BF16) for i in range(2)]
    aT = P.sb("aT", [128, NFC, 512], BF16)
    ost = [P.sb("ost%d" % i, [128, D], F32) for i in range(2)]
    return locals()


def build(L, n_layers=DEPTH, debug=False):
    NT = L // 128
    NB = L // 512
    nc = bass.Bass("TRN2", target_bir_lowering=False)
    P = Prog(nc)
    A = {}

    def din(name, shape, dt=F32):
        if name in PADDED:
            rows = int(np.prod(shape[:-1]))
            t = P.dram(name, [rows + 1, shape[-1]], dt, kind="ExternalInput")
            names = "abcdefg"[:len(shape) - 1]
            pat = "(%s) z -> %s z" % (" ".join(names), " ".join(names))
            A[name] = Tl(t.t[0:rows, :].rearrange(pat, **{n_: int(v_) for n_, v_ in zip(names, shape[:-1])}), name)
            return A[name]
        A[name] = P.dram(name, shape, dt, kind="ExternalInput")
        return A[name]

    x_in = din("x", [L, D])
    c_in = din("c_col", [128, KD])
    pos_in = din("pos", [128, NT], I32)
    inv_in = din("inv_bc", [128, 16])
    for name, shp in LAYER_PARAMS:
        din(name, [DEPTH] + shp)
    din("ffn_wg", [2, D, 2 * DFE])
    din("ffn_wu", [2, D, 2 * DFE])
    din("ffn_wd", [2, 2 * DFE, D])
    din("moe_wr", [2, D, 8])
    din("moe_br", [2, 1, 8])
    din("moe_wg", [2, 8, D, DFE])
    din("moe_wu", [2, 8, D, DFE])
    din("moe_wd", [2, 8, DFE, D])

    y = P.dram("y", [L, D], F32, kind="ExternalOutput")
    skind = "ExternalOutput" if debug else "Internal"
    QTm = P.dram("QTm", [NH, 96, L], BF16, kind=skind)
    KTm = P.dram("KTm", [NH, 96, L], BF16, kind=skind)
    Vm = P.dram("Vm", [L, NH * 65], BF16, kind=skind)
    QTf = P.dram("QTf", [NH, 68, L], BF16, kind=skind)
    KTf = P.dram("KTf", [NH, 68, L], BF16, kind=skind)
    Vf = P.dram("Vf", [L, NH * 65], BF16, kind=skind)
    mssm = P.dram("mssm", [2, 128, L], BF16, kind=skind)
    h2T_d = P.dram("h2T", [KD, 128, L], BF16, kind=skind)
    g12 = P.dram("g12", [DEPTH, 2, 128, D], F32, kind=skind)
    ytile = [Tl(y.t, "y%d" % t) for t in range(NT)]

    ident = P.sb("ident", [128, 128], BF16)
    identf = P.sb("identf", [128, 128], F32)
    ones_f = P.sb("ones_f", [128, 512], F32)
    ones_b = P.sb("ones_b", [128, 128], BF16)
    for t_, dt_ in ((ident, BF16), (identf, F32)):
        P.op("pool", lambda e, t_=t_: e.memset(t_[:], 1.0), writes=[t_])
        P.op("pool", lambda e, t_=t_: e.affine_select(out=t_[:], in_=t_[:], pattern=[[1, 128]], compare_op=ALU.is_equal,
                                                    fill=0.0, base=0, channel_multiplier=-1), reads=[t_], writes=[t_])
    P.op("pool", lambda e: e.memset(ones_f[:], 1.0), writes=[ones_f])
    P.op("pool", lambda e: e.memset(ones_b[:], 1.0), writes=[ones_b])

    TB = [P.ps("TB%d" % i, [128, 1024], BF16) for i in range(2)]
    FB = [P.ps("FB%d" % i, [128, 512], F32) for i in range(6)]

    def rstd_chain(ss_ap, n, nfeat, tmp, out, reads, eng_r="dve"):
        (tt, tv), (ot, ov) = tmp, out
        P.op("act", lambda e: e.activation(out=tv, in_=ss_ap, func=AF.Sqrt, scale=1.0 / nfeat, bias=eps_c[:, 0:1]),
             reads=list(reads) + [eps_c], writes=[tt])
        P.op("dve", lambda e: e.reciprocal(out=ov, in_=tv), reads=[tt], writes=[ot])

    eps_c = P.sb("eps_c", [128, 1], F32)
    P.op("pool", lambda e: e.memset(eps_c[:], EPS), writes=[eps_c])

    G_bc = P.sb("G_bc", [128, D], F32)
    xs = [P.sb("xs%d" % i, [128, D], F32) for i in range(2)]
    sq_junk = P.sb("sq_junk", [128, D], F32)
    xh = [P.sb("xh%d" % i, [128, D], BF16) for i in range(2)]
    stat = [P.sb("stat%d" % i, [128, 16], F32) for i in range(4)]
    comb_all = P.sb("comb_all", [128, NT, 8], F32)

    modc = P.sb("modc", [128, DEPTH, 4, KD], F32)
    nmc = P.sb("nmc", [128, DEPTH, 2, KD], F32)
    cosT = P.sb("cosT", [128, NT, 16], F32)
    sinT = P.sb("sinT", [128, NT, 16], F32)
    P.push()
    c_col = P.sb("c_colsb", [128, KD], F32)
    P.dma("sp", c_col[:], c_in[:], writes=[c_col])
    c_e = P.sb("c_e", [128, KD], F32)
    c_act = P.sb("c_act", [128, KD], F32)
    P.op("act", lambda e: e.activation(out=c_e[:], in_=c_col[:], func=AF.Exp, scale=-1.0), reads=[c_col], writes=[c_e])
    P.op("dve", lambda e: e.tensor_scalar(out=c_e[:], in0=c_e[:], scalar1=1.0, scalar2=None, op0=ALU.add), reads=[c_e], writes=[c_e])
    P.op("dve", lambda e: e.reciprocal(out=c_e[:], in_=c_e[:]), reads=[c_e], writes=[c_e])
    P.op("dve", lambda e: e.tensor_tensor(out=c_act[:], in0=c_col[:], in1=c_e[:], op=ALU.mult), reads=[c_col, c_e], writes=[c_act])
    C_bc = P.sb("C_bc", [128, KD, 128], F32)
    for k in range(KD):
        P.op("dve", lambda e, k=k: e.tensor_scalar(out=C_bc[:, k, :], in0=ones_f[:, 0:128], scalar1=c_act[:, k:k + 1], scalar2=None,
                                                   op0=ALU.mult), reads=[ones_f, c_act], writes=[C_bc])
    for l in range(n_layers):
        P.dma("sp", nmc[:, l, 0, :], A["norm_mix_c"][l], writes=[nmc])
        P.dma("sp", nmc[:, l, 1, :], A["norm_ffn_c"][l], writes=[nmc])
    wst = [P.sb("wst%d" % i, [128, 2048], F32) for i in range(3)]
    wst_i = [0]

    def next_wst():
        t = wst[wst_i[0] % 3]
        wst_i[0] += 1
        return t
    ada_sb = P.sb("ada_sb", [128, 2048], F32)
    brow = P.sb("brow", [128, 2048], F32)
    for l in range(n_layers):
        for ng in range(3):
            P.dma("sp", brow[:], A["b_ada"][l][:, ng * 2048:(ng + 1) * 2048].to_broadcast([128, 2048]), writes=[brow])
            for k in range(KD):
                st = next_wst()
                P.dma("sp", st[:], A["w_ada"][l, k * 128:(k + 1) * 128, ng * 2048:(ng + 1) * 2048], writes=[st])
                for j in range(4):
                    P.op("pe", lambda e, st=st, j=j, k=k: e.matmul(FB[j][:], lhsT=C_bc[:, k, :], rhs=st[:, j * 512:(j + 1) * 512],
                                                                 start=(k == 0), stop=(k == KD - 1)), reads=[st, C_bc], writes=[FB[j]])
            for j in range(4):
                P.op("dve", lambda e, j=j: e.tensor_tensor(out=ada_sb[:, j * 512:(j + 1) * 512], in0=FB[j][:], in1=brow[:, j * 512:(j + 1) * 512], op=ALU.add),
                     reads=[FB[j], brow], writes=[ada_sb])
            for half in range(2):
                seg = 2 * ng + half
                src = ada_sb[:, half * 1024:(half + 1) * 1024]
                if seg in (2, 5):
                    P.dma("sp", g12[l, 0 if seg == 2 else 1], src, reads=[ada_sb], writes=[g12])
                else:
                    v = {0: 1, 1: 0, 3: 3, 4: 2}[seg]
                    for k in range(KD):
                        P.op("pe", lambda e, half=half, k=k: e.transpose(FB[4][:, 0:128], ada_sb[:, half * 1024 + k * 128: half * 1024 + (k + 1) * 128], identf[:]),
                             reads=[ada_sb, identf], writes=[FB[4]])
                        if seg in (1, 4):
                            nm = 0 if seg == 1 else 1
                            P.op("dve", lambda e, l=l, v=v, k=k, nm=nm: e.scalar_tensor_tensor(
                                out=modc[:, l, v, k:k + 1], in0=FB[4][:, 0:1], scalar=1.0, in1=nmc[:, l, nm, k:k + 1],
                                op0=ALU.add, op1=ALU.mult), reads=[FB[4], nmc], writes=[modc])
                        else:
                            P.op("dve", lambda e, l=l, v=v, k=k: e.tensor_copy(out=modc[:, l, v, k:k + 1], in_=FB[4][:, 0:1]),
                                 reads=[FB[4]], writes=[modc])

    posi = P.sb("posi", [128, NT], I32)
    posf = P.sb("posf", [128, NT], F32)
    inv_bc = P.sb("inv_bcs", [128, 16], F32)
    ang = P.sb("ang", [128, NT, 16], F32)
    kk = P.sb("kk", [128, NT, 16], F32)
    P.dma("sp", posi[:], pos_in[:], writes=[posi])
    P.dma("sp", inv_bc[:], inv_in[:], writes=[inv_bc])
    P.op("dve", lambda e: e.tensor_copy(out=posf[:], in_=posi[:]), reads=[posi], writes=[posf])
    for t in range(NT):
        P.op("dve", lambda e, t=t: e.tensor_scalar(out=ang[:, t, :], in0=inv_bc[:], scalar1=posf[:, t:t + 1], scalar2=None, op0=ALU.mult),
             reads=[inv_bc, posf], writes=[ang])

    MAGIC = 12582912.0
    C1 = 6.28125
    C2 = 2.0 * math.pi - C1

    def sin_reduced(dst, src_t, src_v, shape_v, shift, tmp_t):
        tv = tmp_t[:] if shape_v is None else shape_v(tmp_t)
        P.op("dve", lambda e: e.tensor_scalar(out=tv, in0=src_v, scalar1=shift, scalar2=1.0 / (2 * math.pi), op0=ALU.add, op1=ALU.mult),
             reads=[src_t], writes=[tmp_t])
        P.op("dve", lambda e: e.tensor_scalar(out=tv, in0=tv, scalar1=MAGIC, scalar2=None, op0=ALU.add), reads=[tmp_t], writes=[tmp_t])
        P.op("dve", lambda e: e.tensor_scalar(out=tv, in0=tv, scalar1=-MAGIC, scalar2=None, op0=ALU.add), reads=[tmp_t], writes=[tmp_t])
        P.op("dve", lambda e: e.scalar_tensor_tensor(out=dst[0][:] if shape_v is None else shape_v(dst[0]), in0=tv, scalar=-C1, in1=src_v,
                                                     op0=ALU.mult, op1=ALU.add), reads=[tmp_t, src_t], writes=[dst[0]])
        dv = dst[0][:] if shape_v is None else shape_v(dst[0])
        P.op("dve", lambda e: e.scalar_tensor_tensor(out=dv, in0=tv, scalar=-C2, in1=dv, op0=ALU.mult, op1=ALU.add),
             reads=[tmp_t, dst[0]], writes=[dst[0]])
        P.op("dve", lambda e: e.tensor_scalar(out=dv, in0=dv, scalar1=shift, scalar2=3.14159, op0=ALU.add, op1=ALU.min), reads=[dst[0]], writes=[dst[0]])
        P.op("dve", lambda e: e.tensor_scalar(out=dv, in0=dv, scalar1=-3.14159, scalar2=None, op0=ALU.max), reads=[dst[0]], writes=[dst[0]])
        P.op("act", lambda e: e.activation(out=dv, in_=dv, func=AF.Sin), reads=[dst[0]], writes=[dst[0]])

    sin_reduced((sinT,), ang, ang[:], None, 0.0, kk)
    sin_reduced((cosT,), ang, ang[:], None, math.pi / 2, kk)
    onesrow = P.sb("onesrow", [NH, L], BF16)
    P.op("pool", lambda e: e.memset(onesrow[:], 1.0), writes=[onesrow])
    for r_ in (66, 67):
        P.dma("sp", QTf[:, r_, :], onesrow[:], reads=[onesrow], writes=[QTf])
    for r_ in (64, 65):
        P.dma("sp", KTf[:, r_, :], onesrow[:], reads=[onesrow], writes=[KTf])
    P.pop()

    def load_layer(l):
        lp = lambda n: A[n][l]
        P.dma("pool", w_in_sb[:], lp("w_in").rearrange("(k p) n -> p k n", p=128), writes=[w_in_sb])
        P.dma("pool", w_uq_sb[:], lp("w_uq").rearrange("(k p) n -> p k n", p=128), writes=[w_uq_sb])
        P.dma("pool", w_ukv_sb[:], lp("w_ukv"), writes=[w_ukv_sb])
        P.dma("pool", w_glu_sb[:], lp("w_glu").rearrange("(k p) n -> p k n", p=128), writes=[w_glu_sb])
        for t_, n_ in ((qn_c, "q_norm"), (kvn_c, "kv_norm"), (gqm, "gq_m"), (gkm, "gk_m"), (gqf, "gq_f"), (gkf, "gk_f"),
                       (dcol, "ssm_d"), (bglu_c, "b_glu")):
            P.dma("sp", t_[:], lp(n_), writes=[t_])
        P.dma("sp", nbf[:], lp("fox_bf"), writes=[nbf])
        P.op("dve", lambda e: e.tensor_scalar(out=nbf[:], in0=nbf[:], scalar1=-1.0, scalar2=None, op0=ALU.mult), reads=[nbf], writes=[nbf])
        P.op("dve", lambda e: e.tensor_scalar(out=nbglu_c[:], in0=bglu_c[:], scalar1=-1.0, scalar2=None, op0=ALU.mult), reads=[bglu_c], writes=[nbglu_c])
        for m in range(2):
            P.op("dve", lambda e, m=m: e.tensor_scalar(out=Ddiag[:, m, :], in0=identf[:], scalar1=dcol[:, m:m + 1], scalar2=None, op0=ALU.mult),
                 reads=[identf, dcol], writes=[Ddiag])
        V = lambda i: s5p[:, i, :]
        LRE, LIM, LDT, DT, ZRE, ZIM, MAG, SN, CS, LBR, LBI, DEN, KR, KI, NKI, T1, T2, CK, SK, CK2, SK2 = range(21)
        P.dma("sp", V(LRE), lp("lam_re"), writes=[s5p])
        P.dma("sp", V(LIM), lp("lam_im"), writes=[s5p])
        P.dma("sp", V(LDT), lp("log_dt"), writes=[s5p])
        sop = lambda fn: P.op("dve", fn, reads=[s5p], writes=[s5p])
        P.op("act", lambda e: e.activation(out=V(DT), in_=V(LDT), func=AF.Exp), reads=[s5p], writes=[s5p])
        sop(lambda e: e.tensor_tensor(out=V(ZRE), in0=V(LRE), in1=V(DT), op=ALU.mult))
        sop(lambda e: e.tensor_tensor(out=V(ZIM), in0=V(LIM), in1=V(DT), op=ALU.mult))
        P.op("act", lambda e: e.activation(out=V(MAG), in_=V(ZRE), func=AF.Exp), reads=[s5p], writes=[s5p])
        sv = lambda i: (lambda t: t[:, i, :])
        sin_reduced((s5p,), s5p, V(ZIM), sv(SN), 0.0, s5p) if False else None
        for dst_i, shift in ((SN, 0.0), (CS, math.pi / 2)):
            sop(lambda e, shift=shift: e.tensor_scalar(out=V(T1), in0=V(ZIM), scalar1=shift, scalar2=1.0 / (2 * math.pi), op0=ALU.add, op1=ALU.mult))
            sop(lambda e: e.tensor_scalar(out=V(T1), in0=V(T1), scalar1=MAGIC, scalar2=None, op0=ALU.add))
            sop(lambda e: e.tensor_scalar(out=V(T1), in0=V(T1), scalar1=-MAGIC, scalar2=None, op0=ALU.add))
            sop(lambda e, dst_i=dst_i: e.scalar_tensor_tensor(out=V(dst_i), in0=V(T1), scalar=-C1, in1=V(ZIM), op0=ALU.mult, op1=ALU.add))
            sop(lambda e, dst_i=dst_i: e.scalar_tensor_tensor(out=V(dst_i), in0=V(T1), scalar=-C2, in1=V(dst_i), op0=ALU.mult, op1=ALU.add))
            sop(lambda e, dst_i=dst_i, shift=shift: e.tensor_scalar(out=V(dst_i), in0=V(dst_i), scalar1=shift, scalar2=3.14159, op0=ALU.add, op1=ALU.min))
            sop(lambda e, dst_i=dst_i: e.tensor_scalar(out=V(dst_i), in0=V(dst_i), scalar1=-3.14159, scalar2=None, op0=ALU.max))
            P.op("act", lambda e, dst_i=dst_i: e.activation(out=V(dst_i), in_=V(dst_i), func=AF.Sin), reads=[s5p], writes=[s5p])
        sop(lambda e: e.tensor_tensor(out=V(LBR), in0=V(MAG), in1=V(CS), op=ALU.mult))
        sop(lambda e: e.tensor_tensor(out=V(LBI), in0=V(MAG), in1=V(SN), op=ALU.mult))
        sop(lambda e: e.tensor_tensor(out=V(DEN), in0=V(LRE), in1=V(LRE), op=ALU.mult))
        sop(lambda e: e.tensor_tensor(out=V(T1), in0=V(LIM), in1=V(LIM), op=ALU.mult))
        sop(lambda e: e.tensor_tensor(out=V(DEN), in0=V(DEN), in1=V(T1), op=ALU.add))
        sop(lambda e: e.reciprocal(out=V(DEN), in_=V(DEN)))
        sop(lambda e: e.tensor_scalar(out=V(T2), in0=V(LBR), scalar1=-1.0, scalar2=None, op0=ALU.add))
        sop(lambda e: e.tensor_tensor(out=V(KR), in0=V(T2), in1=V(LRE), op=ALU.mult))
        sop(lambda e: e.tensor_tensor(out=V(T1), in0=V(LBI), in1=V(LIM), op=ALU.mult))
        sop(lambda e: e.tensor_tensor(out=V(KR), in0=V(KR), in1=V(T1), op=ALU.add))
        sop(lambda e: e.tensor_tensor(out=V(KR), in0=V(KR), in1=V(DEN), op=ALU.mult))
        sop(lambda e: e.tensor_tensor(out=V(KI), in0=V(LBI), in1=V(LRE), op=ALU.mult))
        sop(lambda e: e.tensor_tensor(out=V(T1), in0=V(T2), in1=V(LIM), op=ALU.mult))
        sop(lambda e: e.tensor_tensor(out=V(KI), in0=V(KI), in1=V(T1), op=ALU.subtract))
        sop(lambda e: e.tensor_tensor(out=V(KI), in0=V(KI), in1=V(DEN), op=ALU.mult))
        sop(lambda e: e.tensor_scalar(out=V(NKI), in0=V(KI), scalar1=-1.0, scalar2=None, op0=ALU.mult))
        for j in range(8):
            P.dma("sp", blk_f[:, 0, :], lp("b_re")[j], writes=[blk_f])
            P.dma("sp", blk_f[:, 1, :], lp("b_im")[j], writes=[blk_f])
            P.op("dve", lambda e, j=j: e.tensor_scalar(out=blk_o[:, 0, :], in0=blk_f[:, 0, :], scalar1=s5p[:, KR, j:j + 1], scalar2=None, op0=ALU.mult),
                 reads=[blk_f, s5p], writes=[blk_o])
            P.op("dve", lambda e, j=j: e.scalar_tensor_tensor(out=blk_o[:, 0, :], in0=blk_f[:, 1, :], scalar=s5p[:, NKI, j:j + 1], in1=blk_o[:, 0, :],
                                                              op0=ALU.mult, op1=ALU.add), reads=[blk_f, s5p, blk_o], writes=[blk_o])
            P.op("dve", lambda e, j=j: e.tensor_scalar(out=blk_o[:, 1, :], in0=blk_f[:, 1, :], scalar1=s5p[:, KR, j:j + 1], scalar2=None, op0=ALU.mult),
                 reads=[blk_f, s5p], writes=[blk_o])
            P.op("dve", lambda e, j=j: e.scalar_tensor_tensor(out=blk_o[:, 1, :], in0=blk_f[:, 0, :], scalar=s5p[:, KI, j:j + 1], in1=blk_o[:, 1, :],
                                                              op0=ALU.mult, op1=ALU.add), reads=[blk_f, s5p, blk_o], writes=[blk_o])
            for ri, dstT in ((0, BreT), (1, BimT)):
                P.op("pe", lambda e, ri=ri: e.transpose(FB[5][:, 0:128], blk_o[:, ri, :], identf[:]), reads=[blk_o, identf], writes=[FB[5]])
                P.op("act", lambda e, dstT=dstT, j=j: e.activation(out=dstT[:, j, :], in_=FB[5][:, 0:128], func=AF.Copy), reads=[FB[5]], writes=[dstT])
        P.dma("pool", CreT[:], lp("c_re").rearrange("j p f -> p j f"), writes=[CreT])
        P.dma("pool", nCimT[:], lp("c_im").rearrange("j p f -> p j f"), writes=[nCimT])
        P.op("dve", lambda e: e.tensor_scalar(out=nCimT[:], in0=nCimT[:], scalar1=-1.0, scalar2=None, op0=ALU.mult), reads=[nCimT], writes=[nCimT])
        P.op("dve", lambda e: e.memset(Ctab[:, :, 0:1], 1.0), writes=[Ctab])
        P.op("dve", lambda e: e.memset(Stab[:, :, 0:1], 0.0), writes=[Stab])
        sop(lambda e: e.tensor_copy(out=V(CK), in_=V(CS)))
        sop(lambda e: e.tensor_copy(out=V(SK), in_=V(SN)))
        n = 1
        while n < SUB:
            tabt_v = pt[0][:].rearrange("p a b -> p (a b)")[:, 0:8 * n].rearrange("p (j n) -> p j n", j=8)
            tabu_v = pt[1][:].rearrange("p a b -> p (a b)")[:, 0:8 * n].rearrange("p (j n) -> p j n", j=8)
            tabt = pt[0]
            tabu = pt[1]
            ckb = s5p[:, CK, :].unsqueeze(2).to_broadcast([128, 8, n])
            skb = s5p[:, SK, :].unsqueeze(2).to_broadcast([128, 8, n])
            P.op("dve", lambda e, n=n, ckb=ckb: e.tensor_tensor(out=tabt_v, in0=Ctab[:, :, 0:n], in1=ckb, op=ALU.mult), reads=[Ctab, s5p], writes=[tabt])
            P.op("dve", lambda e, n=n, skb=skb: e.tensor_tensor(out=tabu_v, in0=Stab[:, :, 0:n], in1=skb, op=ALU.mult), reads=[Stab, s5p], writes=[tabu])
            P.op("dve", lambda e, n=n: e.tensor_tensor(out=Ctab[:, :, n:2 * n], in0=tabt_v, in1=tabu_v, op=ALU.subtract), reads=[tabt, tabu], writes=[Ctab])
            P.op("dve", lambda e, n=n, skb=skb: e.tensor_tensor(out=tabt_v, in0=Ctab[:, :, 0:n], in1=skb, op=ALU.mult), reads=[Ctab, s5p], writes=[tabt])
            P.op("dve", lambda e, n=n, ckb=ckb: e.tensor_tensor(out=tabu_v, in0=Stab[:, :, 0:n], in1=ckb, op=ALU.mult), reads=[Stab, s5p], writes=[tabu])
            P.op("dve", lambda e, n=n: e.tensor_tensor(out=Stab[:, :, n:2 * n], in0=tabt_v, in1=tabu_v, op=ALU.add), reads=[tabt, tabu], writes=[Stab])
            sop(lambda e: e.tensor_tensor(out=V(T1), in0=V(CK), in1=V(CK), op=ALU.mult))
            sop(lambda e: e.tensor_tensor(out=V(T2), in0=V(SK), in1=V(SK), op=ALU.mult))
            sop(lambda e: e.tensor_tensor(out=V(SK2), in0=V(CK), in1=V(SK), op=ALU.mult))
            sop(lambda e: e.tensor_tensor(out=V(CK), in0=V(T1), in1=V(T2), op=ALU.subtract))
            sop(lambda e: e.tensor_scalar(out=V(SK), in0=V(SK2), scalar1=2.0, scalar2=None, op0=ALU.mult))
            n *= 2
        P.op("dve", lambda e: e.tensor_copy(out=Rtab[:], in_=s5p[:, MAG, :].unsqueeze(2).to_broadcast([128, 8, SUB])), reads=[s5p], writes=[Rtab])
        return dict(CK=CK, SK=SK)

    def load_c(l):
        P.dma("sp", onorm_c[:], A["out_norm"][l], writes=[onorm_c])
        P.dma("sp", G_bc[:], g12[l, 0], reads=[g12], writes=[G_bc])
        for k in range(KD):
            st = wstc[k % 2]
            P.dma("sp", st[:], A["w_out"][l][k * 128:(k + 1) * 128, :], writes=[st])
            P.op("dve", lambda e, st=st, k=k: e.scalar_tensor_tensor(out=w_out_sb[:, k, :], in0=st[:], scalar=onorm_c[:, k:k + 1], in1=G_bc[:],
                                                                     op0=ALU.mult, op1=ALU.mult), reads=[st, onorm_c, G_bc], writes=[w_out_sb])

    def norm_transpose(src_t, src_v, l, v_a, v_b, dst_t, dst_slice, si, fp32_path=None):
        st_ = stat[si % 4]
        P.op("act", lambda e: e.activation(out=sq_junk[:], in_=src_v, func=AF.Square, accum_out=st_[:, 0:1]), reads=[src_t], writes=[sq_junk, st_])
        P.op("act", lambda e: e.activation(out=st_[:, 1:2], in_=st_[:, 0:1], func=AF.Sqrt, scale=1.0 / D, bias=eps_c[:, 0:1]), reads=[st_, eps_c], writes=[st_])
        P.op("dve", lambda e: e.reciprocal(out=st_[:, 2:3], in_=st_[:, 1:2]), reads=[st_], writes=[st_])
        xh_ = xh[si % 2]
        P.op("dve", lambda e: e.tensor_scalar(out=xh_[:], in0=src_v, scalar1=st_[:, 2:3], scalar2=None, op0=ALU.mult), reads=[src_t, st_], writes=[xh_])
        tb = TB[si % 2]
        for k in range(KD):
            P.op("pe", lambda e, k=k: e.transpose(tb[:, k * 128:(k + 1) * 128], xh_[:, k * 128:(k + 1) * 128], ident[:]), reads=[xh_, ident], writes=[tb])
        for k in range(KD):
            P.op("act", lambda e, k=k: e.activation(out=dst_t[:, k, dst_slice], in_=tb[:, k * 128:(k + 1) * 128], func=AF.Identity,
                                                    scale=modc[:, l, v_a, k:k + 1], bias=modc[:, l, v_b, k:k + 1]), reads=[tb, modc], writes=[dst_t])
        if fp32_path is not None:
            xhf_, dstf = fp32_path
            P.op("dve", lambda e: e.tensor_scalar(out=xhf_[:], in0=src_v, scalar1=st_[:, 2:3], scalar2=None, op0=ALU.mult), reads=[src_t, st_], writes=[xhf_])
            for k in range(KD):
                fb = FB[k % 2]
                P.op("pe", lambda e, k=k, fb=fb: e.transpose(fb[:, 0:128], xhf_[:, k * 128:(k + 1) * 128], identf[:]), reads=[xhf_, identf], writes=[fb])
                P.op("act", lambda e, k=k, fb=fb: e.activation(out=dstf[:, k, :], in_=fb[:, 0:128], func=AF.Identity,
                                                             scale=modc[:, l, v_a, k:k + 1], bias=modc[:, l, v_b, k:k + 1]), reads=[fb, modc], writes=[dstf])

    def head_rms(src_t, src_v3, nh, hd, gain_t, dst_t, dst_v3, si, extra_ss=None):
        st_ = stat[si % 4]
        P.op("act", lambda e: e.activation(out=sq_junk[:, 0:nh * hd].rearrange("p (h d) -> p h d", h=nh), in_=src_v3, func=AF.Square), reads=[src_t], writes=[sq_junk])
        P.op("dve", lambda e: e.tensor_reduce(out=st_[:, 0:nh], in_=sq_junk[:, 0:nh * hd].rearrange("p (h d) -> p h d", h=nh), axis=AX.X, op=ALU.add),
             reads=[sq_junk], writes=[st_])
        tot = hd
        if extra_ss is not None:
            et, ev, en = extra_ss
            P.op("dve", lambda e: e.tensor_scalar(out=st_[:, 0:nh], in0=st_[:, 0:nh], scalar1=ev, scalar2=None, op0=ALU.add), reads=[st_, et], writes=[st_])
            tot = hd + en
        P.op("act", lambda e: e.activation(out=st_[:, 6:6 + nh], in_=st_[:, 0:nh], func=AF.Sqrt, scale=1.0 / tot, bias=eps_c[:, 0:1]), reads=[st_, eps_c], writes=[st_])
        P.op("dve", lambda e: e.reciprocal(out=st_[:, 6:6 + nh], in_=st_[:, 6:6 + nh]), reads=[st_], writes=[st_])
        return st_

    def rope(src_t, dst_t, cos_v, sin_v):
        x1 = src_t[:, :, 64:80]
        x2 = src_t[:, :, 80:96]
        cb = cos_v.unsqueeze(1).to_broadcast([128, NH, 16])
        sb_ = sin_v.unsqueeze(1).to_broadcast([128, NH, 16])
        P.op("dve", lambda e: e.tensor_copy(out=dst_t[:, :, 0:64], in_=src_t[:, :, 0:64]), reads=[src_t], writes=[dst_t])
        P.op("dve", lambda e: e.tensor_tensor(out=rt[0][:], in0=x1, in1=cb, op=ALU.mult), reads=[src_t, cosT], writes=[rt[0]])
        P.op("dve", lambda e: e.tensor_tensor(out=rt[1][:], in0=x2, in1=sb_, op=ALU.mult), reads=[src_t, sinT], writes=[rt[1]])
        P.op("dve", lambda e: e.tensor_tensor(out=dst_t[:, :, 64:80], in0=rt[0][:], in1=rt[1][:], op=ALU.subtract), reads=[rt[0], rt[1]], writes=[dst_t])
        P.op("dve", lambda e: e.tensor_tensor(out=rt[2][:], in0=x1, in1=sb_, op=ALU.mult), reads=[src_t, sinT], writes=[rt[2]])
        P.op("dve", lambda e: e.tensor_tensor(out=rt[3][:], in0=x2, in1=cb, op=ALU.mult), reads=[src_t, cosT], writes=[rt[3]])
        P.op("dve", lambda e: e.tensor_tensor(out=dst_t[:, :, 80:96], in0=rt[2][:], in1=rt[3][:], op=ALU.add), reads=[rt[2], rt[3]], writes=[dst_t])

    def phase_a(l, s5c):
        src = x_in if l == 0 else None
        def stream_tiles(b):
            hTb = hT[b % 2]
            for ti in range(4):
                t = b * 4 + ti
                xs_ = xs[t % 2]
                if l == 0:
                    P.dma("sp", xs_[:], x_in[t * 128:(t + 1) * 128, :], writes=[xs_])
                else:
                    P.dma("sp", xs_[:], y[t * 128:(t + 1) * 128, :], reads=[ytile[t]], writes=[xs_])
                norm_transpose(xs_, xs_[:], l, 0, 1, hTb, slice(ti * 128, (ti + 1) * 128), t)
            for m in range(2):
                for k in range(KD):
                    P.op("pe", lambda e, m=m, k=k: e.matmul(FB[m][:], lhsT=w_in_sb[:, k, m * 128:(m + 1) * 128], rhs=hTb[:, k, :], start=(k == 0), stop=(k == KD - 1)),
                         reads=[w_in_sb, hTb], writes=[FB[m]])
                P.op("act", lambda e, m=m: e.activation(out=uT_l[b % 2][:, m, :], in_=FB[m][:], func=AF.Copy), reads=[FB[m]], writes=[uT_l[b % 2]])
            for k in range(KD):
                P.op("pe", lambda e, k=k: e.matmul(FB[2][0:NH, :], lhsT=w_in_sb[:, k, 1824:1830], rhs=hTb[:, k, :], start=(k == 0), stop=(k == KD - 1)),
                     reads=[w_in_sb, hTb], writes=[FB[2]])
            P.op("act", lambda e: e.activation(out=fg_e[:], in_=FB[2][0:NH, :], func=AF.Exp, scale=-1.0, bias=nbf[:, 0:1]), reads=[FB[2], nbf], writes=[fg_e])
            P.op("act", lambda e: e.activation(out=fg_sp[:], in_=fg_e[:], func=AF.Ln, bias=ones_f[0:NH, 0:1]), reads=[fg_e, ones_f], writes=[fg_sp])
            if b == 0:
                P.op("dve", lambda e: e.tensor_tensor_scan(out=fg_cum[:], data0=ones_f[0:NH, 0:512], data1=fg_sp[:], initial=0.0, op0=ALU.mult, op1=ALU.add),
                     reads=[ones_f, fg_sp], writes=[fg_cum])
            else:
                P.op("dve", lambda e: e.tensor_tensor_scan(out=fg_cum[:], data0=ones_f[0:NH, 0:512], data1=fg_sp[:], initial=fg_carry[:, 0:1], op0=ALU.mult, op1=ALU.add),
                     reads=[ones_f, fg_sp, fg_carry], writes=[fg_cum])
            P.op("dve", lambda e: e.tensor_copy(out=fg_carry[:], in_=fg_cum[:, 511:512]), reads=[fg_cum], writes=[fg_carry])
            P.op("dve", lambda e: e.tensor_scalar(out=fg_sp[:], in0=fg_cum[:], scalar1=8.0, scalar2=None, op0=ALU.mult), reads=[fg_cum], writes=[fg_sp])
            P.op("dve", lambda e: e.tensor_copy(out=fg_hi[:], in_=fg_sp[:]), reads=[fg_sp], writes=[fg_hi])
            P.op("dve", lambda e: e.tensor_tensor(out=fg_lo[:], in0=fg_sp[:], in1=fg_hi[:], op=ALU.subtract), reads=[fg_sp, fg_hi], writes=[fg_lo])
            P.op("dve", lambda e: e.tensor_scalar(out=fg_nhi[:], in0=fg_hi[:], scalar1=-1.0, scalar2=None, op0=ALU.mult), reads=[fg_hi], writes=[fg_nhi])
            P.op("dve", lambda e: e.tensor_scalar(out=fg_nlo[:], in0=fg_lo[:], scalar1=-1.0, scalar2=None, op0=ALU.mult), reads=[fg_lo], writes=[fg_nlo])
            bs = slice(b * 512, (b + 1) * 512)
            P.dma("sp", QTf[:, 64, bs], fg_nhi[:], reads=[fg_nhi], writes=[QTf])
            P.dma("sp", QTf[:, 65, bs], fg_nlo[:], reads=[fg_nlo], writes=[QTf])
            P.dma("sp", KTf[:, 66, bs], fg_hi[:], reads=[fg_hi], writes=[KTf])
            P.dma("sp", KTf[:, 67, bs], fg_lo[:], reads=[fg_lo], writes=[KTf])

            for ti in range(4):
                t = b * 4 + ti
                tsl = slice(ti * 128, (ti + 1) * 128)
                def proj_seg(fb, c0, w):
                    for k in range(KD):
                        P.op("pe", lambda e: e.matmul(fb[:, 0:w], lhsT=hTb[:, k, tsl], rhs=w_in_sb[:, k, c0:c0 + w], start=(k == 0), stop=(k == KD - 1)),
                             reads=[hTb, w_in_sb], writes=[fb])
                proj_seg(FB[2], 256, 416)
                st_ = stat[0]
                P.op("act", lambda e: e.activation(out=sq_junk[:, 0:256], in_=FB[2][:, 0:256], func=AF.Square, accum_out=st_[:, 12:13]), reads=[FB[2]], writes=[sq_junk, st_])
                P.op("act", lambda e: e.activation(out=st_[:, 13:14], in_=st_[:, 12:13], func=AF.Sqrt, scale=1.0 / 256, bias=eps_c[:, 0:1]), reads=[st_, eps_c], writes=[st_])
                P.op("dve", lambda e: e.reciprocal(out=st_[:, 13:14], in_=st_[:, 13:14]), reads=[st_], writes=[st_])
                P.op("dve", lambda e: e.tensor_scalar(out=cq_h[:], in0=FB[2][:, 0:256], scalar1=st_[:, 13:14], scalar2=None, op0=ALU.mult), reads=[FB[2], st_], writes=[cq_h])
                st1 = stat[1]
                P.op("act", lambda e: e.activation(out=sq_junk[:, 256:384], in_=FB[2][:, 256:384], func=AF.Square, accum_out=st1[:, 12:13]), reads=[FB[2]], writes=[sq_junk, st1])
                P.op("act", lambda e: e.activation(out=st1[:, 13:14], in_=st1[:, 12:13], func=AF.Sqrt, scale=1.0 / 128, bias=eps_c[:, 0:1]), reads=[st1, eps_c], writes=[st1])
                P.op("dve", lambda e: e.reciprocal(out=st1[:, 13:14], in_=st1[:, 13:14]), reads=[st1], writes=[st1])
                P.op("dve", lambda e: e.tensor_scalar(out=ckv_h[:], in0=FB[2][:, 256:384], scalar1=st1[:, 13:14], scalar2=None, op0=ALU.mult), reads=[FB[2], st1], writes=[ckv_h])
                P.op("act", lambda e: e.activation(out=kn[:, 0, 64:96], in_=FB[2][:, 384:416], func=AF.Copy), reads=[FB[2]], writes=[kn])
                P.op("act", lambda e: e.activation(out=sq_junk[:, 384:416], in_=FB[2][:, 384:416], func=AF.Square, accum_out=st1[:, 14:15]), reads=[FB[2]], writes=[sq_junk, st1])
                tb = TB[0]
                for j in range(2):
                    P.op("pe", lambda e, j=j: e.transpose(tb[:, j * 128:(j + 1) * 128], cq_h[:, j * 128:(j + 1) * 128], ident[:]), reads=[cq_h, ident], writes=[tb])
                P.op("pe", lambda e: e.transpose(tb[:, 256:384], ckv_h[:], ident[:]), reads=[ckv_h, ident], writes=[tb])
                for j in range(2):
                    P.op("act", lambda e, j=j: e.activation(out=cqT[:, j, :], in_=tb[:, j * 128:(j + 1) * 128], func=AF.Copy, scale=qn_c[:, j:j + 1]), reads=[tb, qn_c], writes=[cqT])
                P.op("act", lambda e: e.activation(out=ckvT[:], in_=tb[:, 256:384], func=AF.Copy, scale=kvn_c[:, 0:1]), reads=[tb, kvn_c], writes=[ckvT])
                for j in range(2):
                    P.op("pe", lambda e, j=j: e.matmul(FB[0][:], lhsT=cqT[:, j, :], rhs=w_uq_sb[:, j, 0:512], start=(j == 0), stop=(j == 1)), reads=[cqT, w_uq_sb], writes=[FB[0]])
                for j in range(2):
                    P.op("pe", lambda e, j=j: e.matmul(FB[1][:, 0:64], lhsT=cqT[:, j, :], rhs=w_uq_sb[:, j, 512:576], start=(j == 0), stop=(j == 1)), reads=[cqT, w_uq_sb], writes=[FB[1]])
                P.op("act", lambda e: e.activation(out=qn[:].rearrange("p h d -> p (h d)")[:, 0:512], in_=FB[0][:], func=AF.Copy), reads=[FB[0]], writes=[qn])
                P.op("act", lambda e: e.activation(out=qn[:].rearrange("p h d -> p (h d)")[:, 512:576], in_=FB[1][:, 0:64], func=AF.Copy), reads=[FB[1]], writes=[qn])
                sq = head_rms(qn, qn[:], NH, 96, gqm, None, None, 2)
                P.op("dve", lambda e, sq=sq: e.tensor_tensor(out=qn[:], in0=qn[:], in1=sq[:, 6:12].unsqueeze(2).to_broadcast([128, NH, 96]), op=ALU.mult), reads=[qn, sq], writes=[qn])
                P.op("dve", lambda e: e.tensor_tensor(out=qn[:], in0=qn[:], in1=gqm[:].unsqueeze(1).to_broadcast([128, NH, 96]), op=ALU.mult), reads=[qn, gqm], writes=[qn])
                rope(qn, qfin, cosT[:, t, :], sinT[:, t, :])
                P.op("pe", lambda e: e.matmul(FB[0][:], lhsT=ckvT[:], rhs=w_ukv_sb[:, 0:512], start=True, stop=True), reads=[ckvT, w_ukv_sb], writes=[FB[0]])
                P.op("pe", lambda e: e.matmul(FB[1][:, 0:256], lhsT=ckvT[:], rhs=w_ukv_sb[:, 512:768], start=True, stop=True), reads=[ckvT, w_ukv_sb], writes=[FB[1]])
                vst = Vm_st[t % 2]
                kv0 = FB[0][:].rearrange("p (h d) -> p h d", h=4)
                kv1 = FB[1][:, 0:256].rearrange("p (h d) -> p h d", h=2)
                P.op("act", lambda e: e.activation(out=kn[:, 0:4, 0:64], in_=kv0[:, :, 0:64], func=AF.Copy), reads=[FB[0]], writes=[kn])
                P.op("act", lambda e: e.activation(out=kn[:, 4:6, 0:64], in_=kv1[:, :, 0:64], func=AF.Copy), reads=[FB[1]], writes=[kn])
                P.op("dve", lambda e: e.tensor_copy(out=vst[:, 0:4, 0:64], in_=kv0[:, :, 64:128]), reads=[FB[0]], writes=[vst])
                P.op("dve", lambda e: e.tensor_copy(out=vst[:, 4:6, 0:64], in_=kv1[:, :, 64:128]), reads=[FB[1]], writes=[vst])
                P.dma("sp", Vm[t * 128:(t + 1) * 128, :], vst[:].rearrange("p h d -> p (h d)"), reads=[vst], writes=[Vm])
                P.op("dve", lambda e: e.tensor_copy(out=kn[:, 1:6, 64:96], in_=kn[:, 0:1, 64:96].to_broadcast([128, 5, 32])), reads=[kn], writes=[kn])
                st3 = stat[3]
                P.op("act", lambda e: e.activation(out=sq_junk[:, 0:384].rearrange("p (h d) -> p h d", h=NH), in_=kn[:, :, 0:64], func=AF.Square), reads=[kn], writes=[sq_junk])
                P.op("dve", lambda e: e.tensor_reduce(out=st3[:, 0:NH], in_=sq_junk[:, 0:384].rearrange("p (h d) -> p h d", h=NH), axis=AX.X, op=ALU.add), reads=[sq_junk], writes=[st3])
                P.op("dve", lambda e: e.tensor_scalar(out=st3[:, 0:NH], in0=st3[:, 0:NH], scalar1=st1[:, 14:15], scalar2=None, op0=ALU.add), reads=[st3, st1], writes=[st3])
                P.op("act", lambda e: e.activation(out=st3[:, 6:12], in_=st3[:, 0:NH], func=AF.Sqrt, scale=1.0 / 96, bias=eps_c[:, 0:1]), reads=[st3, eps_c], writes=[st3])
                P.op("dve", lambda e: e.reciprocal(out=st3[:, 6:12], in_=st3[:, 6:12]), reads=[st3], writes=[st3])
                P.op("dve", lambda e: e.tensor_tensor(out=kn[:], in0=kn[:], in1=st3[:, 6:12].unsqueeze(2).to_broadcast([128, NH, 96]), op=ALU.mult), reads=[kn, st3], writes=[kn])
                P.op("dve", lambda e: e.tensor_tensor(out=kn[:], in0=kn[:], in1=gkm[:].unsqueeze(1).to_broadcast([128, NH, 96]), op=ALU.mult), reads=[kn, gkm], writes=[kn])
                rope(kn, kfin, cosT[:, t, :], sinT[:, t, :])
                gsl = slice(t * 128, (t + 1) * 128)
                for src_, st_l, dst_d in ((qfin, QTm_st, QTm), (kfin, KTm_st, KTm)):
                    tb2 = TB[1]
                    st_t = st_l[t % 2]
                    for h in range(NH):
                        P.op("pe", lambda e, h=h, src_=src_: e.transpose(tb2[0:96, h * 128:(h + 1) * 128], src_[:, h, :], ident[:]), reads=[src_, ident], writes=[tb2])
                    P.op("act", lambda e, st_t=st_t: e.activation(out=st_t[:], in_=tb2[0:96, 0:768].rearrange("p (h t) -> p h t", h=NH), func=AF.Copy), reads=[tb2], writes=[st_t])
                    P.dma("sp", dst_d[:, :, gsl].rearrange("h p t -> p h t"), st_t[:], reads=[st_t], writes=[dst_d])
                for fb, g_t, dstb, st_l, dst_d, c0_ in ((FB[3], gqf, fqb, QTf_st, QTf, 672), (FB[3], gkf, fkb, KTf_st, KTf, 1056)):
                    st_t = st_l[t % 2]
                    proj_seg(FB[3], c0_, 384)
                    P.op("act", lambda e, fb=fb: e.activation(out=fqn[:].rearrange("p h d -> p (h d)"), in_=fb[:, 0:384], func=AF.Copy), reads=[fb], writes=[fqn])
                    sq = head_rms(fqn, fqn[:], NH, 64, g_t, None, None, 2)
                    P.op("dve", lambda e, sq=sq: e.tensor_tensor(out=fqn[:], in0=fqn[:], in1=sq[:, 6:12].unsqueeze(2).to_broadcast([128, NH, 64]), op=ALU.mult), reads=[fqn, sq], writes=[fqn])
                    P.op("dve", lambda e, g_t=g_t, dstb=dstb: e.tensor_tensor(out=dstb[:], in0=fqn[:], in1=g_t[:].unsqueeze(1).to_broadcast([128, NH, 64]), op=ALU.mult), reads=[fqn, g_t], writes=[dstb])
                    tb2 = TB[1]
                    for h in range(NH):
                        P.op("pe", lambda e, h=h, dstb=dstb: e.transpose(tb2[0:64, h * 128:(h + 1) * 128], dstb[:, h, :], ident[:]), reads=[dstb, ident], writes=[tb2])
                    P.op("act", lambda e, st_t=st_t: e.activation(out=st_t[:], in_=tb2[0:64, 0:768].rearrange("p (h t) -> p h t", h=NH), func=AF.Copy), reads=[tb2], writes=[st_t])
                    P.dma("sp", dst_d[:, 0:64, gsl].rearrange("h p t -> p h t"), st_t[:], reads=[st_t], writes=[dst_d])
                vst = Vf_st[t % 2]
                proj_seg(FB[3], 1440, 384)
                P.op("dve", lambda e, vst=vst: e.tensor_copy(out=vst[:, :, 0:64], in_=FB[3][:, 0:384].rearrange("p (h d) -> p h d", h=NH)), reads=[FB[3]], writes=[vst])
                P.dma("sp", Vf[t * 128:(t + 1) * 128, :], vst[:].rearrange("p h d -> p (h d)"), reads=[vst], writes=[Vf])

        def stream_s5(b):
            for sc in range(512 // SUB):
                first = (b == 0 and sc == 0)
                ss_ = slice(sc * SUB, (sc + 1) * SUB)
                gs = slice(b * 512 + sc * SUB, b * 512 + (sc + 1) * SUB)
                if not first:
                    wl_re = wlast[:, 0, :]
                    wl_im = wlast[:, 1, :]
                    ck = s5p[:, s5c["CK"], :]
                    sk = s5p[:, s5c["SK"], :]
                    P.op("dve", lambda e: e.tensor_tensor(out=w0t[:, 0, :], in0=wl_re, in1=ck, op=ALU.mult), reads=[wlast, s5p], writes=[w0t])
                    P.op("dve", lambda e: e.tensor_tensor(out=w0t[:, 1, :], in0=wl_im, in1=sk, op=ALU.mult), reads=[wlast, s5p], writes=[w0t])
                    P.op("dve", lambda e: e.tensor_tensor(out=w0t[:, 2, :], in0=wl_re, in1=sk, op=ALU.mult), reads=[wlast, s5p], writes=[w0t])
                    P.op("dve", lambda e: e.tensor_tensor(out=w0t[:, 3, :], in0=wl_im, in1=ck, op=ALU.mult), reads=[wlast, s5p], writes=[w0t])
                    P.op("dve", lambda e: e.tensor_tensor(out=w0[:, 0, :], in0=w0t[:, 0, :], in1=w0t[:, 1, :], op=ALU.subtract), reads=[w0t], writes=[w0])
                    P.op("dve", lambda e: e.tensor_tensor(out=w0[:, 1, :], in0=w0t[:, 2, :], in1=w0t[:, 3, :], op=ALU.add), reads=[w0t], writes=[w0])
                for m in range(2):
                    hs = slice(4 * m, 4 * m + 4)
                    for jj in range(4):
                        j = 4 * m + jj
                        fbp = FB[4]
                        P.op("pe", lambda e: e.matmul(fbp[:, 0:SUB], lhsT=BreT[:, j, :], rhs=uT_l[b % 2][:, m, ss_], start=True, stop=True), reads=[BreT, uT_l[b % 2]], writes=[fbp])
                        P.op("pe", lambda e: e.matmul(fbp[:, SUB:2 * SUB], lhsT=BimT[:, j, :], rhs=uT_l[b % 2][:, m, ss_], start=True, stop=True), reads=[BimT, uT_l[b % 2]], writes=[fbp])
                        b2 = fbp[:, 0:2 * SUB].rearrange("p (r t) -> p r t", r=2)
                        cb = Ctab[:, j, :].unsqueeze(1).to_broadcast([128, 2, SUB])
                        sb_ = Stab[:, j, :].unsqueeze(1).to_broadcast([128, 2, SUB])
                        P.op("dve", lambda e: e.tensor_tensor(out=pre_c[:], in0=b2, in1=cb, op=ALU.mult), reads=[fbp, Ctab], writes=[pre_c])
                        P.op("dve", lambda e: e.tensor_tensor(out=pre_s[:], in0=b2, in1=sb_, op=ALU.mult), reads=[fbp, Stab], writes=[pre_s])
                        P.op("dve", lambda e: e.tensor_tensor(out=pin_re[:], in0=pre_c[:, 0, :], in1=pre_s[:, 1, :], op=ALU.add), reads=[pre_c, pre_s], writes=[pin_re])
                        P.op("dve", lambda e: e.tensor_tensor(out=pin_im[:], in0=pre_c[:, 1, :], in1=pre_s[:, 0, :], op=ALU.subtract), reads=[pre_c, pre_s], writes=[pin_im])
                        for pin, Wt, ri in ((pin_re, W_re, 0), (pin_im, W_im, 1)):
                            if first:
                                P.op("dve", lambda e: e.tensor_tensor_scan(out=Wt[:, jj, :], data0=Rtab[:, j, :], data1=pin[:], initial=0.0, op0=ALU.mult, op1=ALU.add),
                                     reads=[Rtab, pin], writes=[Wt])
                            else:
                                P.op("dve", lambda e: e.tensor_tensor_scan(out=Wt[:, jj, :], data0=Rtab[:, j, :], data1=pin[:], initial=w0[:, ri, j:j + 1],
                                                                           op0=ALU.mult, op1=ALU.add), reads=[Rtab, pin, w0], writes=[Wt])
                    P.op("dve", lambda e: e.tensor_copy(out=wlast[:, 0, hs], in_=W_re[:, :, SUB - 1]), reads=[W_re], writes=[wlast])
                    P.op("dve", lambda e: e.tensor_copy(out=wlast[:, 1, hs], in_=W_im[:, :, SUB - 1]), reads=[W_im], writes=[wlast])
                    Ch = Ctab[:, hs, :]
                    Sh = Stab[:, hs, :]
                    P.op("dve", lambda e: e.tensor_tensor(out=pt[0][:], in0=W_re[:], in1=Ch, op=ALU.mult), reads=[W_re, Ctab], writes=[pt[0]])
                    P.op("dve", lambda e: e.tensor_tensor(out=pt[1][:], in0=W_im[:], in1=Sh, op=ALU.mult), reads=[W_im, Stab], writes=[pt[1]])
                    P.op("dve", lambda e: e.tensor_tensor(out=s_re[:], in0=pt[0][:], in1=pt[1][:], op=ALU.subtract), reads=[pt[0], pt[1]], writes=[s_re])
                    P.op("dve", lambda e: e.tensor_tensor(out=pt[0][:], in0=W_re[:], in1=Sh, op=ALU.mult), reads=[W_re, Stab], writes=[pt[0]])
                    P.op("dve", lambda e: e.tensor_tensor(out=pt[1][:], in0=W_im[:], in1=Ch, op=ALU.mult), reads=[W_im, Stab], writes=[pt[1]])
                    P.op("dve", lambda e: e.tensor_tensor(out=s_im[:], in0=pt[0][:], in1=pt[1][:], op=ALU.add), reads=[pt[0], pt[1]], writes=[s_im])
                    osl = slice(m * SUB, (m + 1) * SUB)
                    for jj in range(4):
                        j = 4 * m + jj
                        P.op("pe", lambda e: e.matmul(FB[5][:, osl], lhsT=CreT[:, j, :], rhs=s_re[:, jj, :], start=(jj == 0), stop=False), reads=[CreT, s_re], writes=[FB[5]])
                        P.op("pe", lambda e: e.matmul(FB[5][:, osl], lhsT=nCimT[:, j, :], rhs=s_im[:, jj, :], start=False, stop=False), reads=[nCimT, s_im], writes=[FB[5]])
                    P.op("pe", lambda e: e.matmul(FB[5][:, osl], lhsT=Ddiag[:, m, :], rhs=uT_l[b % 2][:, m, ss_], start=False, stop=True), reads=[Ddiag, uT_l[b % 2]], writes=[FB[5]])
                yv = FB[5][:, 0:2 * SUB].rearrange("p (m t) -> p m t", m=2)
                P.op("act", lambda e: e.activation(out=yg[:], in_=yv, func=AF.Copy), reads=[FB[5]], writes=[yg])
                P.op("dve", lambda e: e.tensor_tensor(out=yt1[:], in0=yg[:], in1=yg[:], op=ALU.mult), reads=[yg], writes=[yt1])
                P.op("dve", lambda e: e.tensor_scalar(out=yt1[:], in0=yt1[:], scalar1=0.044715, scalar2=1.0, op0=ALU.mult, op1=ALU.add), reads=[yt1], writes=[yt1])
                P.op("dve", lambda e: e.tensor_tensor(out=yt1[:], in0=yt1[:], in1=yg[:], op=ALU.mult), reads=[yt1, yg], writes=[yt1])
                P.op("dve", lambda e: e.tensor_scalar(out=yt1[:], in0=yt1[:], scalar1=-45.0, scalar2=None, op0=ALU.max), reads=[yt1], writes=[yt1])
                P.op("act", lambda e: e.activation(out=yt1[:], in_=yt1[:], func=AF.Exp, scale=-1.5957691216), reads=[yt1], writes=[yt1])
                P.op("dve", lambda e: e.tensor_scalar(out=yt1[:], in0=yt1[:], scalar1=1.0, scalar2=None, op0=ALU.add), reads=[yt1], writes=[yt1])
                P.op("dve", lambda e: e.reciprocal(out=yt1[:], in_=yt1[:]), reads=[yt1], writes=[yt1])
                P.op("dve", lambda e: e.tensor_tensor(out=yg[:], in0=yg[:], in1=yt1[:], op=ALU.mult), reads=[yg, yt1], writes=[yg])
                P.op("dve", lambda e: e.tensor_copy(out=yTb[:], in_=yg[:]), reads=[yg], writes=[yTb])
                for mo in range(2):
                    osl = slice(mo * SUB, (mo + 1) * SUB)
                    for k in range(2):
                        P.op("pe", lambda e, mo=mo, k=k, osl=osl: e.matmul(FB[5][:, osl], lhsT=w_glu_sb[:, k, mo * 128:(mo + 1) * 128], rhs=yTb[:, k, :], start=(k == 0), stop=(k == 1)),
                             reads=[w_glu_sb, yTb], writes=[FB[5]])
                    P.op("act", lambda e, mo=mo, osl=osl: e.activation(out=yt2[:, mo, :], in_=FB[5][:, osl], func=AF.Exp, scale=-1.0, bias=nbglu_c[:, mo:mo + 1]), reads=[FB[5], nbglu_c], writes=[yt2])
                P.op("dve", lambda e: e.tensor_scalar(out=yt2[:], in0=yt2[:], scalar1=1.0, scalar2=None, op0=ALU.add), reads=[yt2], writes=[yt2])
                P.op("dve", lambda e: e.reciprocal(out=yt2[:], in_=yt2[:]), reads=[yt2], writes=[yt2])
                P.op("dve", lambda e: e.tensor_tensor(out=osm[:], in0=yg[:], in1=yt2[:], op=ALU.mult), reads=[yg, yt2], writes=[osm])
                P.op("dve", lambda e: e.tensor_tensor(out=o2[:], in0=osm[:], in1=osm[:], op=ALU.mult), reads=[osm], writes=[o2])
                for m in range(2):
                    P.op("pe", lambda e, m=m: e.matmul(FB[5][:, 0:SUB], lhsT=ones_f[:, 0:128], rhs=o2[:, m, :], start=(m == 0), stop=(m == 1)), reads=[ones_f, o2], writes=[FB[5]])
                P.op("act", lambda e: e.activation(out=rs_bc[:], in_=FB[5][:, 0:SUB], func=AF.Sqrt, scale=1.0 / 256, bias=eps_c[:, 0:1]), reads=[FB[5], eps_c], writes=[rs_bc])
                P.op("dve", lambda e: e.reciprocal(out=rs_bc[:], in_=rs_bc[:]), reads=[rs_bc], writes=[rs_bc])
                P.op("dve", lambda e: e.tensor_tensor(out=msm[:], in0=osm[:], in1=rs_bc[:].unsqueeze(1).to_broadcast([128, 2, SUB]), op=ALU.mult), reads=[osm, rs_bc], writes=[msm])
                P.dma("sp", mssm[:, :, gs].rearrange("m p t -> p m t"), msm[:], reads=[msm], writes=[mssm])


        for b in range(NB + 1):
            f1 = (lambda b=b: stream_tiles(b)) if b < NB else None
            f2 = (lambda b=b: stream_s5(b - 1)) if b >= 1 else None
            if f1 is not None and f2 is not None:
                run_pair(P, f1, f2)
            else:
                (f1 or f2)()

    def phase_b(l):
        hi = 0
        ob_i = 0
        it = 0
        for mixer in range(2):
            QTd, KTd, Vd, dk, scale = ((QTm, KTm, Vm, 96, 1.0 / math.sqrt(96.0)), (QTf, KTf, Vf, 68, 0.125))[mixer]
            P.dma("sp", V_sb[:], Vd[:].rearrange("(t p) c -> p t c", p=128), reads=[Vd], writes=[V_sb])
            for h in range(NH):
                Qs = QT_sb[hi % 2]
                Ks = KT_sb[hi % 2]
                hi += 1
                P.dma("sp", Qs[0:dk, :], QTd[h], reads=[QTd], writes=[Qs])
                P.dma("sp", Ks[0:dk, :], KTd[h], reads=[KTd], writes=[Ks])
                units = [(b, kt) for b in range(NB) for kt in range(4 * b + 4)]

                def front(u):
                    nonlocal it
                    b, kt = u
                    j = kt - 4 * b
                    q0 = 0 if j <= 0 else 128 * j
                    Sb = FB[it % 2]
                    pt_ = PT[it % 3]
                    it += 1
                    qsl = slice(b * 512 + q0, (b + 1) * 512)
                    P.op("pe", lambda e: e.matmul(Sb[:, q0:512], lhsT=Ks[0:dk, kt * 128:(kt + 1) * 128], rhs=Qs[0:dk, qsl], start=True, stop=True),
                         reads=[Ks, Qs], writes=[Sb])
                    P.op("act", lambda e: e.activation(out=pt_[:, q0:512], in_=Sb[:, q0:512], func=AF.Exp, scale=scale), reads=[Sb], writes=[pt_])
                    if j >= 0:
                        if mixer == 0:
                            P.op("pool", lambda e: e.memset(pt_[64:128, q0:q0 + 64], 0.0), writes=[pt_])
                        else:
                            P.op("pool", lambda e: e.affine_select(out=pt_[:, q0:q0 + 128], in_=pt_[:, q0:q0 + 128], pattern=[[1, 128]], compare_op=ALU.is_ge,
                                                                 fill=0.0, base=0, channel_multiplier=-1), reads=[pt_], writes=[pt_])
                    return pt_, q0

                def evac(b, OT, ob):
                    Otr = FB[4 + (ob % 2)]
                    osb_ = osb[ob % 2]
                    rd = rden[ob % 2]
                    col = mixer * 384 + h * 64
                    P.op("act", lambda e: e.activation(out=osb_[:], in_=OT[0:65, :], func=AF.Copy), reads=[OT], writes=[osb_])
                    for qi in range(4):
                        P.op("pe", lambda e: e.transpose(Otr[:, qi * 65:(qi + 1) * 65], osb_[:, qi * 128:(qi + 1) * 128], identf[0:65, 0:65]), reads=[osb_, identf], writes=[Otr])
                    o3 = Otr[:, 0:260].rearrange("p (q c) -> p q c", q=4)
                    P.op("dve", lambda e: e.reciprocal(out=rd[:], in_=o3[:, :, 64]), reads=[Otr], writes=[rd])
                    P.op("dve", lambda e: e.tensor_tensor(out=o_attn[:, 4 * b:4 * b + 4, col:col + 64], in0=o3[:, :, 0:64], in1=rd[:].unsqueeze(2).to_broadcast([128, 4, 64]), op=ALU.mult),
                         reads=[Otr, rd], writes=[o_attn])

                cur = front(units[0])
                pending = None
                for i, (b, kt) in enumerate(units):
                    nxt = front(units[i + 1]) if i + 1 < len(units) else None
                    pt_, q0 = cur
                    nk = 4 * b + 4
                    OT = FB[2 + (ob_i % 2)]
                    P.op("pe", lambda e: e.matmul(OT[0:65, q0:512], lhsT=V_sb[:, kt, h * 65:(h + 1) * 65], rhs=pt_[:, q0:512], start=(kt == 0), stop=(kt == nk - 1)),
                         reads=[pt_, V_sb], writes=[OT])
                    if pending is not None:
                        evac(*pending)
                        pending = None
                    if kt == nk - 1:
                        pending = (b, OT, ob_i)
                        ob_i += 1
                    cur = nxt
                if pending is not None:
                    evac(*pending)

    def phase_c(l, moe):
        j2 = l // 2
        if moe:
            P.dma("sp", wr_sb[:], A["moe_wr"][j2].rearrange("(k p) n -> p k n", p=128), writes=[wr_sb])
            P.dma("sp", br_sb[:], A["moe_br"][j2].to_broadcast([128, 8]), writes=[br_sb])
        for t in range(NT):
            xs_ = xs[t % 2]
            mat, mT, h2T_st = mat_l[t % 2], mT_l[t % 2], h2T_st_l[t % 2]
            if l == 0:
                P.dma("sp", xs_[:], x_in[t * 128:(t + 1) * 128, :], writes=[xs_])
            else:
                P.dma("sp", xs_[:], y[t * 128:(t + 1) * 128, :], reads=[ytile[t]], writes=[xs_])
            st_ = stat[t % 4]
            for mx in range(2):
                P.op("act", lambda e, mx=mx: e.activation(out=sq_junk[:, mx * 384:(mx + 1) * 384], in_=o_attn[:, t, mx * 384:(mx + 1) * 384], func=AF.Square, accum_out=st_[:, mx:mx + 1]),
                     reads=[o_attn], writes=[sq_junk, st_])
            P.op("act", lambda e: e.activation(out=st_[:, 2:4], in_=st_[:, 0:2], func=AF.Sqrt, scale=1.0 / 384, bias=eps_c[:, 0:1]), reads=[st_, eps_c], writes=[st_])
            P.op("dve", lambda e: e.reciprocal(out=st_[:, 2:4], in_=st_[:, 2:4]), reads=[st_], writes=[st_])
            for mx in range(2):
                P.op("dve", lambda e, mx=mx: e.tensor_scalar(out=mat[:, mx * 384:(mx + 1) * 384], in0=o_attn[:, t, mx * 384:(mx + 1) * 384], scalar1=st_[:, 2 + mx:3 + mx], scalar2=None, op0=ALU.mult),
                     reads=[o_attn, st_], writes=[mat])
            tb = TB[t % 2]
            for k in range(6):
                P.op("pe", lambda e, k=k: e.transpose(tb[:, k * 128:(k + 1) * 128], mat[:, k * 128:(k + 1) * 128], ident[:]), reads=[mat, ident], writes=[tb])
            P.op("act", lambda e: e.activation(out=mT[:, 2:8, :], in_=tb[:, 0:768].rearrange("p (k t) -> p k t", k=6), func=AF.Copy), reads=[tb], writes=[mT])
            P.dma("sp", mT[:, 0:2, :], mssm[:, :, t * 128:(t + 1) * 128].rearrange("m p t -> p m t"), reads=[mssm], writes=[mT])
            xn = xnew[t % 2]
            for hf in range(2):
                fb = FB[hf]
                for k in range(KD):
                    P.op("pe", lambda e, fb=fb, k=k, hf=hf: e.matmul(fb[:], lhsT=mT[:, k, :], rhs=w_out_sb[:, k, hf * 512:(hf + 1) * 512], start=(k == 0), stop=(k == KD - 1)),
                         reads=[mT, w_out_sb], writes=[fb])
                P.op("dve", lambda e, fb=fb, hf=hf: e.tensor_tensor(out=xn[:, hf * 512:(hf + 1) * 512], in0=fb[:], in1=xs_[:, hf * 512:(hf + 1) * 512], op=ALU.add), reads=[fb, xs_], writes=[xn])
            P.dma("sp", y[t * 128:(t + 1) * 128, :], xn[:], reads=[xn], writes=[ytile[t]])
            norm_transpose(xn, xn[:], l, 2, 3, h2T_st, slice(0, 128), t, fp32_path=(xhf, h2Tf) if moe else None)
            P.dma("sp", h2T_d[:, :, t * 128:(t + 1) * 128].rearrange("k p t -> p k t"), h2T_st[:], reads=[h2T_st], writes=[h2T_d])
            if moe:
                fb = FB[2]
                for k in range(KD):
                    P.op("pe", lambda e, k=k: e.matmul(fb[:, 0:8], lhsT=h2Tf[:, k, :], rhs=wr_sb[:, k, :], start=(k == 0), stop=(k == KD - 1)), reads=[h2Tf, wr_sb], writes=[fb])
                lg, m1, m2, lg2 = rtmp
                s1, s2, s3, s4 = rsc
                P.op("dve", lambda e: e.tensor_tensor(out=lg[:], in0=fb[:, 0:8], in1=br_sb[:], op=ALU.add), reads=[fb, br_sb], writes=[lg])
                P.op("dve", lambda e: e.tensor_reduce(out=s1[:], in_=lg[:], axis=AX.X, op=ALU.max), reads=[lg], writes=[s1])
                P.op("dve", lambda e: e.tensor_scalar(out=m1[:], in0=lg[:], scalar1=s1[:, 0:1], scalar2=None, op0=ALU.is_equal), reads=[lg, s1], writes=[m1])
                P.op("dve", lambda e: e.scalar_tensor_tensor(out=lg2[:], in0=m1[:], scalar=-1e30, in1=lg[:], op0=ALU.mult, op1=ALU.add), reads=[m1, lg], writes=[lg2])
                P.op("dve", lambda e: e.tensor_reduce(out=s2[:], in_=lg2[:], axis=AX.X, op=ALU.max), reads=[lg2], writes=[s2])
                P.op("dve", lambda e: e.tensor_scalar(out=m2[:], in0=lg2[:], scalar1=s2[:, 0:1], scalar2=None, op0=ALU.is_equal), reads=[lg2, s2], writes=[m2])
                P.op("dve", lambda e: e.tensor_tensor(out=s3[:], in0=s2[:], in1=s1[:], op=ALU.subtract), reads=[s1, s2], writes=[s3])
                P.op("act", lambda e: e.activation(out=s3[:], in_=s3[:], func=AF.Exp), reads=[s3], writes=[s3])
                P.op("dve", lambda e: e.tensor_scalar(out=s3[:], in0=s3[:], scalar1=1.0, scalar2=None, op0=ALU.add), reads=[s3], writes=[s3])
                P.op("dve", lambda e: e.reciprocal(out=s3[:], in_=s3[:]), reads=[s3], writes=[s3])
                P.op("dve", lambda e: e.tensor_scalar(out=s4[:], in0=s3[:], scalar1=-1.0, scalar2=1.0, op0=ALU.mult, op1=ALU.add), reads=[s3], writes=[s4])
                P.op("dve", lambda e: e.tensor_scalar(out=m1[:], in0=m1[:], scalar1=s3[:, 0:1], scalar2=None, op0=ALU.mult), reads=[m1, s3], writes=[m1])
                P.op("dve", lambda e, t=t: e.scalar_tensor_tensor(out=comb_all[:, t, :], in0=m2[:], scalar=s4[:, 0:1], in1=m1[:], op0=ALU.mult, op1=ALU.add), reads=[m2, s4, m1], writes=[comb_all])

    def phase_d(l, moe):
        j2 = l // 2
        P.dma("sp", G_bc[:], g12[l, 1], reads=[g12], writes=[G_bc])
        ne = 8 if moe else 2
        it = 0
        oi = 0
        for ex in range(ne):
            if moe:
                wg = A["moe_wg"][j2, ex]
                wu = A["moe_wu"][j2, ex]
                wd = A["moe_wd"][j2, ex]
            else:
                wg = A["ffn_wg"][j2][:, ex * DFE:(ex + 1) * DFE]
                wu = A["ffn_wu"][j2][:, ex * DFE:(ex + 1) * DFE]
                wd = A["ffn_wd"][j2][ex * DFE:(ex + 1) * DFE, :]
            Wg_, Wu_, Wd_ = Wg_sb[ex % 2], Wu_sb[ex % 2], Wd_sb[ex % 2]
            P.dma("pool", Wg_[:], wg.rearrange("(k p) f -> p k f", p=128), writes=[Wg_])
            P.dma("pool", Wu_[:], wu.rearrange("(k p) f -> p k f", p=128), writes=[Wu_])
            P.dma("pool", Wd_[:], wd.rearrange("(c p) d -> p c d", p=128), writes=[Wd_])
            for b in range(NB):
                hb = h2T_sb[(ex * NB + b) % 2]
                P.dma("sp", hb[:], h2T_d[:, :, b * 512:(b + 1) * 512].rearrange("k p t -> p k t"), reads=[h2T_d], writes=[hb])
                for c in range(NFC):
                    gb = FB[(it % 2) * 2]
                    ub = FB[(it % 2) * 2 + 1]
                    sg_ = sg[it % 2]
                    it += 1
                    for k in range(KD):
                        P.op("pe", lambda e, gb=gb, k=k, c=c: e.matmul(gb[:], lhsT=Wg_[:, k, c * 128:(c + 1) * 128], rhs=hb[:, k, :], start=(k == 0), stop=(k == KD - 1)), reads=[Wg_, hb], writes=[gb])
                    for k in range(KD):
                        P.op("pe", lambda e, ub=ub, k=k, c=c: e.matmul(ub[:], lhsT=Wu_[:, k, c * 128:(c + 1) * 128], rhs=hb[:, k, :], start=(k == 0), stop=(k == KD - 1)), reads=[Wu_, hb], writes=[ub])
                    P.op("act", lambda e, gb=gb, sg_=sg_: e.activation(out=sg_[:], in_=gb[:], func=AF.Silu), reads=[gb], writes=[sg_])
                    P.op("dve", lambda e, ub=ub, sg_=sg_, c=c: e.tensor_tensor(out=aT[:, c, :], in0=ub[:], in1=sg_[:], op=ALU.mult), reads=[ub, sg_], writes=[aT])
                for ti in range(4):
                    t = b * 4 + ti
                    os_ = ost[oi % 2]
                    oi += 1
                    for hf in range(2):
                        fb = FB[4 + hf]
                        for c in range(NFC):
                            P.op("pe", lambda e, fb=fb, c=c, ti=ti, hf=hf: e.matmul(fb[:], lhsT=aT[:, c, ti * 128:(ti + 1) * 128], rhs=Wd_[:, c, hf * 512:(hf + 1) * 512], start=(c == 0), stop=(c == NFC - 1)),
                                 reads=[aT, Wd_], writes=[fb])
                        if moe:
                            P.op("dve", lambda e, fb=fb, hf=hf, t=t, ex=ex, os_=os_: e.scalar_tensor_tensor(out=os_[:, hf * 512:(hf + 1) * 512], in0=fb[:], scalar=comb_all[:, t, ex:ex + 1],
                                                                                                       in1=G_bc[:, hf * 512:(hf + 1) * 512], op0=ALU.mult, op1=ALU.mult), reads=[fb, comb_all, G_bc], writes=[os_])
                        else:
                            P.op("dve", lambda e, fb=fb, hf=hf, os_=os_: e.tensor_tensor(out=os_[:, hf * 512:(hf + 1) * 512], in0=fb[:], in1=G_bc[:, hf * 512:(hf + 1) * 512], op=ALU.mult),
                                 reads=[fb, G_bc], writes=[os_])
                    P.dma("pool", y[t * 128:(t + 1) * 128, :], os_[:], reads=[os_, ytile[t]], writes=[ytile[t]], accum_op=ALU.add)

    stop = build.stop_after
    G = globals()

    def use(dct):
        for k_, v_ in dct.items():
            if k_ not in ("P", "L", "NT"):
                G[k_] = v_
    dbg = P.dram("dbg_oattn", [128, NT * 768], BF16, kind="ExternalOutput") if debug else None
    for l in range(n_layers):
        moe = (l % 2 == 1)
        P.push()
        use(_alloc_a(P, L))
        s5c = load_layer(l)
        phase_a(l, s5c)
        P.pop()
        if stop == "a":
            break
        P.push()
        use(_alloc_bc(P, L))
        P.push()
        use(_alloc_b(P, L))
        phase_b(l)
        P.pop()
        if debug and (stop == "b" or l == n_layers - 1):
            P.dma("sp", dbg[:], o_attn[:].rearrange("p t c -> p (t c)"), reads=[o_attn], writes=[dbg])
        if stop == "b":
            P.pop()
            break
        P.push()
        use(_alloc_c(P, L))
        load_c(l)
        phase_c(l, moe)
        P.pop()
        P.pop()
        if stop == "c":
            break
        P.push()
        use(_alloc_d(P, L))
        phase_d(l, moe)
        P.pop()
    P.finish()
    build.n_inst = P.n_inst
    return nc


build.stop_after = None


def prep_shared(inp, n_layers=DEPTH):
    f = lambda a: np.ascontiguousarray(np.asarray(a, dtype=np.float32))
    col = lambda a, k: f(np.asarray(a).reshape(DEPTH, k, 128).transpose(0, 2, 1))
    out = {}
    out["norm_mix_c"] = col(inp["norm_mix"], KD)
    out["norm_ffn_c"] = col(inp["norm_ffn"], KD)
    out["w_ada"] = f(inp["w_ada"])
    out["b_ada"] = f(np.asarray(inp["b_ada"]).reshape(DEPTH, 1, 6 * D))
    out["w_in"] = f(inp["w_in"])
    sm = lambda a: f(np.asarray(a).reshape(DEPTH, 8, 128).transpose(0, 2, 1))
    out["lam_re"] = sm(inp["ssm_lam_re"])
    out["lam_im"] = sm(inp["ssm_lam_im"])
    out["log_dt"] = sm(np.repeat(np.asarray(inp["ssm_log_dt"])[:, :, None], 64, axis=2))
    b_re = np.asarray(inp["ssm_b_re"]); b_im = np.asarray(inp["ssm_b_im"])
    c_re = np.asarray(inp["ssm_c_re"]); c_im = np.asarray(inp["ssm_c_im"])
    blk = {k: np.zeros((DEPTH, 8, 128, 128), np.float32) for k in ("b_re", "b_im", "c_re", "c_im")}
    for g in range(16):
        j, gg = g // 2, g % 2
        fc = (g % 8) * 16
        blk["b_re"][:, j, gg * 64:(gg + 1) * 64, fc:fc + 16] = b_re[:, g]
        blk["b_im"][:, j, gg * 64:(gg + 1) * 64, fc:fc + 16] = b_im[:, g]
        blk["c_re"][:, j, gg * 64:(gg + 1) * 64, fc:fc + 16] = c_re[:, g].transpose(0, 2, 1)
        blk["c_im"][:, j, gg * 64:(gg + 1) * 64, fc:fc + 16] = c_im[:, g].transpose(0, 2, 1)
    out.update(blk)
    out["ssm_d"] = col(inp["ssm_d"], 2)
    out["w_glu"] = f(inp["ssm_w_glu"])
    out["b_glu"] = col(inp["ssm_b_glu"], 2)
    out["q_norm"] = col(inp["mla_q_norm"], 2)
    out["kv_norm"] = col(inp["mla_kv_norm"], 1)
    out["w_uq"] = f(inp["mla_w_uq"])
    out["w_ukv"] = f(inp["mla_w_ukv"])
    rep = lambda a: f(np.broadcast_to(np.asarray(a)[:, None, :], (DEPTH, 128, np.asarray(a).shape[1])))
    out["gq_m"] = rep(inp["mla_qk_gq"])
    out["gk_m"] = rep(inp["mla_qk_gk"])
    out["fox_bf"] = f(np.asarray(inp["fox_b_f"]).reshape(DEPTH, NH, 1))
    out["gq_f"] = rep(inp["fox_qk_gq"])
    out["gk_f"] = rep(inp["fox_qk_gk"])
    out["out_norm"] = col(inp["out_norm"], KD)
    out["w_out"] = f(inp["w_out"])
    out["ffn_wg"] = f(inp["ffn_w_gate"])
    out["ffn_wu"] = f(inp["ffn_w_up"])
    out["ffn_wd"] = f(inp["ffn_w_down"])
    out["moe_wr"] = f(inp["moe_w_router"])
    out["moe_br"] = f(np.asarray(inp["moe_b_router"]).reshape(2, 1, 8))
    out["moe_wg"] = f(inp["moe_w_gate"])
    out["moe_wu"] = f(inp["moe_w_up"])
    out["moe_wd"] = f(inp["moe_w_down"])
    half = 16
    inv = (10000.0 ** (-np.arange(half, dtype=np.float32) / half)).astype(np.float32)
    out["inv_bc"] = f(np.broadcast_to(inv[None, :], (128, 16)))
    return out


def prep_core(inp, b, L):
    NT = L // 128
    m = {}
    m["x"] = np.ascontiguousarray(np.asarray(inp["x"])[b, :L, :], dtype=np.float32)
    m["c_col"] = np.ascontiguousarray(np.asarray(inp["c"], dtype=np.float32)[b].reshape(KD, 128).T)
    m["pos"] = np.ascontiguousarray(np.asarray(inp["positions"])[b, :L].astype(np.int32).reshape(NT, 128).T)
    return m


_CACHE = {}


def run(inp, L, n_layers=DEPTH, debug=False, cores=8, trace=False):
    key = (L, n_layers, debug, build.stop_after)
    if key not in _CACHE:
        _CACHE[key] = build(L, n_layers, debug)
    nc = _CACHE[key]
    shared = prep_shared(inp, n_layers)
    in_maps = []
    for b in range(cores):
        m = dict(shared)
        for n_ in PADDED:
            a = shared[n_]
            a2 = a.reshape(-1, a.shape[-1])
            m[n_] = np.concatenate([a2, np.full((1, a2.shape[1]), float(b), np.float32)], axis=0)
        m.update(prep_core(inp, b, L))
        in_maps.append(m)
    res = run_bass_kernel_spmd(nc, in_maps, core_ids=list(range(cores)), **({"trace": True} if trace else {}))
    return res


def kernel(**inputs):
    L = np.asarray(inputs["x"]).shape[1]
    res = run(inputs, L)
    out = np.stack([np.asarray(r["y"], dtype=np.float32) for r in res.results], axis=0)
    return out
```
